# Optimizing a Trainium2 kernel written in Bass

```python
import math
import jax, jax.numpy as jnp
from jax import lax
import numpy as np

D_MODEL = 1024
BATCH = 16
SEQ = 2048
DEPTH = 1

D_MIX = D_MODEL
D_SSM = D_MIX // 2
D_ATTN = D_MIX - D_SSM
SSM_GROUP = 16
N_SSM_GROUPS = D_SSM // SSM_GROUP
SSM_STATE = 64
DT_MIN = 1e-3
DT_MAX = 1e-1
HEAD_DIM = 64
N_HEADS = D_ATTN // HEAD_DIM
MOBA_BLOCK = 256
MOBA_TOPK = 3
Q_CHUNK = 16
D_FF = -(-8 * D_MODEL // (3 * 256)) * 256
RMS_EPS = 1e-6

kernel_name = "hybrid_s5_moba_block"


def rmsnorm(x, g):
    xf = x.astype(jnp.float32)
    y = xf * lax.rsqrt(jnp.mean(xf * xf, axis=-1, keepdims=True) + RMS_EPS)
    return (y * g.astype(jnp.float32)).astype(x.dtype)


def _complex_affine_combine(e1, e2):
    a1r, a1i, b1r, b1i = e1
    a2r, a2i, b2r, b2i = e2
    ar = a2r * a1r - a2i * a1i
    ai = a2r * a1i + a2i * a1r
    br = a2r * b1r - a2i * b1i + b2r
    bi = a2r * b1i + a2i * b1r + b2i
    return (ar, ai, br, bi)


def s5_mixer(u, a_re, a_im, log_dt, b_re, b_im, c_re, c_im, d_skip, w_glu, b_glu):
    bsz, s, _ = u.shape
    f32 = jnp.float32
    uf = u.astype(f32).reshape(bsz, s, N_SSM_GROUPS, SSM_GROUP)
    a_re = a_re.astype(f32); a_im = a_im.astype(f32)
    dt = jnp.exp(log_dt.astype(f32))[:, None]
    mag = jnp.exp(a_re * dt)
    ang = a_im * dt
    lb_re = mag * jnp.cos(ang)
    lb_im = mag * jnp.sin(ang)
    nr = lb_re - 1.0
    den = a_re * a_re + a_im * a_im
    cr = (nr * a_re + lb_im * a_im) / den
    ci = (lb_im * a_re - nr * a_im) / den
    b_re = b_re.astype(f32); b_im = b_im.astype(f32)
    bb_re = cr[..., None] * b_re - ci[..., None] * b_im
    bb_im = cr[..., None] * b_im + ci[..., None] * b_re
    bu_re = jnp.einsum('bsgh,gph->bsgp', uf, bb_re)
    bu_im = jnp.einsum('bsgh,gph->bsgp', uf, bb_im)
    ar = jnp.broadcast_to(lb_re, (1, s) + lb_re.shape)
    ai = jnp.broadcast_to(lb_im, (1, s) + lb_im.shape)
    _, _, st_re, st_im = lax.associative_scan(
        _complex_affine_combine, (ar, ai, bu_re, bu_im), axis=1)
    y = (jnp.einsum('bsgp,ghp->bsgh', st_re, c_re.astype(f32))
         - jnp.einsum('bsgp,ghp->bsgh', st_im, c_im.astype(f32))
         + d_skip.astype(f32) * uf)
    y = jax.nn.gelu(y.reshape(bsz, s, D_SSM))
    y = y * jax.nn.sigmoid(jnp.einsum('bsc,ce->bse', y, w_glu.astype(f32)) + b_glu.astype(f32))
    return y.astype(u.dtype)


def moba_attention(q, k, v):
    bsz, s, _ = q.shape
    n_blocks = -(-s // MOBA_BLOCK)
    sp = n_blocks * MOBA_BLOCK
    k_sel = min(MOBA_TOPK, n_blocks)
    n_chunks = sp // Q_CHUNK

    def heads(t):
        t = t.astype(jnp.float32).reshape(bsz, s, N_HEADS, HEAD_DIM).transpose(0, 2, 1, 3)
        return jnp.pad(t, ((0, 0), (0, 0), (0, sp - s), (0, 0)))

    qh = heads(q) * (HEAD_DIM ** -0.5)
    kh = heads(k)
    vh = heads(v)
    k_blocks = kh.reshape(bsz, N_HEADS, n_blocks, MOBA_BLOCK, HEAD_DIM)
    v_blocks = vh.reshape(bsz, N_HEADS, n_blocks, MOBA_BLOCK, HEAD_DIM)
    k_mean = jnp.mean(k_blocks, axis=3)

    gate = jnp.einsum('bhsd,bhnd->bhsn', qh, k_mean)
    q_blk = jnp.arange(sp) // MOBA_BLOCK
    past = jnp.arange(n_blocks)[None, :] < q_blk[:, None]
    gate = jnp.where(past, gate, -jnp.inf)
    _, sel_idx = lax.top_k(gate, k_sel)
    sel_valid = jnp.arange(k_sel)[None, :] < q_blk[:, None]

    def to_chunks(t):
        return jnp.moveaxis(t.reshape(bsz, N_HEADS, n_chunks, Q_CHUNK, *t.shape[3:]), 2, 0)

    q_c = to_chunks(qh)
    idx_c = to_chunks(sel_idx)
    valid_c = sel_valid.reshape(n_chunks, Q_CHUNK, k_sel)
    gather = jax.vmap(jax.vmap(lambda blocks, ix: blocks[ix]))

    def chunk_attend(args):
        c, qc, ic, vc = args
        kg = gather(k_blocks, ic)
        vg = gather(v_blocks, ic)
        s_sel = jnp.einsum('bhqd,bhqnjd->bhqnj', qc, kg)
        s_sel = jnp.where(vc[None, None, :, :, None], s_sel, -jnp.inf)
        own = (c * Q_CHUNK) // MOBA_BLOCK
        k_own = lax.dynamic_index_in_dim(k_blocks, own, axis=2, keepdims=False)
        v_own = lax.dynamic_index_in_dim(v_blocks, own, axis=2, keepdims=False)
        s_own = jnp.einsum('bhqd,bhjd->bhqj', qc, k_own)
        q_pos = c * Q_CHUNK + jnp.arange(Q_CHUNK)
        k_pos = own * MOBA_BLOCK + jnp.arange(MOBA_BLOCK)
        s_own = jnp.where(k_pos[None, :] <= q_pos[:, None], s_own, -jnp.inf)
        scores = jnp.concatenate(
            [s_sel.reshape(bsz, N_HEADS, Q_CHUNK, k_sel * MOBA_BLOCK), s_own], axis=-1)
        p = jax.nn.softmax(scores, axis=-1)
        p_sel = p[..., :k_sel * MOBA_BLOCK].reshape(bsz, N_HEADS, Q_CHUNK, k_sel, MOBA_BLOCK)
        p_own = p[..., k_sel * MOBA_BLOCK:]
        return (jnp.einsum('bhqnj,bhqnjd->bhqd', p_sel, vg)
                + jnp.einsum('bhqj,bhjd->bhqd', p_own, v_own))

    out = lax.map(chunk_attend, (jnp.arange(n_chunks), q_c, idx_c, valid_c))
    out = out.transpose(1, 0, 3, 2, 4).reshape(bsz, sp, D_ATTN)
    return out[:, :s].astype(q.dtype)


def hybrid_layer(x, g_pre_mix, w_in, ssm_a_re, ssm_a_im, ssm_log_dt, ssm_b_re, ssm_b_im,
                 ssm_c_re, ssm_c_im, ssm_d, w_glu, b_glu, g_ssm_out, g_attn_out, w_out,
                 g_post_mix, g_pre_ffn, w_gate, w_up, w_down, g_post_ffn):
    h = rmsnorm(x, g_pre_mix)
    z = jnp.einsum('bsd,de->bse', h, w_in)
    u, q, k, v = jnp.split(z, [D_SSM, D_SSM + D_ATTN, D_SSM + 2 * D_ATTN], axis=-1)
    y_ssm = s5_mixer(u, ssm_a_re, ssm_a_im, ssm_log_dt, ssm_b_re, ssm_b_im,
                     ssm_c_re, ssm_c_im, ssm_d, w_glu, b_glu)
    y_attn = moba_attention(q, k, v)
    mix = jnp.concatenate([rmsnorm(y_ssm, g_ssm_out), rmsnorm(y_attn, g_attn_out)], axis=-1)
    x = x + rmsnorm(jnp.einsum('bse,ed->bsd', mix, w_out), g_post_mix)
    h = rmsnorm(x, g_pre_ffn)
    f = jax.nn.silu(jnp.einsum('bsd,df->bsf', h, w_gate)) * jnp.einsum('bsd,df->bsf', h, w_up)
    x = x + rmsnorm(jnp.einsum('bsf,fd->bsd', f, w_down), g_post_ffn)
    return x


def setup_inputs(seed: int = 0) -> dict:
    key = jax.random.key(seed)
    ks = jax.random.split(key, 24)
    f32 = jnp.float32
    G, P, H = N_SSM_GROUPS, SSM_STATE, SSM_GROUP
    L = DEPTH

    def nrm(k, shape, scale):
        return jax.random.normal(k, shape, f32) * scale

    def gain(k, n):
        return 1.0 + 0.01 * jax.random.normal(k, (L, n), f32)

    n_idx = jnp.arange(P, dtype=f32)
    return dict(
        x=nrm(ks[0], (BATCH, SEQ, D_MODEL), 1.0),
        g_pre_mix=gain(ks[1], D_MODEL),
        w_in=nrm(ks[2], (L, D_MODEL, D_SSM + 3 * D_ATTN), D_MODEL ** -0.5),
        ssm_a_re=-0.5 + nrm(ks[3], (L, G, P), 0.01),
        ssm_a_im=math.pi * n_idx[None, None, :] + nrm(ks[4], (L, G, P), 0.01),
        ssm_log_dt=jax.random.uniform(ks[5], (L, G), f32, minval=math.log(DT_MIN), maxval=math.log(DT_MAX)),
        ssm_b_re=nrm(ks[6], (L, G, P, H), (2 * H) ** -0.5),
        ssm_b_im=nrm(ks[7], (L, G, P, H), (2 * H) ** -0.5),
        ssm_c_re=nrm(ks[8], (L, G, H, P), (2 * P) ** -0.5),
        ssm_c_im=nrm(ks[9], (L, G, H, P), (2 * P) ** -0.5),
        ssm_d=nrm(ks[10], (L, G, H), 1.0),
        w_glu=nrm(ks[11], (L, D_SSM, D_SSM), D_SSM ** -0.5),
        b_glu=nrm(ks[12], (L, D_SSM), 0.01),
        g_ssm_out=gain(ks[13], D_SSM),
        g_attn_out=gain(ks[14], D_ATTN),
        w_out=nrm(ks[15], (L, D_MIX, D_MODEL), D_MIX ** -0.5),
        g_post_mix=gain(ks[16], D_MODEL),
        g_pre_ffn=gain(ks[17], D_MODEL),
        w_gate=nrm(ks[18], (L, D_MODEL, D_FF), D_MODEL ** -0.5),
        w_up=nrm(ks[19], (L, D_MODEL, D_FF), D_MODEL ** -0.5),
        w_down=nrm(ks[20], (L, D_FF, D_MODEL), D_FF ** -0.5),
        g_post_ffn=gain(ks[21], D_MODEL),
    )


def reference(x, g_pre_mix, w_in, ssm_a_re, ssm_a_im, ssm_log_dt, ssm_b_re, ssm_b_im,
              ssm_c_re, ssm_c_im, ssm_d, w_glu, b_glu, g_ssm_out, g_attn_out, w_out,
              g_post_mix, g_pre_ffn, w_gate, w_up, w_down, g_post_ffn):
    for l in range(DEPTH):
        x = hybrid_layer(x, g_pre_mix[l], w_in[l], ssm_a_re[l], ssm_a_im[l], ssm_log_dt[l],
                         ssm_b_re[l], ssm_b_im[l], ssm_c_re[l], ssm_c_im[l], ssm_d[l],
                         w_glu[l], b_glu[l], g_ssm_out[l], g_attn_out[l], w_out[l],
                         g_post_mix[l], g_pre_ffn[l], w_gate[l], w_up[l], w_down[l], g_post_ffn[l])
    return x
```

```python
import math
import numpy as np
from contextlib import ExitStack
import concourse.bass as bass
import concourse.mybir as mybir
from concourse.alu_op_type import AluOpType as ALU
from concourse.bass_utils import run_bass_kernel_spmd

F32 = mybir.dt.float32
BF16 = mybir.dt.bfloat16
U8 = mybir.dt.uint8
I32 = mybir.dt.int32
AF = mybir.ActivationFunctionType
AX = mybir.AxisListType

S = 2048
D = 1024
DFF = 2816
NFF = 22
NEG = -30000.0
EPS = 1e-6
ENGS = ['tensor', 'vector', 'scalar', 'gpsimd', 'sync']
DEBUG = False
STAGE = 99
NSTEP = 127
ABQ = 8


class _Stop(Exception):
    pass


class Tk:
    __slots__ = ('w', 'r')

    def __init__(self):
        self.w = {}
        self.r = {}


class FW:
    def __init__(self, nc, es):
        self.nc, self.es = nc, es
        self.prog = {e: [] for e in ENGS}
        self.esem = {e: es.enter_context(nc.semaphore("es_" + e)) for e in ENGS}
        self.ecnt = {e: 0 for e in ENGS}
        self.seen = {e: {} for e in ENGS}
        self.dcnt = {}

    def newsem(self, name):
        s = self.es.enter_context(self.nc.semaphore(name))
        self.dcnt[id(s)] = [s, 0]
        return s

    def _dep(self, eng, reads, writes):
        need = {}

        def add(dct):
            for k, (s, v) in dct.items():
                if need.get(k, (None, 0))[1] < v:
                    need[k] = (s, v)
        for t in reads:
            add(t.w)
        for t in writes:
            add(t.w)
            add(t.r)
        for k, (s, v) in need.items():
            if self.seen[eng].get(k, 0) >= v:
                continue
            self.seen[eng][k] = v
            self.prog[eng].append(('w', s, v))

    def _post(self, tok, reads, writes):
        s, v = tok
        k = id(s)
        for t in reads:
            if t.r.get(k, (None, 0))[1] < v:
                t.r[k] = (s, v)
        for t in writes:
            t.w = {k: (s, v)}
            t.r = {}

    def op(self, eng, fn, reads=(), writes=()):
        self._dep(eng, reads, writes)
        self.ecnt[eng] += 1
        self.prog[eng].append(('o', fn, True))
        self._post((self.esem[eng], self.ecnt[eng]), reads, writes)

    def mm(self, fns, reads=(), writes=()):
        self._dep('tensor', reads, writes)
        for f in fns[:-1]:
            self.prog['tensor'].append(('o', f, False))
        self.ecnt['tensor'] += 1
        self.prog['tensor'].append(('o', fns[-1], True))
        self._post((self.esem['tensor'], self.ecnt['tensor']), reads, writes)

    def dma(self, eng, out, in_, sem, reads=(), writes=()):
        self._dep(eng, reads, writes)
        c = self.dcnt[id(sem)]
        c[1] += 16
        self.prog[eng].append(('d', out, in_, sem))
        self._post((sem, c[1]), reads, writes)

    def barrier(self):
        for e in ENGS:
            for e2 in ENGS:
                if e2 == e or self.ecnt[e2] == 0:
                    continue
                k = id(self.esem[e2])
                if self.seen[e].get(k, 0) < self.ecnt[e2]:
                    self.seen[e][k] = self.ecnt[e2]
                    self.prog[e].append(('w', self.esem[e2], self.ecnt[e2]))
            for k, (s, v) in self.dcnt.items():
                if v > 0 and self.seen[e].get(k, 0) < v:
                    self.seen[e][k] = v
                    self.prog[e].append(('w', s, v))

    def emit(self):
        nc = self.nc
        with nc.Block() as block:
            for e in ENGS:
                prog = self.prog[e]
                sem = self.esem[e]

                def body(E, prog=prog, sem=sem):
                    for it in prog:
                        if it[0] == 'w':
                            E.wait_ge(it[1], it[2])
                        elif it[0] == 'o':
                            ins = it[1](E)
                            if it[2]:
                                ins.then_inc(sem, 1)
                        else:
                            E.dma_start(out=it[1], in_=it[2]).then_inc(it[3], 16)
                getattr(block, e)(body)


def tt(out, in0, in1, op):
    return lambda E: E.tensor_tensor(out=out, in0=in0, in1=in1, op=op)


def ts(out, in0, s1, op0, s2=None, op1=None):
    if op1 is None:
        return lambda E: E.tensor_scalar(out=out, in0=in0, scalar1=s1, scalar2=None, op0=op0)
    return lambda E: E.tensor_scalar(out=out, in0=in0, scalar1=s1, scalar2=s2, op0=op0, op1=op1)


def stt(out, in0, scalar, in1, op0, op1):
    return lambda E: E.scalar_tensor_tensor(out=out, in0=in0, scalar=scalar, in1=in1, op0=op0, op1=op1)


def ttr(out, in0, in1, accum):
    return lambda E: E.activation(out=out, in_=in0, func=AF.Square, accum_out=accum)


def act(out, in_, func, scale=None):
    if scale is None:
        return lambda E: E.activation(out=out, in_=in_, func=func)
    return lambda E: E.activation(out=out, in_=in_, func=func, scale=scale)


def sqa(out, in_, accum):
    return lambda E: E.activation(out=out, in_=in_, func=AF.Square, accum_out=accum)


def rcp(out, in_):
    return lambda E: E.reciprocal(out=out, in_=in_)


def cp(out, in_):
    return lambda E: E.tensor_copy(out=out, in_=in_)


def mmf(out, lhsT, rhs, start, stop):
    assert len(rhs.ap) == 2 and len(lhsT.ap) == 2, ("rhs", rhs.ap, lhsT.ap)
    return lambda E: E.matmul(out, lhsT, rhs, start=start, stop=stop)


def trf(out, in_, ident):
    assert len(ident.ap) == 2 and len(in_.ap) == 2, ("ident", ident.ap, in_.ap)
    return lambda E: E.transpose(out, in_, ident)


def bc(ap, axis, shape):
    return ap.unsqueeze(axis).broadcast_to(list(shape))


def build(dbg=False):
    nc = bass.Bass("TRN2", target_bir_lowering=False)
    es = ExitStack()

    def din(name, shape):
        return nc.dram_tensor(name, list(shape), F32, kind="ExternalInput").ap()

    x = din("x", [2, S, D])
    w_in = din("w_in", [D, 2048])
    w_glu = din("w_glu", [512, 512])
    w_out = din("w_out", [1024, 1024])
    w_gate = din("w_gate", [D, DFF])
    w_up = din("w_up", [D, DFF])
    w_down = din("w_down", [DFF, D])
    areT = din("areT", [128, 16])
    aimT = din("aimT", [128, 16])
    ldtT = din("ldtT", [128, 16])
    breT = din("breT", [128, 16, 16])
    bimT = din("bimT", [128, 16, 16])
    creT = din("creT", [128, 16, 16])
    cimT = din("cimT", [128, 16, 16])
    drep = din("drep", [128, 32, 16])
    g1c = din("g1c", [128, 8])
    g3c = din("g3c", [128, 8])
    g2r = din("g2r", [128, 1024])
    g4r = din("g4r", [128, 1024])
    gsr = din("gsr", [128, 512])
    gar = din("gar", [128, 512])
    bgr = din("bgr", [128, 512])
    c_idf = din("c_idf", [128, 128])
    c_cb = din("c_cb", [128, 2, 256])
    c_tm = din("c_tm", [128, 2, 256])
    c_dm = din("c_dm", [128, 2, 256])
    c_nv = din("c_nv", [128, 32])
    out = nc.dram_tensor("out", [2, S, D], F32, kind="ExternalOutput").ap()
    GT_d = nc.dram_tensor("GT_d", [32, 128, 2, 2, 128], BF16, kind="Internal").ap()
    TOEP_d = nc.dram_tensor("TOEP_d", [32, 128, 2, 256], BF16, kind="Internal").ap()
    CT_d = nc.dram_tensor("CT_d", [16, 128, 2, 256], BF16, kind="Internal").ap()
    dbg_t = {}
    if dbg:
        dbg_t['uc'] = nc.dram_tensor("d_uc", [128, 32, 16, 16], BF16, kind="ExternalOutput").ap()
        dbg_t['qT'] = nc.dram_tensor("d_qT", [128, 4, S], BF16, kind="ExternalOutput").ap()
        dbg_t['vt'] = nc.dram_tensor("d_vt", [128, 16, 8, 65], BF16, kind="ExternalOutput").ap()
        dbg_t['y1'] = nc.dram_tensor("d_y1", [128, 16, 512], BF16, kind="ExternalOutput").ap()
        dbg_t['ssmT'] = nc.dram_tensor("d_ssmT", [128, 4, 16, 128], BF16, kind="ExternalOutput").ap()
        dbg_t['attT'] = nc.dram_tensor("d_attT", [128, 4, S], BF16, kind="ExternalOutput").ap()
        dbg_t['x1'] = nc.dram_tensor("d_x1", [128, 16, 1024], F32, kind="ExternalOutput").ap()
        dbg_t['S'] = nc.dram_tensor("d_S", [128, 2, 16, 128], F32, kind="ExternalOutput").ap()

    ARENA = 206 * 1024
    arena = es.enter_context(nc.sbuf_tensor("arena", [128, ARENA], U8))
    pbanks = [es.enter_context(nc.psum_tensor("pb%d" % i, [128, 512], F32)) for i in range(8)]
    pk = [Tk() for _ in range(8)]
    fw = FW(nc, es)

    def reg(off, shape, dt, p0=0):
        esz = 2 if dt == BF16 else 4
        n = int(np.prod(shape[1:])) * esz
        assert off % 4 == 0 and off + n <= ARENA, (off, n)
        ap = arena[p0:p0 + shape[0], off:off + n].bitcast(dt)
        if len(shape) == 3:
            ap = ap.rearrange("p (a b) -> p a b", a=shape[1])
        elif len(shape) == 4:
            ap = ap.rearrange("p (a b c) -> p a b c", a=shape[1], b=shape[2])
        elif len(shape) == 5:
            ap = ap.rearrange("p (a b c d) -> p a b c d", a=shape[1], b=shape[2], c=shape[3])
        return ap

    class Bump:
        def __init__(self, lo, hi):
            self.o, self.hi = lo, hi

        def __call__(self, shape, dt, p0=0):
            esz = 2 if dt == BF16 else 4
            n = int(np.prod(shape[1:])) * esz
            n4 = (n + 31) // 32 * 32
            a = reg(self.o, shape, dt, p0)
            self.o += n4
            assert self.o <= self.hi, (self.o, self.hi)
            return a

    K = 1024
    V, A, G_, T_, SY = 'vector', 'scalar', 'gpsimd', 'tensor', 'sync'

    cb = Bump(0, 20 * K)
    idf = cb([128, 128], F32)
    idb = cb([128, 128], BF16)
    cbias = cb([128, 2, 256], BF16)
    g1 = cb([128, 8], F32)
    g3 = cb([128, 8], F32)
    g2 = cb([128, 1024], F32)
    g4 = cb([128, 1024], F32)
    gs = cb([128, 512], F32)
    ga = cb([128, 512], F32)
    bg = cb([128, 512], F32)
    stat = cb([128, 64], F32)
    LR = cb([128, 7, 16], F32)
    LI = cb([128, 7, 16], F32)
    LIn = cb([128, 7, 16], F32)
    LAk = cb([128, 7, 2, 16], F32)
    assert cb.o <= 20 * K
    kC = Tk()
    sem_c = fw.newsem("sem_c")
    sem_c2 = fw.newsem("sem_c2")
    for dst, src in [(idf, c_idf), (g1, g1c), (g3, g3c), (g2, g2r), (g4, g4r), (gs, gsr), (ga, gar), (bg, bgr)]:
        fw.dma(SY, dst, src, sem_c, writes=[kC])

    sb = Bump(20 * K, ARENA)
    cb_off = sb.o
    CBre = sb([128, 16, 16, 16], F32)
    cbf = sb([128, 2, 256], F32)
    tmk = sb([128, 2, 256], F32)
    dmk = sb([128, 2, 256], F32)
    nv = sb([128, 32], F32)
    drp = sb([128, 32, 16], F32)
    are = sb([128, 16], F32)
    aim = sb([128, 16], F32)
    ldt = sb([128, 16], F32)
    bre = sb([128, 16, 16], F32)
    bim = sb([128, 16, 16], F32)
    cre = sb([128, 16, 16], F32)
    cim = sb([128, 16, 16], F32)
    kS = Tk()
    for dst, src in [(cbf, c_cb), (tmk, c_tm), (dmk, c_dm), (nv, c_nv), (drp, drep), (are, areT), (aim, aimT),
                     (ldt, ldtT), (bre, breT), (bim, bimT), (cre, creT), (cim, cimT)]:
        fw.dma(SY, dst, src, sem_c2, writes=[kS])
    fw.op(V, cp(idb, idf), [kC], [kC])
    fw.op(V, cp(cbias, cbf), [kS, kC], [kC])

    dtt = sb([128, 16], F32)
    adr = sb([128, 16], F32)
    adi = sb([128, 16], F32)
    ex = sb([128, 16, 32], F32)
    ang = sb([128, 16, 32], F32)
    mag = sb([128, 16, 32], F32)
    magi = sb([128, 16, 32], F32)
    r1 = sb([128, 16, 32], F32)
    r2 = sb([128, 16, 32], F32)
    sn = sb([128, 16, 32], F32)
    cs = sb([128, 16, 32], F32)
    Pre = sb([128, 16, 32], F32)
    Pim = sb([128, 16, 32], F32)
    Qre = sb([128, 16, 32], F32)
    Qim = sb([128, 16, 32], F32)
    sm = [sb([128, 16], F32) for _ in range(10)]
    bbre = sb([128, 16, 16], F32)
    bbim = sb([128, 16, 16], F32)
    tb1 = sb([128, 16, 16], F32)
    tb2 = sb([128, 16, 16], F32)
    k0 = Tk()
    fw.op(A, act(dtt, ldt, AF.Exp), [kS], [k0])
    fw.op(V, tt(adr, are, dtt, ALU.mult), [k0, kS], [k0])
    fw.op(V, tt(adi, aim, dtt, ALU.mult), [k0, kS], [k0])
    fw.op(V, tt(ex, bc(adr, 2, [128, 16, 32]), bc(nv, 1, [128, 16, 32]), ALU.mult), [k0, kS], [k0])
    fw.op(V, tt(ang, bc(adi, 2, [128, 16, 32]), bc(nv, 1, [128, 16, 32]), ALU.mult), [k0, kS], [k0])
    fw.op(A, act(mag, ex, AF.Exp), [k0], [k0])
    fw.op(A, act(magi, ex, AF.Exp, scale=-1.0), [k0], [k0])
    ki = reg(cb_off, [128, 16, 32], I32)
    kf = reg(cb_off + 2048, [128, 16, 32], F32)
    mk = reg(cb_off + 4096, [128, 16, 32], F32)

    def range_reduce(r, shift):
        fw.op(V, ts(r, ang, 1.0 / (2 * math.pi), ALU.mult, shift, ALU.add), [k0], [k0])
        fw.op(V, cp(ki, r), [k0], [k0])
        fw.op(V, cp(kf, ki), [k0], [k0])
        fw.op(V, tt(r, r, kf, ALU.subtract), [k0], [k0])
        fw.op(V, ts(mk, r, -0.5, ALU.is_lt), [k0], [k0])
        fw.op(V, tt(r, r, mk, ALU.add), [k0], [k0])
        fw.op(V, ts(mk, r, 0.5, ALU.is_gt), [k0], [k0])
        fw.op(V, tt(r, r, mk, ALU.subtract), [k0], [k0])
    range_reduce(r1, 0.0)
    range_reduce(r2, 0.25)
    fw.op(A, act(sn, r1, AF.Sin, scale=2 * math.pi), [k0], [k0])
    fw.op(A, act(cs, r2, AF.Sin, scale=2 * math.pi), [k0], [k0])
    fw.op(V, tt(Pre, mag, cs, ALU.mult), [k0], [k0])
    fw.op(V, tt(Pim, mag, sn, ALU.mult), [k0], [k0])
    fw.op(V, tt(Qre, magi, cs, ALU.mult), [k0], [k0])
    fw.op(V, stt(Qim, magi, -1.0, sn, ALU.mult, ALU.mult), [k0], [k0])
    lbre, lbim = Pre[:, :, 0], Pim[:, :, 0]
    nr, den, t0, t1_, cr, ci, rden = sm[0], sm[1], sm[2], sm[3], sm[4], sm[5], sm[6]
    fw.op(V, ts(nr, lbre, -1.0, ALU.add), [k0], [k0])
    fw.op(V, tt(den, are, are, ALU.mult), [k0, kS], [k0])
    fw.op(V, tt(t0, aim, aim, ALU.mult), [k0, kS], [k0])
    fw.op(V, tt(den, den, t0, ALU.add), [k0], [k0])
    fw.op(V, lambda E: E.reciprocal(out=rden, in_=den), [k0], [k0])
    fw.op(V, tt(t0, nr, are, ALU.mult), [k0], [k0])
    fw.op(V, tt(t1_, lbim, aim, ALU.mult), [k0], [k0])
    fw.op(V, tt(t0, t0, t1_, ALU.add), [k0], [k0])
    fw.op(V, tt(cr, t0, rden, ALU.mult), [k0], [k0])
    fw.op(V, tt(t0, lbim, are, ALU.mult), [k0], [k0])
    fw.op(V, tt(t1_, nr, aim, ALU.mult), [k0], [k0])
    fw.op(V, tt(t0, t0, t1_, ALU.subtract), [k0], [k0])
    fw.op(V, tt(ci, t0, rden, ALU.mult), [k0], [k0])
    crb, cib = bc(cr, 2, [128, 16, 16]), bc(ci, 2, [128, 16, 16])
    fw.op(V, tt(tb1, crb, bre, ALU.mult), [k0, kS], [k0])
    fw.op(V, tt(tb2, cib, bim, ALU.mult), [k0, kS], [k0])
    fw.op(V, tt(bbre, tb1, tb2, ALU.subtract), [k0], [k0])
    fw.op(V, tt(tb1, crb, bim, ALU.mult), [k0, kS], [k0])
    fw.op(V, tt(tb2, cib, bre, ALU.mult), [k0, kS], [k0])
    fw.op(V, tt(bbim, tb1, tb2, ALU.add), [k0], [k0])
    fw.op(V, cp(LR[:, 0, :], Pre[:, :, 15]), [k0], [kC])
    fw.op(V, cp(LI[:, 0, :], Pim[:, :, 15]), [k0], [kC])
    for k in range(6):
        fw.op(V, tt(sm[7], LR[:, k, :], LR[:, k, :], ALU.mult), [kC, k0], [k0])
        fw.op(V, tt(sm[8], LI[:, k, :], LI[:, k, :], ALU.mult), [kC, k0], [k0])
        fw.op(V, tt(LR[:, k + 1, :], sm[7], sm[8], ALU.subtract), [k0], [kC])
        fw.op(V, tt(sm[9], LR[:, k, :], LI[:, k, :], ALU.mult), [kC, k0], [k0])
        fw.op(V, ts(LI[:, k + 1, :], sm[9], 2.0, ALU.mult), [k0], [kC])
    fw.op(V, ts(LIn, LI, -1.0, ALU.mult), [kC], [kC])
    fw.op(V, cp(LAk[:, :, 0, :], LR), [kC], [kC])
    fw.op(V, cp(LAk[:, :, 1, :], LR), [kC], [kC])
    SH4 = [128, 16, 16, 16]
    Gre = sb(SH4, F32)
    Gim = sb(SH4, F32)
    CAre = sb(SH4, F32)
    CAim = sb(SH4, F32)
    CBim = sb(SH4, F32)
    u1_off = sb.o
    U1 = sb(SH4, F32)
    U2 = sb(SH4, F32)
    U3, U4 = U1, U2
    CTst = reg(u1_off, [128, 16, 2, 256], BF16)
    kG, kCA, kCB, kU1, kU2, kCT = [Tk() for _ in range(6)]
    kU3, kU4 = kU1, kU2

    def outer(eng, o, a, pn, rd, wr):
        fw.op(eng, tt(o, bc(a, 2, SH4), bc(pn, 3, SH4), ALU.mult), rd, wr)

    def fl(ap):
        return ap.rearrange("p a b c -> p (a b c)")
    outer(V, U1, bbre, Qre[:, :, 0:16], [k0], [kU1])
    outer(G_, U2, bbim, Qim[:, :, 0:16], [k0], [kU2])
    fw.op(V, tt(Gre, U1, U2, ALU.subtract), [kU1, kU2], [kG])
    outer(V, U3, bbre, Qim[:, :, 0:16], [k0], [kU3])
    outer(G_, U4, bbim, Qre[:, :, 0:16], [k0], [kU4])
    fw.op(V, tt(Gim, U3, U4, ALU.add), [kU3, kU4], [kG])
    outer(V, U1, cre, Pre[:, :, 0:16], [k0, kS], [kU1])
    outer(G_, U2, cim, Pim[:, :, 0:16], [k0, kS], [kU2])
    fw.op(V, tt(CAre, U1, U2, ALU.subtract), [kU1, kU2], [kCA])
    outer(V, U3, cre, Pim[:, :, 0:16], [k0, kS], [kU3])
    outer(G_, U4, cim, Pre[:, :, 0:16], [k0, kS], [kU4])
    fw.op(V, stt(fl(CAim), fl(U3), -1.0, fl(U4), ALU.mult, ALU.subtract), [kU3, kU4], [kCA])
    outer(V, U1, cre, Pre[:, :, 16:32], [k0, kS], [kU1])
    outer(G_, U2, cim, Pim[:, :, 16:32], [k0, kS], [kU2])
    fw.op(V, tt(CBre, U1, U2, ALU.subtract), [kU1, kU2], [kCB])
    outer(V, U3, cre, Pim[:, :, 16:32], [k0, kS], [kU3])
    outer(G_, U4, cim, Pre[:, :, 16:32], [k0, kS], [kU4])
    fw.op(V, stt(fl(CBim), fl(U3), -1.0, fl(U4), ALU.mult, ALU.subtract), [kU3, kU4], [kCB])
    fw.op(V, cp(CTst[:, :, 0, :], CBre.rearrange("p g t h -> p g (t h)")), [kCB], [kCT, kU1])
    fw.op(V, cp(CTst[:, :, 1, :], CBim.rearrange("p g t h -> p g (t h)")), [kCB], [kCT, kU1])
    sem_t = fw.newsem("sem_t")
    kDR = Tk()
    fw.dma(SY, CT_d.rearrange("g p r c -> p g r c"), CTst, sem_t, [kCT], [kDR])
    TPst = [sb([128, 2, 256], BF16) for _ in range(2)]
    GTst = [sb([128, 2, 2, 128], BF16) for _ in range(2)]
    TPt = [sb([128, 2, 256], F32) for _ in range(2)]
    TPd = [sb([128, 2, 256], F32) for _ in range(2)]
    kTP = [Tk() for _ in range(2)]
    kGTs = [Tk() for _ in range(2)]
    kTt = [Tk() for _ in range(2)]
    sem_tp = [fw.newsem("sem_tp%d" % i) for i in range(2)]
    sem_gt = [fw.newsem("sem_gt%d" % i) for i in range(2)]
    for gh in range(2):
        pr = slice(64 * gh, 64 * gh + 64)
        for gl in range(16):
            g = 16 * gh + gl
            i = g % 2
            pT, pG = 2 * i, 2 * i + 1
            pt_v = pbanks[pT][:, :].rearrange("p (j c) -> p j c", j=2)
            fns = []
            for j in range(2):
                l_re = Gre[pr, gl, 8 * j:8 * j + 8, :].rearrange("p s h -> p (s h)")
                l_im = Gim[pr, gl, 8 * j:8 * j + 8, :].rearrange("p s h -> p (s h)")
                fns.append(mmf(pt_v[:, j, :], l_re, CAre[pr, gl].rearrange("p t h -> p (t h)"), True, False))
                fns.append(mmf(pt_v[:, j, :], l_im, CAim[pr, gl].rearrange("p t h -> p (t h)"), False, True))
            fw.mm(fns, [kG, kCA], [pk[pT]])
            pg_v = pbanks[pG][:, :].rearrange("p (j r c) -> p j r c", j=2, r=2)
            fns = []
            for j in range(2):
                l_re = Gre[pr, gl, 8 * j:8 * j + 8, :].rearrange("p s h -> p (s h)")
                l_im = Gim[pr, gl, 8 * j:8 * j + 8, :].rearrange("p s h -> p (s h)")
                fns.append(mmf(pg_v[:, j, 0, :], l_re, idf[pr, :], True, True))
                fns.append(mmf(pg_v[:, j, 1, :], l_im, idf[pr, :], True, True))
            fw.mm(fns, [kG, kC], [pk[pG]])
            fw.op(V, tt(TPt[i], pt_v, tmk, ALU.mult), [pk[pT], kS], [kTt[i]])
            fw.op(G_, tt(TPd[i].rearrange("p j (t h) -> p j t h", t=16),
                         dmk.rearrange("p j (t h) -> p j t h", t=16),
                         drp[:, g, :].unsqueeze(1).unsqueeze(1).broadcast_to([128, 2, 16, 16]), ALU.mult),
                  [kS], [kTP[i]])
            fw.op(V, tt(TPst[i], TPt[i], TPd[i], ALU.add), [kTt[i], kTP[i]], [kTP[i]])
            fw.dma(SY, TOEP_d[g], TPst[i], sem_tp[i], [kTP[i]], [kDR])
            fw.op(A, act(GTst[i], pg_v, AF.Copy), [pk[pG]], [kGTs[i]])
            fw.dma(SY, GT_d[g], GTst[i], sem_gt[i], [kGTs[i]], [kDR])
    fw.barrier()

    X1 = reg(20 * K, [128, 16, 1024], F32)
    attT = reg(84 * K, [128, 4, S], BF16)
    ssmT = reg(100 * K, [128, 4, 16, 128], BF16)
    Uc = reg(116 * K, [128, 32, 16, 16], BF16)
    qT = reg(132 * K, [128, 4, S], BF16)
    kT = reg(148 * K, [128, 4, S], BF16)
    Vt = reg(164 * K, [128, 16, 8, 65], BF16)
    sem_x = [fw.newsem("sem_x%d" % i) for i in range(4)]
    sem_w = [fw.newsem("sem_w%d" % i) for i in range(2)]
    sem_o = fw.newsem("sem_o")
    sem_og = fw.newsem("sem_og")
    sem_wq = [fw.newsem("sem_wq%d" % i) for i in range(2)]
    sem_d = fw.newsem("sem_d")
    sem_g = [fw.newsem("sem_g%d" % i) for i in range(2)]
    sem_dn = fw.newsem("sem_dn")
    sem_st = [fw.newsem("sem_st%d" % i) for i in range(2)]
    kOut = Tk()

    try:
      if STAGE < 1:
        raise _Stop()
      for b in range(2):
        a1 = Bump(20 * K, 116 * K)
        Xst = [a1([128, 2, 1024], F32) for _ in range(2)]
        hbf = [a1([128, 2, 1024], BF16) for _ in range(2)]
        hT = a1([128, 8, S], BF16)
        wbuf = [a1([128, 8, 256], BF16) for _ in range(2)]
        junk = a1([128, 1024], F32)
        kX = [Tk(), Tk()]
        kH = [Tk(), Tk()]
        khT = [Tk() for _ in range(8)]
        kW = [Tk(), Tk()]
        kJ = Tk()
        kSt = Tk()
        kUc, kq, kk, kv = Tk(), Tk(), Tk(), Tk()
        xv = x[b].rearrange("(c t) d -> c t d", t=16)
        ss = stat[:, 0:16]
        rs = stat[:, 16:32]
        for tp in range(8):
            i = tp % 2
            fw.dma(SY, Xst[i], xv[:, 2 * tp:2 * tp + 2, :], sem_x[i], writes=[kX[i]])
            for u in range(2):
                t = 2 * tp + u
                fw.op(A, ttr(junk, Xst[i][:, u, :], Xst[i][:, u, :], ss[:, t:t + 1]), [kX[i]], [kJ, kSt])
                fw.op(V, ts(rs[:, t:t + 1], ss[:, t:t + 1], 1.0 / D, ALU.mult, EPS, ALU.add), [kSt], [kSt])
                fw.op(A, act(rs[:, t:t + 1], rs[:, t:t + 1], AF.Ln), [kSt], [kSt])
                fw.op(A, act(rs[:, t:t + 1], rs[:, t:t + 1], AF.Exp, scale=-0.5), [kSt], [kSt])
                fw.op(A, act(hbf[i][:, u, :], Xst[i][:, u, :], AF.Copy, scale=rs[:, t:t + 1]), [kX[i], kSt], [kH[i]])
            for k in range(8):
                pi = k % 4
                pv = pbanks[pi][:, 0:128].bitcast(BF16).rearrange("p (u c) -> p u c", u=2)
                fns = [trf(pv[:, u, :], hbf[i][:, u, k * 128:(k + 1) * 128], idb) for u in range(2)]
                fw.mm(fns, [kH[i], kC], [pk[pi]])
                dst = hT[:, k, :].rearrange("p (c t) -> p t c", t=16)[:, 2 * tp:2 * tp + 2, :]
                if k % 2 == 0:
                    fw.op(V, ts(dst, pv, g1[:, k:k + 1], ALU.mult), [pk[pi], kC], [khT[k]])
                else:
                    fw.op(A, act(dst, pv, AF.Copy, scale=g1[:, k:k + 1]), [pk[pi], kC], [khT[k]])
        win_v = w_in.rearrange("(k p) n -> p k n", p=128)
        pcount = 0
        for piece in range(8):
            i = piece % 2
            fw.dma(G_, wbuf[i], win_v[:, :, piece * 256:(piece + 1) * 256], sem_wq[i], writes=[kW[i]])
            if piece < 2:
                for t in range(16):
                    pi = 4 + (pcount % 4)
                    pcount += 1
                    pv = pbanks[pi][:, 0:256]
                    fns = [mmf(pv, hT[:, k, t::16], wbuf[i][:, k, :], k == 0, k == 7) for k in range(8)]
                    fw.mm(fns, khT + [kW[i]], [pk[pi]])
                    dst = Uc[:, 16 * piece:16 * piece + 16, t, :]
                    srcv = pv.rearrange("p (g h) -> p g h", g=16)
                    if t % 2 == 0:
                        fw.op(V, cp(dst, srcv), [pk[pi]], [kUc])
                    else:
                        fw.op(A, act(dst, srcv, AF.Copy), [pk[pi]], [kUc])
            elif piece < 6:
                dstT, kd, sc = (qT, kq, 0.125) if piece < 4 else (kT, kk, 1.0)
                for mm_ in range(2):
                    m = (piece % 2) * 2 + mm_
                    for n in range(4):
                        pi = 4 + (pcount % 4)
                        pcount += 1
                        pv = pbanks[pi][:, :]
                        fns = [mmf(pv, wbuf[i][:, k, mm_ * 128:(mm_ + 1) * 128], hT[:, k, n * 512:(n + 1) * 512],
                                   k == 0, k == 7) for k in range(8)]
                        fw.mm(fns, khT + [kW[i]], [pk[pi]])
                        dst = dstT[:, m, n * 512:(n + 1) * 512]
                        if n % 2 == 0:
                            fw.op(V, ts(dst, pv, sc, ALU.mult), [pk[pi]], [kd])
                        else:
                            fw.op(A, act(dst, pv, AF.Copy, scale=sc), [pk[pi]], [kd])
            else:
                hh = piece - 6
                for it in range(16):
                    pi = 4 + (pcount % 4)
                    pcount += 1
                    pv = pbanks[pi][:, 0:256]
                    fns = [mmf(pv, hT[:, k, it * 128:(it + 1) * 128], wbuf[i][:, k, :], k == 0, k == 7)
                           for k in range(8)]
                    fw.mm(fns, khT + [kW[i]], [pk[pi]])
                    dst = Vt[:, it, 4 * hh:4 * hh + 4, 0:64]
                    src = pv.rearrange("p (h d) -> p h d", h=4)
                    if it % 2 == 0:
                        fw.op(V, cp(dst, src), [pk[pi]], [kv])
                    else:
                        fw.op(A, act(dst, src, AF.Copy), [pk[pi]], [kv])
        fw.op(G_, lambda E: E.memset(Vt[:, :, :, 64:65], 1.0), [kv], [kv])
        if dbg and b == 0:
            fw.dma(SY, dbg_t['uc'], Uc, sem_d, [kUc], [kOut])
            fw.dma(SY, dbg_t['qT'], qT, sem_d, [kq], [kOut])
            fw.dma(SY, dbg_t['vt'], Vt, sem_d, [kv], [kOut])
        fw.barrier()

        if STAGE < 2:
            raise _Stop()
        a2 = Bump(181 * K, ARENA)
        Sbf = a2([128, 2, 16, 130], BF16)
        a2b = Bump(20 * K, 84 * K)
        UT = a2b([128, 32, 2, 128], BF16)
        Wf = a2b([128, 2, 16, 128], F32)
        GTb = [a2b([128, 2, 2, 2, 128], BF16) for _ in range(2)]
        T1 = a2b([128, 2, 16], F32)
        T2 = a2b([128, 2, 16], F32)
        kUT, kWf, kSb, kT1, kT2 = Tk(), Tk(), Tk(), Tk(), Tk()
        kGTb = [Tk(), Tk()]
        for g in range(32):
            pi = g % 4
            pv = pbanks[pi][:, 0:128].bitcast(BF16).rearrange("p (j c) -> p j c", j=2)
            fns = [trf(pv[:, j, :], Uc[:, g, 8 * j:8 * j + 8, :].rearrange("p s h -> p (s h)"), idb) for j in range(2)]
            fw.mm(fns, [kUc, kC], [pk[pi]])
            if g % 2 == 0:
                fw.op(V, cp(UT[:, g], pv), [pk[pi]], [kUT])
            else:
                fw.op(A, act(UT[:, g], pv, AF.Copy), [pk[pi]], [kUT])
        if STAGE < 2.2:
            raise _Stop()
        for gl in range(16):
            i = gl % 2
            for gh in range(2):
                fw.dma(SY, GTb[i][:, gh], GT_d[16 * gh + gl], sem_w[i], writes=[kGTb[i]])
            pi = 4 + gl % 4
            pv = pbanks[pi][:, 0:256].rearrange("p (r c) -> p r c", r=2)
            fns = []
            for r in range(2):
                n = 0
                for gh in range(2):
                    for j in range(2):
                        fns.append(mmf(pv[:, r, :], GTb[i][:, gh, j, r, :], UT[:, 16 * gh + gl, j, :], n == 0, n == 3))
                        n += 1
            fw.mm(fns, [kUT, kGTb[i]], [pk[pi]])
            fw.op(V, cp(Wf[:, :, gl, :], pv), [pk[pi]], [kWf])
        if STAGE < 2.5:
            raise _Stop()
        WfB = reg(84 * K, [128, 2, 16, 128], F32)
        TT = reg(100 * K, [128, 2, 16, 128], F32)
        kWB, kTT = Tk(), Tk()
        cur, nxt, kcur, knxt = Wf, WfB, kWf, kWB
        for k, d in enumerate([1, 2, 4, 8, 16, 32, 64]):
            n = 128 - d
            fw.op(V, tt(TT[:, :, :, 0:n], cur[:, :, :, 0:n], bc(LAk[:, k], 3, [128, 2, 16, n]), ALU.mult),
                  [kcur, kC], [kTT])
            fw.op(V, tt(nxt[:, :, :, d:128], cur[:, :, :, d:128], TT[:, :, :, 0:n], ALU.add), [kcur, kTT], [knxt])
            fw.op(V, tt(TT[:, 0, :, 0:n], cur[:, 1, :, 0:n], bc(LIn[:, k, :], 2, [128, 16, n]), ALU.mult),
                  [kcur, kC], [kTT])
            fw.op(V, tt(TT[:, 1, :, 0:n], cur[:, 0, :, 0:n], bc(LI[:, k, :], 2, [128, 16, n]), ALU.mult),
                  [kcur, kC], [kTT])
            fw.op(V, tt(nxt[:, :, :, d:128], nxt[:, :, :, d:128], TT[:, :, :, 0:n], ALU.add), [knxt, kTT], [knxt])
            fw.op(V, cp(nxt[:, :, :, 0:d], cur[:, :, :, 0:d]), [kcur], [knxt])
            cur, nxt, kcur, knxt = nxt, cur, knxt, kcur
        Wf, kWf = cur, kcur
        if STAGE < 2.8:
            raise _Stop()
        fw.op(V, lambda E: E.memset(Sbf[:, :, :, 0:2], 0.0), [], [kSb])
        if STAGE < 2.9:
            raise _Stop()
        for r in range(2):
            fw.op(A, act(Sbf[:, r, :, 2:130], Wf[:, r], AF.Copy), [kWf], [kSb])
        if dbg and b == 0:
            fw.dma(SY, dbg_t['S'], Wf, sem_d, [kWf], [kOut])
        fw.barrier()

        if STAGE < 3:
            raise _Stop()
        a3 = Bump(52 * K, 84 * K)
        MBT = a3([128, S], BF16)
        kmT = a3([128, 4, 8], BF16)
        kmf = a3([128, 4, 8], F32)
        gt = a3([128, 8, 8], F32)
        top8 = a3([128, 8, 8], F32)
        mbt = a3([128, 2, 64], BF16)
        PT = [a3([128, 256], BF16) for _ in range(3)]
        Ao = a3([128, 2, 512], F32)
        Aj = a3([128, 512], F32)
        Ab = a3([128, 2, 512], BF16)
        rden_a = a3([128, 4], F32)
        kMB, kkm, kgt, kmb = Tk(), Tk(), Tk(), Tk()
        Esel = reg(100 * K, [128, 64, 128], BF16)
        kEs = Tk()
        fw.op(V, cp(Esel[0:64], bc(idb[0:64, 0:64], 2, [64, 64, 128])), [kC], [kEs])
        fw.op(V, cp(Esel[64:128], bc(idb[64:128, 64:128], 2, [64, 64, 128])), [kC], [kEs])
        kPT = [Tk() for _ in range(3)]
        kAo, kAj, kAb, kat, krd = Tk(), Tk(), Tk(), Tk(), Tk()
        if STAGE < 3.05:
            raise _Stop()
        kjunk = a3([128, 256], BF16)
        kkj = Tk()
        for m in range(4):
            for n in range(8):
                fw.op(A, (lambda m=m, n=n: (lambda E: E.activation(out=kjunk, in_=kT[:, m, n * 256:(n + 1) * 256],
                                                                   func=AF.Copy, accum_out=kmf[:, m, n:n + 1])))(),
                      [kk], [kkj, kkm])
        if STAGE < 3.08:
            raise _Stop()
        fw.op(A, act(kmT, kmf, AF.Copy, scale=1.0 / 256), [kkm], [kkm])
        if STAGE < 3.1:
            raise _Stop()
        fw.op(G_, lambda E: E.memset(MBT, 0.0), [], [kMB])
        for qt in range(8, 16):
            bq = qt // 2
            pve = pbanks[0][:, 0:32].rearrange("p (h n) -> p h n", h=4)
            pvo = pbanks[1][:, 0:32].rearrange("p (h n) -> p h n", h=4)
            fe = [mmf(pve[:, h2, :], qT[0:64, h2, qt * 128:(qt + 1) * 128], kmT[0:64, h2, :], True, True)
                  for h2 in range(4)]
            fw.mm(fe, [kq, kkm], [pk[0]])
            fo = [mmf(pvo[:, h2, :], qT[64:128, h2, qt * 128:(qt + 1) * 128], kmT[64:128, h2, :], True, True)
                  for h2 in range(4)]
            fw.mm(fo, [kq, kkm], [pk[1]])
            fw.op(G_, lambda E: E.memset(gt, NEG), [kgt], [kgt])
            gtv = gt.rearrange("p (h2 e) n -> p h2 e n", e=2)
            fw.op(V, cp(gtv[:, :, 0, 0:bq], pve[:, :, 0:bq]), [pk[0], kgt], [kgt])
            fw.op(V, cp(gtv[:, :, 1, 0:bq], pvo[:, :, 0:bq]), [pk[1], kgt], [kgt])
            for h in range(8):
                fw.op(V, (lambda hh: (lambda E: E.max(out=top8[:, hh, :], in_=gt[:, hh, :])))(h), [kgt], [kmb])
            for dup in range(2):
                fw.op(V, tt(mbt[:, dup, :].rearrange("p (h n) -> p h n", h=8), gt,
                            top8[:, :, 2:3].broadcast_to([128, 8, 8]), ALU.is_lt), [kgt, kmb], [kmb])
            pi2 = 2 + qt % 2
            pv2 = pbanks[pi2][:, 0:64].bitcast(BF16)
            fw.mm([trf(pv2, mbt.rearrange("p d c -> p (d c)"), idb)], [kmb, kC], [pk[pi2]])
            fw.op(V, ts(MBT[:, qt * 128:(qt + 1) * 128], pv2, NEG, ALU.mult), [pk[pi2]], [kMB])
        if STAGE < 3.2:
            raise _Stop()
        ucount = 0
        for bq in range(ABQ):
            for h in range(8):
                pr = slice(64 * (h % 2), 64 * (h % 2) + 64)
                m = h // 2
                pos = [4 + 2 * (h % 2), 5 + 2 * (h % 2)]
                povs = [pbanks[pos[0]][:, 0:65], pbanks[pos[1]][:, 0:65]]
                nkt = 2 * bq + 2
                for kt in range(nkt):
                    n = kt // 2
                    pi = ucount % 3
                    ucount += 1
                    pv = pbanks[pi][:, 0:256]
                    fns = [mmf(pv, kT[pr, m, kt * 128:(kt + 1) * 128], qT[pr, m, bq * 256:(bq + 1) * 256], True, False)]
                    rd = [kq, kk]
                    if n == bq:
                        fns.append(mmf(pv, idb, cbias[:, kt - 2 * bq, :], False, True))
                        rd.append(kC)
                    elif bq >= 4:
                        r = h * 8 + n
                        fns.append(mmf(pv, Esel[pr, r, :], MBT[pr, bq * 256:(bq + 1) * 256],
                                       False, True))
                        rd += [kEs, kMB]
                    else:
                        fns[0] = mmf(pv, kT[pr, m, kt * 128:(kt + 1) * 128], qT[pr, m, bq * 256:(bq + 1) * 256], True, True)
                    fw.mm(fns, rd, [pk[pi]])
                    fw.op(A, act(PT[pi], pv, AF.Exp), [pk[pi]], [kPT[pi]])
                    fns = [mmf(povs[u], PT[pi][:, u * 128:(u + 1) * 128], Vt[:, kt, h, :], kt == 0, kt == nkt - 1)
                           for u in range(2)]
                    fw.mm(fns, [kPT[pi], kv], [pk[pos[0]], pk[pos[1]]])
                for u in range(2):
                    fw.op(V, rcp(rden_a[:, u:u + 1], povs[u][:, 64:65]), [pk[pos[u]]], [krd])
                    fw.op(V, ts(Ao[:, u, h * 64:(h + 1) * 64], povs[u][:, 0:64], rden_a[:, u:u + 1], ALU.mult),
                          [pk[pos[u]], krd], [kAo])
            for u in range(2):
                fw.op(A, ttr(Aj, Ao[:, u, :], Ao[:, u, :], rden_a[:, 2:3]), [kAo], [kAj, krd])
                fw.op(V, ts(rden_a[:, 3:4], rden_a[:, 2:3], 1.0 / 512, ALU.mult, EPS, ALU.add), [krd], [krd])
                fw.op(A, act(rden_a[:, 3:4], rden_a[:, 3:4], AF.Ln), [krd], [krd])
                fw.op(A, act(rden_a[:, 3:4], rden_a[:, 3:4], AF.Exp, scale=-0.5), [krd], [krd])
                fw.op(V, stt(Ab[:, u, :], Ao[:, u, :], rden_a[:, 3:4], ga, ALU.mult, ALU.mult), [kAo, krd, kC], [kAb])
            for u in range(2):
                pi = 3
                pv = pbanks[pi][:, 0:256].bitcast(BF16).rearrange("p (f c) -> p f c", f=4)
                fns = [trf(pv[:, f, :], Ab[:, u, f * 128:(f + 1) * 128], idb) for f in range(4)]
                fw.mm(fns, [kAb, kC], [pk[pi]])
                tok0 = bq * 256 + u * 128
                fw.op(A, act(attT[:, :, tok0:tok0 + 128], pv, AF.Copy), [pk[pi]], [kat])
        if dbg and b == 0:
            fw.dma(SY, dbg_t['attT'], attT, sem_d, [kat], [kOut])
        fw.barrier()

        if STAGE < 4:
            raise _Stop()
        a4 = Bump(132 * K, 181 * K)
        Y1 = a4([128, 16, 512], BF16)
        TCb = [(a4([128, 2, 2, 256], BF16), a4([128, 2, 256], BF16)) for _ in range(2)]
        wgl = a4([128, 4, 512], BF16)
        Y1T = [a4([128, 4, 128], BF16) for _ in range(2)]
        zt = a4([128, 512], F32)
        zs = a4([128, 512], F32)
        y2 = a4([128, 512], F32)
        y2b = a4([128, 512], BF16)
        yq = a4([128, 256], F32)
        yc = a4([128, 256], F32)
        kY1, kwg, kzt, kzs, ky2, ky2b, kss, kyq, kyc, kssm = [Tk() for _ in range(10)]
        kTC = [Tk(), Tk()]
        kY1T = [Tk(), Tk()]
        fw.dma(G_, wgl, w_glu.rearrange("(k p) n -> p k n", p=128), sem_og, writes=[kwg])
        C1 = 0.7978845608028654 * 2.0
        for gl in range(16):
            i = gl % 2
            for gh in range(2):
                fw.dma(SY, TCb[i][0][:, gh], TOEP_d[16 * gh + gl], sem_w[i], writes=[kTC[i]])
            fw.dma(SY, TCb[i][1], CT_d[gl], sem_w[i], writes=[kTC[i]])
            for gh in range(2):
                g = 16 * gh + gl
                pr = slice(64 * gh, 64 * gh + 64)
                pi = (2 * gl + gh) % 4
                pv = pbanks[pi][:, 0:256]
                fns = [mmf(pv, UT[:, g, 0, :], TCb[i][0][:, gh, 0, :], True, False),
                       mmf(pv, UT[:, g, 1, :], TCb[i][0][:, gh, 1, :], False, False),
                       mmf(pv, Sbf[pr, 0, gl, 1:129], TCb[i][1][pr, 0, :], False, False),
                       mmf(pv, Sbf[pr, 1, gl, 1:129], TCb[i][1][pr, 1, :], False, True)]
                fw.mm(fns, [kUT, kSb, kTC[i]], [pk[pi]])
                fw.op(V, cp(yc, pv), [pk[pi]], [kyc])
                fw.op(V, tt(yq, yc, yc, ALU.mult), [kyc], [kyq])
                fw.op(V, ts(yq, yq, 0.044715, ALU.mult, 1.0, ALU.add), [kyq], [kyq])
                fw.op(V, tt(yq, yq, yc, ALU.mult), [kyq, kyc], [kyq])
                fw.op(V, ts(yq, yq, -40.0, ALU.max), [kyq], [kyq])
                fw.op(A, act(yq, yq, AF.Exp, scale=-C1), [kyq], [kyq])
                fw.op(V, ts(yq, yq, 1.0, ALU.add), [kyq], [kyq])
                fw.op(V, rcp(yq, yq), [kyq], [kyq])
                fw.op(V, tt(Y1[:, :, 16 * g:16 * g + 16], yq.rearrange("p (t h) -> p t h", t=16),
                            yc.rearrange("p (t h) -> p t h", t=16), ALU.mult), [kyq, kyc], [kY1])
        if dbg and b == 0:
            fw.dma(SY, dbg_t['y1'], Y1, sem_d, [kY1], [kOut])
        for t in range(16):
            i = t % 2
            pi = 4 + i
            pv = pbanks[pi][:, 0:256].bitcast(BF16).rearrange("p (f c) -> p f c", f=4)
            fns = [trf(pv[:, f, :], Y1[:, t, f * 128:(f + 1) * 128], idb) for f in range(4)]
            fw.mm(fns, [kY1, kC], [pk[pi]])
            fw.op(A, act(Y1T[i], pv, AF.Copy), [pk[pi]], [kY1T[i]])
            pz = 6 + i
            pzv = pbanks[pz][:, :]
            fns = [mmf(pzv, Y1T[i][:, f, :], wgl[:, f, :], f == 0, f == 3) for f in range(4)]
            fw.mm(fns, [kY1T[i], kwg], [pk[pz]])
            fw.op(V, tt(zt, pzv, bg, ALU.add), [pk[pz], kC], [kzt])
            fw.op(A, act(zs, zt, AF.Exp, scale=-1.0), [kzt], [kzs])
            fw.op(V, ts(zs, zs, 1.0, ALU.add), [kzs], [kzs])
            fw.op(V, rcp(zs, zs), [kzs], [kzs])
            fw.op(V, tt(y2, zs, Y1[:, t, :], ALU.mult), [kzs, kY1], [ky2])
            fw.op(A, ttr(zt, y2, y2, stat[:, 32:33]), [ky2, kzt], [kzt, kss])
            fw.op(V, ts(stat[:, 33:34], stat[:, 32:33], 1.0 / 512, ALU.mult, EPS, ALU.add), [kss], [kss])
            fw.op(A, act(stat[:, 33:34], stat[:, 33:34], AF.Ln), [kss], [kss])
            fw.op(A, act(stat[:, 33:34], stat[:, 33:34], AF.Exp, scale=-0.5), [kss], [kss])
            fw.op(V, stt(y2b, y2, stat[:, 33:34], gs, ALU.mult, ALU.mult), [ky2, kss, kC], [ky2b])
            pi2 = i
            pv2 = pbanks[pi2][:, 0:256].bitcast(BF16).rearrange("p (f c) -> p f c", f=4)
            fns = [trf(pv2[:, f, :], y2b[:, f * 128:(f + 1) * 128], idb) for f in range(4)]
            fw.mm(fns, [ky2b, kC], [pk[pi2]])
            fw.op(A, act(ssmT[:, :, t, :], pv2, AF.Copy), [pk[pi2]], [kssm])
        if dbg and b == 0:
            fw.dma(SY, dbg_t['ssmT'], ssmT, sem_d, [kssm], [kOut])
        fw.barrier()

        if STAGE < 5:
            raise _Stop()
        a5 = Bump(116 * K, ARENA)
        wo = a5([128, 8, 1024], BF16)
        otmp = a5([128, 1024], F32)
        oj = a5([128, 512], F32)
        kwo, kot, koj, kst4 = Tk(), Tk(), Tk(), Tk()
        kX1 = [Tk() for _ in range(16)]
        fw.dma(G_, wo, w_out.rearrange("(k p) n -> p k n", p=128), sem_og, writes=[kwo])
        for q4 in range(4):
            fw.dma(SY, X1[:, 4 * q4:4 * q4 + 4, :], xv[:, 4 * q4:4 * q4 + 4, :], sem_x[q4],
                   writes=[kX1[4 * q4 + j] for j in range(4)])
        for t in range(16):
            pis = [2 * (t % 2), 2 * (t % 2) + 1]
            for hf in range(2):
                pv = pbanks[pis[hf]][:, :]
                fns = []
                for k in range(8):
                    lhs = ssmT[:, k, t, :] if k < 4 else attT[:, k - 4, t::16]
                    fns.append(mmf(pv, lhs, wo[:, k, hf * 512:(hf + 1) * 512], k == 0, k == 7))
                fw.mm(fns, [kssm, kat, kwo], [pk[pis[hf]]])
                fw.op(A, sqa(oj, pv, stat[:, 40 + hf:41 + hf]), [pk[pis[hf]]], [koj, kst4])
            fw.op(V, tt(stat[:, 42:43], stat[:, 40:41], stat[:, 41:42], ALU.add), [kst4], [kst4])
            fw.op(V, ts(stat[:, 42:43], stat[:, 42:43], 1.0 / D, ALU.mult, EPS, ALU.add), [kst4], [kst4])
            fw.op(A, act(stat[:, 42:43], stat[:, 42:43], AF.Ln), [kst4], [kst4])
            fw.op(A, act(stat[:, 42:43], stat[:, 42:43], AF.Exp, scale=-0.5), [kst4], [kst4])
            for hf in range(2):
                pv = pbanks[pis[hf]][:, :]
                sl = slice(hf * 512, (hf + 1) * 512)
                fw.op(V, stt(otmp[:, sl], pv, stat[:, 42:43], g2[:, sl], ALU.mult, ALU.mult),
                      [pk[pis[hf]], kst4, kC], [kot])
            fw.op(G_, tt(X1[:, t, :], X1[:, t, :], otmp, ALU.add), [kot, kX1[t]], [kX1[t]])
        if dbg and b == 0:
            fw.dma(SY, dbg_t['x1'], X1, sem_d, kX1, [kOut])
        fw.barrier()

        if STAGE < 6:
            raise _Stop()
        f0 = Bump(86 * K, ARENA)
        h2T = f0([128, 8, 16, 128], BF16)
        fT = f0([128, NFF, 16, 128], BF16)
        f1 = Bump(84 * K, 86 * K)
        hb2 = f1([128, 1024], BF16)
        khb, kfj, kh2T, kfT, kst5, kot2 = Tk(), Tk(), Tk(), Tk(), Tk(), Tk()
        ov = out[b].rearrange("(c t) d -> c t d", t=16)
        fj1 = reg(118 * K, [128, 1024], F32)
        for t in range(16):
            fw.op(A, ttr(fj1, X1[:, t, :], X1[:, t, :], stat[:, 44:45]), [kX1[t]], [kfj, kst5])
            fw.op(V, ts(stat[:, 45:46], stat[:, 44:45], 1.0 / D, ALU.mult, EPS, ALU.add), [kst5], [kst5])
            fw.op(A, act(stat[:, 45:46], stat[:, 45:46], AF.Ln), [kst5], [kst5])
            fw.op(A, act(stat[:, 45:46], stat[:, 45:46], AF.Exp, scale=-0.5), [kst5], [kst5])
            fw.op(A, act(hb2, X1[:, t, :], AF.Copy, scale=stat[:, 45:46]), [kX1[t], kst5], [khb])
            for kk_ in range(2):
                pi = kk_ + 2 * (t % 2)
                pv = pbanks[pi][:, 0:256].bitcast(BF16).rearrange("p (f c) -> p f c", f=4)
                fns = [trf(pv[:, f, :], hb2[:, (4 * kk_ + f) * 128:(4 * kk_ + f + 1) * 128], idb) for f in range(4)]
                fw.mm(fns, [khb, kC], [pk[pi]])
                for f in range(4):
                    k = 4 * kk_ + f
                    if f % 2 == 0:
                        fw.op(V, ts(h2T[:, k, t, :], pv[:, f, :], g3[:, k:k + 1], ALU.mult), [pk[pi], kC], [kh2T])
                    else:
                        fw.op(A, act(h2T[:, k, t, :], pv[:, f, :], AF.Copy, scale=g3[:, k:k + 1]),
                              [pk[pi], kC], [kh2T])
        kO = Tk()
        fw.dma(SY, ov, X1, sem_o, kX1, [kO])
        fw.barrier()
        f2 = Bump(64 * K, 86 * K)
        wg = [f2([128, 8, 128], BF16) for _ in range(2)]
        wu = [f2([128, 8, 128], BF16) for _ in range(2)]
        sg = [f2([128, 512], F32) for _ in range(2)]
        wd = reg(20 * K, [128, NFF, 1024], BF16)
        kwg = [Tk(), Tk()]
        kwd = Tk()
        ksg = [Tk(), Tk()]
        wg_v = w_gate.rearrange("(k p) n -> p k n", p=128)
        wu_v = w_up.rearrange("(k p) n -> p k n", p=128)
        wd_v = w_down.rearrange("(j p) n -> p j n", p=128)
        hv = h2T.rearrange("p k t c -> p k (t c)")
        pc = 0
        for j in range(NFF):
            i = j % 2
            fw.dma(G_, wg[i], wg_v[:, :, j * 128:(j + 1) * 128], sem_g[i], writes=[kwg[i]])
            fw.dma(G_, wu[i], wu_v[:, :, j * 128:(j + 1) * 128], sem_g[i], writes=[kwg[i]])
            fw.dma(G_, wd[:, j, :], wd_v[:, j, :], sem_dn, writes=[kwd])
            for n in range(4):
                pg_, pu_ = 2 * (pc % 4), 2 * (pc % 4) + 1
                si = pc % 2
                pc += 1
                pgv, puv = pbanks[pg_][:, :], pbanks[pu_][:, :]
                fns = [mmf(pgv, wg[i][:, k, :], hv[:, k, n * 512:(n + 1) * 512], k == 0, k == 7) for k in range(8)]
                fw.mm(fns, [kh2T, kwg[i]], [pk[pg_]])
                fns = [mmf(puv, wu[i][:, k, :], hv[:, k, n * 512:(n + 1) * 512], k == 0, k == 7) for k in range(8)]
                fw.mm(fns, [kh2T, kwg[i]], [pk[pu_]])
                fw.op(A, act(sg[si], pgv, AF.Exp, scale=-1.0), [pk[pg_]], [ksg[si]])
                fw.op(V, ts(sg[si], sg[si], 1.0, ALU.add), [ksg[si]], [ksg[si]])
                fw.op(V, rcp(sg[si], sg[si]), [ksg[si]], [ksg[si]])
                fw.op(V, tt(sg[si], sg[si], pgv, ALU.mult), [ksg[si], pk[pg_]], [ksg[si]])
                fw.op(V, tt(fT[:, j].rearrange("p t c -> p (t c)")[:, n * 512:(n + 1) * 512], sg[si], puv, ALU.mult),
                      [ksg[si], pk[pu_]], [kfT])
        fw.barrier()
        f3 = Bump(64 * K, 86 * K)
        xr = [f3([128, 1024], F32) for _ in range(2)]
        ot2 = f3([128, 1024], F32)
        fj3 = f3([128, 512], F32)
        kxr = [Tk(), Tk()]
        for tg in range(4):
            for tl in range(4):
                t = 4 * tg + tl
                if tl < 2:
                    pass
            for j in range(NFF):
                fns = []
                for tl in range(4):
                    t = 4 * tg + tl
                    for hf in range(2):
                        fns.append(mmf(pbanks[2 * tl + hf][:, :], fT[:, j, t, :], wd[:, j, hf * 512:(hf + 1) * 512],
                                       j == 0, j == NFF - 1))
                fw.mm(fns, [kfT, kwd], pk)
            for tl in range(4):
                t = 4 * tg + tl
                xi = t % 2
                fw.dma(SY, xr[xi], ov[:, t, :], sem_x[xi], [kO], [kxr[xi]])
                for hf in range(2):
                    pv = pbanks[2 * tl + hf][:, :]
                    fw.op(A, sqa(fj3, pv, stat[:, 48 + hf:49 + hf]), [pk[2 * tl + hf]], [kfj, kst5])
                fw.op(V, tt(stat[:, 50:51], stat[:, 48:49], stat[:, 49:50], ALU.add), [kst5], [kst5])
                fw.op(V, ts(stat[:, 50:51], stat[:, 50:51], 1.0 / D, ALU.mult, EPS, ALU.add), [kst5], [kst5])
                fw.op(A, act(stat[:, 50:51], stat[:, 50:51], AF.Ln), [kst5], [kst5])
                fw.op(A, act(stat[:, 50:51], stat[:, 50:51], AF.Exp, scale=-0.5), [kst5], [kst5])
                for hf in range(2):
                    pv = pbanks[2 * tl + hf][:, :]
                    sl = slice(hf * 512, (hf + 1) * 512)
                    fw.op(V, stt(ot2[:, sl], pv, stat[:, 50:51], g4[:, sl], ALU.mult, ALU.mult),
                          [pk[2 * tl + hf], kst5, kC], [kot2])
                fw.op(G_, tt(xr[xi], xr[xi], ot2, ALU.add), [kot2, kxr[xi]], [kxr[xi]])
                fw.dma(SY, ov[:, t, :], xr[xi], sem_st[xi], [kxr[xi]], [kO])
        fw.barrier()
    except _Stop:
        pass
    fw.barrier()
    fw.emit()
    return nc, es


_CACHE = {}


def _consts():
    idf = np.eye(128, dtype=np.float32)
    cb = np.zeros((128, 2, 256), np.float32)
    for r in range(2):
        kpos = r * 128 + np.arange(128)[:, None]
        qpos = np.arange(256)[None, :]
        cb[:, r, :] = np.where(kpos <= qpos, 0.0, NEG)
    tm = np.zeros((128, 2, 256), np.float32)
    dm = np.zeros((128, 2, 256), np.float32)
    for j in range(2):
        for s8 in range(8):
            for hi in range(16):
                sp = 8 * j + s8
                row = s8 * 16 + hi
                for jj in range(2):
                    pass
                for tp in range(16):
                    if tp >= sp:
                        tm[row, j, tp * 16:(tp + 1) * 16] = 1.0
                dm[row, j, sp * 16 + hi] = 1.0
    nv = np.tile(np.arange(1, 33, dtype=np.float32)[None, :], (128, 1))
    return idf, cb, tm, dm, nv


def kernel(**inp):
    f32 = np.float32
    x = np.ascontiguousarray(inp['x'], dtype=f32)

    def sq(k):
        return np.ascontiguousarray(inp[k][0], dtype=f32)

    def gp(a):
        a = a.reshape((2, 16) + a.shape[1:])
        a = np.moveaxis(a, 2, 1)
        return np.ascontiguousarray(a.reshape((128, 16) + a.shape[3:]))
    idf, cb, tm, dm, nv = _consts()
    shared = dict(
        w_in=sq('w_in'), w_glu=sq('w_glu'), w_out=sq('w_out'), w_gate=sq('w_gate'), w_up=sq('w_up'),
        w_down=sq('w_down'),
        areT=gp(sq('ssm_a_re')), aimT=gp(sq('ssm_a_im')),
        ldtT=gp(np.ascontiguousarray(np.broadcast_to(sq('ssm_log_dt')[:, None], (32, 64)))),
        breT=gp(sq('ssm_b_re')), bimT=gp(sq('ssm_b_im')),
        creT=gp(np.ascontiguousarray(sq('ssm_c_re').transpose(0, 2, 1))),
        cimT=gp(np.ascontiguousarray(sq('ssm_c_im').transpose(0, 2, 1))),
        drep=np.ascontiguousarray(np.broadcast_to(sq('ssm_d')[None], (128, 32, 16))),
        g1c=np.ascontiguousarray(sq('g_pre_mix').reshape(8, 128).T),
        g3c=np.ascontiguousarray(sq('g_pre_ffn').reshape(8, 128).T),
        g2r=np.ascontiguousarray(np.broadcast_to(sq('g_post_mix')[None], (128, 1024))),
        g4r=np.ascontiguousarray(np.broadcast_to(sq('g_post_ffn')[None], (128, 1024))),
        gsr=np.ascontiguousarray(np.broadcast_to(sq('g_ssm_out')[None], (128, 512))),
        gar=np.ascontiguousarray(np.broadcast_to(sq('g_attn_out')[None], (128, 512))),
        bgr=np.ascontiguousarray(np.broadcast_to(sq('b_glu')[None], (128, 512))),
        c_idf=idf, c_cb=cb, c_tm=tm, c_dm=dm, c_nv=nv,
    )
    if 'nc' not in _CACHE:
        _CACHE['nc'] = build(DEBUG)
    nc, _es = _CACHE['nc']
    in_maps = []
    for c in range(8):
        m = dict(shared)
        m['x'] = np.ascontiguousarray(x[2 * c:2 * c + 2])
        in_maps.append(m)
    res = run_bass_kernel_spmd(nc, in_maps, core_ids=list(range(8)))
    _CACHE['res'] = res
    return np.concatenate([np.asarray(r['out'], dtype=f32) for r in res.results], axis=0)
```

```python
import math
import numpy as np
from contextlib import ExitStack
import concourse.bass as bass
import concourse.mybir as mybir
from concourse.alu_op_type import AluOpType as ALU
from concourse.bass_utils import run_bass_kernel_spmd

F32 = mybir.dt.float32
BF16 = mybir.dt.bfloat16
U8 = mybir.dt.uint8
I32 = mybir.dt.int32
AF = mybir.ActivationFunctionType
AX = mybir.AxisListType

S = 2048
D = 1024
DFF = 2816
NFF = 22
NEG = -30000.0
EPS = 1e-6
ENGS = ['tensor', 'vector', 'scalar', 'gpsimd', 'sync']
DEBUG = False
STAGE = 99
NSTEP = 127
ABQ = 8


class _Stop(Exception):
    pass


class Tk:
    __slots__ = ('w', 'r')

    def __init__(self):
        self.w = {}
        self.r = {}


class FW:
    def __init__(self, nc, es):
        self.nc, self.es = nc, es
        self.prog = {e: [] for e in ENGS}
        self.esem = {e: es.enter_context(nc.semaphore("es_" + e)) for e in ENGS}
        self.ecnt = {e: 0 for e in ENGS}
        self.seen = {e: {} for e in ENGS}
        self.dcnt = {}

    def newsem(self, name):
        s = self.es.enter_context(self.nc.semaphore(name))
        self.dcnt[id(s)] = [s, 0]
        return s

    def _dep(self, eng, reads, writes):
        need = {}

        def add(dct):
            for k, (s, v) in dct.items():
                if need.get(k, (None, 0))[1] < v:
                    need[k] = (s, v)
        for t in reads:
            add(t.w)
        for t in writes:
            add(t.w)
            add(t.r)
        for k, (s, v) in need.items():
            if self.seen[eng].get(k, 0) >= v:
                continue
            self.seen[eng][k] = v
            self.prog[eng].append(('w', s, v))

    def _post(self, tok, reads, writes):
        s, v = tok
        k = id(s)
        for t in reads:
            if t.r.get(k, (None, 0))[1] < v:
                t.r[k] = (s, v)
        for t in writes:
            t.w = {k: (s, v)}
            t.r = {}

    def op(self, eng, fn, reads=(), writes=()):
        self._dep(eng, reads, writes)
        self.ecnt[eng] += 1
        self.prog[eng].append(('o', fn, True))
        self._post((self.esem[eng], self.ecnt[eng]), reads, writes)

    def mm(self, fns, reads=(), writes=()):
        self._dep('tensor', reads, writes)
        for f in fns[:-1]:
            self.prog['tensor'].append(('o', f, False))
        self.ecnt['tensor'] += 1
        self.prog['tensor'].append(('o', fns[-1], True))
        self._post((self.esem['tensor'], self.ecnt['tensor']), reads, writes)

    def dma(self, eng, out, in_, sem, reads=(), writes=()):
        self._dep(eng, reads, writes)
        c = self.dcnt[id(sem)]
        c[1] += 16
        self.prog[eng].append(('d', out, in_, sem))
        self._post((sem, c[1]), reads, writes)

    def barrier(self):
        for e in ENGS:
            for e2 in ENGS:
                if e2 == e or self.ecnt[e2] == 0:
                    continue
                k = id(self.esem[e2])
                if self.seen[e].get(k, 0) < self.ecnt[e2]:
                    self.seen[e][k] = self.ecnt[e2]
                    self.prog[e].append(('w', self.esem[e2], self.ecnt[e2]))
            for k, (s, v) in self.dcnt.items():
                if v > 0 and self.seen[e].get(k, 0) < v:
                    self.seen[e][k] = v
                    self.prog[e].append(('w', s, v))

    def emit(self):
        nc = self.nc
        with nc.Block() as block:
            for e in ENGS:
                prog = self.prog[e]
                sem = self.esem[e]

                def body(E, prog=prog, sem=sem):
                    for it in prog:
                        if it[0] == 'w':
                            E.wait_ge(it[1], it[2])
                        elif it[0] == 'o':
                            ins = it[1](E)
                            if it[2]:
                                ins.then_inc(sem, 1)
                        else:
                            E.dma_start(out=it[1], in_=it[2]).then_inc(it[3], 16)
                getattr(block, e)(body)


def tt(out, in0, in1, op):
    return lambda E: E.tensor_tensor(out=out, in0=in0, in1=in1, op=op)


def ts(out, in0, s1, op0, s2=None, op1=None):
    if op1 is None:
        return lambda E: E.tensor_scalar(out=out, in0=in0, scalar1=s1, scalar2=None, op0=op0)
    return lambda E: E.tensor_scalar(out=out, in0=in0, scalar1=s1, scalar2=s2, op0=op0, op1=op1)


def stt(out, in0, scalar, in1, op0, op1):
    return lambda E: E.scalar_tensor_tensor(out=out, in0=in0, scalar=scalar, in1=in1, op0=op0, op1=op1)


def ttr(out, in0, in1, accum):
    return lambda E: E.activation(out=out, in_=in0, func=AF.Square, accum_out=accum)


def act(out, in_, func, scale=None):
    if scale is None:
        return lambda E: E.activation(out=out, in_=in_, func=func)
    return lambda E: E.activation(out=out, in_=in_, func=func, scale=scale)


def sqa(out, in_, accum):
    return lambda E: E.activation(out=out, in_=in_, func=AF.Square, accum_out=accum)


def rcp(out, in_):
    return lambda E: E.reciprocal(out=out, in_=in_)


def cp(out, in_):
    return lambda E: E.tensor_copy(out=out, in_=in_)


def mmf(out, lhsT, rhs, start, stop):
    assert len(rhs.ap) == 2 and len(lhsT.ap) == 2, ("rhs", rhs.ap, lhsT.ap)
    return lambda E: E.matmul(out, lhsT, rhs, start=start, stop=stop)


def trf(out, in_, ident):
    assert len(ident.ap) == 2 and len(in_.ap) == 2, ("ident", ident.ap, in_.ap)
    return lambda E: E.transpose(out, in_, ident)


def bc(ap, axis, shape):
    return ap.unsqueeze(axis).broadcast_to(list(shape))


def build(dbg=False):
    nc = bass.Bass("TRN2", target_bir_lowering=False)
    es = ExitStack()

    def din(name, shape):
        return nc.dram_tensor(name, list(shape), F32, kind="ExternalInput").ap()

    x = din("x", [2, S, D])
    w_in = din("w_in", [D, 2048])
    w_glu = din("w_glu", [512, 512])
    w_out = din("w_out", [1024, 1024])
    w_gate = din("w_gate", [D, DFF])
    w_up = din("w_up", [D, DFF])
    w_down = din("w_down", [DFF, D])
    areT = din("areT", [128, 16])
    aimT = din("aimT", [128, 16])
    ldtT = din("ldtT", [128, 16])
    breT = din("breT", [128, 16, 16])
    bimT = din("bimT", [128, 16, 16])
    creT = din("creT", [128, 16, 16])
    cimT = din("cimT", [128, 16, 16])
    drep = din("drep", [128, 32, 16])
    g1c = din("g1c", [128, 8])
    g3c = din("g3c", [128, 8])
    g2r = din("g2r", [128, 1024])
    g4r = din("g4r", [128, 1024])
    gsr = din("gsr", [128, 512])
    gar = din("gar", [128, 512])
    bgr = din("bgr", [128, 512])
    c_idf = din("c_idf", [128, 128])
    c_cb = din("c_cb", [128, 2, 256])
    c_tm = din("c_tm", [128, 2, 256])
    c_dm = din("c_dm", [128, 2, 256])
    c_nv = din("c_nv", [128, 32])
    out = nc.dram_tensor("out", [2, S, D], F32, kind="ExternalOutput").ap()
    GT_d = nc.dram_tensor("GT_d", [32, 128, 2, 2, 128], BF16, kind="Internal").ap()
    TOEP_d = nc.dram_tensor("TOEP_d", [32, 128, 2, 256], BF16, kind="Internal").ap()
    CT_d = nc.dram_tensor("CT_d", [16, 128, 2, 256], BF16, kind="Internal").ap()
    dbg_t = {}
    if dbg:
        dbg_t['uc'] = nc.dram_tensor("d_uc", [128, 32, 16, 16], BF16, kind="ExternalOutput").ap()
        dbg_t['qT'] = nc.dram_tensor("d_qT", [128, 4, S], BF16, kind="ExternalOutput").ap()
        dbg_t['vt'] = nc.dram_tensor("d_vt", [128, 16, 8, 65], BF16, kind="ExternalOutput").ap()
        dbg_t['y1'] = nc.dram_tensor("d_y1", [128, 16, 512], BF16, kind="ExternalOutput").ap()
        dbg_t['ssmT'] = nc.dram_tensor("d_ssmT", [128, 4, 16, 128], BF16, kind="ExternalOutput").ap()
        dbg_t['attT'] = nc.dram_tensor("d_attT", [128, 4, S], BF16, kind="ExternalOutput").ap()
        dbg_t['x1'] = nc.dram_tensor("d_x1", [128, 16, 1024], F32, kind="ExternalOutput").ap()
        dbg_t['S'] = nc.dram_tensor("d_S", [128, 2, 16, 128], F32, kind="ExternalOutput").ap()

    ARENA = 206 * 1024
    arena = es.enter_context(nc.sbuf_tensor("arena", [128, ARENA], U8))
    pbanks = [es.enter_context(nc.psum_tensor("pb%d" % i, [128, 512], F32)) for i in range(8)]
    pk = [Tk() for _ in range(8)]
    fw = FW(nc, es)

    def reg(off, shape, dt, p0=0):
        esz = 2 if dt == BF16 else 4
        n = int(np.prod(shape[1:])) * esz
        assert off % 4 == 0 and off + n <= ARENA, (off, n)
        ap = arena[p0:p0 + shape[0], off:off + n].bitcast(dt)
        if len(shape) == 3:
            ap = ap.rearrange("p (a b) -> p a b", a=shape[1])
        elif len(shape) == 4:
            ap = ap.rearrange("p (a b c) -> p a b c", a=shape[1], b=shape[2])
        elif len(shape) == 5:
            ap = ap.rearrange("p (a b c d) -> p a b c d", a=shape[1], b=shape[2], c=shape[3])
        return ap

    class Bump:
        def __init__(self, lo, hi):
            self.o, self.hi = lo, hi

        def __call__(self, shape, dt, p0=0):
            esz = 2 if dt == BF16 else 4
            n = int(np.prod(shape[1:])) * esz
            n4 = (n + 31) // 32 * 32
            a = reg(self.o, shape, dt, p0)
            self.o += n4
            assert self.o <= self.hi, (self.o, self.hi)
            return a

    K = 1024
    V, A, G_, T_, SY = 'vector', 'scalar', 'gpsimd', 'tensor', 'sync'

    cb = Bump(0, 20 * K)
    idf = cb([128, 128], F32)
    idb = cb([128, 128], BF16)
    cbias = cb([128, 2, 256], BF16)
    g1 = cb([128, 8], F32)
    g3 = cb([128, 8], F32)
    g2 = cb([128, 1024], F32)
    g4 = cb([128, 1024], F32)
    gs = cb([128, 512], F32)
    ga = cb([128, 512], F32)
    bg = cb([128, 512], F32)
    stat = cb([128, 64], F32)
    LR = cb([128, 7, 16], F32)
    LI = cb([128, 7, 16], F32)
    LIn = cb([128, 7, 16], F32)
    LAk = cb([128, 7, 2, 16], F32)
    assert cb.o <= 20 * K
    kC = Tk()
    sem_c = fw.newsem("sem_c")
    sem_c2 = fw.newsem("sem_c2")
    for dst, src in [(idf, c_idf), (g1, g1c), (g3, g3c), (g2, g2r), (g4, g4r), (gs, gsr), (ga, gar), (bg, bgr)]:
        fw.dma(SY, dst, src, sem_c, writes=[kC])

    sb = Bump(20 * K, ARENA)
    cb_off = sb.o
    CBre = sb([128, 16, 16, 16], F32)
    cbf = sb([128, 2, 256], F32)
    tmk = sb([128, 2, 256], F32)
    dmk = sb([128, 2, 256], F32)
    nv = sb([128, 32], F32)
    drp = sb([128, 32, 16], F32)
    are = sb([128, 16], F32)
    aim = sb([128, 16], F32)
    ldt = sb([128, 16], F32)
    bre = sb([128, 16, 16], F32)
    bim = sb([128, 16, 16], F32)
    cre = sb([128, 16, 16], F32)
    cim = sb([128, 16, 16], F32)
    kS = Tk()
    for dst, src in [(cbf, c_cb), (tmk, c_tm), (dmk, c_dm), (nv, c_nv), (drp, drep), (are, areT), (aim, aimT),
                     (ldt, ldtT), (bre, breT), (bim, bimT), (cre, creT), (cim, cimT)]:
        fw.dma(SY, dst, src, sem_c2, writes=[kS])
    fw.op(V, cp(idb, idf), [kC], [kC])
    fw.op(V, cp(cbias, cbf), [kS, kC], [kC])

    dtt = sb([128, 16], F32)
    adr = sb([128, 16], F32)
    adi = sb([128, 16], F32)
    ex = sb([128, 16, 32], F32)
    ang = sb([128, 16, 32], F32)
    mag = sb([128, 16, 32], F32)
    magi = sb([128, 16, 32], F32)
    r1 = sb([128, 16, 32], F32)
    r2 = sb([128, 16, 32], F32)
    sn = sb([128, 16, 32], F32)
    cs = sb([128, 16, 32], F32)
    Pre = sb([128, 16, 32], F32)
    Pim = sb([128, 16, 32], F32)
    Qre = sb([128, 16, 32], F32)
    Qim = sb([128, 16, 32], F32)
    sm = [sb([128, 16], F32) for _ in range(10)]
    bbre = sb([128, 16, 16], F32)
    bbim = sb([128, 16, 16], F32)
    tb1 = sb([128, 16, 16], F32)
    tb2 = sb([128, 16, 16], F32)
    k0 = Tk()
    fw.op(A, act(dtt, ldt, AF.Exp), [kS], [k0])
    fw.op(V, tt(adr, are, dtt, ALU.mult), [k0, kS], [k0])
    fw.op(V, tt(adi, aim, dtt, ALU.mult), [k0, kS], [k0])
    fw.op(V, tt(ex, bc(adr, 2, [128, 16, 32]), bc(nv, 1, [128, 16, 32]), ALU.mult), [k0, kS], [k0])
    fw.op(V, tt(ang, bc(adi, 2, [128, 16, 32]), bc(nv, 1, [128, 16, 32]), ALU.mult), [k0, kS], [k0])
    fw.op(A, act(mag, ex, AF.Exp), [k0], [k0])
    fw.op(A, act(magi, ex, AF.Exp, scale=-1.0), [k0], [k0])
    ki = reg(cb_off, [128, 16, 32], I32)
    kf = reg(cb_off + 2048, [128, 16, 32], F32)
    mk = reg(cb_off + 4096, [128, 16, 32], F32)

    def range_reduce(r, shift):
        fw.op(V, ts(r, ang, 1.0 / (2 * math.pi), ALU.mult, shift, ALU.add), [k0], [k0])
        fw.op(V, cp(ki, r), [k0], [k0])
        fw.op(V, cp(kf, ki), [k0], [k0])
        fw.op(V, tt(r, r, kf, ALU.subtract), [k0], [k0])
        fw.op(V, ts(mk, r, -0.5, ALU.is_lt), [k0], [k0])
        fw.op(V, tt(r, r, mk, ALU.add), [k0], [k0])
        fw.op(V, ts(mk, r, 0.5, ALU.is_gt), [k0], [k0])
        fw.op(V, tt(r, r, mk, ALU.subtract), [k0], [k0])
    range_reduce(r1, 0.0)
    range_reduce(r2, 0.25)
    fw.op(A, act(sn, r1, AF.Sin, scale=2 * math.pi), [k0], [k0])
    fw.op(A, act(cs, r2, AF.Sin, scale=2 * math.pi), [k0], [k0])
    fw.op(V, tt(Pre, mag, cs, ALU.mult), [k0], [k0])
    fw.op(V, tt(Pim, mag, sn, ALU.mult), [k0], [k0])
    fw.op(V, tt(Qre, magi, cs, ALU.mult), [k0], [k0])
    fw.op(V, stt(Qim, magi, -1.0, sn, ALU.mult, ALU.mult), [k0], [k0])
    lbre, lbim = Pre[:, :, 0], Pim[:, :, 0]
    nr, den, t0, t1_, cr, ci, rden = sm[0], sm[1], sm[2], sm[3], sm[4], sm[5], sm[6]
    fw.op(V, ts(nr, lbre, -1.0, ALU.add), [k0], [k0])
    fw.op(V, tt(den, are, are, ALU.mult), [k0, kS], [k0])
    fw.op(V, tt(t0, aim, aim, ALU.mult), [k0, kS], [k0])
    fw.op(V, tt(den, den, t0, ALU.add), [k0], [k0])
    fw.op(V, lambda E: E.reciprocal(out=rden, in_=den), [k0], [k0])
    fw.op(V, tt(t0, nr, are, ALU.mult), [k0], [k0])
    fw.op(V, tt(t1_, lbim, aim, ALU.mult), [k0], [k0])
    fw.op(V, tt(t0, t0, t1_, ALU.add), [k0], [k0])
    fw.op(V, tt(cr, t0, rden, ALU.mult), [k0], [k0])
    fw.op(V, tt(t0, lbim, are, ALU.mult), [k0], [k0])
    fw.op(V, tt(t1_, nr, aim, ALU.mult), [k0], [k0])
    fw.op(V, tt(t0, t0, t1_, ALU.subtract), [k0], [k0])
    fw.op(V, tt(ci, t0, rden, ALU.mult), [k0], [k0])
    crb, cib = bc(cr, 2, [128, 16, 16]), bc(ci, 2, [128, 16, 16])
    fw.op(V, tt(tb1, crb, bre, ALU.mult), [k0, kS], [k0])
    fw.op(V, tt(tb2, cib, bim, ALU.mult), [k0, kS], [k0])
    fw.op(V, tt(bbre, tb1, tb2, ALU.subtract), [k0], [k0])
    fw.op(V, tt(tb1, crb, bim, ALU.mult), [k0, kS], [k0])
    fw.op(V, tt(tb2, cib, bre, ALU.mult), [k0, kS], [k0])
    fw.op(V, tt(bbim, tb1, tb2, ALU.add), [k0], [k0])
    fw.op(V, cp(LR[:, 0, :], Pre[:, :, 15]), [k0], [kC])
    fw.op(V, cp(LI[:, 0, :], Pim[:, :, 15]), [k0], [kC])
    for k in range(6):
        fw.op(V, tt(sm[7], LR[:, k, :], LR[:, k, :], ALU.mult), [kC, k0], [k0])
        fw.op(V, tt(sm[8], LI[:, k, :], LI[:, k, :], ALU.mult), [kC, k0], [k0])
        fw.op(V, tt(LR[:, k + 1, :], sm[7], sm[8], ALU.subtract), [k0], [kC])
        fw.op(V, tt(sm[9], LR[:, k, :], LI[:, k, :], ALU.mult), [kC, k0], [k0])
        fw.op(V, ts(LI[:, k + 1, :], sm[9], 2.0, ALU.mult), [k0], [kC])
    fw.op(V, ts(LIn, LI, -1.0, ALU.mult), [kC], [kC])
    fw.op(V, cp(LAk[:, :, 0, :], LR), [kC], [kC])
    fw.op(V, cp(LAk[:, :, 1, :], LR), [kC], [kC])
    SH4 = [128, 16, 16, 16]
    Gre = sb(SH4, F32)
    Gim = sb(SH4, F32)
    CAre = sb(SH4, F32)
    CAim = sb(SH4, F32)
    CBim = sb(SH4, F32)
    u1_off = sb.o
    U1 = sb(SH4, F32)
    U2 = sb(SH4, F32)
    U3, U4 = U1, U2
    CTst = reg(u1_off, [128, 16, 2, 256], BF16)
    kG, kCA, kCB, kU1, kU2, kCT = [Tk() for _ in range(6)]
    kU3, kU4 = kU1, kU2

    def outer(eng, o, a, pn, rd, wr):
        fw.op(eng, tt(o, bc(a, 2, SH4), bc(pn, 3, SH4), ALU.mult), rd, wr)

    def fl(ap):
        return ap.rearrange("p a b c -> p (a b c)")
    outer(V, U1, bbre, Qre[:, :, 0:16], [k0], [kU1])
    outer(G_, U2, bbim, Qim[:, :, 0:16], [k0], [kU2])
    fw.op(V, tt(Gre, U1, U2, ALU.subtract), [kU1, kU2], [kG])
    outer(V, U3, bbre, Qim[:, :, 0:16], [k0], [kU3])
    outer(G_, U4, bbim, Qre[:, :, 0:16], [k0], [kU4])
    fw.op(V, tt(Gim, U3, U4, ALU.add), [kU3, kU4], [kG])
    outer(V, U1, cre, Pre[:, :, 0:16], [k0, kS], [kU1])
    outer(G_, U2, cim, Pim[:, :, 0:16], [k0, kS], [kU2])
    fw.op(V, tt(CAre, U1, U2, ALU.subtract), [kU1, kU2], [kCA])
    outer(V, U3, cre, Pim[:, :, 0:16], [k0, kS], [kU3])
    outer(G_, U4, cim, Pre[:, :, 0:16], [k0, kS], [kU4])
    fw.op(V, stt(fl(CAim), fl(U3), -1.0, fl(U4), ALU.mult, ALU.subtract), [kU3, kU4], [kCA])
    outer(V, U1, cre, Pre[:, :, 16:32], [k0, kS], [kU1])
    outer(G_, U2, cim, Pim[:, :, 16:32], [k0, kS], [kU2])
    fw.op(V, tt(CBre, U1, U2, ALU.subtract), [kU1, kU2], [kCB])
    outer(V, U3, cre, Pim[:, :, 16:32], [k0, kS], [kU3])
    outer(G_, U4, cim, Pre[:, :, 16:32], [k0, kS], [kU4])
    fw.op(V, stt(fl(CBim), fl(U3), -1.0, fl(U4), ALU.mult, ALU.subtract), [kU3, kU4], [kCB])
    fw.op(V, cp(CTst[:, :, 0, :], CBre.rearrange("p g t h -> p g (t h)")), [kCB], [kCT, kU1])
    fw.op(V, cp(CTst[:, :, 1, :], CBim.rearrange("p g t h -> p g (t h)")), [kCB], [kCT, kU1])
    sem_t = fw.newsem("sem_t")
    kDR = Tk()
    fw.dma(SY, CT_d.rearrange("g p r c -> p g r c"), CTst, sem_t, [kCT], [kDR])
    TPst = [sb([128, 2, 256], BF16) for _ in range(2)]
    GTst = [sb([128, 2, 2, 128], BF16) for _ in range(2)]
    TPt = [sb([128, 2, 256], F32) for _ in range(2)]
    TPd = [sb([128, 2, 256], F32) for _ in range(2)]
    kTP = [Tk() for _ in range(2)]
    kGTs = [Tk() for _ in range(2)]
    kTt = [Tk() for _ in range(2)]
    sem_tp = [fw.newsem("sem_tp%d" % i) for i in range(2)]
    sem_gt = [fw.newsem("sem_gt%d" % i) for i in range(2)]
    for gh in range(2):
        pr = slice(64 * gh, 64 * gh + 64)
        for gl in range(16):
            g = 16 * gh + gl
            i = g % 2
            pT, pG = 2 * i, 2 * i + 1
            pt_v = pbanks[pT][:, :].rearrange("p (j c) -> p j c", j=2)
            fns = []
            for j in range(2):
                l_re = Gre[pr, gl, 8 * j:8 * j + 8, :].rearrange("p s h -> p (s h)")
                l_im = Gim[pr, gl, 8 * j:8 * j + 8, :].rearrange("p s h -> p (s h)")
                fns.append(mmf(pt_v[:, j, :], l_re, CAre[pr, gl].rearrange("p t h -> p (t h)"), True, False))
                fns.append(mmf(pt_v[:, j, :], l_im, CAim[pr, gl].rearrange("p t h -> p (t h)"), False, True))
            fw.mm(fns, [kG, kCA], [pk[pT]])
            pg_v = pbanks[pG][:, :].rearrange("p (j r c) -> p j r c", j=2, r=2)
            fns = []
            for j in range(2):
                l_re = Gre[pr, gl, 8 * j:8 * j + 8, :].rearrange("p s h -> p (s h)")
                l_im = Gim[pr, gl, 8 * j:8 * j + 8, :].rearrange("p s h -> p (s h)")
                fns.append(mmf(pg_v[:, j, 0, :], l_re, idf[pr, :], True, True))
                fns.append(mmf(pg_v[:, j, 1, :], l_im, idf[pr, :], True, True))
            fw.mm(fns, [kG, kC], [pk[pG]])
            fw.op(V, tt(TPt[i], pt_v, tmk, ALU.mult), [pk[pT], kS], [kTt[i]])
            fw.op(G_, tt(TPd[i].rearrange("p j (t h) -> p j t h", t=16),
                         dmk.rearrange("p j (t h) -> p j t h", t=16),
                         drp[:, g, :].unsqueeze(1).unsqueeze(1).broadcast_to([128, 2, 16, 16]), ALU.mult),
                  [kS], [kTP[i]])
            fw.op(V, tt(TPst[i], TPt[i], TPd[i], ALU.add), [kTt[i], kTP[i]], [kTP[i]])
            fw.dma(SY, TOEP_d[g], TPst[i], sem_tp[i], [kTP[i]], [kDR])
            fw.op(A, act(GTst[i], pg_v, AF.Copy), [pk[pG]], [kGTs[i]])
            fw.dma(SY, GT_d[g], GTst[i], sem_gt[i], [kGTs[i]], [kDR])
    fw.barrier()

    X1 = reg(20 * K, [128, 16, 1024], F32)
    attT = reg(84 * K, [128, 4, S], BF16)
    ssmT = reg(100 * K, [128, 4, 16, 128], BF16)
    Uc = reg(116 * K, [128, 32, 16, 16], BF16)
    qT = reg(132 * K, [128, 4, S], BF16)
    kT = reg(148 * K, [128, 4, S], BF16)
    Vt = reg(164 * K, [128, 16, 8, 65], BF16)
    sem_x = [fw.newsem("sem_x%d" % i) for i in range(4)]
    sem_w = [fw.newsem("sem_w%d" % i) for i in range(2)]
    sem_o = fw.newsem("sem_o")
    sem_og = fw.newsem("sem_og")
    sem_wq = [fw.newsem("sem_wq%d" % i) for i in range(2)]
    sem_d = fw.newsem("sem_d")
    sem_g = [fw.newsem("sem_g%d" % i) for i in range(2)]
    sem_dn = fw.newsem("sem_dn")
    sem_st = [fw.newsem("sem_st%d" % i) for i in range(2)]
    kOut = Tk()

    try:
      if STAGE < 1:
        raise _Stop()
      for b in range(2):
        a1 = Bump(20 * K, 116 * K)
        Xst = [a1([128, 2, 1024], F32) for _ in range(2)]
        hbf = [a1([128, 2, 1024], BF16) for _ in range(2)]
        hT = a1([128, 8, S], BF16)
        wbuf = [a1([128, 8, 256], BF16) for _ in range(2)]
        junk = a1([128, 1024], F32)
        kX = [Tk(), Tk()]
        kH = [Tk(), Tk()]
        khT = [Tk() for _ in range(8)]
        kW = [Tk(), Tk()]
        kJ = Tk()
        kSt = Tk()
        kUc, kq, kk, kv = Tk(), Tk(), Tk(), Tk()
        xv = x[b].rearrange("(c t) d -> c t d", t=16)
        ss = stat[:, 0:16]
        rs = stat[:, 16:32]
        for tp in range(8):
            i = tp % 2
            fw.dma(SY, Xst[i], xv[:, 2 * tp:2 * tp + 2, :], sem_x[i], writes=[kX[i]])
            for u in range(2):
                t = 2 * tp + u
                fw.op(A, ttr(junk, Xst[i][:, u, :], Xst[i][:, u, :], ss[:, t:t + 1]), [kX[i]], [kJ, kSt])
                fw.op(V, ts(rs[:, t:t + 1], ss[:, t:t + 1], 1.0 / D, ALU.mult, EPS, ALU.add), [kSt], [kSt])
                fw.op(A, act(rs[:, t:t + 1], rs[:, t:t + 1], AF.Ln), [kSt], [kSt])
                fw.op(A, act(rs[:, t:t + 1], rs[:, t:t + 1], AF.Exp, scale=-0.5), [kSt], [kSt])
                fw.op(A, act(hbf[i][:, u, :], Xst[i][:, u, :], AF.Copy, scale=rs[:, t:t + 1]), [kX[i], kSt], [kH[i]])
            for k in range(8):
                pi = k % 4
                pv = pbanks[pi][:, 0:128].bitcast(BF16).rearrange("p (u c) -> p u c", u=2)
                fns = [trf(pv[:, u, :], hbf[i][:, u, k * 128:(k + 1) * 128], idb) for u in range(2)]
                fw.mm(fns, [kH[i], kC], [pk[pi]])
                dst = hT[:, k, :].rearrange("p (c t) -> p t c", t=16)[:, 2 * tp:2 * tp + 2, :]
                if k % 2 == 0:
                    fw.op(V, ts(dst, pv, g1[:, k:k + 1], ALU.mult), [pk[pi], kC], [khT[k]])
                else:
                    fw.op(A, act(dst, pv, AF.Copy, scale=g1[:, k:k + 1]), [pk[pi], kC], [khT[k]])
        win_v = w_in.rearrange("(k p) n -> p k n", p=128)
        pcount = 0
        for piece in range(8):
            i = piece % 2
            fw.dma(G_, wbuf[i], win_v[:, :, piece * 256:(piece + 1) * 256], sem_wq[i], writes=[kW[i]])
            if piece < 2:
                for t in range(16):
                    pi = 4 + (pcount % 4)
                    pcount += 1
                    pv = pbanks[pi][:, 0:256]
                    fns = [mmf(pv, hT[:, k, t::16], wbuf[i][:, k, :], k == 0, k == 7) for k in range(8)]
                    fw.mm(fns, khT + [kW[i]], [pk[pi]])
                    dst = Uc[:, 16 * piece:16 * piece + 16, t, :]
                    srcv = pv.rearrange("p (g h) -> p g h", g=16)
                    if t % 2 == 0:
                        fw.op(V, cp(dst, srcv), [pk[pi]], [kUc])
                    else:
                        fw.op(A, act(dst, srcv, AF.Copy), [pk[pi]], [kUc])
            elif piece < 6:
                dstT, kd, sc = (qT, kq, 0.125) if piece < 4 else (kT, kk, 1.0)
                for mm_ in range(2):
                    m = (piece % 2) * 2 + mm_
                    for n in range(4):
                        pi = 4 + (pcount % 4)
                        pcount += 1
                        pv = pbanks[pi][:, :]
                        fns = [mmf(pv, wbuf[i][:, k, mm_ * 128:(mm_ + 1) * 128], hT[:, k, n * 512:(n + 1) * 512],
                                   k == 0, k == 7) for k in range(8)]
                        fw.mm(fns, khT + [kW[i]], [pk[pi]])
                        dst = dstT[:, m, n * 512:(n + 1) * 512]
                        if n % 2 == 0:
                            fw.op(V, ts(dst, pv, sc, ALU.mult), [pk[pi]], [kd])
                        else:
                            fw.op(A, act(dst, pv, AF.Copy, scale=sc), [pk[pi]], [kd])
            else:
                hh = piece - 6
                for it in range(16):
                    pi = 4 + (pcount % 4)
                    pcount += 1
                    pv = pbanks[pi][:, 0:256]
                    fns = [mmf(pv, hT[:, k, it * 128:(it + 1) * 128], wbuf[i][:, k, :], k == 0, k == 7)
                           for k in range(8)]
                    fw.mm(fns, khT + [kW[i]], [pk[pi]])
                    dst = Vt[:, it, 4 * hh:4 * hh + 4, 0:64]
                    src = pv.rearrange("p (h d) -> p h d", h=4)
                    if it % 2 == 0:
                        fw.op(V, cp(dst, src), [pk[pi]], [kv])
                    else:
                        fw.op(A, act(dst, src, AF.Copy), [pk[pi]], [kv])
        fw.op(G_, lambda E: E.memset(Vt[:, :, :, 64:65], 1.0), [kv], [kv])
        if dbg and b == 0:
            fw.dma(SY, dbg_t['uc'], Uc, sem_d, [kUc], [kOut])
            fw.dma(SY, dbg_t['qT'], qT, sem_d, [kq], [kOut])
            fw.dma(SY, dbg_t['vt'], Vt, sem_d, [kv], [kOut])
        fw.barrier()

        if STAGE < 2:
            raise _Stop()
        a2 = Bump(181 * K, ARENA)
        Sbf = a2([128, 2, 16, 130], BF16)
        a2b = Bump(20 * K, 84 * K)
        UT = a2b([128, 32, 2, 128], BF16)
        Wf = a2b([128, 2, 16, 128], F32)
        GTb = [a2b([128, 2, 2, 2, 128], BF16) for _ in range(2)]
        T1 = a2b([128, 2, 16], F32)
        T2 = a2b([128, 2, 16], F32)
        kUT, kWf, kSb, kT1, kT2 = Tk(), Tk(), Tk(), Tk(), Tk()
        kGTb = [Tk(), Tk()]
        for g in range(32):
            pi = g % 4
            pv = pbanks[pi][:, 0:128].bitcast(BF16).rearrange("p (j c) -> p j c", j=2)
            fns = [trf(pv[:, j, :], Uc[:, g, 8 * j:8 * j + 8, :].rearrange("p s h -> p (s h)"), idb) for j in range(2)]
            fw.mm(fns, [kUc, kC], [pk[pi]])
            if g % 2 == 0:
                fw.op(V, cp(UT[:, g], pv), [pk[pi]], [kUT])
            else:
                fw.op(A, act(UT[:, g], pv, AF.Copy), [pk[pi]], [kUT])
        if STAGE < 2.2:
            raise _Stop()
        for gl in range(16):
            i = gl % 2
            for gh in range(2):
                fw.dma(SY, GTb[i][:, gh], GT_d[16 * gh + gl], sem_w[i], writes=[kGTb[i]])
            pi = 4 + gl % 4
            pv = pbanks[pi][:, 0:256].rearrange("p (r c) -> p r c", r=2)
            fns = []
            for r in range(2):
                n = 0
                for gh in range(2):
                    for j in range(2):
                        fns.append(mmf(pv[:, r, :], GTb[i][:, gh, j, r, :], UT[:, 16 * gh + gl, j, :], n == 0, n == 3))
                        n += 1
            fw.mm(fns, [kUT, kGTb[i]], [pk[pi]])
            fw.op(V, cp(Wf[:, :, gl, :], pv), [pk[pi]], [kWf])
        if STAGE < 2.5:
            raise _Stop()
        WfB = reg(84 * K, [128, 2, 16, 128], F32)
        TT = reg(100 * K, [128, 2, 16, 128], F32)
        kWB, kTT = Tk(), Tk()
        cur, nxt, kcur, knxt = Wf, WfB, kWf, kWB
        for k, d in enumerate([1, 2, 4, 8, 16, 32, 64]):
            n = 128 - d
            fw.op(V, tt(TT[:, :, :, 0:n], cur[:, :, :, 0:n], bc(LAk[:, k], 3, [128, 2, 16, n]), ALU.mult),
                  [kcur, kC], [kTT])
            fw.op(V, tt(nxt[:, :, :, d:128], cur[:, :, :, d:128], TT[:, :, :, 0:n], ALU.add), [kcur, kTT], [knxt])
            fw.op(V, tt(TT[:, 0, :, 0:n], cur[:, 1, :, 0:n], bc(LIn[:, k, :], 2, [128, 16, n]), ALU.mult),
                  [kcur, kC], [kTT])
            fw.op(V, tt(TT[:, 1, :, 0:n], cur[:, 0, :, 0:n], bc(LI[:, k, :], 2, [128, 16, n]), ALU.mult),
                  [kcur, kC], [kTT])
            fw.op(V, tt(nxt[:, :, :, d:128], nxt[:, :, :, d:128], TT[:, :, :, 0:n], ALU.add), [knxt, kTT], [knxt])
            fw.op(V, cp(nxt[:, :, :, 0:d], cur[:, :, :, 0:d]), [kcur], [knxt])
            cur, nxt, kcur, knxt = nxt, cur, knxt, kcur
        Wf, kWf = cur, kcur
        if STAGE < 2.8:
            raise _Stop()
        fw.op(V, lambda E: E.memset(Sbf[:, :, :, 0:2], 0.0), [], [kSb])
        if STAGE < 2.9:
            raise _Stop()
        for r in range(2):
            fw.op(A, act(Sbf[:, r, :, 2:130], Wf[:, r], AF.Copy), [kWf], [kSb])
        if dbg and b == 0:
            fw.dma(SY, dbg_t['S'], Wf, sem_d, [kWf], [kOut])
        fw.barrier()

        if STAGE < 3:
            raise _Stop()
        a3 = Bump(52 * K, 84 * K)
        MBT = a3([128, S], BF16)
        kmT = a3([128, 4, 8], BF16)
        kmf = a3([128, 4, 8], F32)
        gt = a3([128, 8, 8], F32)
        top8 = a3([128, 8, 8], F32)
        mbt = a3([128, 2, 64], BF16)
        PT = [a3([128, 256], BF16) for _ in range(3)]
        Ao = a3([128, 2, 512], F32)
        Aj = a3([128, 512], F32)
        Ab = a3([128, 2, 512], BF16)
        rden_a = a3([128, 4], F32)
        kMB, kkm, kgt, kmb = Tk(), Tk(), Tk(), Tk()
        Esel = reg(100 * K, [128, 64, 128], BF16)
        kEs = Tk()
        fw.op(V, cp(Esel[0:64], bc(idb[0:64, 0:64], 2, [64, 64, 128])), [kC], [kEs])
        fw.op(V, cp(Esel[64:128], bc(idb[64:128, 64:128], 2, [64, 64, 128])), [kC], [kEs])
        kPT = [Tk() for _ in range(3)]
        kAo, kAj, kAb, kat, krd = Tk(), Tk(), Tk(), Tk(), Tk()
        if STAGE < 3.05:
            raise _Stop()
        kjunk = a3([128, 256], BF16)
        kkj = Tk()
        for m in range(4):
            for n in range(8):
                fw.op(A, (lambda m=m, n=n: (lambda E: E.activation(out=kjunk, in_=kT[:, m, n * 256:(n + 1) * 256],
                                                                   func=AF.Copy, accum_out=kmf[:, m, n:n + 1])))(),
                      [kk], [kkj, kkm])
        if STAGE < 3.08:
            raise _Stop()
        fw.op(A, act(kmT, kmf, AF.Copy, scale=1.0 / 256), [kkm], [kkm])
        if STAGE < 3.1:
            raise _Stop()
        fw.op(G_, lambda E: E.memset(MBT, 0.0), [], [kMB])
        for qt in range(8, 16):
            bq = qt // 2
            pve = pbanks[0][:, 0:32].rearrange("p (h n) -> p h n", h=4)
            pvo = pbanks[1][:, 0:32].rearrange("p (h n) -> p h n", h=4)
            fe = [mmf(pve[:, h2, :], qT[0:64, h2, qt * 128:(qt + 1) * 128], kmT[0:64, h2, :], True, True)
                  for h2 in range(4)]
            fw.mm(fe, [kq, kkm], [pk[0]])
            fo = [mmf(pvo[:, h2, :], qT[64:128, h2, qt * 128:(qt + 1) * 128], kmT[64:128, h2, :], True, True)
                  for h2 in range(4)]
            fw.mm(fo, [kq, kkm], [pk[1]])
            fw.op(G_, lambda E: E.memset(gt, NEG), [kgt], [kgt])
            gtv = gt.rearrange("p (h2 e) n -> p h2 e n", e=2)
            fw.op(V, cp(gtv[:, :, 0, 0:bq], pve[:, :, 0:bq]), [pk[0], kgt], [kgt])
            fw.op(V, cp(gtv[:, :, 1, 0:bq], pvo[:, :, 0:bq]), [pk[1], kgt], [kgt])
            for h in range(8):
                fw.op(V, (lambda hh: (lambda E: E.max(out=top8[:, hh, :], in_=gt[:, hh, :])))(h), [kgt], [kmb])
            for dup in range(2):
                fw.op(V, tt(mbt[:, dup, :].rearrange("p (h n) -> p h n", h=8), gt,
                            top8[:, :, 2:3].broadcast_to([128, 8, 8]), ALU.is_lt), [kgt, kmb], [kmb])
            pi2 = 2 + qt % 2
            pv2 = pbanks[pi2][:, 0:64].bitcast(BF16)
            fw.mm([trf(pv2, mbt.rearrange("p d c -> p (d c)"), idb)], [kmb, kC], [pk[pi2]])
            fw.op(V, ts(MBT[:, qt * 128:(qt + 1) * 128], pv2, NEG, ALU.mult), [pk[pi2]], [kMB])
        if STAGE < 3.2:
            raise _Stop()
        ucount = 0
        LAG = 2
        for bq in range(ABQ):
            nkt = 2 * bq + 2

            def stage_a(h, kt, pi, bq=bq):
                pr = slice(64 * (h % 2), 64 * (h % 2) + 64)
                m = h // 2
                n = kt // 2
                pv = pbanks[pi][:, 0:256]
                fns = [mmf(pv, kT[pr, m, kt * 128:(kt + 1) * 128], qT[pr, m, bq * 256:(bq + 1) * 256], True, False)]
                rd = [kq, kk]
                if n == bq:
                    fns.append(mmf(pv, idb, cbias[:, kt - 2 * bq, :], False, True))
                    rd.append(kC)
                elif bq >= 4:
                    r = h * 8 + n
                    fns.append(mmf(pv, Esel[pr, r, :], MBT[pr, bq * 256:(bq + 1) * 256], False, True))
                    rd += [kEs, kMB]
                else:
                    fns[0] = mmf(pv, kT[pr, m, kt * 128:(kt + 1) * 128], qT[pr, m, bq * 256:(bq + 1) * 256], True, True)
                fw.mm(fns, rd, [pk[pi]])
                fw.op(A, act(PT[pi], pv, AF.Exp), [pk[pi]], [kPT[pi]])

            def stage_b(h, kt, pi, nkt=nkt):
                pos = [4 + 2 * (h % 2), 5 + 2 * (h % 2)]
                povs = [pbanks[pos[0]][:, 0:65], pbanks[pos[1]][:, 0:65]]
                fns = [mmf(povs[u], PT[pi][:, u * 128:(u + 1) * 128], Vt[:, kt, h, :], kt == 0, kt == nkt - 1)
                       for u in range(2)]
                fw.mm(fns, [kPT[pi], kv], [pk[pos[0]], pk[pos[1]]])
                if kt == nkt - 1:
                    for u in range(2):
                        fw.op(V, rcp(rden_a[:, u:u + 1], povs[u][:, 64:65]), [pk[pos[u]]], [krd])
                        fw.op(V, ts(Ao[:, u, h * 64:(h + 1) * 64], povs[u][:, 0:64], rden_a[:, u:u + 1], ALU.mult),
                              [pk[pos[u]], krd], [kAo])

            pend = []
            for h in range(8):
                for kt in range(nkt):
                    pi = ucount % 3
                    ucount += 1
                    stage_a(h, kt, pi)
                    pend.append((h, kt, pi))
                    if len(pend) > LAG:
                        stage_b(*pend.pop(0))
            while pend:
                stage_b(*pend.pop(0))
            for u in range(2):
                fw.op(A, ttr(Aj, Ao[:, u, :], Ao[:, u, :], rden_a[:, 2:3]), [kAo], [kAj, krd])
                fw.op(V, ts(rden_a[:, 3:4], rden_a[:, 2:3], 1.0 / 512, ALU.mult, EPS, ALU.add), [krd], [krd])
                fw.op(A, act(rden_a[:, 3:4], rden_a[:, 3:4], AF.Ln), [krd], [krd])
                fw.op(A, act(rden_a[:, 3:4], rden_a[:, 3:4], AF.Exp, scale=-0.5), [krd], [krd])
                fw.op(V, stt(Ab[:, u, :], Ao[:, u, :], rden_a[:, 3:4], ga, ALU.mult, ALU.mult), [kAo, krd, kC], [kAb])
            for u in range(2):
                pi = 3
                pv = pbanks[pi][:, 0:256].bitcast(BF16).rearrange("p (f c) -> p f c", f=4)
                fns = [trf(pv[:, f, :], Ab[:, u, f * 128:(f + 1) * 128], idb) for f in range(4)]
                fw.mm(fns, [kAb, kC], [pk[pi]])
                tok0 = bq * 256 + u * 128
                fw.op(A, act(attT[:, :, tok0:tok0 + 128], pv, AF.Copy), [pk[pi]], [kat])
        if dbg and b == 0:
            fw.dma(SY, dbg_t['attT'], attT, sem_d, [kat], [kOut])
        fw.barrier()

        if STAGE < 4:
            raise _Stop()
        a4 = Bump(132 * K, 181 * K)
        Y1 = a4([128, 16, 512], BF16)
        TCb = [(a4([128, 2, 2, 256], BF16), a4([128, 2, 256], BF16)) for _ in range(2)]
        wgl = a4([128, 4, 512], BF16)
        Y1T = [a4([128, 4, 128], BF16) for _ in range(2)]
        zt = a4([128, 512], F32)
        zs = a4([128, 512], F32)
        y2 = a4([128, 512], F32)
        y2b = a4([128, 512], BF16)
        yq = a4([128, 256], F32)
        yc = a4([128, 256], F32)
        kY1, kwg, kzt, kzs, ky2, ky2b, kss, kyq, kyc, kssm = [Tk() for _ in range(10)]
        kTC = [Tk(), Tk()]
        kY1T = [Tk(), Tk()]
        fw.dma(G_, wgl, w_glu.rearrange("(k p) n -> p k n", p=128), sem_og, writes=[kwg])
        C1 = 0.7978845608028654 * 2.0
        for gl in range(16):
            i = gl % 2
            for gh in range(2):
                fw.dma(SY, TCb[i][0][:, gh], TOEP_d[16 * gh + gl], sem_w[i], writes=[kTC[i]])
            fw.dma(SY, TCb[i][1], CT_d[gl], sem_w[i], writes=[kTC[i]])
            for gh in range(2):
                g = 16 * gh + gl
                pr = slice(64 * gh, 64 * gh + 64)
                pi = (2 * gl + gh) % 4
                pv = pbanks[pi][:, 0:256]
                fns = [mmf(pv, UT[:, g, 0, :], TCb[i][0][:, gh, 0, :], True, False),
                       mmf(pv, UT[:, g, 1, :], TCb[i][0][:, gh, 1, :], False, False),
                       mmf(pv, Sbf[pr, 0, gl, 1:129], TCb[i][1][pr, 0, :], False, False),
                       mmf(pv, Sbf[pr, 1, gl, 1:129], TCb[i][1][pr, 1, :], False, True)]
                fw.mm(fns, [kUT, kSb, kTC[i]], [pk[pi]])
                fw.op(V, cp(yc, pv), [pk[pi]], [kyc])
                fw.op(V, tt(yq, yc, yc, ALU.mult), [kyc], [kyq])
                fw.op(V, ts(yq, yq, 0.044715, ALU.mult, 1.0, ALU.add), [kyq], [kyq])
                fw.op(V, tt(yq, yq, yc, ALU.mult), [kyq, kyc], [kyq])
                fw.op(V, ts(yq, yq, -40.0, ALU.max), [kyq], [kyq])
                fw.op(A, act(yq, yq, AF.Exp, scale=-C1), [kyq], [kyq])
                fw.op(V, ts(yq, yq, 1.0, ALU.add), [kyq], [kyq])
                fw.op(V, rcp(yq, yq), [kyq], [kyq])
                fw.op(V, tt(Y1[:, :, 16 * g:16 * g + 16], yq.rearrange("p (t h) -> p t h", t=16),
                            yc.rearrange("p (t h) -> p t h", t=16), ALU.mult), [kyq, kyc], [kY1])
        if dbg and b == 0:
            fw.dma(SY, dbg_t['y1'], Y1, sem_d, [kY1], [kOut])
        for t in range(16):
            i = t % 2
            pi = 4 + i
            pv = pbanks[pi][:, 0:256].bitcast(BF16).rearrange("p (f c) -> p f c", f=4)
            fns = [trf(pv[:, f, :], Y1[:, t, f * 128:(f + 1) * 128], idb) for f in range(4)]
            fw.mm(fns, [kY1, kC], [pk[pi]])
            fw.op(A, act(Y1T[i], pv, AF.Copy), [pk[pi]], [kY1T[i]])
            pz = 6 + i
            pzv = pbanks[pz][:, :]
            fns = [mmf(pzv, Y1T[i][:, f, :], wgl[:, f, :], f == 0, f == 3) for f in range(4)]
            fw.mm(fns, [kY1T[i], kwg], [pk[pz]])
            fw.op(V, tt(zt, pzv, bg, ALU.add), [pk[pz], kC], [kzt])
            fw.op(A, act(zs, zt, AF.Exp, scale=-1.0), [kzt], [kzs])
            fw.op(V, ts(zs, zs, 1.0, ALU.add), [kzs], [kzs])
            fw.op(V, rcp(zs, zs), [kzs], [kzs])
            fw.op(V, tt(y2, zs, Y1[:, t, :], ALU.mult), [kzs, kY1], [ky2])
            fw.op(A, ttr(zt, y2, y2, stat[:, 32:33]), [ky2, kzt], [kzt, kss])
            fw.op(V, ts(stat[:, 33:34], stat[:, 32:33], 1.0 / 512, ALU.mult, EPS, ALU.add), [kss], [kss])
            fw.op(A, act(stat[:, 33:34], stat[:, 33:34], AF.Ln), [kss], [kss])
            fw.op(A, act(stat[:, 33:34], stat[:, 33:34], AF.Exp, scale=-0.5), [kss], [kss])
            fw.op(V, stt(y2b, y2, stat[:, 33:34], gs, ALU.mult, ALU.mult), [ky2, kss, kC], [ky2b])
            pi2 = i
            pv2 = pbanks[pi2][:, 0:256].bitcast(BF16).rearrange("p (f c) -> p f c", f=4)
            fns = [trf(pv2[:, f, :], y2b[:, f * 128:(f + 1) * 128], idb) for f in range(4)]
            fw.mm(fns, [ky2b, kC], [pk[pi2]])
            fw.op(A, act(ssmT[:, :, t, :], pv2, AF.Copy), [pk[pi2]], [kssm])
        if dbg and b == 0:
            fw.dma(SY, dbg_t['ssmT'], ssmT, sem_d, [kssm], [kOut])
        fw.barrier()

        if STAGE < 5:
            raise _Stop()
        a5 = Bump(116 * K, ARENA)
        wo = a5([128, 8, 1024], BF16)
        otmp = a5([128, 1024], F32)
        oj = a5([128, 512], F32)
        kwo, kot, koj, kst4 = Tk(), Tk(), Tk(), Tk()
        kX1 = [Tk() for _ in range(16)]
        fw.dma(G_, wo, w_out.rearrange("(k p) n -> p k n", p=128), sem_og, writes=[kwo])
        for q4 in range(4):
            fw.dma(SY, X1[:, 4 * q4:4 * q4 + 4, :], xv[:, 4 * q4:4 * q4 + 4, :], sem_x[q4],
                   writes=[kX1[4 * q4 + j] for j in range(4)])
        for t in range(16):
            pis = [2 * (t % 2), 2 * (t % 2) + 1]
            for hf in range(2):
                pv = pbanks[pis[hf]][:, :]
                fns = []
                for k in range(8):
                    lhs = ssmT[:, k, t, :] if k < 4 else attT[:, k - 4, t::16]
                    fns.append(mmf(pv, lhs, wo[:, k, hf * 512:(hf + 1) * 512], k == 0, k == 7))
                fw.mm(fns, [kssm, kat, kwo], [pk[pis[hf]]])
                fw.op(A, sqa(oj, pv, stat[:, 40 + hf:41 + hf]), [pk[pis[hf]]], [koj, kst4])
            fw.op(V, tt(stat[:, 42:43], stat[:, 40:41], stat[:, 41:42], ALU.add), [kst4], [kst4])
            fw.op(V, ts(stat[:, 42:43], stat[:, 42:43], 1.0 / D, ALU.mult, EPS, ALU.add), [kst4], [kst4])
            fw.op(A, act(stat[:, 42:43], stat[:, 42:43], AF.Ln), [kst4], [kst4])
            fw.op(A, act(stat[:, 42:43], stat[:, 42:43], AF.Exp, scale=-0.5), [kst4], [kst4])
            for hf in range(2):
                pv = pbanks[pis[hf]][:, :]
                sl = slice(hf * 512, (hf + 1) * 512)
                fw.op(V, stt(otmp[:, sl], pv, stat[:, 42:43], g2[:, sl], ALU.mult, ALU.mult),
                      [pk[pis[hf]], kst4, kC], [kot])
            fw.op(G_, tt(X1[:, t, :], X1[:, t, :], otmp, ALU.add), [kot, kX1[t]], [kX1[t]])
        if dbg and b == 0:
            fw.dma(SY, dbg_t['x1'], X1, sem_d, kX1, [kOut])
        fw.barrier()

        if STAGE < 6:
            raise _Stop()
        f0 = Bump(86 * K, ARENA)
        h2T = f0([128, 8, 16, 128], BF16)
        fT = f0([128, NFF, 16, 128], BF16)
        f1 = Bump(84 * K, 86 * K)
        hb2 = f1([128, 1024], BF16)
        khb, kfj, kh2T, kfT, kst5, kot2 = Tk(), Tk(), Tk(), Tk(), Tk(), Tk()
        ov = out[b].rearrange("(c t) d -> c t d", t=16)
        fj1 = reg(118 * K, [128, 1024], F32)
        for t in range(16):
            fw.op(A, ttr(fj1, X1[:, t, :], X1[:, t, :], stat[:, 44:45]), [kX1[t]], [kfj, kst5])
            fw.op(V, ts(stat[:, 45:46], stat[:, 44:45], 1.0 / D, ALU.mult, EPS, ALU.add), [kst5], [kst5])
            fw.op(A, act(stat[:, 45:46], stat[:, 45:46], AF.Ln), [kst5], [kst5])
            fw.op(A, act(stat[:, 45:46], stat[:, 45:46], AF.Exp, scale=-0.5), [kst5], [kst5])
            fw.op(A, act(hb2, X1[:, t, :], AF.Copy, scale=stat[:, 45:46]), [kX1[t], kst5], [khb])
            for kk_ in range(2):
                pi = kk_ + 2 * (t % 2)
                pv = pbanks[pi][:, 0:256].bitcast(BF16).rearrange("p (f c) -> p f c", f=4)
                fns = [trf(pv[:, f, :], hb2[:, (4 * kk_ + f) * 128:(4 * kk_ + f + 1) * 128], idb) for f in range(4)]
                fw.mm(fns, [khb, kC], [pk[pi]])
                for f in range(4):
                    k = 4 * kk_ + f
                    if f % 2 == 0:
                        fw.op(V, ts(h2T[:, k, t, :], pv[:, f, :], g3[:, k:k + 1], ALU.mult), [pk[pi], kC], [kh2T])
                    else:
                        fw.op(A, act(h2T[:, k, t, :], pv[:, f, :], AF.Copy, scale=g3[:, k:k + 1]),
                              [pk[pi], kC], [kh2T])
        kO = Tk()
        fw.dma(SY, ov, X1, sem_o, kX1, [kO])
        fw.barrier()
        f2 = Bump(64 * K, 86 * K)
        wg = [f2([128, 8, 128], BF16) for _ in range(2)]
        wu = [f2([128, 8, 128], BF16) for _ in range(2)]
        sg = [f2([128, 512], F32) for _ in range(2)]
        wd = reg(20 * K, [128, NFF, 1024], BF16)
        kwg = [Tk(), Tk()]
        kwd = Tk()
        ksg = [Tk(), Tk()]
        wg_v = w_gate.rearrange("(k p) n -> p k n", p=128)
        wu_v = w_up.rearrange("(k p) n -> p k n", p=128)
        wd_v = w_down.rearrange("(j p) n -> p j n", p=128)
        hv = h2T.rearrange("p k t c -> p k (t c)")
        pc = 0
        for j in range(NFF):
            i = j % 2
            fw.dma(G_, wg[i], wg_v[:, :, j * 128:(j + 1) * 128], sem_g[i], writes=[kwg[i]])
            fw.dma(G_, wu[i], wu_v[:, :, j * 128:(j + 1) * 128], sem_g[i], writes=[kwg[i]])
            fw.dma(G_, wd[:, j, :], wd_v[:, j, :], sem_dn, writes=[kwd])
            for n in range(4):
                pg_, pu_ = 2 * (pc % 4), 2 * (pc % 4) + 1
                si = pc % 2
                pc += 1
                pgv, puv = pbanks[pg_][:, :], pbanks[pu_][:, :]
                fns = [mmf(pgv, wg[i][:, k, :], hv[:, k, n * 512:(n + 1) * 512], k == 0, k == 7) for k in range(8)]
                fw.mm(fns, [kh2T, kwg[i]], [pk[pg_]])
                fns = [mmf(puv, wu[i][:, k, :], hv[:, k, n * 512:(n + 1) * 512], k == 0, k == 7) for k in range(8)]
                fw.mm(fns, [kh2T, kwg[i]], [pk[pu_]])
                fw.op(A, act(sg[si], pgv, AF.Silu), [pk[pg_]], [ksg[si]])
                fw.op(V, tt(fT[:, j].rearrange("p t c -> p (t c)")[:, n * 512:(n + 1) * 512], sg[si], puv, ALU.mult),
                      [ksg[si], pk[pu_]], [kfT])
        fw.barrier()
        f3 = Bump(64 * K, 86 * K)
        xr = [f3([128, 1024], F32) for _ in range(2)]
        ot2 = f3([128, 1024], F32)
        fj3 = f3([128, 512], F32)
        kxr = [Tk(), Tk()]
        for tg in range(4):
            for tl in range(4):
                t = 4 * tg + tl
                if tl < 2:
                    pass
            for j in range(NFF):
                fns = []
                for tl in range(4):
                    t = 4 * tg + tl
                    for hf in range(2):
                        fns.append(mmf(pbanks[2 * tl + hf][:, :], fT[:, j, t, :], wd[:, j, hf * 512:(hf + 1) * 512],
                                       j == 0, j == NFF - 1))
                fw.mm(fns, [kfT, kwd], pk)
            for tl in range(4):
                t = 4 * tg + tl
                xi = t % 2
                fw.dma(SY, xr[xi], ov[:, t, :], sem_x[xi], [kO], [kxr[xi]])
                for hf in range(2):
                    pv = pbanks[2 * tl + hf][:, :]
                    fw.op(A, sqa(fj3, pv, stat[:, 48 + hf:49 + hf]), [pk[2 * tl + hf]], [kfj, kst5])
                fw.op(V, tt(stat[:, 50:51], stat[:, 48:49], stat[:, 49:50], ALU.add), [kst5], [kst5])
                fw.op(V, ts(stat[:, 50:51], stat[:, 50:51], 1.0 / D, ALU.mult, EPS, ALU.add), [kst5], [kst5])
                fw.op(A, act(stat[:, 50:51], stat[:, 50:51], AF.Ln), [kst5], [kst5])
                fw.op(A, act(stat[:, 50:51], stat[:, 50:51], AF.Exp, scale=-0.5), [kst5], [kst5])
                for hf in range(2):
                    pv = pbanks[2 * tl + hf][:, :]
                    sl = slice(hf * 512, (hf + 1) * 512)
                    fw.op(V, stt(ot2[:, sl], pv, stat[:, 50:51], g4[:, sl], ALU.mult, ALU.mult),
                          [pk[2 * tl + hf], kst5, kC], [kot2])
                fw.op(G_, tt(xr[xi], xr[xi], ot2, ALU.add), [kot2, kxr[xi]], [kxr[xi]])
                fw.dma(SY, ov[:, t, :], xr[xi], sem_st[xi], [kxr[xi]], [kO])
        fw.barrier()
    except _Stop:
        pass
    fw.barrier()
    fw.emit()
    return nc, es


_CACHE = {}


def _consts():
    idf = np.eye(128, dtype=np.float32)
    cb = np.zeros((128, 2, 256), np.float32)
    for r in range(2):
        kpos = r * 128 + np.arange(128)[:, None]
        qpos = np.arange(256)[None, :]
        cb[:, r, :] = np.where(kpos <= qpos, 0.0, NEG)
    tm = np.zeros((128, 2, 256), np.float32)
    dm = np.zeros((128, 2, 256), np.float32)
    for j in range(2):
        for s8 in range(8):
            for hi in range(16):
                sp = 8 * j + s8
                row = s8 * 16 + hi
                for jj in range(2):
                    pass
                for tp in range(16):
                    if tp >= sp:
                        tm[row, j, tp * 16:(tp + 1) * 16] = 1.0
                dm[row, j, sp * 16 + hi] = 1.0
    nv = np.tile(np.arange(1, 33, dtype=np.float32)[None, :], (128, 1))
    return idf, cb, tm, dm, nv


def kernel(**inp):
    f32 = np.float32
    x = np.ascontiguousarray(inp['x'], dtype=f32)

    def sq(k):
        return np.ascontiguousarray(inp[k][0], dtype=f32)

    def gp(a):
        a = a.reshape((2, 16) + a.shape[1:])
        a = np.moveaxis(a, 2, 1)
        return np.ascontiguousarray(a.reshape((128, 16) + a.shape[3:]))
    idf, cb, tm, dm, nv = _consts()
    shared = dict(
        w_in=sq('w_in'), w_glu=sq('w_glu'), w_out=sq('w_out'), w_gate=sq('w_gate'), w_up=sq('w_up'),
        w_down=sq('w_down'),
        areT=gp(sq('ssm_a_re')), aimT=gp(sq('ssm_a_im')),
        ldtT=gp(np.ascontiguousarray(np.broadcast_to(sq('ssm_log_dt')[:, None], (32, 64)))),
        breT=gp(sq('ssm_b_re')), bimT=gp(sq('ssm_b_im')),
        creT=gp(np.ascontiguousarray(sq('ssm_c_re').transpose(0, 2, 1))),
        cimT=gp(np.ascontiguousarray(sq('ssm_c_im').transpose(0, 2, 1))),
        drep=np.ascontiguousarray(np.broadcast_to(sq('ssm_d')[None], (128, 32, 16))),
        g1c=np.ascontiguousarray(sq('g_pre_mix').reshape(8, 128).T),
        g3c=np.ascontiguousarray(sq('g_pre_ffn').reshape(8, 128).T),
        g2r=np.ascontiguousarray(np.broadcast_to(sq('g_post_mix')[None], (128, 1024))),
        g4r=np.ascontiguousarray(np.broadcast_to(sq('g_post_ffn')[None], (128, 1024))),
        gsr=np.ascontiguousarray(np.broadcast_to(sq('g_ssm_out')[None], (128, 512))),
        gar=np.ascontiguousarray(np.broadcast_to(sq('g_attn_out')[None], (128, 512))),
        bgr=np.ascontiguousarray(np.broadcast_to(sq('b_glu')[None], (128, 512))),
        c_idf=idf, c_cb=cb, c_tm=tm, c_dm=dm, c_nv=nv,
    )
    if 'nc' not in _CACHE:
        _CACHE['nc'] = build(DEBUG)
    nc, _es = _CACHE['nc']
    in_maps = []
    for c in range(8):
        m = dict(shared)
        m['x'] = np.ascontiguousarray(x[2 * c:2 * c + 2])
        in_maps.append(m)
    res = run_bass_kernel_spmd(nc, in_maps, core_ids=list(range(8)))
    _CACHE['res'] = res
    return np.concatenate([np.asarray(r['out'], dtype=f32) for r in res.results], axis=0)
```

```python
import math
import numpy as np
from contextlib import ExitStack
import concourse.bass as bass
import concourse.mybir as mybir
from concourse.alu_op_type import AluOpType as ALU
from concourse.bass_utils import run_bass_kernel_spmd

F32 = mybir.dt.float32
BF16 = mybir.dt.bfloat16
U8 = mybir.dt.uint8
I32 = mybir.dt.int32
AF = mybir.ActivationFunctionType
AX = mybir.AxisListType

S = 2048
D = 1024
DFF = 2816
NFF = 22
NEG = -30000.0
EPS = 1e-6
ENGS = ['tensor', 'vector', 'scalar', 'gpsimd', 'sync']
DEBUG = False
STAGE = 99
NSTEP = 127
ABQ = 8


class _Stop(Exception):
    pass


class Tk:
    __slots__ = ('w', 'r')

    def __init__(self):
        self.w = {}
        self.r = {}


class FW:
    def __init__(self, nc, es):
        self.nc, self.es = nc, es
        self.prog = {e: [] for e in ENGS}
        self.esem = {e: es.enter_context(nc.semaphore("es_" + e)) for e in ENGS}
        self.ecnt = {e: 0 for e in ENGS}
        self.seen = {e: {} for e in ENGS}
        self.dcnt = {}

    def newsem(self, name):
        s = self.es.enter_context(self.nc.semaphore(name))
        self.dcnt[id(s)] = [s, 0]
        return s

    def _dep(self, eng, reads, writes):
        need = {}

        def add(dct):
            for k, (s, v) in dct.items():
                if need.get(k, (None, 0))[1] < v:
                    need[k] = (s, v)
        for t in reads:
            add(t.w)
        for t in writes:
            add(t.w)
            add(t.r)
        for k, (s, v) in need.items():
            if self.seen[eng].get(k, 0) >= v:
                continue
            self.seen[eng][k] = v
            self.prog[eng].append(('w', s, v))

    def _post(self, tok, reads, writes):
        s, v = tok
        k = id(s)
        for t in reads:
            if t.r.get(k, (None, 0))[1] < v:
                t.r[k] = (s, v)
        for t in writes:
            t.w = {k: (s, v)}
            t.r = {}

    def op(self, eng, fn, reads=(), writes=()):
        self._dep(eng, reads, writes)
        self.ecnt[eng] += 1
        self.prog[eng].append(('o', fn, True))
        self._post((self.esem[eng], self.ecnt[eng]), reads, writes)

    def mm(self, fns, reads=(), writes=()):
        self._dep('tensor', reads, writes)
        for f in fns[:-1]:
            self.prog['tensor'].append(('o', f, False))
        self.ecnt['tensor'] += 1
        self.prog['tensor'].append(('o', fns[-1], True))
        self._post((self.esem['tensor'], self.ecnt['tensor']), reads, writes)

    def dma(self, eng, out, in_, sem, reads=(), writes=()):
        self._dep(eng, reads, writes)
        c = self.dcnt[id(sem)]
        c[1] += 16
        self.prog[eng].append(('d', out, in_, sem))
        self._post((sem, c[1]), reads, writes)

    def barrier(self):
        for e in ENGS:
            for e2 in ENGS:
                if e2 == e or self.ecnt[e2] == 0:
                    continue
                k = id(self.esem[e2])
                if self.seen[e].get(k, 0) < self.ecnt[e2]:
                    self.seen[e][k] = self.ecnt[e2]
                    self.prog[e].append(('w', self.esem[e2], self.ecnt[e2]))
            for k, (s, v) in self.dcnt.items():
                if v > 0 and self.seen[e].get(k, 0) < v:
                    self.seen[e][k] = v
                    self.prog[e].append(('w', s, v))

    def emit(self):
        nc = self.nc
        with nc.Block() as block:
            for e in ENGS:
                prog = self.prog[e]
                sem = self.esem[e]

                def body(E, prog=prog, sem=sem):
                    for it in prog:
                        if it[0] == 'w':
                            E.wait_ge(it[1], it[2])
                        elif it[0] == 'o':
                            ins = it[1](E)
                            if it[2]:
                                ins.then_inc(sem, 1)
                        else:
                            E.dma_start(out=it[1], in_=it[2]).then_inc(it[3], 16)
                getattr(block, e)(body)


def tt(out, in0, in1, op):
    return lambda E: E.tensor_tensor(out=out, in0=in0, in1=in1, op=op)


def ts(out, in0, s1, op0, s2=None, op1=None):
    if op1 is None:
        return lambda E: E.tensor_scalar(out=out, in0=in0, scalar1=s1, scalar2=None, op0=op0)
    return lambda E: E.tensor_scalar(out=out, in0=in0, scalar1=s1, scalar2=s2, op0=op0, op1=op1)


def stt(out, in0, scalar, in1, op0, op1):
    return lambda E: E.scalar_tensor_tensor(out=out, in0=in0, scalar=scalar, in1=in1, op0=op0, op1=op1)


def ttr(out, in0, in1, accum):
    return lambda E: E.activation(out=out, in_=in0, func=AF.Square, accum_out=accum)


def act(out, in_, func, scale=None):
    if scale is None:
        return lambda E: E.activation(out=out, in_=in_, func=func)
    return lambda E: E.activation(out=out, in_=in_, func=func, scale=scale)


def sqa(out, in_, accum):
    return lambda E: E.activation(out=out, in_=in_, func=AF.Square, accum_out=accum)


def rcp(out, in_):
    return lambda E: E.reciprocal(out=out, in_=in_)


def cp(out, in_):
    return lambda E: E.tensor_copy(out=out, in_=in_)


def mmf(out, lhsT, rhs, start, stop):
    assert len(rhs.ap) == 2 and len(lhsT.ap) == 2, ("rhs", rhs.ap, lhsT.ap)
    return lambda E: E.matmul(out, lhsT, rhs, start=start, stop=stop)


def trf(out, in_, ident):
    assert len(ident.ap) == 2 and len(in_.ap) == 2, ("ident", ident.ap, in_.ap)
    return lambda E: E.transpose(out, in_, ident)


def bc(ap, axis, shape):
    return ap.unsqueeze(axis).broadcast_to(list(shape))


def build(dbg=False):
    nc = bass.Bass("TRN2", target_bir_lowering=False)
    es = ExitStack()

    def din(name, shape):
        return nc.dram_tensor(name, list(shape), F32, kind="ExternalInput").ap()

    x = din("x", [2, S, D])
    w_in = din("w_in", [D, 2048])
    w_glu = din("w_glu", [512, 512])
    w_out = din("w_out", [1024, 1024])
    w_gate = din("w_gate", [D, DFF])
    w_up = din("w_up", [D, DFF])
    w_down = din("w_down", [DFF, D])
    areT = din("areT", [128, 16])
    aimT = din("aimT", [128, 16])
    ldtT = din("ldtT", [128, 16])
    breT = din("breT", [128, 16, 16])
    bimT = din("bimT", [128, 16, 16])
    creT = din("creT", [128, 16, 16])
    cimT = din("cimT", [128, 16, 16])
    drep = din("drep", [128, 32, 16])
    g1c = din("g1c", [128, 8])
    g3c = din("g3c", [128, 8])
    g2r = din("g2r", [128, 1024])
    g4r = din("g4r", [128, 1024])
    gsr = din("gsr", [128, 512])
    gar = din("gar", [128, 512])
    bgr = din("bgr", [128, 512])
    c_idf = din("c_idf", [128, 128])
    c_cb = din("c_cb", [128, 2, 256])
    c_tm = din("c_tm", [128, 2, 256])
    c_dm = din("c_dm", [128, 2, 256])
    c_nv = din("c_nv", [128, 32])
    out = nc.dram_tensor("out", [2, S, D], F32, kind="ExternalOutput").ap()
    GT_d = nc.dram_tensor("GT_d", [32, 128, 2, 2, 128], BF16, kind="Internal").ap()
    TOEP_d = nc.dram_tensor("TOEP_d", [32, 128, 2, 256], BF16, kind="Internal").ap()
    CT_d = nc.dram_tensor("CT_d", [16, 128, 2, 256], BF16, kind="Internal").ap()
    dbg_t = {}
    if dbg:
        dbg_t['uc'] = nc.dram_tensor("d_uc", [128, 32, 16, 16], BF16, kind="ExternalOutput").ap()
        dbg_t['qT'] = nc.dram_tensor("d_qT", [128, 4, S], BF16, kind="ExternalOutput").ap()
        dbg_t['vt'] = nc.dram_tensor("d_vt", [128, 16, 8, 65], BF16, kind="ExternalOutput").ap()
        dbg_t['y1'] = nc.dram_tensor("d_y1", [128, 16, 512], BF16, kind="ExternalOutput").ap()
        dbg_t['ssmT'] = nc.dram_tensor("d_ssmT", [128, 4, 16, 128], BF16, kind="ExternalOutput").ap()
        dbg_t['attT'] = nc.dram_tensor("d_attT", [128, 4, S], BF16, kind="ExternalOutput").ap()
        dbg_t['x1'] = nc.dram_tensor("d_x1", [128, 16, 1024], F32, kind="ExternalOutput").ap()
        dbg_t['S'] = nc.dram_tensor("d_S", [128, 2, 16, 128], F32, kind="ExternalOutput").ap()

    ARENA = 206 * 1024
    arena = es.enter_context(nc.sbuf_tensor("arena", [128, ARENA], U8))
    pbanks = [es.enter_context(nc.psum_tensor("pb%d" % i, [128, 512], F32)) for i in range(8)]
    pk = [Tk() for _ in range(8)]
    fw = FW(nc, es)

    def reg(off, shape, dt, p0=0):
        esz = 2 if dt == BF16 else 4
        n = int(np.prod(shape[1:])) * esz
        assert off % 4 == 0 and off + n <= ARENA, (off, n)
        ap = arena[p0:p0 + shape[0], off:off + n].bitcast(dt)
        if len(shape) == 3:
            ap = ap.rearrange("p (a b) -> p a b", a=shape[1])
        elif len(shape) == 4:
            ap = ap.rearrange("p (a b c) -> p a b c", a=shape[1], b=shape[2])
        elif len(shape) == 5:
            ap = ap.rearrange("p (a b c d) -> p a b c d", a=shape[1], b=shape[2], c=shape[3])
        return ap

    class Bump:
        def __init__(self, lo, hi):
            self.o, self.hi = lo, hi

        def __call__(self, shape, dt, p0=0):
            esz = 2 if dt == BF16 else 4
            n = int(np.prod(shape[1:])) * esz
            n4 = (n + 31) // 32 * 32
            a = reg(self.o, shape, dt, p0)
            self.o += n4
            assert self.o <= self.hi, (self.o, self.hi)
            return a

    K = 1024
    V, A, G_, T_, SY = 'vector', 'scalar', 'gpsimd', 'tensor', 'sync'

    cb = Bump(0, 20 * K)
    idf = cb([128, 128], F32)
    idb = cb([128, 128], BF16)
    cbias = cb([128, 2, 256], BF16)
    g1 = cb([128, 8], F32)
    g3 = cb([128, 8], F32)
    g2 = cb([128, 1024], F32)
    g4 = cb([128, 1024], F32)
    gs = cb([128, 512], F32)
    ga = cb([128, 512], F32)
    bg = cb([128, 512], F32)
    stat = cb([128, 64], F32)
    LR = cb([128, 7, 16], F32)
    LI = cb([128, 7, 16], F32)
    LIn = cb([128, 7, 16], F32)
    LAk = cb([128, 7, 2, 16], F32)
    assert cb.o <= 20 * K
    kC = Tk()
    sem_c = fw.newsem("sem_c")
    sem_c2 = fw.newsem("sem_c2")
    for dst, src in [(idf, c_idf), (g1, g1c), (g3, g3c), (g2, g2r), (g4, g4r), (gs, gsr), (ga, gar), (bg, bgr)]:
        fw.dma(SY, dst, src, sem_c, writes=[kC])

    sb = Bump(20 * K, ARENA)
    cb_off = sb.o
    CBre = sb([128, 16, 16, 16], F32)
    cbf = sb([128, 2, 256], F32)
    tmk = sb([128, 2, 256], F32)
    dmk = sb([128, 2, 256], F32)
    nv = sb([128, 32], F32)
    drp = sb([128, 32, 16], F32)
    are = sb([128, 16], F32)
    aim = sb([128, 16], F32)
    ldt = sb([128, 16], F32)
    bre = sb([128, 16, 16], F32)
    bim = sb([128, 16, 16], F32)
    cre = sb([128, 16, 16], F32)
    cim = sb([128, 16, 16], F32)
    kS = Tk()
    for dst, src in [(cbf, c_cb), (tmk, c_tm), (dmk, c_dm), (nv, c_nv), (drp, drep), (are, areT), (aim, aimT),
                     (ldt, ldtT), (bre, breT), (bim, bimT), (cre, creT), (cim, cimT)]:
        fw.dma(SY, dst, src, sem_c2, writes=[kS])
    fw.op(V, cp(idb, idf), [kC], [kC])
    fw.op(V, cp(cbias, cbf), [kS, kC], [kC])

    dtt = sb([128, 16], F32)
    adr = sb([128, 16], F32)
    adi = sb([128, 16], F32)
    ex = sb([128, 16, 32], F32)
    ang = sb([128, 16, 32], F32)
    mag = sb([128, 16, 32], F32)
    magi = sb([128, 16, 32], F32)
    r1 = sb([128, 16, 32], F32)
    r2 = sb([128, 16, 32], F32)
    sn = sb([128, 16, 32], F32)
    cs = sb([128, 16, 32], F32)
    Pre = sb([128, 16, 32], F32)
    Pim = sb([128, 16, 32], F32)
    Qre = sb([128, 16, 32], F32)
    Qim = sb([128, 16, 32], F32)
    sm = [sb([128, 16], F32) for _ in range(10)]
    bbre = sb([128, 16, 16], F32)
    bbim = sb([128, 16, 16], F32)
    tb1 = sb([128, 16, 16], F32)
    tb2 = sb([128, 16, 16], F32)
    k0 = Tk()
    fw.op(A, act(dtt, ldt, AF.Exp), [kS], [k0])
    fw.op(V, tt(adr, are, dtt, ALU.mult), [k0, kS], [k0])
    fw.op(V, tt(adi, aim, dtt, ALU.mult), [k0, kS], [k0])
    fw.op(V, tt(ex, bc(adr, 2, [128, 16, 32]), bc(nv, 1, [128, 16, 32]), ALU.mult), [k0, kS], [k0])
    fw.op(V, tt(ang, bc(adi, 2, [128, 16, 32]), bc(nv, 1, [128, 16, 32]), ALU.mult), [k0, kS], [k0])
    fw.op(A, act(mag, ex, AF.Exp), [k0], [k0])
    fw.op(A, act(magi, ex, AF.Exp, scale=-1.0), [k0], [k0])
    ki = reg(cb_off, [128, 16, 32], I32)
    kf = reg(cb_off + 2048, [128, 16, 32], F32)
    mk = reg(cb_off + 4096, [128, 16, 32], F32)

    def range_reduce(r, shift):
        fw.op(V, ts(r, ang, 1.0 / (2 * math.pi), ALU.mult, shift, ALU.add), [k0], [k0])
        fw.op(V, cp(ki, r), [k0], [k0])
        fw.op(V, cp(kf, ki), [k0], [k0])
        fw.op(V, tt(r, r, kf, ALU.subtract), [k0], [k0])
        fw.op(V, ts(mk, r, -0.5, ALU.is_lt), [k0], [k0])
        fw.op(V, tt(r, r, mk, ALU.add), [k0], [k0])
        fw.op(V, ts(mk, r, 0.5, ALU.is_gt), [k0], [k0])
        fw.op(V, tt(r, r, mk, ALU.subtract), [k0], [k0])
    range_reduce(r1, 0.0)
    range_reduce(r2, 0.25)
    fw.op(A, act(sn, r1, AF.Sin, scale=2 * math.pi), [k0], [k0])
    fw.op(A, act(cs, r2, AF.Sin, scale=2 * math.pi), [k0], [k0])
    fw.op(V, tt(Pre, mag, cs, ALU.mult), [k0], [k0])
    fw.op(V, tt(Pim, mag, sn, ALU.mult), [k0], [k0])
    fw.op(V, tt(Qre, magi, cs, ALU.mult), [k0], [k0])
    fw.op(V, stt(Qim, magi, -1.0, sn, ALU.mult, ALU.mult), [k0], [k0])
    lbre, lbim = Pre[:, :, 0], Pim[:, :, 0]
    nr, den, t0, t1_, cr, ci, rden = sm[0], sm[1], sm[2], sm[3], sm[4], sm[5], sm[6]
    fw.op(V, ts(nr, lbre, -1.0, ALU.add), [k0], [k0])
    fw.op(V, tt(den, are, are, ALU.mult), [k0, kS], [k0])
    fw.op(V, tt(t0, aim, aim, ALU.mult), [k0, kS], [k0])
    fw.op(V, tt(den, den, t0, ALU.add), [k0], [k0])
    fw.op(V, lambda E: E.reciprocal(out=rden, in_=den), [k0], [k0])
    fw.op(V, tt(t0, nr, are, ALU.mult), [k0], [k0])
    fw.op(V, tt(t1_, lbim, aim, ALU.mult), [k0], [k0])
    fw.op(V, tt(t0, t0, t1_, ALU.add), [k0], [k0])
    fw.op(V, tt(cr, t0, rden, ALU.mult), [k0], [k0])
    fw.op(V, tt(t0, lbim, are, ALU.mult), [k0], [k0])
    fw.op(V, tt(t1_, nr, aim, ALU.mult), [k0], [k0])
    fw.op(V, tt(t0, t0, t1_, ALU.subtract), [k0], [k0])
    fw.op(V, tt(ci, t0, rden, ALU.mult), [k0], [k0])
    crb, cib = bc(cr, 2, [128, 16, 16]), bc(ci, 2, [128, 16, 16])
    fw.op(V, tt(tb1, crb, bre, ALU.mult), [k0, kS], [k0])
    fw.op(V, tt(tb2, cib, bim, ALU.mult), [k0, kS], [k0])
    fw.op(V, tt(bbre, tb1, tb2, ALU.subtract), [k0], [k0])
    fw.op(V, tt(tb1, crb, bim, ALU.mult), [k0, kS], [k0])
    fw.op(V, tt(tb2, cib, bre, ALU.mult), [k0, kS], [k0])
    fw.op(V, tt(bbim, tb1, tb2, ALU.add), [k0], [k0])
    fw.op(V, cp(LR[:, 0, :], Pre[:, :, 15]), [k0], [kC])
    fw.op(V, cp(LI[:, 0, :], Pim[:, :, 15]), [k0], [kC])
    for k in range(6):
        fw.op(V, tt(sm[7], LR[:, k, :], LR[:, k, :], ALU.mult), [kC, k0], [k0])
        fw.op(V, tt(sm[8], LI[:, k, :], LI[:, k, :], ALU.mult), [kC, k0], [k0])
        fw.op(V, tt(LR[:, k + 1, :], sm[7], sm[8], ALU.subtract), [k0], [kC])
        fw.op(V, tt(sm[9], LR[:, k, :], LI[:, k, :], ALU.mult), [kC, k0], [k0])
        fw.op(V, ts(LI[:, k + 1, :], sm[9], 2.0, ALU.mult), [k0], [kC])
    fw.op(V, ts(LIn, LI, -1.0, ALU.mult), [kC], [kC])
    fw.op(V, cp(LAk[:, :, 0, :], LR), [kC], [kC])
    fw.op(V, cp(LAk[:, :, 1, :], LR), [kC], [kC])
    SH4 = [128, 16, 16, 16]
    Gre = sb(SH4, F32)
    Gim = sb(SH4, F32)
    CAre = sb(SH4, F32)
    CAim = sb(SH4, F32)
    CBim = sb(SH4, F32)
    u1_off = sb.o
    U1 = sb(SH4, F32)
    U2 = sb(SH4, F32)
    U3, U4 = U1, U2
    CTst = reg(u1_off, [128, 16, 2, 256], BF16)
    kG, kCA, kCB, kU1, kU2, kCT = [Tk() for _ in range(6)]
    kU3, kU4 = kU1, kU2

    def outer(eng, o, a, pn, rd, wr):
        fw.op(eng, tt(o, bc(a, 2, SH4), bc(pn, 3, SH4), ALU.mult), rd, wr)

    def fl(ap):
        return ap.rearrange("p a b c -> p (a b c)")
    outer(V, U1, bbre, Qre[:, :, 0:16], [k0], [kU1])
    outer(G_, U2, bbim, Qim[:, :, 0:16], [k0], [kU2])
    fw.op(V, tt(Gre, U1, U2, ALU.subtract), [kU1, kU2], [kG])
    outer(V, U3, bbre, Qim[:, :, 0:16], [k0], [kU3])
    outer(G_, U4, bbim, Qre[:, :, 0:16], [k0], [kU4])
    fw.op(V, tt(Gim, U3, U4, ALU.add), [kU3, kU4], [kG])
    outer(V, U1, cre, Pre[:, :, 0:16], [k0, kS], [kU1])
    outer(G_, U2, cim, Pim[:, :, 0:16], [k0, kS], [kU2])
    fw.op(V, tt(CAre, U1, U2, ALU.subtract), [kU1, kU2], [kCA])
    outer(V, U3, cre, Pim[:, :, 0:16], [k0, kS], [kU3])
    outer(G_, U4, cim, Pre[:, :, 0:16], [k0, kS], [kU4])
    fw.op(V, stt(fl(CAim), fl(U3), -1.0, fl(U4), ALU.mult, ALU.subtract), [kU3, kU4], [kCA])
    outer(V, U1, cre, Pre[:, :, 16:32], [k0, kS], [kU1])
    outer(G_, U2, cim, Pim[:, :, 16:32], [k0, kS], [kU2])
    fw.op(V, tt(CBre, U1, U2, ALU.subtract), [kU1, kU2], [kCB])
    outer(V, U3, cre, Pim[:, :, 16:32], [k0, kS], [kU3])
    outer(G_, U4, cim, Pre[:, :, 16:32], [k0, kS], [kU4])
    fw.op(V, stt(fl(CBim), fl(U3), -1.0, fl(U4), ALU.mult, ALU.subtract), [kU3, kU4], [kCB])
    fw.op(V, cp(CTst[:, :, 0, :], CBre.rearrange("p g t h -> p g (t h)")), [kCB], [kCT, kU1])
    fw.op(V, cp(CTst[:, :, 1, :], CBim.rearrange("p g t h -> p g (t h)")), [kCB], [kCT, kU1])
    sem_t = fw.newsem("sem_t")
    kDR = Tk()
    fw.dma(SY, CT_d.rearrange("g p r c -> p g r c"), CTst, sem_t, [kCT], [kDR])
    TPst = [sb([128, 2, 256], BF16) for _ in range(2)]
    GTst = [sb([128, 2, 2, 128], BF16) for _ in range(2)]
    TPt = [sb([128, 2, 256], F32) for _ in range(2)]
    TPd = [sb([128, 2, 256], F32) for _ in range(2)]
    kTP = [Tk() for _ in range(2)]
    kGTs = [Tk() for _ in range(2)]
    kTt = [Tk() for _ in range(2)]
    sem_tp = [fw.newsem("sem_tp%d" % i) for i in range(2)]
    sem_gt = [fw.newsem("sem_gt%d" % i) for i in range(2)]
    for gh in range(2):
        pr = slice(64 * gh, 64 * gh + 64)
        for gl in range(16):
            g = 16 * gh + gl
            i = g % 2
            pT, pG = 2 * i, 2 * i + 1
            pt_v = pbanks[pT][:, :].rearrange("p (j c) -> p j c", j=2)
            fns = []
            for j in range(2):
                l_re = Gre[pr, gl, 8 * j:8 * j + 8, :].rearrange("p s h -> p (s h)")
                l_im = Gim[pr, gl, 8 * j:8 * j + 8, :].rearrange("p s h -> p (s h)")
                fns.append(mmf(pt_v[:, j, :], l_re, CAre[pr, gl].rearrange("p t h -> p (t h)"), True, False))
                fns.append(mmf(pt_v[:, j, :], l_im, CAim[pr, gl].rearrange("p t h -> p (t h)"), False, True))
            fw.mm(fns, [kG, kCA], [pk[pT]])
            pg_v = pbanks[pG][:, :].rearrange("p (j r c) -> p j r c", j=2, r=2)
            fns = []
            for j in range(2):
                l_re = Gre[pr, gl, 8 * j:8 * j + 8, :].rearrange("p s h -> p (s h)")
                l_im = Gim[pr, gl, 8 * j:8 * j + 8, :].rearrange("p s h -> p (s h)")
                fns.append(mmf(pg_v[:, j, 0, :], l_re, idf[pr, :], True, True))
                fns.append(mmf(pg_v[:, j, 1, :], l_im, idf[pr, :], True, True))
            fw.mm(fns, [kG, kC], [pk[pG]])
            fw.op(V, tt(TPt[i], pt_v, tmk, ALU.mult), [pk[pT], kS], [kTt[i]])
            fw.op(G_, tt(TPd[i].rearrange("p j (t h) -> p j t h", t=16),
                         dmk.rearrange("p j (t h) -> p j t h", t=16),
                         drp[:, g, :].unsqueeze(1).unsqueeze(1).broadcast_to([128, 2, 16, 16]), ALU.mult),
                  [kS], [kTP[i]])
            fw.op(V, tt(TPst[i], TPt[i], TPd[i], ALU.add), [kTt[i], kTP[i]], [kTP[i]])
            fw.dma(SY, TOEP_d[g], TPst[i], sem_tp[i], [kTP[i]], [kDR])
            fw.op(A, act(GTst[i], pg_v, AF.Copy), [pk[pG]], [kGTs[i]])
            fw.dma(SY, GT_d[g], GTst[i], sem_gt[i], [kGTs[i]], [kDR])
    fw.barrier()

    X1 = reg(20 * K, [128, 16, 1024], F32)
    attT = reg(84 * K, [128, 4, S], BF16)
    ssmT = reg(100 * K, [128, 4, 16, 128], BF16)
    Uc = reg(116 * K, [128, 32, 16, 16], BF16)
    qT = reg(132 * K, [128, 4, S], BF16)
    kT = reg(148 * K, [128, 4, S], BF16)
    Vt = reg(164 * K, [128, 16, 8, 65], BF16)
    sem_x = [fw.newsem("sem_x%d" % i) for i in range(4)]
    sem_w = [fw.newsem("sem_w%d" % i) for i in range(2)]
    sem_o = fw.newsem("sem_o")
    sem_og = fw.newsem("sem_og")
    sem_wq = [fw.newsem("sem_wq%d" % i) for i in range(2)]
    sem_d = fw.newsem("sem_d")
    sem_g = [fw.newsem("sem_g%d" % i) for i in range(2)]
    sem_dn = fw.newsem("sem_dn")
    sem_st = [fw.newsem("sem_st%d" % i) for i in range(2)]
    kOut = Tk()

    try:
      if STAGE < 1:
        raise _Stop()
      for b in range(2):
        a1 = Bump(20 * K, 116 * K)
        Xst = [a1([128, 2, 1024], F32) for _ in range(2)]
        hbf = [a1([128, 2, 1024], BF16) for _ in range(2)]
        hT = a1([128, 8, S], BF16)
        wbuf = [a1([128, 8, 256], BF16) for _ in range(2)]
        junk = a1([128, 1024], F32)
        kX = [Tk(), Tk()]
        kH = [Tk(), Tk()]
        khT = [Tk() for _ in range(8)]
        kW = [Tk(), Tk()]
        kJ = Tk()
        kSt = Tk()
        kUc, kq, kk, kv = Tk(), Tk(), Tk(), Tk()
        xv = x[b].rearrange("(c t) d -> c t d", t=16)
        ss = stat[:, 0:16]
        rs = stat[:, 16:32]
        for tp in range(8):
            i = tp % 2
            fw.dma(SY, Xst[i], xv[:, 2 * tp:2 * tp + 2, :], sem_x[i], writes=[kX[i]])
            for u in range(2):
                t = 2 * tp + u
                fw.op(A, ttr(junk, Xst[i][:, u, :], Xst[i][:, u, :], ss[:, t:t + 1]), [kX[i]], [kJ, kSt])
                fw.op(V, ts(rs[:, t:t + 1], ss[:, t:t + 1], 1.0 / D, ALU.mult, EPS, ALU.add), [kSt], [kSt])
                fw.op(A, act(rs[:, t:t + 1], rs[:, t:t + 1], AF.Ln), [kSt], [kSt])
                fw.op(A, act(rs[:, t:t + 1], rs[:, t:t + 1], AF.Exp, scale=-0.5), [kSt], [kSt])
                fw.op(A, act(hbf[i][:, u, :], Xst[i][:, u, :], AF.Copy, scale=rs[:, t:t + 1]), [kX[i], kSt], [kH[i]])
            for k in range(8):
                pi = k % 4
                pv = pbanks[pi][:, 0:128].bitcast(BF16).rearrange("p (u c) -> p u c", u=2)
                fns = [trf(pv[:, u, :], hbf[i][:, u, k * 128:(k + 1) * 128], idb) for u in range(2)]
                fw.mm(fns, [kH[i], kC], [pk[pi]])
                dst = hT[:, k, :].rearrange("p (c t) -> p t c", t=16)[:, 2 * tp:2 * tp + 2, :]
                if k % 2 == 0:
                    fw.op(V, ts(dst, pv, g1[:, k:k + 1], ALU.mult), [pk[pi], kC], [khT[k]])
                else:
                    fw.op(A, act(dst, pv, AF.Copy, scale=g1[:, k:k + 1]), [pk[pi], kC], [khT[k]])
        win_v = w_in.rearrange("(k p) n -> p k n", p=128)
        pcount = 0
        for piece in range(8):
            i = piece % 2
            fw.dma(G_, wbuf[i], win_v[:, :, piece * 256:(piece + 1) * 256], sem_wq[i], writes=[kW[i]])
            if piece < 2:
                for t in range(16):
                    pi = 4 + (pcount % 4)
                    pcount += 1
                    pv = pbanks[pi][:, 0:256]
                    fns = [mmf(pv, hT[:, k, t::16], wbuf[i][:, k, :], k == 0, k == 7) for k in range(8)]
                    fw.mm(fns, khT + [kW[i]], [pk[pi]])
                    dst = Uc[:, 16 * piece:16 * piece + 16, t, :]
                    srcv = pv.rearrange("p (g h) -> p g h", g=16)
                    if t % 2 == 0:
                        fw.op(V, cp(dst, srcv), [pk[pi]], [kUc])
                    else:
                        fw.op(A, act(dst, srcv, AF.Copy), [pk[pi]], [kUc])
            elif piece < 6:
                dstT, kd, sc = (qT, kq, 0.125) if piece < 4 else (kT, kk, 1.0)
                for mm_ in range(2):
                    m = (piece % 2) * 2 + mm_
                    for n in range(4):
                        pi = 4 + (pcount % 4)
                        pcount += 1
                        pv = pbanks[pi][:, :]
                        fns = [mmf(pv, wbuf[i][:, k, mm_ * 128:(mm_ + 1) * 128], hT[:, k, n * 512:(n + 1) * 512],
                                   k == 0, k == 7) for k in range(8)]
                        fw.mm(fns, khT + [kW[i]], [pk[pi]])
                        dst = dstT[:, m, n * 512:(n + 1) * 512]
                        if n % 2 == 0:
                            fw.op(V, ts(dst, pv, sc, ALU.mult), [pk[pi]], [kd])
                        else:
                            fw.op(A, act(dst, pv, AF.Copy, scale=sc), [pk[pi]], [kd])
            else:
                hh = piece - 6
                for it in range(16):
                    pi = 4 + (pcount % 4)
                    pcount += 1
                    pv = pbanks[pi][:, 0:256]
                    fns = [mmf(pv, hT[:, k, it * 128:(it + 1) * 128], wbuf[i][:, k, :], k == 0, k == 7)
                           for k in range(8)]
                    fw.mm(fns, khT + [kW[i]], [pk[pi]])
                    dst = Vt[:, it, 4 * hh:4 * hh + 4, 0:64]
                    src = pv.rearrange("p (h d) -> p h d", h=4)
                    if it % 2 == 0:
                        fw.op(V, cp(dst, src), [pk[pi]], [kv])
                    else:
                        fw.op(A, act(dst, src, AF.Copy), [pk[pi]], [kv])
        fw.op(G_, lambda E: E.memset(Vt[:, :, :, 64:65], 1.0), [kv], [kv])
        if dbg and b == 0:
            fw.dma(SY, dbg_t['uc'], Uc, sem_d, [kUc], [kOut])
            fw.dma(SY, dbg_t['qT'], qT, sem_d, [kq], [kOut])
            fw.dma(SY, dbg_t['vt'], Vt, sem_d, [kv], [kOut])
        fw.barrier()

        if STAGE < 2:
            raise _Stop()
        a2 = Bump(181 * K, ARENA)
        Sbf = a2([128, 2, 16, 130], BF16)
        a2b = Bump(20 * K, 84 * K)
        UT = a2b([128, 32, 2, 128], BF16)
        Wf = a2b([128, 2, 16, 128], F32)
        GTb = [a2b([128, 2, 2, 2, 128], BF16) for _ in range(2)]
        T1 = a2b([128, 2, 16], F32)
        T2 = a2b([128, 2, 16], F32)
        kUT, kWf, kSb, kT1, kT2 = Tk(), Tk(), Tk(), Tk(), Tk()
        kGTb = [Tk(), Tk()]
        for g in range(32):
            pi = g % 4
            pv = pbanks[pi][:, 0:128].bitcast(BF16).rearrange("p (j c) -> p j c", j=2)
            fns = [trf(pv[:, j, :], Uc[:, g, 8 * j:8 * j + 8, :].rearrange("p s h -> p (s h)"), idb) for j in range(2)]
            fw.mm(fns, [kUc, kC], [pk[pi]])
            if g % 2 == 0:
                fw.op(V, cp(UT[:, g], pv), [pk[pi]], [kUT])
            else:
                fw.op(A, act(UT[:, g], pv, AF.Copy), [pk[pi]], [kUT])
        if STAGE < 2.2:
            raise _Stop()
        for gl in range(16):
            i = gl % 2
            for gh in range(2):
                fw.dma(SY, GTb[i][:, gh], GT_d[16 * gh + gl], sem_w[i], writes=[kGTb[i]])
            pi = 4 + gl % 4
            pv = pbanks[pi][:, 0:256].rearrange("p (r c) -> p r c", r=2)
            fns = []
            for r in range(2):
                n = 0
                for gh in range(2):
                    for j in range(2):
                        fns.append(mmf(pv[:, r, :], GTb[i][:, gh, j, r, :], UT[:, 16 * gh + gl, j, :], n == 0, n == 3))
                        n += 1
            fw.mm(fns, [kUT, kGTb[i]], [pk[pi]])
            fw.op(V, cp(Wf[:, :, gl, :], pv), [pk[pi]], [kWf])
        if STAGE < 2.5:
            raise _Stop()
        WfB = reg(116 * K, [128, 2, 16, 128], F32)
        TT = reg(190 * K, [128, 2, 16, 128], F32)
        kWB, kTT = kUc, Tk()
        ks = {'cur': Wf, 'nxt': WfB, 'kcur': kWf, 'knxt': kWB}

        def ks_level(k):
            d = 1 << k
            n = 128 - d
            cur, nxt, kcur, knxt = ks['cur'], ks['nxt'], ks['kcur'], ks['knxt']
            fw.op(V, tt(TT[:, :, :, 0:n], cur[:, :, :, 0:n], bc(LAk[:, k], 3, [128, 2, 16, n]), ALU.mult),
                  [kcur, kC], [kTT])
            fw.op(V, tt(nxt[:, :, :, d:128], cur[:, :, :, d:128], TT[:, :, :, 0:n], ALU.add), [kcur, kTT], [knxt])
            fw.op(V, tt(TT[:, 0, :, 0:n], cur[:, 1, :, 0:n], bc(LIn[:, k, :], 2, [128, 16, n]), ALU.mult),
                  [kcur, kC], [kTT])
            fw.op(V, tt(TT[:, 1, :, 0:n], cur[:, 0, :, 0:n], bc(LI[:, k, :], 2, [128, 16, n]), ALU.mult),
                  [kcur, kC], [kTT])
            fw.op(V, tt(nxt[:, :, :, d:128], nxt[:, :, :, d:128], TT[:, :, :, 0:n], ALU.add), [knxt, kTT], [knxt])
            fw.op(V, cp(nxt[:, :, :, 0:d], cur[:, :, :, 0:d]), [kcur], [knxt])
            ks['cur'], ks['nxt'], ks['kcur'], ks['knxt'] = nxt, cur, knxt, kcur

        def ks_finish():
            Wfin, kWfin = ks['cur'], ks['kcur']
            fw.op(V, lambda E: E.memset(Sbf[:, :, :, 0:2], 0.0), [], [kSb])
            for r in range(2):
                fw.op(A, act(Sbf[:, r, :, 2:130], Wfin[:, r], AF.Copy), [kWfin], [kSb])
            if dbg and b == 0:
                fw.dma(SY, dbg_t['S'], Wfin, sem_d, [kWfin], [kOut])

        if STAGE < 3:
            raise _Stop()
        a3 = Bump(52 * K, 84 * K)
        MBT = a3([128, S], BF16)
        kmT = a3([128, 4, 8], BF16)
        kmf = a3([128, 4, 8], F32)
        gt = a3([128, 8, 8], F32)
        top8 = a3([128, 8, 8], F32)
        mbt = a3([128, 2, 64], BF16)
        PT = [a3([128, 256], BF16) for _ in range(3)]
        Ao = a3([128, 2, 512], F32)
        Aj = a3([128, 512], F32)
        Ab = a3([128, 2, 512], BF16)
        rden_a = a3([128, 4], F32)
        kMB, kkm, kgt, kmb = Tk(), Tk(), Tk(), Tk()
        Esel = reg(100 * K, [128, 64, 128], BF16)
        kEs = Tk()
        fw.op(V, cp(Esel[0:64], bc(idb[0:64, 0:64], 2, [64, 64, 128])), [kC], [kEs])
        fw.op(V, cp(Esel[64:128], bc(idb[64:128, 64:128], 2, [64, 64, 128])), [kC], [kEs])
        kPT = [Tk() for _ in range(3)]
        kAo, kAj, kAb, kat, krd = Tk(), Tk(), Tk(), Tk(), Tk()
        if STAGE < 3.05:
            raise _Stop()
        kjunk = a3([128, 256], BF16)
        kkj = Tk()
        for m in range(4):
            for n in range(8):
                fw.op(A, (lambda m=m, n=n: (lambda E: E.activation(out=kjunk, in_=kT[:, m, n * 256:(n + 1) * 256],
                                                                   func=AF.Copy, accum_out=kmf[:, m, n:n + 1])))(),
                      [kk], [kkj, kkm])
        if STAGE < 3.08:
            raise _Stop()
        fw.op(A, act(kmT, kmf, AF.Copy, scale=1.0 / 256), [kkm], [kkm])
        if STAGE < 3.1:
            raise _Stop()
        fw.op(G_, lambda E: E.memset(MBT, 0.0), [], [kMB, kGTb[0], kGTb[1]])
        for qt in range(8, 16):
            bq = qt // 2
            pve = pbanks[0][:, 0:32].rearrange("p (h n) -> p h n", h=4)
            pvo = pbanks[1][:, 0:32].rearrange("p (h n) -> p h n", h=4)
            fe = [mmf(pve[:, h2, :], qT[0:64, h2, qt * 128:(qt + 1) * 128], kmT[0:64, h2, :], True, True)
                  for h2 in range(4)]
            fw.mm(fe, [kq, kkm], [pk[0]])
            fo = [mmf(pvo[:, h2, :], qT[64:128, h2, qt * 128:(qt + 1) * 128], kmT[64:128, h2, :], True, True)
                  for h2 in range(4)]
            fw.mm(fo, [kq, kkm], [pk[1]])
            fw.op(G_, lambda E: E.memset(gt, NEG), [kgt], [kgt])
            gtv = gt.rearrange("p (h2 e) n -> p h2 e n", e=2)
            fw.op(V, cp(gtv[:, :, 0, 0:bq], pve[:, :, 0:bq]), [pk[0], kgt], [kgt])
            fw.op(V, cp(gtv[:, :, 1, 0:bq], pvo[:, :, 0:bq]), [pk[1], kgt], [kgt])
            for h in range(8):
                fw.op(V, (lambda hh: (lambda E: E.max(out=top8[:, hh, :], in_=gt[:, hh, :])))(h), [kgt], [kmb])
            for dup in range(2):
                fw.op(V, tt(mbt[:, dup, :].rearrange("p (h n) -> p h n", h=8), gt,
                            top8[:, :, 2:3].broadcast_to([128, 8, 8]), ALU.is_lt), [kgt, kmb], [kmb])
            pi2 = 2 + qt % 2
            pv2 = pbanks[pi2][:, 0:64].bitcast(BF16)
            fw.mm([trf(pv2, mbt.rearrange("p d c -> p (d c)"), idb)], [kmb, kC], [pk[pi2]])
            fw.op(V, ts(MBT[:, qt * 128:(qt + 1) * 128], pv2, NEG, ALU.mult), [pk[pi2]], [kMB])
        if STAGE < 3.2:
            raise _Stop()
        ucount = 0
        LAG = 2
        for bq in range(ABQ):
            nkt = 2 * bq + 2

            def stage_a(h, kt, pi, bq=bq):
                pr = slice(64 * (h % 2), 64 * (h % 2) + 64)
                m = h // 2
                n = kt // 2
                pv = pbanks[pi][:, 0:256]
                fns = [mmf(pv, kT[pr, m, kt * 128:(kt + 1) * 128], qT[pr, m, bq * 256:(bq + 1) * 256], True, False)]
                rd = [kq, kk]
                if n == bq:
                    fns.append(mmf(pv, idb, cbias[:, kt - 2 * bq, :], False, True))
                    rd.append(kC)
                elif bq >= 4:
                    r = h * 8 + n
                    fns.append(mmf(pv, Esel[pr, r, :], MBT[pr, bq * 256:(bq + 1) * 256], False, True))
                    rd += [kEs, kMB]
                else:
                    fns[0] = mmf(pv, kT[pr, m, kt * 128:(kt + 1) * 128], qT[pr, m, bq * 256:(bq + 1) * 256], True, True)
                fw.mm(fns, rd, [pk[pi]])
                fw.op(A, act(PT[pi], pv, AF.Exp), [pk[pi]], [kPT[pi]])

            def stage_b(h, kt, pi, nkt=nkt):
                pos = [4 + 2 * (h % 2), 5 + 2 * (h % 2)]
                povs = [pbanks[pos[0]][:, 0:65], pbanks[pos[1]][:, 0:65]]
                fns = [mmf(povs[u], PT[pi][:, u * 128:(u + 1) * 128], Vt[:, kt, h, :], kt == 0, kt == nkt - 1)
                       for u in range(2)]
                fw.mm(fns, [kPT[pi], kv], [pk[pos[0]], pk[pos[1]]])
                if kt == nkt - 1:
                    for u in range(2):
                        fw.op(V, rcp(rden_a[:, u:u + 1], povs[u][:, 64:65]), [pk[pos[u]]], [krd])
                        fw.op(V, ts(Ao[:, u, h * 64:(h + 1) * 64], povs[u][:, 0:64], rden_a[:, u:u + 1], ALU.mult),
                              [pk[pos[u]], krd], [kAo])

            pend = []
            for h in range(8):
                for kt in range(nkt):
                    pi = ucount % 3
                    ucount += 1
                    stage_a(h, kt, pi)
                    pend.append((h, kt, pi))
                    if len(pend) > LAG:
                        stage_b(*pend.pop(0))
            while pend:
                stage_b(*pend.pop(0))
            if bq < 7:
                ks_level(bq)
            else:
                ks_finish()
            for u in range(2):
                fw.op(A, ttr(Aj, Ao[:, u, :], Ao[:, u, :], rden_a[:, 2:3]), [kAo], [kAj, krd])
                fw.op(V, ts(rden_a[:, 3:4], rden_a[:, 2:3], 1.0 / 512, ALU.mult, EPS, ALU.add), [krd], [krd])
                fw.op(A, act(rden_a[:, 3:4], rden_a[:, 3:4], AF.Ln), [krd], [krd])
                fw.op(A, act(rden_a[:, 3:4], rden_a[:, 3:4], AF.Exp, scale=-0.5), [krd], [krd])
                fw.op(V, stt(Ab[:, u, :], Ao[:, u, :], rden_a[:, 3:4], ga, ALU.mult, ALU.mult), [kAo, krd, kC], [kAb])
            for u in range(2):
                pi = 3
                pv = pbanks[pi][:, 0:256].bitcast(BF16).rearrange("p (f c) -> p f c", f=4)
                fns = [trf(pv[:, f, :], Ab[:, u, f * 128:(f + 1) * 128], idb) for f in range(4)]
                fw.mm(fns, [kAb, kC], [pk[pi]])
                tok0 = bq * 256 + u * 128
                fw.op(A, act(attT[:, :, tok0:tok0 + 128], pv, AF.Copy), [pk[pi]], [kat])
        if dbg and b == 0:
            fw.dma(SY, dbg_t['attT'], attT, sem_d, [kat], [kOut])
        fw.barrier()

        if STAGE < 4:
            raise _Stop()
        a4 = Bump(132 * K, 181 * K)
        Y1 = a4([128, 16, 512], BF16)
        TCb = [(a4([128, 2, 2, 256], BF16), a4([128, 2, 256], BF16)) for _ in range(2)]
        wgl = a4([128, 4, 512], BF16)
        Y1T = [a4([128, 4, 128], BF16) for _ in range(2)]
        zt = a4([128, 512], F32)
        zs = a4([128, 512], F32)
        y2 = a4([128, 512], F32)
        y2b = a4([128, 512], BF16)
        yq = a4([128, 256], F32)
        yc = a4([128, 256], F32)
        kY1, kwg, kzt, kzs, ky2, ky2b, kss, kyq, kyc, kssm = [Tk() for _ in range(10)]
        kTC = [Tk(), Tk()]
        kY1T = [Tk(), Tk()]
        fw.dma(G_, wgl, w_glu.rearrange("(k p) n -> p k n", p=128), sem_og, writes=[kwg])
        C1 = 0.7978845608028654 * 2.0
        for gl in range(16):
            i = gl % 2
            for gh in range(2):
                fw.dma(SY, TCb[i][0][:, gh], TOEP_d[16 * gh + gl], sem_w[i], writes=[kTC[i]])
            fw.dma(SY, TCb[i][1], CT_d[gl], sem_w[i], writes=[kTC[i]])
            for gh in range(2):
                g = 16 * gh + gl
                pr = slice(64 * gh, 64 * gh + 64)
                pi = (2 * gl + gh) % 4
                pv = pbanks[pi][:, 0:256]
                fns = [mmf(pv, UT[:, g, 0, :], TCb[i][0][:, gh, 0, :], True, False),
                       mmf(pv, UT[:, g, 1, :], TCb[i][0][:, gh, 1, :], False, False),
                       mmf(pv, Sbf[pr, 0, gl, 1:129], TCb[i][1][pr, 0, :], False, False),
                       mmf(pv, Sbf[pr, 1, gl, 1:129], TCb[i][1][pr, 1, :], False, True)]
                fw.mm(fns, [kUT, kSb, kTC[i]], [pk[pi]])
                fw.op(V, cp(yc, pv), [pk[pi]], [kyc])
                fw.op(V, tt(yq, yc, yc, ALU.mult), [kyc], [kyq])
                fw.op(V, ts(yq, yq, 0.044715, ALU.mult, 1.0, ALU.add), [kyq], [kyq])
                fw.op(V, tt(yq, yq, yc, ALU.mult), [kyq, kyc], [kyq])
                fw.op(V, ts(yq, yq, -40.0, ALU.max), [kyq], [kyq])
                fw.op(A, act(yq, yq, AF.Exp, scale=-C1), [kyq], [kyq])
                fw.op(V, ts(yq, yq, 1.0, ALU.add), [kyq], [kyq])
                fw.op(V, rcp(yq, yq), [kyq], [kyq])
                fw.op(V, tt(Y1[:, :, 16 * g:16 * g + 16], yq.rearrange("p (t h) -> p t h", t=16),
                            yc.rearrange("p (t h) -> p t h", t=16), ALU.mult), [kyq, kyc], [kY1])
        if dbg and b == 0:
            fw.dma(SY, dbg_t['y1'], Y1, sem_d, [kY1], [kOut])
        for t in range(16):
            i = t % 2
            pi = 4 + i
            pv = pbanks[pi][:, 0:256].bitcast(BF16).rearrange("p (f c) -> p f c", f=4)
            fns = [trf(pv[:, f, :], Y1[:, t, f * 128:(f + 1) * 128], idb) for f in range(4)]
            fw.mm(fns, [kY1, kC], [pk[pi]])
            fw.op(A, act(Y1T[i], pv, AF.Copy), [pk[pi]], [kY1T[i]])
            pz = 6 + i
            pzv = pbanks[pz][:, :]
            fns = [mmf(pzv, Y1T[i][:, f, :], wgl[:, f, :], f == 0, f == 3) for f in range(4)]
            fw.mm(fns, [kY1T[i], kwg], [pk[pz]])
            fw.op(V, tt(zt, pzv, bg, ALU.add), [pk[pz], kC], [kzt])
            fw.op(A, act(zs, zt, AF.Exp, scale=-1.0), [kzt], [kzs])
            fw.op(V, ts(zs, zs, 1.0, ALU.add), [kzs], [kzs])
            fw.op(V, rcp(zs, zs), [kzs], [kzs])
            fw.op(V, tt(y2, zs, Y1[:, t, :], ALU.mult), [kzs, kY1], [ky2])
            fw.op(A, ttr(zt, y2, y2, stat[:, 32:33]), [ky2, kzt], [kzt, kss])
            fw.op(V, ts(stat[:, 33:34], stat[:, 32:33], 1.0 / 512, ALU.mult, EPS, ALU.add), [kss], [kss])
            fw.op(A, act(stat[:, 33:34], stat[:, 33:34], AF.Ln), [kss], [kss])
            fw.op(A, act(stat[:, 33:34], stat[:, 33:34], AF.Exp, scale=-0.5), [kss], [kss])
            fw.op(V, stt(y2b, y2, stat[:, 33:34], gs, ALU.mult, ALU.mult), [ky2, kss, kC], [ky2b])
            pi2 = i
            pv2 = pbanks[pi2][:, 0:256].bitcast(BF16).rearrange("p (f c) -> p f c", f=4)
            fns = [trf(pv2[:, f, :], y2b[:, f * 128:(f + 1) * 128], idb) for f in range(4)]
            fw.mm(fns, [ky2b, kC], [pk[pi2]])
            fw.op(A, act(ssmT[:, :, t, :], pv2, AF.Copy), [pk[pi2]], [kssm])
        if dbg and b == 0:
            fw.dma(SY, dbg_t['ssmT'], ssmT, sem_d, [kssm], [kOut])
        fw.barrier()

        if STAGE < 5:
            raise _Stop()
        a5 = Bump(116 * K, ARENA)
        wo = a5([128, 8, 1024], BF16)
        otmp = a5([128, 1024], F32)
        oj = a5([128, 512], F32)
        kwo, kot, koj, kst4 = Tk(), Tk(), Tk(), Tk()
        kX1 = [Tk() for _ in range(16)]
        fw.dma(G_, wo, w_out.rearrange("(k p) n -> p k n", p=128), sem_og, writes=[kwo])
        for q4 in range(4):
            fw.dma(SY, X1[:, 4 * q4:4 * q4 + 4, :], xv[:, 4 * q4:4 * q4 + 4, :], sem_x[q4],
                   writes=[kX1[4 * q4 + j] for j in range(4)])
        for t in range(16):
            pis = [2 * (t % 2), 2 * (t % 2) + 1]
            for hf in range(2):
                pv = pbanks[pis[hf]][:, :]
                fns = []
                for k in range(8):
                    lhs = ssmT[:, k, t, :] if k < 4 else attT[:, k - 4, t::16]
                    fns.append(mmf(pv, lhs, wo[:, k, hf * 512:(hf + 1) * 512], k == 0, k == 7))
                fw.mm(fns, [kssm, kat, kwo], [pk[pis[hf]]])
                fw.op(A, sqa(oj, pv, stat[:, 40 + hf:41 + hf]), [pk[pis[hf]]], [koj, kst4])
            fw.op(V, tt(stat[:, 42:43], stat[:, 40:41], stat[:, 41:42], ALU.add), [kst4], [kst4])
            fw.op(V, ts(stat[:, 42:43], stat[:, 42:43], 1.0 / D, ALU.mult, EPS, ALU.add), [kst4], [kst4])
            fw.op(A, act(stat[:, 42:43], stat[:, 42:43], AF.Ln), [kst4], [kst4])
            fw.op(A, act(stat[:, 42:43], stat[:, 42:43], AF.Exp, scale=-0.5), [kst4], [kst4])
            for hf in range(2):
                pv = pbanks[pis[hf]][:, :]
                sl = slice(hf * 512, (hf + 1) * 512)
                fw.op(V, stt(otmp[:, sl], pv, stat[:, 42:43], g2[:, sl], ALU.mult, ALU.mult),
                      [pk[pis[hf]], kst4, kC], [kot])
            fw.op(G_, tt(X1[:, t, :], X1[:, t, :], otmp, ALU.add), [kot, kX1[t]], [kX1[t]])
        if dbg and b == 0:
            fw.dma(SY, dbg_t['x1'], X1, sem_d, kX1, [kOut])
        fw.barrier()

        if STAGE < 6:
            raise _Stop()
        f0 = Bump(86 * K, ARENA)
        h2T = f0([128, 8, 16, 128], BF16)
        fT = f0([128, NFF, 16, 128], BF16)
        f1 = Bump(84 * K, 86 * K)
        hb2 = f1([128, 1024], BF16)
        khb, kfj, kh2T, kfT, kst5, kot2 = Tk(), Tk(), Tk(), Tk(), Tk(), Tk()
        ov = out[b].rearrange("(c t) d -> c t d", t=16)
        fj1 = reg(118 * K, [128, 1024], F32)
        for t in range(16):
            fw.op(A, ttr(fj1, X1[:, t, :], X1[:, t, :], stat[:, 44:45]), [kX1[t]], [kfj, kst5])
            fw.op(V, ts(stat[:, 45:46], stat[:, 44:45], 1.0 / D, ALU.mult, EPS, ALU.add), [kst5], [kst5])
            fw.op(A, act(stat[:, 45:46], stat[:, 45:46], AF.Ln), [kst5], [kst5])
            fw.op(A, act(stat[:, 45:46], stat[:, 45:46], AF.Exp, scale=-0.5), [kst5], [kst5])
            fw.op(A, act(hb2, X1[:, t, :], AF.Copy, scale=stat[:, 45:46]), [kX1[t], kst5], [khb])
            for kk_ in range(2):
                pi = kk_ + 2 * (t % 2)
                pv = pbanks[pi][:, 0:256].bitcast(BF16).rearrange("p (f c) -> p f c", f=4)
                fns = [trf(pv[:, f, :], hb2[:, (4 * kk_ + f) * 128:(4 * kk_ + f + 1) * 128], idb) for f in range(4)]
                fw.mm(fns, [khb, kC], [pk[pi]])
                for f in range(4):
                    k = 4 * kk_ + f
                    if f % 2 == 0:
                        fw.op(V, ts(h2T[:, k, t, :], pv[:, f, :], g3[:, k:k + 1], ALU.mult), [pk[pi], kC], [kh2T])
                    else:
                        fw.op(A, act(h2T[:, k, t, :], pv[:, f, :], AF.Copy, scale=g3[:, k:k + 1]),
                              [pk[pi], kC], [kh2T])
        kO = Tk()
        fw.dma(SY, ov, X1, sem_o, kX1, [kO])
        fw.barrier()
        f2 = Bump(64 * K, 86 * K)
        wg = [f2([128, 8, 128], BF16) for _ in range(2)]
        wu = [f2([128, 8, 128], BF16) for _ in range(2)]
        sg = [f2([128, 512], F32) for _ in range(2)]
        wd = reg(20 * K, [128, NFF, 1024], BF16)
        kwg = [Tk(), Tk()]
        kwd = Tk()
        ksg = [Tk(), Tk()]
        wg_v = w_gate.rearrange("(k p) n -> p k n", p=128)
        wu_v = w_up.rearrange("(k p) n -> p k n", p=128)
        wd_v = w_down.rearrange("(j p) n -> p j n", p=128)
        hv = h2T.rearrange("p k t c -> p k (t c)")
        pc = 0
        for j in range(NFF):
            i = j % 2
            fw.dma(G_, wg[i], wg_v[:, :, j * 128:(j + 1) * 128], sem_g[i], writes=[kwg[i]])
            fw.dma(G_, wu[i], wu_v[:, :, j * 128:(j + 1) * 128], sem_g[i], writes=[kwg[i]])
            fw.dma(G_, wd[:, j, :], wd_v[:, j, :], sem_dn, writes=[kwd])
            for n in range(4):
                pg_, pu_ = 2 * (pc % 4), 2 * (pc % 4) + 1
                si = pc % 2
                pc += 1
                pgv, puv = pbanks[pg_][:, :], pbanks[pu_][:, :]
                fns = [mmf(pgv, wg[i][:, k, :], hv[:, k, n * 512:(n + 1) * 512], k == 0, k == 7) for k in range(8)]
                fw.mm(fns, [kh2T, kwg[i]], [pk[pg_]])
                fns = [mmf(puv, wu[i][:, k, :], hv[:, k, n * 512:(n + 1) * 512], k == 0, k == 7) for k in range(8)]
                fw.mm(fns, [kh2T, kwg[i]], [pk[pu_]])
                fw.op(A, act(sg[si], pgv, AF.Silu), [pk[pg_]], [ksg[si]])
                fw.op(V, tt(fT[:, j].rearrange("p t c -> p (t c)")[:, n * 512:(n + 1) * 512], sg[si], puv, ALU.mult),
                      [ksg[si], pk[pu_]], [kfT])
        fw.barrier()
        f3 = Bump(64 * K, 86 * K)
        xr = [f3([128, 1024], F32) for _ in range(2)]
        ot2 = f3([128, 1024], F32)
        fj3 = f3([128, 512], F32)
        kxr = [Tk(), Tk()]
        for tg in range(4):
            for tl in range(4):
                t = 4 * tg + tl
                if tl < 2:
                    pass
            for j in range(NFF):
                fns = []
                for tl in range(4):
                    t = 4 * tg + tl
                    for hf in range(2):
                        fns.append(mmf(pbanks[2 * tl + hf][:, :], fT[:, j, t, :], wd[:, j, hf * 512:(hf + 1) * 512],
                                       j == 0, j == NFF - 1))
                fw.mm(fns, [kfT, kwd], pk)
            for tl in range(4):
                t = 4 * tg + tl
                xi = t % 2
                fw.dma(SY, xr[xi], ov[:, t, :], sem_x[xi], [kO], [kxr[xi]])
                for hf in range(2):
                    pv = pbanks[2 * tl + hf][:, :]
                    fw.op(A, sqa(fj3, pv, stat[:, 48 + hf:49 + hf]), [pk[2 * tl + hf]], [kfj, kst5])
                fw.op(V, tt(stat[:, 50:51], stat[:, 48:49], stat[:, 49:50], ALU.add), [kst5], [kst5])
                fw.op(V, ts(stat[:, 50:51], stat[:, 50:51], 1.0 / D, ALU.mult, EPS, ALU.add), [kst5], [kst5])
                fw.op(A, act(stat[:, 50:51], stat[:, 50:51], AF.Ln), [kst5], [kst5])
                fw.op(A, act(stat[:, 50:51], stat[:, 50:51], AF.Exp, scale=-0.5), [kst5], [kst5])
                for hf in range(2):
                    pv = pbanks[2 * tl + hf][:, :]
                    sl = slice(hf * 512, (hf + 1) * 512)
                    fw.op(V, stt(ot2[:, sl], pv, stat[:, 50:51], g4[:, sl], ALU.mult, ALU.mult),
                          [pk[2 * tl + hf], kst5, kC], [kot2])
                fw.op(G_, tt(xr[xi], xr[xi], ot2, ALU.add), [kot2, kxr[xi]], [kxr[xi]])
                fw.dma(SY, ov[:, t, :], xr[xi], sem_st[xi], [kxr[xi]], [kO])
        fw.barrier()
    except _Stop:
        pass
    fw.barrier()
    fw.emit()
    return nc, es


_CACHE = {}


def _consts():
    idf = np.eye(128, dtype=np.float32)
    cb = np.zeros((128, 2, 256), np.float32)
    for r in range(2):
        kpos = r * 128 + np.arange(128)[:, None]
        qpos = np.arange(256)[None, :]
        cb[:, r, :] = np.where(kpos <= qpos, 0.0, NEG)
    tm = np.zeros((128, 2, 256), np.float32)
    dm = np.zeros((128, 2, 256), np.float32)
    for j in range(2):
        for s8 in range(8):
            for hi in range(16):
                sp = 8 * j + s8
                row = s8 * 16 + hi
                for jj in range(2):
                    pass
                for tp in range(16):
                    if tp >= sp:
                        tm[row, j, tp * 16:(tp + 1) * 16] = 1.0
                dm[row, j, sp * 16 + hi] = 1.0
    nv = np.tile(np.arange(1, 33, dtype=np.float32)[None, :], (128, 1))
    return idf, cb, tm, dm, nv


def kernel(**inp):
    f32 = np.float32
    x = np.ascontiguousarray(inp['x'], dtype=f32)

    def sq(k):
        return np.ascontiguousarray(inp[k][0], dtype=f32)

    def gp(a):
        a = a.reshape((2, 16) + a.shape[1:])
        a = np.moveaxis(a, 2, 1)
        return np.ascontiguousarray(a.reshape((128, 16) + a.shape[3:]))
    idf, cb, tm, dm, nv = _consts()
    shared = dict(
        w_in=sq('w_in'), w_glu=sq('w_glu'), w_out=sq('w_out'), w_gate=sq('w_gate'), w_up=sq('w_up'),
        w_down=sq('w_down'),
        areT=gp(sq('ssm_a_re')), aimT=gp(sq('ssm_a_im')),
        ldtT=gp(np.ascontiguousarray(np.broadcast_to(sq('ssm_log_dt')[:, None], (32, 64)))),
        breT=gp(sq('ssm_b_re')), bimT=gp(sq('ssm_b_im')),
        creT=gp(np.ascontiguousarray(sq('ssm_c_re').transpose(0, 2, 1))),
        cimT=gp(np.ascontiguousarray(sq('ssm_c_im').transpose(0, 2, 1))),
        drep=np.ascontiguousarray(np.broadcast_to(sq('ssm_d')[None], (128, 32, 16))),
        g1c=np.ascontiguousarray(sq('g_pre_mix').reshape(8, 128).T),
        g3c=np.ascontiguousarray(sq('g_pre_ffn').reshape(8, 128).T),
        g2r=np.ascontiguousarray(np.broadcast_to(sq('g_post_mix')[None], (128, 1024))),
        g4r=np.ascontiguousarray(np.broadcast_to(sq('g_post_ffn')[None], (128, 1024))),
        gsr=np.ascontiguousarray(np.broadcast_to(sq('g_ssm_out')[None], (128, 512))),
        gar=np.ascontiguousarray(np.broadcast_to(sq('g_attn_out')[None], (128, 512))),
        bgr=np.ascontiguousarray(np.broadcast_to(sq('b_glu')[None], (128, 512))),
        c_idf=idf, c_cb=cb, c_tm=tm, c_dm=dm, c_nv=nv,
    )
    if 'nc' not in _CACHE:
        _CACHE['nc'] = build(DEBUG)
    nc, _es = _CACHE['nc']
    in_maps = []
    for c in range(8):
        m = dict(shared)
        m['x'] = np.ascontiguousarray(x[2 * c:2 * c + 2])
        in_maps.append(m)
    res = run_bass_kernel_spmd(nc, in_maps, core_ids=list(range(8)))
    _CACHE['res'] = res
    return np.concatenate([np.asarray(r['out'], dtype=f32) for r in res.results], axis=0)
```

```python
import math
import numpy as np
from contextlib import ExitStack
import concourse.bass as bass
import concourse.mybir as mybir
from concourse.alu_op_type import AluOpType as ALU
from concourse.bass_utils import run_bass_kernel_spmd

F32 = mybir.dt.float32
BF16 = mybir.dt.bfloat16
U8 = mybir.dt.uint8
I32 = mybir.dt.int32
AF = mybir.ActivationFunctionType
AX = mybir.AxisListType

S = 2048
D = 1024
DFF = 2816
NFF = 22
NEG = -30000.0
EPS = 1e-6
ENGS = ['tensor', 'vector', 'scalar', 'gpsimd', 'sync']
DEBUG = False
STAGE = 99
NSTEP = 127
ABQ = 8


class _Stop(Exception):
    pass


class Tk:
    __slots__ = ('w', 'r')

    def __init__(self):
        self.w = {}
        self.r = {}


class FW:
    def __init__(self, nc, es):
        self.nc, self.es = nc, es
        self.prog = {e: [] for e in ENGS}
        self.esem = {e: es.enter_context(nc.semaphore("es_" + e)) for e in ENGS}
        self.ecnt = {e: 0 for e in ENGS}
        self.seen = {e: {} for e in ENGS}
        self.dcnt = {}

    def newsem(self, name):
        s = self.es.enter_context(self.nc.semaphore(name))
        self.dcnt[id(s)] = [s, 0]
        return s

    def _dep(self, eng, reads, writes):
        need = {}

        def add(dct):
            for k, (s, v) in dct.items():
                if need.get(k, (None, 0))[1] < v:
                    need[k] = (s, v)
        for t in reads:
            add(t.w)
        for t in writes:
            add(t.w)
            add(t.r)
        for k, (s, v) in need.items():
            if self.seen[eng].get(k, 0) >= v:
                continue
            self.seen[eng][k] = v
            self.prog[eng].append(('w', s, v))

    def _post(self, tok, reads, writes):
        s, v = tok
        k = id(s)
        for t in reads:
            if t.r.get(k, (None, 0))[1] < v:
                t.r[k] = (s, v)
        for t in writes:
            t.w = {k: (s, v)}
            t.r = {}

    def op(self, eng, fn, reads=(), writes=()):
        self._dep(eng, reads, writes)
        self.ecnt[eng] += 1
        self.prog[eng].append(('o', fn, True))
        self._post((self.esem[eng], self.ecnt[eng]), reads, writes)

    def mm(self, fns, reads=(), writes=()):
        self._dep('tensor', reads, writes)
        for f in fns[:-1]:
            self.prog['tensor'].append(('o', f, False))
        self.ecnt['tensor'] += 1
        self.prog['tensor'].append(('o', fns[-1], True))
        self._post((self.esem['tensor'], self.ecnt['tensor']), reads, writes)

    def dma(self, eng, out, in_, sem, reads=(), writes=()):
        self._dep(eng, reads, writes)
        c = self.dcnt[id(sem)]
        c[1] += 16
        self.prog[eng].append(('d', out, in_, sem))
        self._post((sem, c[1]), reads, writes)

    def barrier(self):
        for e in ENGS:
            for e2 in ENGS:
                if e2 == e or self.ecnt[e2] == 0:
                    continue
                k = id(self.esem[e2])
                if self.seen[e].get(k, 0) < self.ecnt[e2]:
                    self.seen[e][k] = self.ecnt[e2]
                    self.prog[e].append(('w', self.esem[e2], self.ecnt[e2]))
            for k, (s, v) in self.dcnt.items():
                if v > 0 and self.seen[e].get(k, 0) < v:
                    self.seen[e][k] = v
                    self.prog[e].append(('w', s, v))

    def emit(self):
        nc = self.nc
        with nc.Block() as block:
            for e in ENGS:
                prog = self.prog[e]
                sem = self.esem[e]

                def body(E, prog=prog, sem=sem):
                    for it in prog:
                        if it[0] == 'w':
                            E.wait_ge(it[1], it[2])
                        elif it[0] == 'o':
                            ins = it[1](E)
                            if it[2]:
                                ins.then_inc(sem, 1)
                        else:
                            E.dma_start(out=it[1], in_=it[2]).then_inc(it[3], 16)
                getattr(block, e)(body)


def tt(out, in0, in1, op):
    return lambda E: E.tensor_tensor(out=out, in0=in0, in1=in1, op=op)


def ts(out, in0, s1, op0, s2=None, op1=None):
    if op1 is None:
        return lambda E: E.tensor_scalar(out=out, in0=in0, scalar1=s1, scalar2=None, op0=op0)
    return lambda E: E.tensor_scalar(out=out, in0=in0, scalar1=s1, scalar2=s2, op0=op0, op1=op1)


def stt(out, in0, scalar, in1, op0, op1):
    return lambda E: E.scalar_tensor_tensor(out=out, in0=in0, scalar=scalar, in1=in1, op0=op0, op1=op1)


def ttr(out, in0, in1, accum):
    return lambda E: E.activation(out=out, in_=in0, func=AF.Square, accum_out=accum)


def act(out, in_, func, scale=None):
    if scale is None:
        return lambda E: E.activation(out=out, in_=in_, func=func)
    return lambda E: E.activation(out=out, in_=in_, func=func, scale=scale)


def sqa(out, in_, accum):
    return lambda E: E.activation(out=out, in_=in_, func=AF.Square, accum_out=accum)


def rcp(out, in_):
    return lambda E: E.reciprocal(out=out, in_=in_)


def cp(out, in_):
    return lambda E: E.tensor_copy(out=out, in_=in_)


def mmf(out, lhsT, rhs, start, stop):
    assert len(rhs.ap) == 2 and len(lhsT.ap) == 2, ("rhs", rhs.ap, lhsT.ap)
    return lambda E: E.matmul(out, lhsT, rhs, start=start, stop=stop)


def trf(out, in_, ident):
    assert len(ident.ap) == 2 and len(in_.ap) == 2, ("ident", ident.ap, in_.ap)
    return lambda E: E.transpose(out, in_, ident)


def bc(ap, axis, shape):
    return ap.unsqueeze(axis).broadcast_to(list(shape))


def build(dbg=False):
    nc = bass.Bass("TRN2", target_bir_lowering=False)
    es = ExitStack()

    def din(name, shape):
        return nc.dram_tensor(name, list(shape), F32, kind="ExternalInput").ap()

    x = din("x", [2, S, D])
    w_in = din("w_in", [D, 2048])
    w_glu = din("w_glu", [512, 512])
    w_out = din("w_out", [1024, 1024])
    w_gate = din("w_gate", [D, DFF])
    w_up = din("w_up", [D, DFF])
    w_down = din("w_down", [DFF, D])
    areT = din("areT", [128, 16])
    aimT = din("aimT", [128, 16])
    ldtT = din("ldtT", [128, 16])
    breT = din("breT", [128, 16, 16])
    bimT = din("bimT", [128, 16, 16])
    creT = din("creT", [128, 16, 16])
    cimT = din("cimT", [128, 16, 16])
    drep = din("drep", [128, 32, 16])
    g1c = din("g1c", [128, 8])
    g3c = din("g3c", [128, 8])
    g2r = din("g2r", [128, 1024])
    g4r = din("g4r", [128, 1024])
    gsr = din("gsr", [128, 512])
    gar = din("gar", [128, 512])
    bgr = din("bgr", [128, 512])
    c_idf = din("c_idf", [128, 128])
    c_cb = din("c_cb", [128, 2, 256])
    c_tm = din("c_tm", [128, 2, 256])
    c_dm = din("c_dm", [128, 2, 256])
    c_nv = din("c_nv", [128, 32])
    out = nc.dram_tensor("out", [2, S, D], F32, kind="ExternalOutput").ap()
    GT_d = nc.dram_tensor("GT_d", [32, 128, 2, 2, 128], BF16, kind="Internal").ap()
    TOEP_d = nc.dram_tensor("TOEP_d", [32, 128, 2, 256], BF16, kind="Internal").ap()
    CT_d = nc.dram_tensor("CT_d", [16, 128, 2, 256], BF16, kind="Internal").ap()
    dbg_t = {}
    if dbg:
        dbg_t['uc'] = nc.dram_tensor("d_uc", [128, 32, 16, 16], BF16, kind="ExternalOutput").ap()
        dbg_t['qT'] = nc.dram_tensor("d_qT", [128, 4, S], BF16, kind="ExternalOutput").ap()
        dbg_t['vt'] = nc.dram_tensor("d_vt", [128, 16, 8, 65], BF16, kind="ExternalOutput").ap()
        dbg_t['y1'] = nc.dram_tensor("d_y1", [128, 16, 512], BF16, kind="ExternalOutput").ap()
        dbg_t['ssmT'] = nc.dram_tensor("d_ssmT", [128, 4, 16, 128], BF16, kind="ExternalOutput").ap()
        dbg_t['attT'] = nc.dram_tensor("d_attT", [128, 4, S], BF16, kind="ExternalOutput").ap()
        dbg_t['x1'] = nc.dram_tensor("d_x1", [128, 16, 1024], F32, kind="ExternalOutput").ap()
        dbg_t['S'] = nc.dram_tensor("d_S", [128, 2, 16, 128], F32, kind="ExternalOutput").ap()

    ARENA = 206 * 1024
    arena = es.enter_context(nc.sbuf_tensor("arena", [128, ARENA], U8))
    pbanks = [es.enter_context(nc.psum_tensor("pb%d" % i, [128, 512], F32)) for i in range(8)]
    pk = [Tk() for _ in range(8)]
    fw = FW(nc, es)

    def reg(off, shape, dt, p0=0):
        esz = 2 if dt == BF16 else 4
        n = int(np.prod(shape[1:])) * esz
        assert off % 4 == 0 and off + n <= ARENA, (off, n)
        ap = arena[p0:p0 + shape[0], off:off + n].bitcast(dt)
        if len(shape) == 3:
            ap = ap.rearrange("p (a b) -> p a b", a=shape[1])
        elif len(shape) == 4:
            ap = ap.rearrange("p (a b c) -> p a b c", a=shape[1], b=shape[2])
        elif len(shape) == 5:
            ap = ap.rearrange("p (a b c d) -> p a b c d", a=shape[1], b=shape[2], c=shape[3])
        return ap

    class Bump:
        def __init__(self, lo, hi):
            self.o, self.hi = lo, hi

        def __call__(self, shape, dt, p0=0):
            esz = 2 if dt == BF16 else 4
            n = int(np.prod(shape[1:])) * esz
            n4 = (n + 31) // 32 * 32
            a = reg(self.o, shape, dt, p0)
            self.o += n4
            assert self.o <= self.hi, (self.o, self.hi)
            return a

    K = 1024
    V, A, G_, T_, SY = 'vector', 'scalar', 'gpsimd', 'tensor', 'sync'

    cb = Bump(0, 20 * K)
    idf = cb([128, 128], F32)
    idb = cb([128, 128], BF16)
    cbias = cb([128, 2, 256], BF16)
    g1 = cb([128, 8], F32)
    g3 = cb([128, 8], F32)
    g2 = cb([128, 1024], F32)
    g4 = cb([128, 1024], F32)
    gs = cb([128, 512], F32)
    ga = cb([128, 512], F32)
    bg = cb([128, 512], F32)
    stat = cb([128, 64], F32)
    LR = cb([128, 7, 16], F32)
    LI = cb([128, 7, 16], F32)
    LIn = cb([128, 7, 16], F32)
    LAk = cb([128, 7, 2, 16], F32)
    assert cb.o <= 20 * K
    kC = Tk()
    sem_c = fw.newsem("sem_c")
    sem_c2 = fw.newsem("sem_c2")
    for dst, src in [(idf, c_idf), (g1, g1c), (g3, g3c), (g2, g2r), (g4, g4r), (gs, gsr), (ga, gar), (bg, bgr)]:
        fw.dma(SY, dst, src, sem_c, writes=[kC])

    sb = Bump(20 * K, ARENA)
    cb_off = sb.o
    CBre = sb([128, 16, 16, 16], F32)
    cbf = sb([128, 2, 256], F32)
    tmk = sb([128, 2, 256], F32)
    dmk = sb([128, 2, 256], F32)
    nv = sb([128, 32], F32)
    drp = sb([128, 32, 16], F32)
    are = sb([128, 16], F32)
    aim = sb([128, 16], F32)
    ldt = sb([128, 16], F32)
    bre = sb([128, 16, 16], F32)
    bim = sb([128, 16, 16], F32)
    cre = sb([128, 16, 16], F32)
    cim = sb([128, 16, 16], F32)
    kS = Tk()
    for dst, src in [(cbf, c_cb), (tmk, c_tm), (dmk, c_dm), (nv, c_nv), (drp, drep), (are, areT), (aim, aimT),
                     (ldt, ldtT), (bre, breT), (bim, bimT), (cre, creT), (cim, cimT)]:
        fw.dma(SY, dst, src, sem_c2, writes=[kS])
    fw.op(V, cp(idb, idf), [kC], [kC])
    fw.op(V, cp(cbias, cbf), [kS, kC], [kC])

    dtt = sb([128, 16], F32)
    adr = sb([128, 16], F32)
    adi = sb([128, 16], F32)
    ex = sb([128, 16, 32], F32)
    ang = sb([128, 16, 32], F32)
    mag = sb([128, 16, 32], F32)
    magi = sb([128, 16, 32], F32)
    r1 = sb([128, 16, 32], F32)
    r2 = sb([128, 16, 32], F32)
    sn = sb([128, 16, 32], F32)
    cs = sb([128, 16, 32], F32)
    Pre = sb([128, 16, 32], F32)
    Pim = sb([128, 16, 32], F32)
    Qre = sb([128, 16, 32], F32)
    Qim = sb([128, 16, 32], F32)
    sm = [sb([128, 16], F32) for _ in range(10)]
    bbre = sb([128, 16, 16], F32)
    bbim = sb([128, 16, 16], F32)
    tb1 = sb([128, 16, 16], F32)
    tb2 = sb([128, 16, 16], F32)
    k0 = Tk()
    fw.op(A, act(dtt, ldt, AF.Exp), [kS], [k0])
    fw.op(V, tt(adr, are, dtt, ALU.mult), [k0, kS], [k0])
    fw.op(V, tt(adi, aim, dtt, ALU.mult), [k0, kS], [k0])
    fw.op(V, tt(ex, bc(adr, 2, [128, 16, 32]), bc(nv, 1, [128, 16, 32]), ALU.mult), [k0, kS], [k0])
    fw.op(V, tt(ang, bc(adi, 2, [128, 16, 32]), bc(nv, 1, [128, 16, 32]), ALU.mult), [k0, kS], [k0])
    fw.op(A, act(mag, ex, AF.Exp), [k0], [k0])
    fw.op(A, act(magi, ex, AF.Exp, scale=-1.0), [k0], [k0])
    ki = reg(cb_off, [128, 16, 32], I32)
    kf = reg(cb_off + 2048, [128, 16, 32], F32)
    mk = reg(cb_off + 4096, [128, 16, 32], F32)

    def range_reduce(r, shift):
        fw.op(V, ts(r, ang, 1.0 / (2 * math.pi), ALU.mult, shift, ALU.add), [k0], [k0])
        fw.op(V, cp(ki, r), [k0], [k0])
        fw.op(V, cp(kf, ki), [k0], [k0])
        fw.op(V, tt(r, r, kf, ALU.subtract), [k0], [k0])
        fw.op(V, ts(mk, r, -0.5, ALU.is_lt), [k0], [k0])
        fw.op(V, tt(r, r, mk, ALU.add), [k0], [k0])
        fw.op(V, ts(mk, r, 0.5, ALU.is_gt), [k0], [k0])
        fw.op(V, tt(r, r, mk, ALU.subtract), [k0], [k0])
    range_reduce(r1, 0.0)
    range_reduce(r2, 0.25)
    fw.op(A, act(sn, r1, AF.Sin, scale=2 * math.pi), [k0], [k0])
    fw.op(A, act(cs, r2, AF.Sin, scale=2 * math.pi), [k0], [k0])
    fw.op(V, tt(Pre, mag, cs, ALU.mult), [k0], [k0])
    fw.op(V, tt(Pim, mag, sn, ALU.mult), [k0], [k0])
    fw.op(V, tt(Qre, magi, cs, ALU.mult), [k0], [k0])
    fw.op(V, stt(Qim, magi, -1.0, sn, ALU.mult, ALU.mult), [k0], [k0])
    lbre, lbim = Pre[:, :, 0], Pim[:, :, 0]
    nr, den, t0, t1_, cr, ci, rden = sm[0], sm[1], sm[2], sm[3], sm[4], sm[5], sm[6]
    fw.op(V, ts(nr, lbre, -1.0, ALU.add), [k0], [k0])
    fw.op(V, tt(den, are, are, ALU.mult), [k0, kS], [k0])
    fw.op(V, tt(t0, aim, aim, ALU.mult), [k0, kS], [k0])
    fw.op(V, tt(den, den, t0, ALU.add), [k0], [k0])
    fw.op(V, lambda E: E.reciprocal(out=rden, in_=den), [k0], [k0])
    fw.op(V, tt(t0, nr, are, ALU.mult), [k0], [k0])
    fw.op(V, tt(t1_, lbim, aim, ALU.mult), [k0], [k0])
    fw.op(V, tt(t0, t0, t1_, ALU.add), [k0], [k0])
    fw.op(V, tt(cr, t0, rden, ALU.mult), [k0], [k0])
    fw.op(V, tt(t0, lbim, are, ALU.mult), [k0], [k0])
    fw.op(V, tt(t1_, nr, aim, ALU.mult), [k0], [k0])
    fw.op(V, tt(t0, t0, t1_, ALU.subtract), [k0], [k0])
    fw.op(V, tt(ci, t0, rden, ALU.mult), [k0], [k0])
    crb, cib = bc(cr, 2, [128, 16, 16]), bc(ci, 2, [128, 16, 16])
    fw.op(V, tt(tb1, crb, bre, ALU.mult), [k0, kS], [k0])
    fw.op(V, tt(tb2, cib, bim, ALU.mult), [k0, kS], [k0])
    fw.op(V, tt(bbre, tb1, tb2, ALU.subtract), [k0], [k0])
    fw.op(V, tt(tb1, crb, bim, ALU.mult), [k0, kS], [k0])
    fw.op(V, tt(tb2, cib, bre, ALU.mult), [k0, kS], [k0])
    fw.op(V, tt(bbim, tb1, tb2, ALU.add), [k0], [k0])
    fw.op(V, cp(LR[:, 0, :], Pre[:, :, 15]), [k0], [kC])
    fw.op(V, cp(LI[:, 0, :], Pim[:, :, 15]), [k0], [kC])
    for k in range(6):
        fw.op(V, tt(sm[7], LR[:, k, :], LR[:, k, :], ALU.mult), [kC, k0], [k0])
        fw.op(V, tt(sm[8], LI[:, k, :], LI[:, k, :], ALU.mult), [kC, k0], [k0])
        fw.op(V, tt(LR[:, k + 1, :], sm[7], sm[8], ALU.subtract), [k0], [kC])
        fw.op(V, tt(sm[9], LR[:, k, :], LI[:, k, :], ALU.mult), [kC, k0], [k0])
        fw.op(V, ts(LI[:, k + 1, :], sm[9], 2.0, ALU.mult), [k0], [kC])
    fw.op(V, ts(LIn, LI, -1.0, ALU.mult), [kC], [kC])
    fw.op(V, cp(LAk[:, :, 0, :], LR), [kC], [kC])
    fw.op(V, cp(LAk[:, :, 1, :], LR), [kC], [kC])
    SH4 = [128, 16, 16, 16]
    Gre = sb(SH4, F32)
    Gim = sb(SH4, F32)
    CAre = sb(SH4, F32)
    CAim = sb(SH4, F32)
    CBim = sb(SH4, F32)
    u1_off = sb.o
    U1 = sb(SH4, F32)
    U2 = sb(SH4, F32)
    U3, U4 = U1, U2
    CTst = reg(u1_off, [128, 16, 2, 256], BF16)
    kG, kCA, kCB, kU1, kU2, kCT = [Tk() for _ in range(6)]
    kU3, kU4 = kU1, kU2

    def outer(eng, o, a, pn, rd, wr):
        fw.op(eng, tt(o, bc(a, 2, SH4), bc(pn, 3, SH4), ALU.mult), rd, wr)

    def fl(ap):
        return ap.rearrange("p a b c -> p (a b c)")
    outer(V, U1, bbre, Qre[:, :, 0:16], [k0], [kU1])
    outer(G_, U2, bbim, Qim[:, :, 0:16], [k0], [kU2])
    fw.op(V, tt(Gre, U1, U2, ALU.subtract), [kU1, kU2], [kG])
    outer(V, U3, bbre, Qim[:, :, 0:16], [k0], [kU3])
    outer(G_, U4, bbim, Qre[:, :, 0:16], [k0], [kU4])
    fw.op(V, tt(Gim, U3, U4, ALU.add), [kU3, kU4], [kG])
    outer(V, U1, cre, Pre[:, :, 0:16], [k0, kS], [kU1])
    outer(G_, U2, cim, Pim[:, :, 0:16], [k0, kS], [kU2])
    fw.op(V, tt(CAre, U1, U2, ALU.subtract), [kU1, kU2], [kCA])
    outer(V, U3, cre, Pim[:, :, 0:16], [k0, kS], [kU3])
    outer(G_, U4, cim, Pre[:, :, 0:16], [k0, kS], [kU4])
    fw.op(V, stt(fl(CAim), fl(U3), -1.0, fl(U4), ALU.mult, ALU.subtract), [kU3, kU4], [kCA])
    outer(V, U1, cre, Pre[:, :, 16:32], [k0, kS], [kU1])
    outer(G_, U2, cim, Pim[:, :, 16:32], [k0, kS], [kU2])
    fw.op(V, tt(CBre, U1, U2, ALU.subtract), [kU1, kU2], [kCB])
    outer(V, U3, cre, Pim[:, :, 16:32], [k0, kS], [kU3])
    outer(G_, U4, cim, Pre[:, :, 16:32], [k0, kS], [kU4])
    fw.op(V, stt(fl(CBim), fl(U3), -1.0, fl(U4), ALU.mult, ALU.subtract), [kU3, kU4], [kCB])
    fw.op(V, cp(CTst[:, :, 0, :], CBre.rearrange("p g t h -> p g (t h)")), [kCB], [kCT, kU1])
    fw.op(V, cp(CTst[:, :, 1, :], CBim.rearrange("p g t h -> p g (t h)")), [kCB], [kCT, kU1])
    sem_t = fw.newsem("sem_t")
    kDR = Tk()
    fw.dma(SY, CT_d.rearrange("g p r c -> p g r c"), CTst, sem_t, [kCT], [kDR])
    TPst = [sb([128, 2, 256], BF16) for _ in range(2)]
    GTst = [sb([128, 2, 2, 128], BF16) for _ in range(2)]
    TPt = [sb([128, 2, 256], F32) for _ in range(2)]
    TPd = [sb([128, 2, 256], F32) for _ in range(2)]
    kTP = [Tk() for _ in range(2)]
    kGTs = [Tk() for _ in range(2)]
    kTt = [Tk() for _ in range(2)]
    sem_tp = [fw.newsem("sem_tp%d" % i) for i in range(2)]
    sem_gt = [fw.newsem("sem_gt%d" % i) for i in range(2)]
    for gh in range(2):
        pr = slice(64 * gh, 64 * gh + 64)
        for gl in range(16):
            g = 16 * gh + gl
            i = g % 2
            pT, pG = 2 * i, 2 * i + 1
            pt_v = pbanks[pT][:, :].rearrange("p (j c) -> p j c", j=2)
            fns = []
            for j in range(2):
                l_re = Gre[pr, gl, 8 * j:8 * j + 8, :].rearrange("p s h -> p (s h)")
                l_im = Gim[pr, gl, 8 * j:8 * j + 8, :].rearrange("p s h -> p (s h)")
                fns.append(mmf(pt_v[:, j, :], l_re, CAre[pr, gl].rearrange("p t h -> p (t h)"), True, False))
                fns.append(mmf(pt_v[:, j, :], l_im, CAim[pr, gl].rearrange("p t h -> p (t h)"), False, True))
            fw.mm(fns, [kG, kCA], [pk[pT]])
            pg_v = pbanks[pG][:, :].rearrange("p (j r c) -> p j r c", j=2, r=2)
            fns = []
            for j in range(2):
                l_re = Gre[pr, gl, 8 * j:8 * j + 8, :].rearrange("p s h -> p (s h)")
                l_im = Gim[pr, gl, 8 * j:8 * j + 8, :].rearrange("p s h -> p (s h)")
                fns.append(mmf(pg_v[:, j, 0, :], l_re, idf[pr, :], True, True))
                fns.append(mmf(pg_v[:, j, 1, :], l_im, idf[pr, :], True, True))
            fw.mm(fns, [kG, kC], [pk[pG]])
            fw.op(V, tt(TPt[i], pt_v, tmk, ALU.mult), [pk[pT], kS], [kTt[i]])
            fw.op(G_, tt(TPd[i].rearrange("p j (t h) -> p j t h", t=16),
                         dmk.rearrange("p j (t h) -> p j t h", t=16),
                         drp[:, g, :].unsqueeze(1).unsqueeze(1).broadcast_to([128, 2, 16, 16]), ALU.mult),
                  [kS], [kTP[i]])
            fw.op(V, tt(TPst[i], TPt[i], TPd[i], ALU.add), [kTt[i], kTP[i]], [kTP[i]])
            fw.dma(SY, TOEP_d[g], TPst[i], sem_tp[i], [kTP[i]], [kDR])
            fw.op(A, act(GTst[i], pg_v, AF.Copy), [pk[pG]], [kGTs[i]])
            fw.dma(SY, GT_d[g], GTst[i], sem_gt[i], [kGTs[i]], [kDR])
    fw.barrier()

    X1 = reg(20 * K, [128, 16, 1024], F32)
    attT = reg(84 * K, [128, 4, S], BF16)
    ssmT = reg(100 * K, [128, 4, 16, 128], BF16)
    Uc = reg(116 * K, [128, 32, 16, 16], BF16)
    qT = reg(132 * K, [128, 4, S], BF16)
    kT = reg(148 * K, [128, 4, S], BF16)
    Vt = reg(164 * K, [128, 16, 8, 65], BF16)
    sem_x = [fw.newsem("sem_x%d" % i) for i in range(4)]
    sem_w = [fw.newsem("sem_w%d" % i) for i in range(2)]
    sem_o = fw.newsem("sem_o")
    sem_og = fw.newsem("sem_og")
    sem_wq = [fw.newsem("sem_wq%d" % i) for i in range(2)]
    sem_d = fw.newsem("sem_d")
    sem_g = [fw.newsem("sem_g%d" % i) for i in range(2)]
    sem_dn = fw.newsem("sem_dn")
    sem_st = [fw.newsem("sem_st%d" % i) for i in range(2)]
    kOut = Tk()

    try:
      if STAGE < 1:
        raise _Stop()
      for b in range(2):
        a1 = Bump(20 * K, 116 * K)
        Xst = [a1([128, 2, 1024], F32) for _ in range(2)]
        hbf = [a1([128, 2, 1024], BF16) for _ in range(2)]
        hT = a1([128, 8, S], BF16)
        wbuf = [a1([128, 8, 256], BF16) for _ in range(2)]
        junk = a1([128, 1024], F32)
        kX = [Tk(), Tk()]
        kH = [Tk(), Tk()]
        khT = [Tk() for _ in range(8)]
        kW = [Tk(), Tk()]
        kJ = Tk()
        kSt = Tk()
        kUc, kq, kk, kv = Tk(), Tk(), Tk(), Tk()
        xv = x[b].rearrange("(c t) d -> c t d", t=16)
        ss = stat[:, 0:16]
        rs = stat[:, 16:32]
        for tp in range(8):
            i = tp % 2
            fw.dma(SY, Xst[i], xv[:, 2 * tp:2 * tp + 2, :], sem_x[i], writes=[kX[i]])
            for u in range(2):
                t = 2 * tp + u
                fw.op(A, ttr(junk, Xst[i][:, u, :], Xst[i][:, u, :], ss[:, t:t + 1]), [kX[i]], [kJ, kSt])
                fw.op(V, ts(rs[:, t:t + 1], ss[:, t:t + 1], 1.0 / D, ALU.mult, EPS, ALU.add), [kSt], [kSt])
                fw.op(A, act(rs[:, t:t + 1], rs[:, t:t + 1], AF.Ln), [kSt], [kSt])
                fw.op(A, act(rs[:, t:t + 1], rs[:, t:t + 1], AF.Exp, scale=-0.5), [kSt], [kSt])
                fw.op(A, act(hbf[i][:, u, :], Xst[i][:, u, :], AF.Copy, scale=rs[:, t:t + 1]), [kX[i], kSt], [kH[i]])
            for k in range(8):
                pi = k % 4
                pv = pbanks[pi][:, 0:128].bitcast(BF16).rearrange("p (u c) -> p u c", u=2)
                fns = [trf(pv[:, u, :], hbf[i][:, u, k * 128:(k + 1) * 128], idb) for u in range(2)]
                fw.mm(fns, [kH[i], kC], [pk[pi]])
                dst = hT[:, k, :].rearrange("p (c t) -> p t c", t=16)[:, 2 * tp:2 * tp + 2, :]
                if k % 2 == 0:
                    fw.op(V, ts(dst, pv, g1[:, k:k + 1], ALU.mult), [pk[pi], kC], [khT[k]])
                else:
                    fw.op(A, act(dst, pv, AF.Copy, scale=g1[:, k:k + 1]), [pk[pi], kC], [khT[k]])
        win_v = w_in.rearrange("(k p) n -> p k n", p=128)
        pcount = 0
        for piece in range(8):
            i = piece % 2
            fw.dma(G_, wbuf[i], win_v[:, :, piece * 256:(piece + 1) * 256], sem_wq[i], writes=[kW[i]])
            if piece < 2:
                for t in range(16):
                    pi = 4 + (pcount % 4)
                    pcount += 1
                    pv = pbanks[pi][:, 0:256]
                    fns = [mmf(pv, hT[:, k, t::16], wbuf[i][:, k, :], k == 0, k == 7) for k in range(8)]
                    fw.mm(fns, khT + [kW[i]], [pk[pi]])
                    dst = Uc[:, 16 * piece:16 * piece + 16, t, :]
                    srcv = pv.rearrange("p (g h) -> p g h", g=16)
                    if t % 2 == 0:
                        fw.op(V, cp(dst, srcv), [pk[pi]], [kUc])
                    else:
                        fw.op(A, act(dst, srcv, AF.Copy), [pk[pi]], [kUc])
            elif piece < 6:
                dstT, kd, sc = (qT, kq, 0.125) if piece < 4 else (kT, kk, 1.0)
                for mm_ in range(2):
                    m = (piece % 2) * 2 + mm_
                    for n in range(4):
                        pi = 4 + (pcount % 4)
                        pcount += 1
                        pv = pbanks[pi][:, :]
                        fns = [mmf(pv, wbuf[i][:, k, mm_ * 128:(mm_ + 1) * 128], hT[:, k, n * 512:(n + 1) * 512],
                                   k == 0, k == 7) for k in range(8)]
                        fw.mm(fns, khT + [kW[i]], [pk[pi]])
                        dst = dstT[:, m, n * 512:(n + 1) * 512]
                        if n % 2 == 0:
                            fw.op(V, ts(dst, pv, sc, ALU.mult), [pk[pi]], [kd])
                        else:
                            fw.op(A, act(dst, pv, AF.Copy, scale=sc), [pk[pi]], [kd])
            else:
                hh = piece - 6
                for it in range(16):
                    pi = 4 + (pcount % 4)
                    pcount += 1
                    pv = pbanks[pi][:, 0:256]
                    fns = [mmf(pv, hT[:, k, it * 128:(it + 1) * 128], wbuf[i][:, k, :], k == 0, k == 7)
                           for k in range(8)]
                    fw.mm(fns, khT + [kW[i]], [pk[pi]])
                    dst = Vt[:, it, 4 * hh:4 * hh + 4, 0:64]
                    src = pv.rearrange("p (h d) -> p h d", h=4)
                    if it % 2 == 0:
                        fw.op(V, cp(dst, src), [pk[pi]], [kv])
                    else:
                        fw.op(A, act(dst, src, AF.Copy), [pk[pi]], [kv])
        fw.op(G_, lambda E: E.memset(Vt[:, :, :, 64:65], 1.0), [kv], [kv])
        if dbg and b == 0:
            fw.dma(SY, dbg_t['uc'], Uc, sem_d, [kUc], [kOut])
            fw.dma(SY, dbg_t['qT'], qT, sem_d, [kq], [kOut])
            fw.dma(SY, dbg_t['vt'], Vt, sem_d, [kv], [kOut])
        fw.barrier()

        if STAGE < 2:
            raise _Stop()
        a2 = Bump(181 * K, ARENA)
        Sbf = a2([128, 2, 16, 130], BF16)
        a2b = Bump(20 * K, 84 * K)
        UT = a2b([128, 32, 2, 128], BF16)
        Wf = a2b([128, 2, 16, 128], F32)
        GTb = [a2b([128, 2, 2, 2, 128], BF16) for _ in range(2)]
        T1 = a2b([128, 2, 16], F32)
        T2 = a2b([128, 2, 16], F32)
        kUT, kWf, kSb, kT1, kT2 = Tk(), Tk(), Tk(), Tk(), Tk()
        kGTb = [Tk(), Tk()]
        for g in range(32):
            pi = g % 4
            pv = pbanks[pi][:, 0:128].bitcast(BF16).rearrange("p (j c) -> p j c", j=2)
            fns = [trf(pv[:, j, :], Uc[:, g, 8 * j:8 * j + 8, :].rearrange("p s h -> p (s h)"), idb) for j in range(2)]
            fw.mm(fns, [kUc, kC], [pk[pi]])
            if g % 2 == 0:
                fw.op(V, cp(UT[:, g], pv), [pk[pi]], [kUT])
            else:
                fw.op(A, act(UT[:, g], pv, AF.Copy), [pk[pi]], [kUT])
        if STAGE < 2.2:
            raise _Stop()
        for gl in range(16):
            i = gl % 2
            for gh in range(2):
                fw.dma(SY, GTb[i][:, gh], GT_d[16 * gh + gl], sem_w[i], writes=[kGTb[i]])
            pi = 4 + gl % 4
            pv = pbanks[pi][:, 0:256].rearrange("p (r c) -> p r c", r=2)
            fns = []
            for r in range(2):
                n = 0
                for gh in range(2):
                    for j in range(2):
                        fns.append(mmf(pv[:, r, :], GTb[i][:, gh, j, r, :], UT[:, 16 * gh + gl, j, :], n == 0, n == 3))
                        n += 1
            fw.mm(fns, [kUT, kGTb[i]], [pk[pi]])
            fw.op(V, cp(Wf[:, :, gl, :], pv), [pk[pi]], [kWf])
        if STAGE < 2.5:
            raise _Stop()
        WfB = reg(116 * K, [128, 2, 16, 128], F32)
        TT = reg(190 * K, [128, 2, 16, 128], F32)
        kWB, kTT = kUc, Tk()
        ks = {'cur': Wf, 'nxt': WfB, 'kcur': kWf, 'knxt': kWB}

        def ks_level(k):
            d = 1 << k
            n = 128 - d
            cur, nxt, kcur, knxt = ks['cur'], ks['nxt'], ks['kcur'], ks['knxt']
            fw.op(V, tt(TT[:, :, :, 0:n], cur[:, :, :, 0:n], bc(LAk[:, k], 3, [128, 2, 16, n]), ALU.mult),
                  [kcur, kC], [kTT])
            fw.op(V, tt(nxt[:, :, :, d:128], cur[:, :, :, d:128], TT[:, :, :, 0:n], ALU.add), [kcur, kTT], [knxt])
            fw.op(V, tt(TT[:, 0, :, 0:n], cur[:, 1, :, 0:n], bc(LIn[:, k, :], 2, [128, 16, n]), ALU.mult),
                  [kcur, kC], [kTT])
            fw.op(V, tt(TT[:, 1, :, 0:n], cur[:, 0, :, 0:n], bc(LI[:, k, :], 2, [128, 16, n]), ALU.mult),
                  [kcur, kC], [kTT])
            fw.op(V, tt(nxt[:, :, :, d:128], nxt[:, :, :, d:128], TT[:, :, :, 0:n], ALU.add), [knxt, kTT], [knxt])
            fw.op(V, cp(nxt[:, :, :, 0:d], cur[:, :, :, 0:d]), [kcur], [knxt])
            ks['cur'], ks['nxt'], ks['kcur'], ks['knxt'] = nxt, cur, knxt, kcur

        def ks_finish():
            Wfin, kWfin = ks['cur'], ks['kcur']
            fw.op(V, lambda E: E.memset(Sbf[:, :, :, 0:2], 0.0), [], [kSb])
            for r in range(2):
                fw.op(A, act(Sbf[:, r, :, 2:130], Wfin[:, r], AF.Copy), [kWfin], [kSb])
            if dbg and b == 0:
                fw.dma(SY, dbg_t['S'], Wfin, sem_d, [kWfin], [kOut])

        if STAGE < 3:
            raise _Stop()
        a3 = Bump(52 * K, 84 * K)
        MBT = a3([128, S], BF16)
        kmT = a3([128, 4, 8], BF16)
        kmf = a3([128, 4, 8], F32)
        gt = a3([128, 8, 8], F32)
        top8 = a3([128, 8, 8], F32)
        mbt = a3([128, 2, 64], BF16)
        PT = [a3([128, 256], BF16) for _ in range(3)]
        Ao = a3([128, 2, 512], F32)
        Aj = a3([128, 512], F32)
        Ab = a3([128, 2, 512], BF16)
        rden_a = a3([128, 4], F32)
        kMB, kkm, kgt, kmb = Tk(), Tk(), Tk(), Tk()
        Esel = reg(100 * K, [128, 64, 128], BF16)
        kEs = Tk()
        fw.op(V, cp(Esel[0:64], bc(idb[0:64, 0:64], 2, [64, 64, 128])), [kC], [kEs])
        fw.op(V, cp(Esel[64:128], bc(idb[64:128, 64:128], 2, [64, 64, 128])), [kC], [kEs])
        kPT = [Tk() for _ in range(3)]
        kAo, kAj, kAb, kat, krd = Tk(), Tk(), Tk(), Tk(), Tk()
        if STAGE < 3.05:
            raise _Stop()
        kjunk = a3([128, 256], BF16)
        kkj = Tk()
        for m in range(4):
            for n in range(8):
                fw.op(A, (lambda m=m, n=n: (lambda E: E.activation(out=kjunk, in_=kT[:, m, n * 256:(n + 1) * 256],
                                                                   func=AF.Copy, accum_out=kmf[:, m, n:n + 1])))(),
                      [kk], [kkj, kkm])
        if STAGE < 3.08:
            raise _Stop()
        fw.op(A, act(kmT, kmf, AF.Copy, scale=1.0 / 256), [kkm], [kkm])
        if STAGE < 3.1:
            raise _Stop()
        fw.op(G_, lambda E: E.memset(MBT, 0.0), [], [kMB, kGTb[0], kGTb[1]])
        for qt in range(8, 16):
            bq = qt // 2
            pve = pbanks[0][:, 0:32].rearrange("p (h n) -> p h n", h=4)
            pvo = pbanks[1][:, 0:32].rearrange("p (h n) -> p h n", h=4)
            fe = [mmf(pve[:, h2, :], qT[0:64, h2, qt * 128:(qt + 1) * 128], kmT[0:64, h2, :], True, True)
                  for h2 in range(4)]
            fw.mm(fe, [kq, kkm], [pk[0]])
            fo = [mmf(pvo[:, h2, :], qT[64:128, h2, qt * 128:(qt + 1) * 128], kmT[64:128, h2, :], True, True)
                  for h2 in range(4)]
            fw.mm(fo, [kq, kkm], [pk[1]])
            fw.op(G_, lambda E: E.memset(gt, NEG), [kgt], [kgt])
            gtv = gt.rearrange("p (h2 e) n -> p h2 e n", e=2)
            fw.op(V, cp(gtv[:, :, 0, 0:bq], pve[:, :, 0:bq]), [pk[0], kgt], [kgt])
            fw.op(V, cp(gtv[:, :, 1, 0:bq], pvo[:, :, 0:bq]), [pk[1], kgt], [kgt])
            for h in range(8):
                fw.op(V, (lambda hh: (lambda E: E.max(out=top8[:, hh, :], in_=gt[:, hh, :])))(h), [kgt], [kmb])
            for dup in range(2):
                fw.op(V, tt(mbt[:, dup, :].rearrange("p (h n) -> p h n", h=8), gt,
                            top8[:, :, 2:3].broadcast_to([128, 8, 8]), ALU.is_lt), [kgt, kmb], [kmb])
            pi2 = 2 + qt % 2
            pv2 = pbanks[pi2][:, 0:64].bitcast(BF16)
            fw.mm([trf(pv2, mbt.rearrange("p d c -> p (d c)"), idb)], [kmb, kC], [pk[pi2]])
            fw.op(V, ts(MBT[:, qt * 128:(qt + 1) * 128], pv2, NEG, ALU.mult), [pk[pi2]], [kMB])
        if STAGE < 3.2:
            raise _Stop()
        ucount = 0
        LAG = 2
        for bq in range(ABQ):
            nkt = 2 * bq + 2

            def stage_a(h, kt, pi, bq=bq):
                pr = slice(64 * (h % 2), 64 * (h % 2) + 64)
                m = h // 2
                n = kt // 2
                pv = pbanks[pi][:, 0:256]
                fns = [mmf(pv, kT[pr, m, kt * 128:(kt + 1) * 128], qT[pr, m, bq * 256:(bq + 1) * 256], True, False)]
                rd = [kq, kk]
                if n == bq:
                    fns.append(mmf(pv, idb, cbias[:, kt - 2 * bq, :], False, True))
                    rd.append(kC)
                elif bq >= 4:
                    r = h * 8 + n
                    fns.append(mmf(pv, Esel[pr, r, :], MBT[pr, bq * 256:(bq + 1) * 256], False, True))
                    rd += [kEs, kMB]
                else:
                    fns[0] = mmf(pv, kT[pr, m, kt * 128:(kt + 1) * 128], qT[pr, m, bq * 256:(bq + 1) * 256], True, True)
                fw.mm(fns, rd, [pk[pi]])
                fw.op(A, act(PT[pi], pv, AF.Exp), [pk[pi]], [kPT[pi]])

            def stage_b(h, kt, pi, nkt=nkt):
                pos = [4 + 2 * (h % 2), 5 + 2 * (h % 2)]
                povs = [pbanks[pos[0]][:, 0:65], pbanks[pos[1]][:, 0:65]]
                fns = [mmf(povs[u], PT[pi][:, u * 128:(u + 1) * 128], Vt[:, kt, h, :], kt == 0, kt == nkt - 1)
                       for u in range(2)]
                fw.mm(fns, [kPT[pi], kv], [pk[pos[0]], pk[pos[1]]])
                if kt == nkt - 1:
                    for u in range(2):
                        fw.op(V, rcp(rden_a[:, u:u + 1], povs[u][:, 64:65]), [pk[pos[u]]], [krd])
                        fw.op(V, ts(Ao[:, u, h * 64:(h + 1) * 64], povs[u][:, 0:64], rden_a[:, u:u + 1], ALU.mult),
                              [pk[pos[u]], krd], [kAo])

            pend = []
            for h in range(8):
                for kt in range(nkt):
                    pi = ucount % 3
                    ucount += 1
                    stage_a(h, kt, pi)
                    pend.append((h, kt, pi))
                    if len(pend) > LAG:
                        stage_b(*pend.pop(0))
            while pend:
                stage_b(*pend.pop(0))
            if bq < 7:
                ks_level(bq)
            else:
                ks_finish()
            for u in range(2):
                fw.op(A, ttr(Aj, Ao[:, u, :], Ao[:, u, :], rden_a[:, 2:3]), [kAo], [kAj, krd])
                fw.op(V, ts(rden_a[:, 3:4], rden_a[:, 2:3], 1.0 / 512, ALU.mult, EPS, ALU.add), [krd], [krd])
                fw.op(A, act(rden_a[:, 3:4], rden_a[:, 3:4], AF.Ln), [krd], [krd])
                fw.op(A, act(rden_a[:, 3:4], rden_a[:, 3:4], AF.Exp, scale=-0.5), [krd], [krd])
                fw.op(V, stt(Ab[:, u, :], Ao[:, u, :], rden_a[:, 3:4], ga, ALU.mult, ALU.mult), [kAo, krd, kC], [kAb])
            for u in range(2):
                pi = 3
                pv = pbanks[pi][:, 0:256].bitcast(BF16).rearrange("p (f c) -> p f c", f=4)
                fns = [trf(pv[:, f, :], Ab[:, u, f * 128:(f + 1) * 128], idb) for f in range(4)]
                fw.mm(fns, [kAb, kC], [pk[pi]])
                tok0 = bq * 256 + u * 128
                fw.op(A, act(attT[:, :, tok0:tok0 + 128], pv, AF.Copy), [pk[pi]], [kat])
        if dbg and b == 0:
            fw.dma(SY, dbg_t['attT'], attT, sem_d, [kat], [kOut])
        fw.barrier()

        if STAGE < 4:
            raise _Stop()
        a4 = Bump(132 * K, 181 * K)
        Y1 = a4([128, 16, 512], BF16)
        TCb = [(a4([128, 2, 2, 256], BF16), a4([128, 2, 256], BF16)) for _ in range(2)]
        wgl = a4([128, 4, 512], BF16)
        Y1T = [a4([128, 4, 128], BF16) for _ in range(2)]
        zt = a4([128, 512], F32)
        zs = a4([128, 512], F32)
        y2 = a4([128, 512], F32)
        y2b = a4([128, 512], BF16)
        yq = a4([128, 256], F32)
        yc = a4([128, 256], F32)
        kY1, kwg, kzt, kzs, ky2, ky2b, kss, kyq, kyc, kssm = [Tk() for _ in range(10)]
        kTC = [Tk(), Tk()]
        kY1T = [Tk(), Tk()]
        fw.dma(G_, wgl, w_glu.rearrange("(k p) n -> p k n", p=128), sem_og, writes=[kwg])
        C1 = 0.7978845608028654 * 2.0
        for gl in range(16):
            i = gl % 2
            for gh in range(2):
                fw.dma(SY, TCb[i][0][:, gh], TOEP_d[16 * gh + gl], sem_w[i], writes=[kTC[i]])
            fw.dma(SY, TCb[i][1], CT_d[gl], sem_w[i], writes=[kTC[i]])
            for gh in range(2):
                g = 16 * gh + gl
                pr = slice(64 * gh, 64 * gh + 64)
                pi = (2 * gl + gh) % 4
                pv = pbanks[pi][:, 0:256]
                fns = [mmf(pv, UT[:, g, 0, :], TCb[i][0][:, gh, 0, :], True, False),
                       mmf(pv, UT[:, g, 1, :], TCb[i][0][:, gh, 1, :], False, False),
                       mmf(pv, Sbf[pr, 0, gl, 1:129], TCb[i][1][pr, 0, :], False, False),
                       mmf(pv, Sbf[pr, 1, gl, 1:129], TCb[i][1][pr, 1, :], False, True)]
                fw.mm(fns, [kUT, kSb, kTC[i]], [pk[pi]])
                fw.op(A, act(Y1[:, :, 16 * g:16 * g + 16], pv.rearrange("p (t h) -> p t h", t=16), AF.Gelu_apprx_tanh),
                      [pk[pi]], [kY1])
        if dbg and b == 0:
            fw.dma(SY, dbg_t['y1'], Y1, sem_d, [kY1], [kOut])
        for t in range(16):
            i = t % 2
            pi = 4 + i
            pv = pbanks[pi][:, 0:256].bitcast(BF16).rearrange("p (f c) -> p f c", f=4)
            fns = [trf(pv[:, f, :], Y1[:, t, f * 128:(f + 1) * 128], idb) for f in range(4)]
            fw.mm(fns, [kY1, kC], [pk[pi]])
            fw.op(A, act(Y1T[i], pv, AF.Copy), [pk[pi]], [kY1T[i]])
            pz = 6 + i
            pzv = pbanks[pz][:, :]
            fns = [mmf(pzv, Y1T[i][:, f, :], wgl[:, f, :], f == 0, f == 3) for f in range(4)]
            fw.mm(fns, [kY1T[i], kwg], [pk[pz]])
            fw.op(V, tt(zt, pzv, bg, ALU.add), [pk[pz], kC], [kzt])
            fw.op(A, act(zs, zt, AF.Exp, scale=-1.0), [kzt], [kzs])
            fw.op(V, ts(zs, zs, 1.0, ALU.add), [kzs], [kzs])
            fw.op(V, rcp(zs, zs), [kzs], [kzs])
            fw.op(V, tt(y2, zs, Y1[:, t, :], ALU.mult), [kzs, kY1], [ky2])
            fw.op(A, ttr(zt, y2, y2, stat[:, 32:33]), [ky2, kzt], [kzt, kss])
            fw.op(V, ts(stat[:, 33:34], stat[:, 32:33], 1.0 / 512, ALU.mult, EPS, ALU.add), [kss], [kss])
            fw.op(A, act(stat[:, 33:34], stat[:, 33:34], AF.Ln), [kss], [kss])
            fw.op(A, act(stat[:, 33:34], stat[:, 33:34], AF.Exp, scale=-0.5), [kss], [kss])
            fw.op(V, stt(y2b, y2, stat[:, 33:34], gs, ALU.mult, ALU.mult), [ky2, kss, kC], [ky2b])
            pi2 = i
            pv2 = pbanks[pi2][:, 0:256].bitcast(BF16).rearrange("p (f c) -> p f c", f=4)
            fns = [trf(pv2[:, f, :], y2b[:, f * 128:(f + 1) * 128], idb) for f in range(4)]
            fw.mm(fns, [ky2b, kC], [pk[pi2]])
            fw.op(A, act(ssmT[:, :, t, :], pv2, AF.Copy), [pk[pi2]], [kssm])
        if dbg and b == 0:
            fw.dma(SY, dbg_t['ssmT'], ssmT, sem_d, [kssm], [kOut])
        fw.barrier()

        if STAGE < 5:
            raise _Stop()
        a5 = Bump(116 * K, ARENA)
        wo = a5([128, 8, 1024], BF16)
        otmp = a5([128, 1024], F32)
        oj = a5([128, 512], F32)
        kwo, kot, koj, kst4 = Tk(), Tk(), Tk(), Tk()
        kX1 = [Tk() for _ in range(16)]
        fw.dma(G_, wo, w_out.rearrange("(k p) n -> p k n", p=128), sem_og, writes=[kwo])
        for q4 in range(4):
            fw.dma(SY, X1[:, 4 * q4:4 * q4 + 4, :], xv[:, 4 * q4:4 * q4 + 4, :], sem_x[q4],
                   writes=[kX1[4 * q4 + j] for j in range(4)])
        for t in range(16):
            pis = [2 * (t % 4), 2 * (t % 4) + 1]
            for hf in range(2):
                pv = pbanks[pis[hf]][:, :]
                fns = []
                for k in range(8):
                    lhs = ssmT[:, k, t, :] if k < 4 else attT[:, k - 4, t::16]
                    fns.append(mmf(pv, lhs, wo[:, k, hf * 512:(hf + 1) * 512], k == 0, k == 7))
                fw.mm(fns, [kssm, kat, kwo], [pk[pis[hf]]])
                fw.op(A, sqa(oj, pv, stat[:, 40 + hf:41 + hf]), [pk[pis[hf]]], [koj, kst4])
            fw.op(V, tt(stat[:, 42:43], stat[:, 40:41], stat[:, 41:42], ALU.add), [kst4], [kst4])
            fw.op(V, ts(stat[:, 42:43], stat[:, 42:43], 1.0 / D, ALU.mult, EPS, ALU.add), [kst4], [kst4])
            fw.op(A, act(stat[:, 42:43], stat[:, 42:43], AF.Ln), [kst4], [kst4])
            fw.op(A, act(stat[:, 42:43], stat[:, 42:43], AF.Exp, scale=-0.5), [kst4], [kst4])
            for hf in range(2):
                pv = pbanks[pis[hf]][:, :]
                sl = slice(hf * 512, (hf + 1) * 512)
                fw.op(V, stt(otmp[:, sl], pv, stat[:, 42:43], g2[:, sl], ALU.mult, ALU.mult),
                      [pk[pis[hf]], kst4, kC], [kot])
            fw.op(G_, tt(X1[:, t, :], X1[:, t, :], otmp, ALU.add), [kot, kX1[t]], [kX1[t]])
        if dbg and b == 0:
            fw.dma(SY, dbg_t['x1'], X1, sem_d, kX1, [kOut])
        fw.barrier()

        if STAGE < 6:
            raise _Stop()
        f0 = Bump(86 * K, ARENA)
        h2T = f0([128, 8, 16, 128], BF16)
        fT = f0([128, NFF, 16, 128], BF16)
        f1 = Bump(84 * K, 86 * K)
        hb2 = f1([128, 1024], BF16)
        khb, kfj, kh2T, kfT, kst5, kot2 = Tk(), Tk(), Tk(), Tk(), Tk(), Tk()
        ov = out[b].rearrange("(c t) d -> c t d", t=16)
        fj1 = reg(118 * K, [128, 1024], F32)
        for t in range(16):
            fw.op(A, ttr(fj1, X1[:, t, :], X1[:, t, :], stat[:, 44:45]), [kX1[t]], [kfj, kst5])
            fw.op(V, ts(stat[:, 45:46], stat[:, 44:45], 1.0 / D, ALU.mult, EPS, ALU.add), [kst5], [kst5])
            fw.op(A, act(stat[:, 45:46], stat[:, 45:46], AF.Ln), [kst5], [kst5])
            fw.op(A, act(stat[:, 45:46], stat[:, 45:46], AF.Exp, scale=-0.5), [kst5], [kst5])
            fw.op(A, act(hb2, X1[:, t, :], AF.Copy, scale=stat[:, 45:46]), [kX1[t], kst5], [khb])
            for kk_ in range(2):
                pi = kk_ + 2 * (t % 2)
                pv = pbanks[pi][:, 0:256].bitcast(BF16).rearrange("p (f c) -> p f c", f=4)
                fns = [trf(pv[:, f, :], hb2[:, (4 * kk_ + f) * 128:(4 * kk_ + f + 1) * 128], idb) for f in range(4)]
                fw.mm(fns, [khb, kC], [pk[pi]])
                for f in range(4):
                    k = 4 * kk_ + f
                    if f % 2 == 0:
                        fw.op(V, ts(h2T[:, k, t, :], pv[:, f, :], g3[:, k:k + 1], ALU.mult), [pk[pi], kC], [kh2T])
                    else:
                        fw.op(A, act(h2T[:, k, t, :], pv[:, f, :], AF.Copy, scale=g3[:, k:k + 1]),
                              [pk[pi], kC], [kh2T])
        kO = Tk()
        fw.dma(SY, ov, X1, sem_o, kX1, [kO])
        fw.barrier()
        f2 = Bump(64 * K, 86 * K)
        wg = [f2([128, 8, 128], BF16) for _ in range(2)]
        wu = [f2([128, 8, 128], BF16) for _ in range(2)]
        sg = [f2([128, 512], F32) for _ in range(2)]
        wd = reg(20 * K, [128, NFF, 1024], BF16)
        kwg = [Tk(), Tk()]
        kwd = Tk()
        ksg = [Tk(), Tk()]
        wg_v = w_gate.rearrange("(k p) n -> p k n", p=128)
        wu_v = w_up.rearrange("(k p) n -> p k n", p=128)
        wd_v = w_down.rearrange("(j p) n -> p j n", p=128)
        hv = h2T.rearrange("p k t c -> p k (t c)")
        pc = 0
        for j in range(NFF):
            i = j % 2
            fw.dma(G_, wg[i], wg_v[:, :, j * 128:(j + 1) * 128], sem_g[i], writes=[kwg[i]])
            fw.dma(G_, wu[i], wu_v[:, :, j * 128:(j + 1) * 128], sem_g[i], writes=[kwg[i]])
            fw.dma(G_, wd[:, j, :], wd_v[:, j, :], sem_dn, writes=[kwd])
            for n in range(4):
                pg_, pu_ = 2 * (pc % 4), 2 * (pc % 4) + 1
                si = pc % 2
                pc += 1
                pgv, puv = pbanks[pg_][:, :], pbanks[pu_][:, :]
                fns = [mmf(pgv, wg[i][:, k, :], hv[:, k, n * 512:(n + 1) * 512], k == 0, k == 7) for k in range(8)]
                fw.mm(fns, [kh2T, kwg[i]], [pk[pg_]])
                fns = [mmf(puv, wu[i][:, k, :], hv[:, k, n * 512:(n + 1) * 512], k == 0, k == 7) for k in range(8)]
                fw.mm(fns, [kh2T, kwg[i]], [pk[pu_]])
                fw.op(A, act(sg[si], pgv, AF.Silu), [pk[pg_]], [ksg[si]])
                fw.op(V, tt(fT[:, j].rearrange("p t c -> p (t c)")[:, n * 512:(n + 1) * 512], sg[si], puv, ALU.mult),
                      [ksg[si], pk[pu_]], [kfT])
        fw.barrier()
        f3 = Bump(64 * K, 86 * K)
        xr = [f3([128, 1024], F32) for _ in range(2)]
        ot2 = f3([128, 1024], F32)
        fj3 = f3([128, 512], F32)
        kxr = [Tk(), Tk()]
        for tg in range(4):
            for tl in range(4):
                t = 4 * tg + tl
                if tl < 2:
                    pass
            for j in range(NFF):
                fns = []
                for tl in range(4):
                    t = 4 * tg + tl
                    for hf in range(2):
                        fns.append(mmf(pbanks[2 * tl + hf][:, :], fT[:, j, t, :], wd[:, j, hf * 512:(hf + 1) * 512],
                                       j == 0, j == NFF - 1))
                fw.mm(fns, [kfT, kwd], pk)
            for tl in range(4):
                t = 4 * tg + tl
                xi = t % 2
                fw.dma(SY, xr[xi], ov[:, t, :], sem_x[xi], [kO], [kxr[xi]])
                for hf in range(2):
                    pv = pbanks[2 * tl + hf][:, :]
                    fw.op(A, sqa(fj3, pv, stat[:, 48 + hf:49 + hf]), [pk[2 * tl + hf]], [kfj, kst5])
                fw.op(V, tt(stat[:, 50:51], stat[:, 48:49], stat[:, 49:50], ALU.add), [kst5], [kst5])
                fw.op(V, ts(stat[:, 50:51], stat[:, 50:51], 1.0 / D, ALU.mult, EPS, ALU.add), [kst5], [kst5])
                fw.op(A, act(stat[:, 50:51], stat[:, 50:51], AF.Ln), [kst5], [kst5])
                fw.op(A, act(stat[:, 50:51], stat[:, 50:51], AF.Exp, scale=-0.5), [kst5], [kst5])
                for hf in range(2):
                    pv = pbanks[2 * tl + hf][:, :]
                    sl = slice(hf * 512, (hf + 1) * 512)
                    fw.op(V, stt(ot2[:, sl], pv, stat[:, 50:51], g4[:, sl], ALU.mult, ALU.mult),
                          [pk[2 * tl + hf], kst5, kC], [kot2])
                fw.op(G_, tt(xr[xi], xr[xi], ot2, ALU.add), [kot2, kxr[xi]], [kxr[xi]])
                fw.dma(SY, ov[:, t, :], xr[xi], sem_st[xi], [kxr[xi]], [kO])
        fw.barrier()
    except _Stop:
        pass
    fw.barrier()
    fw.emit()
    return nc, es


_CACHE = {}


def _consts():
    idf = np.eye(128, dtype=np.float32)
    cb = np.zeros((128, 2, 256), np.float32)
    for r in range(2):
        kpos = r * 128 + np.arange(128)[:, None]
        qpos = np.arange(256)[None, :]
        cb[:, r, :] = np.where(kpos <= qpos, 0.0, NEG)
    tm = np.zeros((128, 2, 256), np.float32)
    dm = np.zeros((128, 2, 256), np.float32)
    for j in range(2):
        for s8 in range(8):
            for hi in range(16):
                sp = 8 * j + s8
                row = s8 * 16 + hi
                for jj in range(2):
                    pass
                for tp in range(16):
                    if tp >= sp:
                        tm[row, j, tp * 16:(tp + 1) * 16] = 1.0
                dm[row, j, sp * 16 + hi] = 1.0
    nv = np.tile(np.arange(1, 33, dtype=np.float32)[None, :], (128, 1))
    return idf, cb, tm, dm, nv


def kernel(**inp):
    f32 = np.float32
    x = np.ascontiguousarray(inp['x'], dtype=f32)

    def sq(k):
        return np.ascontiguousarray(inp[k][0], dtype=f32)

    def gp(a):
        a = a.reshape((2, 16) + a.shape[1:])
        a = np.moveaxis(a, 2, 1)
        return np.ascontiguousarray(a.reshape((128, 16) + a.shape[3:]))
    idf, cb, tm, dm, nv = _consts()
    shared = dict(
        w_in=sq('w_in'), w_glu=sq('w_glu'), w_out=sq('w_out'), w_gate=sq('w_gate'), w_up=sq('w_up'),
        w_down=sq('w_down'),
        areT=gp(sq('ssm_a_re')), aimT=gp(sq('ssm_a_im')),
        ldtT=gp(np.ascontiguousarray(np.broadcast_to(sq('ssm_log_dt')[:, None], (32, 64)))),
        breT=gp(sq('ssm_b_re')), bimT=gp(sq('ssm_b_im')),
        creT=gp(np.ascontiguousarray(sq('ssm_c_re').transpose(0, 2, 1))),
        cimT=gp(np.ascontiguousarray(sq('ssm_c_im').transpose(0, 2, 1))),
        drep=np.ascontiguousarray(np.broadcast_to(sq('ssm_d')[None], (128, 32, 16))),
        g1c=np.ascontiguousarray(sq('g_pre_mix').reshape(8, 128).T),
        g3c=np.ascontiguousarray(sq('g_pre_ffn').reshape(8, 128).T),
        g2r=np.ascontiguousarray(np.broadcast_to(sq('g_post_mix')[None], (128, 1024))),
        g4r=np.ascontiguousarray(np.broadcast_to(sq('g_post_ffn')[None], (128, 1024))),
        gsr=np.ascontiguousarray(np.broadcast_to(sq('g_ssm_out')[None], (128, 512))),
        gar=np.ascontiguousarray(np.broadcast_to(sq('g_attn_out')[None], (128, 512))),
        bgr=np.ascontiguousarray(np.broadcast_to(sq('b_glu')[None], (128, 512))),
        c_idf=idf, c_cb=cb, c_tm=tm, c_dm=dm, c_nv=nv,
    )
    if 'nc' not in _CACHE:
        _CACHE['nc'] = build(DEBUG)
    nc, _es = _CACHE['nc']
    in_maps = []
    for c in range(8):
        m = dict(shared)
        m['x'] = np.ascontiguousarray(x[2 * c:2 * c + 2])
        in_maps.append(m)
    res = run_bass_kernel_spmd(nc, in_maps, core_ids=list(range(8)))
    _CACHE['res'] = res
    return np.concatenate([np.asarray(r['out'], dtype=f32) for r in res.results], axis=0)
```

```python
import math
import numpy as np
from contextlib import ExitStack
import concourse.bass as bass
import concourse.mybir as mybir
from concourse.alu_op_type import AluOpType as ALU
from concourse.bass_utils import run_bass_kernel_spmd

F32 = mybir.dt.float32
BF16 = mybir.dt.bfloat16
U8 = mybir.dt.uint8
I32 = mybir.dt.int32
AF = mybir.ActivationFunctionType
AX = mybir.AxisListType

S = 2048
D = 1024
DFF = 2816
NFF = 22
NEG = -30000.0
EPS = 1e-6
ENGS = ['tensor', 'vector', 'scalar', 'gpsimd', 'sync']
DEBUG = False
STAGE = 99
NSTEP = 127
ABQ = 8


class _Stop(Exception):
    pass


class Tk:
    __slots__ = ('w', 'r')

    def __init__(self):
        self.w = {}
        self.r = {}


class FW:
    def __init__(self, nc, es):
        self.nc, self.es = nc, es
        self.prog = {e: [] for e in ENGS}
        self.esem = {e: es.enter_context(nc.semaphore("es_" + e)) for e in ENGS}
        self.ecnt = {e: 0 for e in ENGS}
        self.seen = {e: {} for e in ENGS}
        self.dcnt = {}

    def newsem(self, name):
        s = self.es.enter_context(self.nc.semaphore(name))
        self.dcnt[id(s)] = [s, 0]
        return s

    def _dep(self, eng, reads, writes):
        need = {}

        def add(dct):
            for k, (s, v) in dct.items():
                if need.get(k, (None, 0))[1] < v:
                    need[k] = (s, v)
        for t in reads:
            add(t.w)
        for t in writes:
            add(t.w)
            add(t.r)
        for k, (s, v) in need.items():
            if self.seen[eng].get(k, 0) >= v:
                continue
            self.seen[eng][k] = v
            self.prog[eng].append(('w', s, v))

    def _post(self, tok, reads, writes):
        s, v = tok
        k = id(s)
        for t in reads:
            if t.r.get(k, (None, 0))[1] < v:
                t.r[k] = (s, v)
        for t in writes:
            t.w = {k: (s, v)}
            t.r = {}

    def op(self, eng, fn, reads=(), writes=()):
        self._dep(eng, reads, writes)
        self.ecnt[eng] += 1
        self.prog[eng].append(('o', fn, True))
        self._post((self.esem[eng], self.ecnt[eng]), reads, writes)

    def mm(self, fns, reads=(), writes=()):
        self._dep('tensor', reads, writes)
        for f in fns[:-1]:
            self.prog['tensor'].append(('o', f, False))
        self.ecnt['tensor'] += 1
        self.prog['tensor'].append(('o', fns[-1], True))
        self._post((self.esem['tensor'], self.ecnt['tensor']), reads, writes)

    def dma(self, eng, out, in_, sem, reads=(), writes=()):
        self._dep(eng, reads, writes)
        c = self.dcnt[id(sem)]
        c[1] += 16
        self.prog[eng].append(('d', out, in_, sem))
        self._post((sem, c[1]), reads, writes)

    def barrier(self):
        for e in ENGS:
            for e2 in ENGS:
                if e2 == e or self.ecnt[e2] == 0:
                    continue
                k = id(self.esem[e2])
                if self.seen[e].get(k, 0) < self.ecnt[e2]:
                    self.seen[e][k] = self.ecnt[e2]
                    self.prog[e].append(('w', self.esem[e2], self.ecnt[e2]))
            for k, (s, v) in self.dcnt.items():
                if v > 0 and self.seen[e].get(k, 0) < v:
                    self.seen[e][k] = v
                    self.prog[e].append(('w', s, v))

    def emit(self):
        nc = self.nc
        with nc.Block() as block:
            for e in ENGS:
                prog = self.prog[e]
                sem = self.esem[e]

                def body(E, prog=prog, sem=sem):
                    for it in prog:
                        if it[0] == 'w':
                            E.wait_ge(it[1], it[2])
                        elif it[0] == 'o':
                            ins = it[1](E)
                            if it[2]:
                                ins.then_inc(sem, 1)
                        else:
                            E.dma_start(out=it[1], in_=it[2]).then_inc(it[3], 16)
                getattr(block, e)(body)


def tt(out, in0, in1, op):
    return lambda E: E.tensor_tensor(out=out, in0=in0, in1=in1, op=op)


def ts(out, in0, s1, op0, s2=None, op1=None):
    if op1 is None:
        return lambda E: E.tensor_scalar(out=out, in0=in0, scalar1=s1, scalar2=None, op0=op0)
    return lambda E: E.tensor_scalar(out=out, in0=in0, scalar1=s1, scalar2=s2, op0=op0, op1=op1)


def stt(out, in0, scalar, in1, op0, op1):
    return lambda E: E.scalar_tensor_tensor(out=out, in0=in0, scalar=scalar, in1=in1, op0=op0, op1=op1)


def ttr(out, in0, in1, accum):
    return lambda E: E.activation(out=out, in_=in0, func=AF.Square, accum_out=accum)


def act(out, in_, func, scale=None):
    if scale is None:
        return lambda E: E.activation(out=out, in_=in_, func=func)
    return lambda E: E.activation(out=out, in_=in_, func=func, scale=scale)


def sqa(out, in_, accum):
    return lambda E: E.activation(out=out, in_=in_, func=AF.Square, accum_out=accum)


def rcp(out, in_):
    return lambda E: E.reciprocal(out=out, in_=in_)


def cp(out, in_):
    return lambda E: E.tensor_copy(out=out, in_=in_)


def mmf(out, lhsT, rhs, start, stop):
    assert len(rhs.ap) == 2 and len(lhsT.ap) == 2, ("rhs", rhs.ap, lhsT.ap)
    return lambda E: E.matmul(out, lhsT, rhs, start=start, stop=stop)


def trf(out, in_, ident):
    assert len(ident.ap) == 2 and len(in_.ap) == 2, ("ident", ident.ap, in_.ap)
    return lambda E: E.transpose(out, in_, ident)


def bc(ap, axis, shape):
    return ap.unsqueeze(axis).broadcast_to(list(shape))


def build(dbg=False):
    nc = bass.Bass("TRN2", target_bir_lowering=False)
    es = ExitStack()

    def din(name, shape):
        return nc.dram_tensor(name, list(shape), F32, kind="ExternalInput").ap()

    x = din("x", [2, S, D])
    w_in = din("w_in", [D, 2048])
    w_glu = din("w_glu", [512, 512])
    w_out = din("w_out", [1024, 1024])
    w_gate = din("w_gate", [D, DFF])
    w_up = din("w_up", [D, DFF])
    w_down = din("w_down", [DFF, D])
    areT = din("areT", [128, 16])
    aimT = din("aimT", [128, 16])
    ldtT = din("ldtT", [128, 16])
    breT = din("breT", [128, 16, 16])
    bimT = din("bimT", [128, 16, 16])
    creT = din("creT", [128, 16, 16])
    cimT = din("cimT", [128, 16, 16])
    drep = din("drep", [128, 32, 16])
    g1c = din("g1c", [128, 8])
    g3c = din("g3c", [128, 8])
    g2r = din("g2r", [128, 1024])
    g4r = din("g4r", [128, 1024])
    gsr = din("gsr", [128, 512])
    gar = din("gar", [128, 512])
    bgr = din("bgr", [128, 512])
    c_idf = din("c_idf", [128, 128])
    c_cb = din("c_cb", [128, 2, 256])
    c_tm = din("c_tm", [128, 2, 256])
    c_dm = din("c_dm", [128, 2, 256])
    c_nv = din("c_nv", [128, 32])
    out = nc.dram_tensor("out", [2, S, D], F32, kind="ExternalOutput").ap()
    GT_d = nc.dram_tensor("GT_d", [32, 128, 2, 2, 128], BF16, kind="Internal").ap()
    TOEP_d = nc.dram_tensor("TOEP_d", [32, 128, 2, 256], BF16, kind="Internal").ap()
    CT_d = nc.dram_tensor("CT_d", [16, 128, 2, 256], BF16, kind="Internal").ap()
    dbg_t = {}
    if dbg:
        dbg_t['uc'] = nc.dram_tensor("d_uc", [128, 32, 16, 16], BF16, kind="ExternalOutput").ap()
        dbg_t['qT'] = nc.dram_tensor("d_qT", [128, 4, S], BF16, kind="ExternalOutput").ap()
        dbg_t['vt'] = nc.dram_tensor("d_vt", [128, 16, 8, 65], BF16, kind="ExternalOutput").ap()
        dbg_t['y1'] = nc.dram_tensor("d_y1", [128, 16, 512], BF16, kind="ExternalOutput").ap()
        dbg_t['ssmT'] = nc.dram_tensor("d_ssmT", [128, 4, 16, 128], BF16, kind="ExternalOutput").ap()
        dbg_t['attT'] = nc.dram_tensor("d_attT", [128, 4, S], BF16, kind="ExternalOutput").ap()
        dbg_t['x1'] = nc.dram_tensor("d_x1", [128, 16, 1024], F32, kind="ExternalOutput").ap()
        dbg_t['S'] = nc.dram_tensor("d_S", [128, 2, 16, 128], F32, kind="ExternalOutput").ap()

    ARENA = 206 * 1024
    arena = es.enter_context(nc.sbuf_tensor("arena", [128, ARENA], U8))
    pbanks = [es.enter_context(nc.psum_tensor("pb%d" % i, [128, 512], F32)) for i in range(8)]
    pk = [Tk() for _ in range(8)]
    fw = FW(nc, es)

    def reg(off, shape, dt, p0=0):
        esz = 2 if dt == BF16 else 4
        n = int(np.prod(shape[1:])) * esz
        assert off % 4 == 0 and off + n <= ARENA, (off, n)
        ap = arena[p0:p0 + shape[0], off:off + n].bitcast(dt)
        if len(shape) == 3:
            ap = ap.rearrange("p (a b) -> p a b", a=shape[1])
        elif len(shape) == 4:
            ap = ap.rearrange("p (a b c) -> p a b c", a=shape[1], b=shape[2])
        elif len(shape) == 5:
            ap = ap.rearrange("p (a b c d) -> p a b c d", a=shape[1], b=shape[2], c=shape[3])
        return ap

    class Bump:
        def __init__(self, lo, hi):
            self.o, self.hi = lo, hi

        def __call__(self, shape, dt, p0=0):
            esz = 2 if dt == BF16 else 4
            n = int(np.prod(shape[1:])) * esz
            n4 = (n + 31) // 32 * 32
            a = reg(self.o, shape, dt, p0)
            self.o += n4
            assert self.o <= self.hi, (self.o, self.hi)
            return a

    K = 1024
    V, A, G_, T_, SY = 'vector', 'scalar', 'gpsimd', 'tensor', 'sync'

    cb = Bump(0, 20 * K)
    idf = cb([128, 128], F32)
    idb = cb([128, 128], BF16)
    cbias = cb([128, 2, 256], BF16)
    g1 = cb([128, 8], F32)
    g3 = cb([128, 8], F32)
    g2 = cb([128, 1024], F32)
    g4 = cb([128, 1024], F32)
    gs = cb([128, 512], F32)
    ga = cb([128, 512], F32)
    bg = cb([128, 512], F32)
    stat = cb([128, 64], F32)
    LR = cb([128, 7, 16], F32)
    LI = cb([128, 7, 16], F32)
    LIn = cb([128, 7, 16], F32)
    LAk = cb([128, 7, 2, 16], F32)
    assert cb.o <= 20 * K
    kC = Tk()
    sem_c = fw.newsem("sem_c")
    sem_c2 = fw.newsem("sem_c2")
    for dst, src in [(idf, c_idf), (g1, g1c), (g3, g3c), (g2, g2r), (g4, g4r), (gs, gsr), (ga, gar), (bg, bgr)]:
        fw.dma(SY, dst, src, sem_c, writes=[kC])

    sb = Bump(20 * K, ARENA)
    cb_off = sb.o
    CBre = sb([128, 16, 16, 16], F32)
    cbf = sb([128, 2, 256], F32)
    tmk = sb([128, 2, 256], F32)
    dmk = sb([128, 2, 256], F32)
    nv = sb([128, 32], F32)
    drp = sb([128, 32, 16], F32)
    are = sb([128, 16], F32)
    aim = sb([128, 16], F32)
    ldt = sb([128, 16], F32)
    bre = sb([128, 16, 16], F32)
    bim = sb([128, 16, 16], F32)
    cre = sb([128, 16, 16], F32)
    cim = sb([128, 16, 16], F32)
    kS = Tk()
    for dst, src in [(cbf, c_cb), (tmk, c_tm), (dmk, c_dm), (nv, c_nv), (drp, drep), (are, areT), (aim, aimT),
                     (ldt, ldtT), (bre, breT), (bim, bimT), (cre, creT), (cim, cimT)]:
        fw.dma(SY, dst, src, sem_c2, writes=[kS])
    fw.op(V, cp(idb, idf), [kC], [kC])
    fw.op(V, cp(cbias, cbf), [kS, kC], [kC])

    dtt = sb([128, 16], F32)
    adr = sb([128, 16], F32)
    adi = sb([128, 16], F32)
    ex = sb([128, 16, 32], F32)
    ang = sb([128, 16, 32], F32)
    mag = sb([128, 16, 32], F32)
    magi = sb([128, 16, 32], F32)
    r1 = sb([128, 16, 32], F32)
    r2 = sb([128, 16, 32], F32)
    sn = sb([128, 16, 32], F32)
    cs = sb([128, 16, 32], F32)
    Pre = sb([128, 16, 32], F32)
    Pim = sb([128, 16, 32], F32)
    Qre = sb([128, 16, 32], F32)
    Qim = sb([128, 16, 32], F32)
    sm = [sb([128, 16], F32) for _ in range(10)]
    bbre = sb([128, 16, 16], F32)
    bbim = sb([128, 16, 16], F32)
    tb1 = sb([128, 16, 16], F32)
    tb2 = sb([128, 16, 16], F32)
    k0 = Tk()
    fw.op(A, act(dtt, ldt, AF.Exp), [kS], [k0])
    fw.op(V, tt(adr, are, dtt, ALU.mult), [k0, kS], [k0])
    fw.op(V, tt(adi, aim, dtt, ALU.mult), [k0, kS], [k0])
    fw.op(V, tt(ex, bc(adr, 2, [128, 16, 32]), bc(nv, 1, [128, 16, 32]), ALU.mult), [k0, kS], [k0])
    fw.op(V, tt(ang, bc(adi, 2, [128, 16, 32]), bc(nv, 1, [128, 16, 32]), ALU.mult), [k0, kS], [k0])
    fw.op(A, act(mag, ex, AF.Exp), [k0], [k0])
    fw.op(A, act(magi, ex, AF.Exp, scale=-1.0), [k0], [k0])
    ki = reg(cb_off, [128, 16, 32], I32)
    kf = reg(cb_off + 2048, [128, 16, 32], F32)
    mk = reg(cb_off + 4096, [128, 16, 32], F32)

    def range_reduce(r, shift):
        fw.op(V, ts(r, ang, 1.0 / (2 * math.pi), ALU.mult, shift, ALU.add), [k0], [k0])
        fw.op(V, cp(ki, r), [k0], [k0])
        fw.op(V, cp(kf, ki), [k0], [k0])
        fw.op(V, tt(r, r, kf, ALU.subtract), [k0], [k0])
        fw.op(V, ts(mk, r, -0.5, ALU.is_lt), [k0], [k0])
        fw.op(V, tt(r, r, mk, ALU.add), [k0], [k0])
        fw.op(V, ts(mk, r, 0.5, ALU.is_gt), [k0], [k0])
        fw.op(V, tt(r, r, mk, ALU.subtract), [k0], [k0])
    range_reduce(r1, 0.0)
    range_reduce(r2, 0.25)
    fw.op(A, act(sn, r1, AF.Sin, scale=2 * math.pi), [k0], [k0])
    fw.op(A, act(cs, r2, AF.Sin, scale=2 * math.pi), [k0], [k0])
    fw.op(V, tt(Pre, mag, cs, ALU.mult), [k0], [k0])
    fw.op(V, tt(Pim, mag, sn, ALU.mult), [k0], [k0])
    fw.op(V, tt(Qre, magi, cs, ALU.mult), [k0], [k0])
    fw.op(V, stt(Qim, magi, -1.0, sn, ALU.mult, ALU.mult), [k0], [k0])
    lbre, lbim = Pre[:, :, 0], Pim[:, :, 0]
    nr, den, t0, t1_, cr, ci, rden = sm[0], sm[1], sm[2], sm[3], sm[4], sm[5], sm[6]
    fw.op(V, ts(nr, lbre, -1.0, ALU.add), [k0], [k0])
    fw.op(V, tt(den, are, are, ALU.mult), [k0, kS], [k0])
    fw.op(V, tt(t0, aim, aim, ALU.mult), [k0, kS], [k0])
    fw.op(V, tt(den, den, t0, ALU.add), [k0], [k0])
    fw.op(V, lambda E: E.reciprocal(out=rden, in_=den), [k0], [k0])
    fw.op(V, tt(t0, nr, are, ALU.mult), [k0], [k0])
    fw.op(V, tt(t1_, lbim, aim, ALU.mult), [k0], [k0])
    fw.op(V, tt(t0, t0, t1_, ALU.add), [k0], [k0])
    fw.op(V, tt(cr, t0, rden, ALU.mult), [k0], [k0])
    fw.op(V, tt(t0, lbim, are, ALU.mult), [k0], [k0])
    fw.op(V, tt(t1_, nr, aim, ALU.mult), [k0], [k0])
    fw.op(V, tt(t0, t0, t1_, ALU.subtract), [k0], [k0])
    fw.op(V, tt(ci, t0, rden, ALU.mult), [k0], [k0])
    crb, cib = bc(cr, 2, [128, 16, 16]), bc(ci, 2, [128, 16, 16])
    fw.op(V, tt(tb1, crb, bre, ALU.mult), [k0, kS], [k0])
    fw.op(V, tt(tb2, cib, bim, ALU.mult), [k0, kS], [k0])
    fw.op(V, tt(bbre, tb1, tb2, ALU.subtract), [k0], [k0])
    fw.op(V, tt(tb1, crb, bim, ALU.mult), [k0, kS], [k0])
    fw.op(V, tt(tb2, cib, bre, ALU.mult), [k0, kS], [k0])
    fw.op(V, tt(bbim, tb1, tb2, ALU.add), [k0], [k0])
    fw.op(V, cp(LR[:, 0, :], Pre[:, :, 15]), [k0], [kC])
    fw.op(V, cp(LI[:, 0, :], Pim[:, :, 15]), [k0], [kC])
    for k in range(6):
        fw.op(V, tt(sm[7], LR[:, k, :], LR[:, k, :], ALU.mult), [kC, k0], [k0])
        fw.op(V, tt(sm[8], LI[:, k, :], LI[:, k, :], ALU.mult), [kC, k0], [k0])
        fw.op(V, tt(LR[:, k + 1, :], sm[7], sm[8], ALU.subtract), [k0], [kC])
        fw.op(V, tt(sm[9], LR[:, k, :], LI[:, k, :], ALU.mult), [kC, k0], [k0])
        fw.op(V, ts(LI[:, k + 1, :], sm[9], 2.0, ALU.mult), [k0], [kC])
    fw.op(V, ts(LIn, LI, -1.0, ALU.mult), [kC], [kC])
    fw.op(V, cp(LAk[:, :, 0, :], LR), [kC], [kC])
    fw.op(V, cp(LAk[:, :, 1, :], LR), [kC], [kC])
    SH4 = [128, 16, 16, 16]
    Gre = sb(SH4, F32)
    Gim = sb(SH4, F32)
    CAre = sb(SH4, F32)
    CAim = sb(SH4, F32)
    CBim = sb(SH4, F32)
    u1_off = sb.o
    U1 = sb(SH4, F32)
    U2 = sb(SH4, F32)
    U3, U4 = U1, U2
    CTst = reg(u1_off, [128, 16, 2, 256], BF16)
    kG, kCA, kCB, kU1, kU2, kCT = [Tk() for _ in range(6)]
    kU3, kU4 = kU1, kU2

    def outer(eng, o, a, pn, rd, wr):
        fw.op(eng, tt(o, bc(a, 2, SH4), bc(pn, 3, SH4), ALU.mult), rd, wr)

    def fl(ap):
        return ap.rearrange("p a b c -> p (a b c)")
    outer(V, U1, bbre, Qre[:, :, 0:16], [k0], [kU1])
    outer(G_, U2, bbim, Qim[:, :, 0:16], [k0], [kU2])
    fw.op(V, tt(Gre, U1, U2, ALU.subtract), [kU1, kU2], [kG])
    outer(V, U3, bbre, Qim[:, :, 0:16], [k0], [kU3])
    outer(G_, U4, bbim, Qre[:, :, 0:16], [k0], [kU4])
    fw.op(V, tt(Gim, U3, U4, ALU.add), [kU3, kU4], [kG])
    outer(V, U1, cre, Pre[:, :, 0:16], [k0, kS], [kU1])
    outer(G_, U2, cim, Pim[:, :, 0:16], [k0, kS], [kU2])
    fw.op(V, tt(CAre, U1, U2, ALU.subtract), [kU1, kU2], [kCA])
    outer(V, U3, cre, Pim[:, :, 0:16], [k0, kS], [kU3])
    outer(G_, U4, cim, Pre[:, :, 0:16], [k0, kS], [kU4])
    fw.op(V, stt(fl(CAim), fl(U3), -1.0, fl(U4), ALU.mult, ALU.subtract), [kU3, kU4], [kCA])
    outer(V, U1, cre, Pre[:, :, 16:32], [k0, kS], [kU1])
    outer(G_, U2, cim, Pim[:, :, 16:32], [k0, kS], [kU2])
    fw.op(V, tt(CBre, U1, U2, ALU.subtract), [kU1, kU2], [kCB])
    outer(V, U3, cre, Pim[:, :, 16:32], [k0, kS], [kU3])
    outer(G_, U4, cim, Pre[:, :, 16:32], [k0, kS], [kU4])
    fw.op(V, stt(fl(CBim), fl(U3), -1.0, fl(U4), ALU.mult, ALU.subtract), [kU3, kU4], [kCB])
    fw.op(V, cp(CTst[:, :, 0, :], CBre.rearrange("p g t h -> p g (t h)")), [kCB], [kCT, kU1])
    fw.op(V, cp(CTst[:, :, 1, :], CBim.rearrange("p g t h -> p g (t h)")), [kCB], [kCT, kU1])
    sem_t = fw.newsem("sem_t")
    kDR = Tk()
    fw.dma(SY, CT_d.rearrange("g p r c -> p g r c"), CTst, sem_t, [kCT], [kDR])
    TPst = [sb([128, 2, 256], BF16) for _ in range(2)]
    GTst = [sb([128, 2, 2, 128], BF16) for _ in range(2)]
    TPt = [sb([128, 2, 256], F32) for _ in range(2)]
    TPd = [sb([128, 2, 256], F32) for _ in range(2)]
    kTP = [Tk() for _ in range(2)]
    kGTs = [Tk() for _ in range(2)]
    kTt = [Tk() for _ in range(2)]
    sem_tp = [fw.newsem("sem_tp%d" % i) for i in range(2)]
    sem_gt = [fw.newsem("sem_gt%d" % i) for i in range(2)]
    for gh in range(2):
        pr = slice(64 * gh, 64 * gh + 64)
        for gl in range(16):
            g = 16 * gh + gl
            i = g % 2
            pT, pG = 2 * i, 2 * i + 1
            pt_v = pbanks[pT][:, :].rearrange("p (j c) -> p j c", j=2)
            fns = []
            for j in range(2):
                l_re = Gre[pr, gl, 8 * j:8 * j + 8, :].rearrange("p s h -> p (s h)")
                l_im = Gim[pr, gl, 8 * j:8 * j + 8, :].rearrange("p s h -> p (s h)")
                fns.append(mmf(pt_v[:, j, :], l_re, CAre[pr, gl].rearrange("p t h -> p (t h)"), True, False))
                fns.append(mmf(pt_v[:, j, :], l_im, CAim[pr, gl].rearrange("p t h -> p (t h)"), False, True))
            fw.mm(fns, [kG, kCA], [pk[pT]])
            pg_v = pbanks[pG][:, :].rearrange("p (j r c) -> p j r c", j=2, r=2)
            fns = []
            for j in range(2):
                l_re = Gre[pr, gl, 8 * j:8 * j + 8, :].rearrange("p s h -> p (s h)")
                l_im = Gim[pr, gl, 8 * j:8 * j + 8, :].rearrange("p s h -> p (s h)")
                fns.append(mmf(pg_v[:, j, 0, :], l_re, idf[pr, :], True, True))
                fns.append(mmf(pg_v[:, j, 1, :], l_im, idf[pr, :], True, True))
            fw.mm(fns, [kG, kC], [pk[pG]])
            fw.op(V, tt(TPt[i], pt_v, tmk, ALU.mult), [pk[pT], kS], [kTt[i]])
            fw.op(G_, tt(TPd[i].rearrange("p j (t h) -> p j t h", t=16),
                         dmk.rearrange("p j (t h) -> p j t h", t=16),
                         drp[:, g, :].unsqueeze(1).unsqueeze(1).broadcast_to([128, 2, 16, 16]), ALU.mult),
                  [kS], [kTP[i]])
            fw.op(V, tt(TPst[i], TPt[i], TPd[i], ALU.add), [kTt[i], kTP[i]], [kTP[i]])
            fw.dma(SY, TOEP_d[g], TPst[i], sem_tp[i], [kTP[i]], [kDR])
            fw.op(A, act(GTst[i], pg_v, AF.Copy), [pk[pG]], [kGTs[i]])
            fw.dma(SY, GT_d[g], GTst[i], sem_gt[i], [kGTs[i]], [kDR])
    fw.barrier()

    X1 = reg(20 * K, [128, 16, 1024], F32)
    attT = reg(84 * K, [128, 4, S], BF16)
    ssmT = reg(100 * K, [128, 4, 16, 128], BF16)
    Uc = reg(116 * K, [128, 32, 16, 16], BF16)
    qT = reg(132 * K, [128, 4, S], BF16)
    kT = reg(148 * K, [128, 4, S], BF16)
    Vt = reg(164 * K, [128, 16, 8, 65], BF16)
    sem_x = [fw.newsem("sem_x%d" % i) for i in range(4)]
    sem_w = [fw.newsem("sem_w%d" % i) for i in range(2)]
    sem_o = fw.newsem("sem_o")
    sem_og = fw.newsem("sem_og")
    sem_wq = [fw.newsem("sem_wq%d" % i) for i in range(2)]
    sem_d = fw.newsem("sem_d")
    sem_g = [fw.newsem("sem_g%d" % i) for i in range(2)]
    sem_dn = fw.newsem("sem_dn")
    sem_st = [fw.newsem("sem_st%d" % i) for i in range(4)]
    kOut = Tk()

    try:
      if STAGE < 1:
        raise _Stop()
      for b in range(2):
        a1 = Bump(20 * K, 116 * K)
        Xst = [a1([128, 2, 1024], F32) for _ in range(2)]
        hbf = [a1([128, 2, 1024], BF16) for _ in range(2)]
        hT = a1([128, 8, S], BF16)
        wbuf = [a1([128, 8, 256], BF16) for _ in range(2)]
        junk = a1([128, 1024], F32)
        kX = [Tk(), Tk()]
        kH = [Tk(), Tk()]
        khT = [Tk() for _ in range(8)]
        kW = [Tk(), Tk()]
        kJ = Tk()
        kSt = Tk()
        kUc, kq, kk, kv = Tk(), Tk(), Tk(), Tk()
        xv = x[b].rearrange("(c t) d -> c t d", t=16)
        ss = stat[:, 0:16]
        rs = stat[:, 16:32]
        for tp in range(8):
            i = tp % 2
            fw.dma(SY, Xst[i], xv[:, 2 * tp:2 * tp + 2, :], sem_x[i], writes=[kX[i]])
            for u in range(2):
                t = 2 * tp + u
                fw.op(A, ttr(junk, Xst[i][:, u, :], Xst[i][:, u, :], ss[:, t:t + 1]), [kX[i]], [kJ, kSt])
                fw.op(V, ts(rs[:, t:t + 1], ss[:, t:t + 1], 1.0 / D, ALU.mult, EPS, ALU.add), [kSt], [kSt])
                fw.op(A, act(rs[:, t:t + 1], rs[:, t:t + 1], AF.Ln), [kSt], [kSt])
                fw.op(A, act(rs[:, t:t + 1], rs[:, t:t + 1], AF.Exp, scale=-0.5), [kSt], [kSt])
                fw.op(A, act(hbf[i][:, u, :], Xst[i][:, u, :], AF.Copy, scale=rs[:, t:t + 1]), [kX[i], kSt], [kH[i]])
            for k in range(8):
                pi = k % 4
                pv = pbanks[pi][:, 0:128].bitcast(BF16).rearrange("p (u c) -> p u c", u=2)
                fns = [trf(pv[:, u, :], hbf[i][:, u, k * 128:(k + 1) * 128], idb) for u in range(2)]
                fw.mm(fns, [kH[i], kC], [pk[pi]])
                dst = hT[:, k, :].rearrange("p (c t) -> p t c", t=16)[:, 2 * tp:2 * tp + 2, :]
                if k % 2 == 0:
                    fw.op(V, ts(dst, pv, g1[:, k:k + 1], ALU.mult), [pk[pi], kC], [khT[k]])
                else:
                    fw.op(A, act(dst, pv, AF.Copy, scale=g1[:, k:k + 1]), [pk[pi], kC], [khT[k]])
        win_v = w_in.rearrange("(k p) n -> p k n", p=128)
        pcount = 0
        for piece in range(8):
            i = piece % 2
            fw.dma(G_, wbuf[i], win_v[:, :, piece * 256:(piece + 1) * 256], sem_wq[i], writes=[kW[i]])
            if piece < 2:
                for t in range(16):
                    pi = 4 + (pcount % 4)
                    pcount += 1
                    pv = pbanks[pi][:, 0:256]
                    fns = [mmf(pv, hT[:, k, t::16], wbuf[i][:, k, :], k == 0, k == 7) for k in range(8)]
                    fw.mm(fns, khT + [kW[i]], [pk[pi]])
                    dst = Uc[:, 16 * piece:16 * piece + 16, t, :]
                    srcv = pv.rearrange("p (g h) -> p g h", g=16)
                    if t % 2 == 0:
                        fw.op(V, cp(dst, srcv), [pk[pi]], [kUc])
                    else:
                        fw.op(A, act(dst, srcv, AF.Copy), [pk[pi]], [kUc])
            elif piece < 6:
                dstT, kd, sc = (qT, kq, 0.125) if piece < 4 else (kT, kk, 1.0)
                for mm_ in range(2):
                    m = (piece % 2) * 2 + mm_
                    for n in range(4):
                        pi = 4 + (pcount % 4)
                        pcount += 1
                        pv = pbanks[pi][:, :]
                        fns = [mmf(pv, wbuf[i][:, k, mm_ * 128:(mm_ + 1) * 128], hT[:, k, n * 512:(n + 1) * 512],
                                   k == 0, k == 7) for k in range(8)]
                        fw.mm(fns, khT + [kW[i]], [pk[pi]])
                        dst = dstT[:, m, n * 512:(n + 1) * 512]
                        if n % 2 == 0:
                            fw.op(V, ts(dst, pv, sc, ALU.mult), [pk[pi]], [kd])
                        else:
                            fw.op(A, act(dst, pv, AF.Copy, scale=sc), [pk[pi]], [kd])
            else:
                hh = piece - 6
                for it in range(16):
                    pi = 4 + (pcount % 4)
                    pcount += 1
                    pv = pbanks[pi][:, 0:256]
                    fns = [mmf(pv, hT[:, k, it * 128:(it + 1) * 128], wbuf[i][:, k, :], k == 0, k == 7)
                           for k in range(8)]
                    fw.mm(fns, khT + [kW[i]], [pk[pi]])
                    dst = Vt[:, it, 4 * hh:4 * hh + 4, 0:64]
                    src = pv.rearrange("p (h d) -> p h d", h=4)
                    if it % 2 == 0:
                        fw.op(V, cp(dst, src), [pk[pi]], [kv])
                    else:
                        fw.op(A, act(dst, src, AF.Copy), [pk[pi]], [kv])
        fw.op(G_, lambda E: E.memset(Vt[:, :, :, 64:65], 1.0), [kv], [kv])
        if dbg and b == 0:
            fw.dma(SY, dbg_t['uc'], Uc, sem_d, [kUc], [kOut])
            fw.dma(SY, dbg_t['qT'], qT, sem_d, [kq], [kOut])
            fw.dma(SY, dbg_t['vt'], Vt, sem_d, [kv], [kOut])
        fw.barrier()

        if STAGE < 2:
            raise _Stop()
        a2 = Bump(181 * K, ARENA)
        Sbf = a2([128, 2, 16, 130], BF16)
        a2b = Bump(20 * K, 84 * K)
        UT = a2b([128, 32, 2, 128], BF16)
        Wf = a2b([128, 2, 16, 128], F32)
        GTb = [a2b([128, 2, 2, 2, 128], BF16) for _ in range(2)]
        T1 = a2b([128, 2, 16], F32)
        T2 = a2b([128, 2, 16], F32)
        kUT, kWf, kSb, kT1, kT2 = Tk(), Tk(), Tk(), Tk(), Tk()
        kGTb = [Tk(), Tk()]
        for g in range(32):
            pi = g % 4
            pv = pbanks[pi][:, 0:128].bitcast(BF16).rearrange("p (j c) -> p j c", j=2)
            fns = [trf(pv[:, j, :], Uc[:, g, 8 * j:8 * j + 8, :].rearrange("p s h -> p (s h)"), idb) for j in range(2)]
            fw.mm(fns, [kUc, kC], [pk[pi]])
            if g % 2 == 0:
                fw.op(V, cp(UT[:, g], pv), [pk[pi]], [kUT])
            else:
                fw.op(A, act(UT[:, g], pv, AF.Copy), [pk[pi]], [kUT])
        if STAGE < 2.2:
            raise _Stop()
        for gl in range(16):
            i = gl % 2
            for gh in range(2):
                fw.dma(SY, GTb[i][:, gh], GT_d[16 * gh + gl], sem_w[i], writes=[kGTb[i]])
            pi = 4 + gl % 4
            pv = pbanks[pi][:, 0:256].rearrange("p (r c) -> p r c", r=2)
            fns = []
            for r in range(2):
                n = 0
                for gh in range(2):
                    for j in range(2):
                        fns.append(mmf(pv[:, r, :], GTb[i][:, gh, j, r, :], UT[:, 16 * gh + gl, j, :], n == 0, n == 3))
                        n += 1
            fw.mm(fns, [kUT, kGTb[i]], [pk[pi]])
            fw.op(V, cp(Wf[:, :, gl, :], pv), [pk[pi]], [kWf])
        if STAGE < 2.5:
            raise _Stop()
        WfB = reg(116 * K, [128, 2, 16, 128], F32)
        TT = reg(190 * K, [128, 2, 16, 128], F32)
        kWB, kTT = kUc, Tk()
        ks = {'cur': Wf, 'nxt': WfB, 'kcur': kWf, 'knxt': kWB}

        def ks_level(k):
            d = 1 << k
            n = 128 - d
            cur, nxt, kcur, knxt = ks['cur'], ks['nxt'], ks['kcur'], ks['knxt']
            fw.op(V, tt(TT[:, :, :, 0:n], cur[:, :, :, 0:n], bc(LAk[:, k], 3, [128, 2, 16, n]), ALU.mult),
                  [kcur, kC], [kTT])
            fw.op(V, tt(nxt[:, :, :, d:128], cur[:, :, :, d:128], TT[:, :, :, 0:n], ALU.add), [kcur, kTT], [knxt])
            fw.op(V, tt(TT[:, 0, :, 0:n], cur[:, 1, :, 0:n], bc(LIn[:, k, :], 2, [128, 16, n]), ALU.mult),
                  [kcur, kC], [kTT])
            fw.op(V, tt(TT[:, 1, :, 0:n], cur[:, 0, :, 0:n], bc(LI[:, k, :], 2, [128, 16, n]), ALU.mult),
                  [kcur, kC], [kTT])
            fw.op(V, tt(nxt[:, :, :, d:128], nxt[:, :, :, d:128], TT[:, :, :, 0:n], ALU.add), [knxt, kTT], [knxt])
            fw.op(V, cp(nxt[:, :, :, 0:d], cur[:, :, :, 0:d]), [kcur], [knxt])
            ks['cur'], ks['nxt'], ks['kcur'], ks['knxt'] = nxt, cur, knxt, kcur

        def ks_finish():
            Wfin, kWfin = ks['cur'], ks['kcur']
            fw.op(V, lambda E: E.memset(Sbf[:, :, :, 0:2], 0.0), [], [kSb])
            for r in range(2):
                fw.op(A, act(Sbf[:, r, :, 2:130], Wfin[:, r], AF.Copy), [kWfin], [kSb])
            if dbg and b == 0:
                fw.dma(SY, dbg_t['S'], Wfin, sem_d, [kWfin], [kOut])

        if STAGE < 3:
            raise _Stop()
        a3 = Bump(52 * K, 84 * K)
        MBT = a3([128, S], BF16)
        kmT = a3([128, 4, 8], BF16)
        kmf = a3([128, 4, 8], F32)
        gt = a3([128, 8, 8], F32)
        top8 = a3([128, 8, 8], F32)
        mbt = a3([128, 2, 64], BF16)
        PT = [a3([128, 256], BF16) for _ in range(3)]
        Ao = a3([128, 2, 512], F32)
        Aj = a3([128, 512], F32)
        Ab = a3([128, 2, 512], BF16)
        rden_a = a3([128, 4], F32)
        kMB, kkm, kgt, kmb = Tk(), Tk(), Tk(), Tk()
        Esel = reg(100 * K, [128, 64, 128], BF16)
        kEs = Tk()
        fw.op(V, cp(Esel[0:64], bc(idb[0:64, 0:64], 2, [64, 64, 128])), [kC], [kEs])
        fw.op(V, cp(Esel[64:128], bc(idb[64:128, 64:128], 2, [64, 64, 128])), [kC], [kEs])
        kPT = [Tk() for _ in range(3)]
        kAo, kAj, kAb, kat, krd = Tk(), Tk(), Tk(), Tk(), Tk()
        if STAGE < 3.05:
            raise _Stop()
        kjunk = a3([128, 256], BF16)
        kkj = Tk()
        for m in range(4):
            for n in range(8):
                fw.op(A, (lambda m=m, n=n: (lambda E: E.activation(out=kjunk, in_=kT[:, m, n * 256:(n + 1) * 256],
                                                                   func=AF.Copy, accum_out=kmf[:, m, n:n + 1])))(),
                      [kk], [kkj, kkm])
        if STAGE < 3.08:
            raise _Stop()
        fw.op(A, act(kmT, kmf, AF.Copy, scale=1.0 / 256), [kkm], [kkm])
        if STAGE < 3.1:
            raise _Stop()
        fw.op(G_, lambda E: E.memset(MBT, 0.0), [], [kMB, kGTb[0], kGTb[1]])
        for qt in range(8, 16):
            bq = qt // 2
            pve = pbanks[0][:, 0:32].rearrange("p (h n) -> p h n", h=4)
            pvo = pbanks[1][:, 0:32].rearrange("p (h n) -> p h n", h=4)
            fe = [mmf(pve[:, h2, :], qT[0:64, h2, qt * 128:(qt + 1) * 128], kmT[0:64, h2, :], True, True)
                  for h2 in range(4)]
            fw.mm(fe, [kq, kkm], [pk[0]])
            fo = [mmf(pvo[:, h2, :], qT[64:128, h2, qt * 128:(qt + 1) * 128], kmT[64:128, h2, :], True, True)
                  for h2 in range(4)]
            fw.mm(fo, [kq, kkm], [pk[1]])
            fw.op(G_, lambda E: E.memset(gt, NEG), [kgt], [kgt])
            gtv = gt.rearrange("p (h2 e) n -> p h2 e n", e=2)
            fw.op(V, cp(gtv[:, :, 0, 0:bq], pve[:, :, 0:bq]), [pk[0], kgt], [kgt])
            fw.op(V, cp(gtv[:, :, 1, 0:bq], pvo[:, :, 0:bq]), [pk[1], kgt], [kgt])
            for h in range(8):
                fw.op(V, (lambda hh: (lambda E: E.max(out=top8[:, hh, :], in_=gt[:, hh, :])))(h), [kgt], [kmb])
            for dup in range(2):
                fw.op(V, tt(mbt[:, dup, :].rearrange("p (h n) -> p h n", h=8), gt,
                            top8[:, :, 2:3].broadcast_to([128, 8, 8]), ALU.is_lt), [kgt, kmb], [kmb])
            pi2 = 2 + qt % 2
            pv2 = pbanks[pi2][:, 0:64].bitcast(BF16)
            fw.mm([trf(pv2, mbt.rearrange("p d c -> p (d c)"), idb)], [kmb, kC], [pk[pi2]])
            fw.op(V, ts(MBT[:, qt * 128:(qt + 1) * 128], pv2, NEG, ALU.mult), [pk[pi2]], [kMB])
        if STAGE < 3.2:
            raise _Stop()
        ucount = 0
        LAG = 2
        for bq in range(ABQ):
            nkt = 2 * bq + 2

            def stage_a(h, kt, pi, bq=bq):
                pr = slice(64 * (h % 2), 64 * (h % 2) + 64)
                m = h // 2
                n = kt // 2
                pv = pbanks[pi][:, 0:256]
                fns = [mmf(pv, kT[pr, m, kt * 128:(kt + 1) * 128], qT[pr, m, bq * 256:(bq + 1) * 256], True, False)]
                rd = [kq, kk]
                if n == bq:
                    fns.append(mmf(pv, idb, cbias[:, kt - 2 * bq, :], False, True))
                    rd.append(kC)
                elif bq >= 4:
                    r = h * 8 + n
                    fns.append(mmf(pv, Esel[pr, r, :], MBT[pr, bq * 256:(bq + 1) * 256], False, True))
                    rd += [kEs, kMB]
                else:
                    fns[0] = mmf(pv, kT[pr, m, kt * 128:(kt + 1) * 128], qT[pr, m, bq * 256:(bq + 1) * 256], True, True)
                fw.mm(fns, rd, [pk[pi]])
                fw.op(A, act(PT[pi], pv, AF.Exp), [pk[pi]], [kPT[pi]])

            def stage_b(h, kt, pi, nkt=nkt):
                pos = [4 + 2 * (h % 2), 5 + 2 * (h % 2)]
                povs = [pbanks[pos[0]][:, 0:65], pbanks[pos[1]][:, 0:65]]
                fns = [mmf(povs[u], PT[pi][:, u * 128:(u + 1) * 128], Vt[:, kt, h, :], kt == 0, kt == nkt - 1)
                       for u in range(2)]
                fw.mm(fns, [kPT[pi], kv], [pk[pos[0]], pk[pos[1]]])
                if kt == nkt - 1:
                    for u in range(2):
                        fw.op(V, rcp(rden_a[:, u:u + 1], povs[u][:, 64:65]), [pk[pos[u]]], [krd])
                        fw.op(V, ts(Ao[:, u, h * 64:(h + 1) * 64], povs[u][:, 0:64], rden_a[:, u:u + 1], ALU.mult),
                              [pk[pos[u]], krd], [kAo])

            pend = []
            for h in range(8):
                for kt in range(nkt):
                    pi = ucount % 3
                    ucount += 1
                    stage_a(h, kt, pi)
                    pend.append((h, kt, pi))
                    if len(pend) > LAG:
                        stage_b(*pend.pop(0))
            while pend:
                stage_b(*pend.pop(0))
            if bq < 7:
                ks_level(bq)
            else:
                ks_finish()
            for u in range(2):
                fw.op(A, ttr(Aj, Ao[:, u, :], Ao[:, u, :], rden_a[:, 2:3]), [kAo], [kAj, krd])
                fw.op(V, ts(rden_a[:, 3:4], rden_a[:, 2:3], 1.0 / 512, ALU.mult, EPS, ALU.add), [krd], [krd])
                fw.op(A, act(rden_a[:, 3:4], rden_a[:, 3:4], AF.Ln), [krd], [krd])
                fw.op(A, act(rden_a[:, 3:4], rden_a[:, 3:4], AF.Exp, scale=-0.5), [krd], [krd])
                fw.op(V, stt(Ab[:, u, :], Ao[:, u, :], rden_a[:, 3:4], ga, ALU.mult, ALU.mult), [kAo, krd, kC], [kAb])
            for u in range(2):
                pi = 3
                pv = pbanks[pi][:, 0:256].bitcast(BF16).rearrange("p (f c) -> p f c", f=4)
                fns = [trf(pv[:, f, :], Ab[:, u, f * 128:(f + 1) * 128], idb) for f in range(4)]
                fw.mm(fns, [kAb, kC], [pk[pi]])
                tok0 = bq * 256 + u * 128
                fw.op(A, act(attT[:, :, tok0:tok0 + 128], pv, AF.Copy), [pk[pi]], [kat])
        if dbg and b == 0:
            fw.dma(SY, dbg_t['attT'], attT, sem_d, [kat], [kOut])
        fw.barrier()

        if STAGE < 4:
            raise _Stop()
        a4 = Bump(132 * K, 181 * K)
        Y1 = a4([128, 16, 512], BF16)
        TCb = [(a4([128, 2, 2, 256], BF16), a4([128, 2, 256], BF16)) for _ in range(2)]
        wgl = a4([128, 4, 512], BF16)
        Y1T = [a4([128, 4, 128], BF16) for _ in range(2)]
        zt = [a4([128, 512], F32) for _ in range(2)]
        zs = [a4([128, 512], F32) for _ in range(2)]
        y2 = [a4([128, 512], F32) for _ in range(2)]
        y2b = [a4([128, 512], BF16) for _ in range(2)]
        kY1, kwg, kssm = Tk(), Tk(), Tk()
        kzt, kzs, ky2, ky2b, kss = [[Tk(), Tk()] for _ in range(5)]
        kTC = [Tk(), Tk()]
        kY1T = [Tk(), Tk()]
        fw.dma(G_, wgl, w_glu.rearrange("(k p) n -> p k n", p=128), sem_og, writes=[kwg])
        C1 = 0.7978845608028654 * 2.0
        for gl in range(16):
            i = gl % 2
            for gh in range(2):
                fw.dma(SY, TCb[i][0][:, gh], TOEP_d[16 * gh + gl], sem_w[i], writes=[kTC[i]])
            fw.dma(SY, TCb[i][1], CT_d[gl], sem_w[i], writes=[kTC[i]])
            for gh in range(2):
                g = 16 * gh + gl
                pr = slice(64 * gh, 64 * gh + 64)
                pi = (2 * gl + gh) % 4
                pv = pbanks[pi][:, 0:256]
                fns = [mmf(pv, UT[:, g, 0, :], TCb[i][0][:, gh, 0, :], True, False),
                       mmf(pv, UT[:, g, 1, :], TCb[i][0][:, gh, 1, :], False, False),
                       mmf(pv, Sbf[pr, 0, gl, 1:129], TCb[i][1][pr, 0, :], False, False),
                       mmf(pv, Sbf[pr, 1, gl, 1:129], TCb[i][1][pr, 1, :], False, True)]
                fw.mm(fns, [kUT, kSb, kTC[i]], [pk[pi]])
                fw.op(A, act(Y1[:, :, 16 * g:16 * g + 16], pv.rearrange("p (t h) -> p t h", t=16), AF.Gelu_apprx_tanh),
                      [pk[pi]], [kY1])
        if dbg and b == 0:
            fw.dma(SY, dbg_t['y1'], Y1, sem_d, [kY1], [kOut])
        for t in range(16):
            i = t % 2
            pi = 4 + i
            pv = pbanks[pi][:, 0:256].bitcast(BF16).rearrange("p (f c) -> p f c", f=4)
            fns = [trf(pv[:, f, :], Y1[:, t, f * 128:(f + 1) * 128], idb) for f in range(4)]
            fw.mm(fns, [kY1, kC], [pk[pi]])
            fw.op(A, act(Y1T[i], pv, AF.Copy), [pk[pi]], [kY1T[i]])
            pz = 6 + i
            pzv = pbanks[pz][:, :]
            fns = [mmf(pzv, Y1T[i][:, f, :], wgl[:, f, :], f == 0, f == 3) for f in range(4)]
            fw.mm(fns, [kY1T[i], kwg], [pk[pz]])
            s0, s1 = stat[:, 32 + 2 * i:33 + 2 * i], stat[:, 33 + 2 * i:34 + 2 * i]
            fw.op(V, tt(zt[i], pzv, bg, ALU.add), [pk[pz], kC], [kzt[i]])
            fw.op(A, act(zs[i], zt[i], AF.Exp, scale=-1.0), [kzt[i]], [kzs[i]])
            fw.op(V, ts(zs[i], zs[i], 1.0, ALU.add), [kzs[i]], [kzs[i]])
            fw.op(V, rcp(zs[i], zs[i]), [kzs[i]], [kzs[i]])
            fw.op(V, tt(y2[i], zs[i], Y1[:, t, :], ALU.mult), [kzs[i], kY1], [ky2[i]])
            fw.op(A, ttr(zt[i], y2[i], y2[i], s0), [ky2[i], kzt[i]], [kzt[i], kss[i]])
            fw.op(V, ts(s1, s0, 1.0 / 512, ALU.mult, EPS, ALU.add), [kss[i]], [kss[i]])
            fw.op(A, act(s1, s1, AF.Ln), [kss[i]], [kss[i]])
            fw.op(A, act(s1, s1, AF.Exp, scale=-0.5), [kss[i]], [kss[i]])
            fw.op(V, stt(y2b[i], y2[i], s1, gs, ALU.mult, ALU.mult), [ky2[i], kss[i], kC], [ky2b[i]])
            pi2 = i
            pv2 = pbanks[pi2][:, 0:256].bitcast(BF16).rearrange("p (f c) -> p f c", f=4)
            fns = [trf(pv2[:, f, :], y2b[i][:, f * 128:(f + 1) * 128], idb) for f in range(4)]
            fw.mm(fns, [ky2b[i], kC], [pk[pi2]])
            fw.op(A, act(ssmT[:, :, t, :], pv2, AF.Copy), [pk[pi2]], [kssm])
        if dbg and b == 0:
            fw.dma(SY, dbg_t['ssmT'], ssmT, sem_d, [kssm], [kOut])
        fw.barrier()

        if STAGE < 5:
            raise _Stop()
        a5 = Bump(116 * K, ARENA)
        wo = a5([128, 8, 1024], BF16)
        otmp = [a5([128, 1024], F32) for _ in range(4)]
        oj = a5([128, 512], F32)
        kwo = Tk()
        kot = [Tk() for _ in range(4)]
        kst4 = [Tk() for _ in range(4)]
        kX1 = [Tk() for _ in range(16)]
        fw.dma(G_, wo, w_out.rearrange("(k p) n -> p k n", p=128), sem_og, writes=[kwo])
        for q4 in range(4):
            fw.dma(SY, X1[:, 4 * q4:4 * q4 + 4, :], xv[:, 4 * q4:4 * q4 + 4, :], sem_x[q4],
                   writes=[kX1[4 * q4 + j] for j in range(4)])
        for t in range(16):
            pis = [2 * (t % 4), 2 * (t % 4) + 1]
            for hf in range(2):
                pv = pbanks[pis[hf]][:, :]
                fns = []
                for k in range(8):
                    lhs = ssmT[:, k, t, :] if k < 4 else attT[:, k - 4, t::16]
                    fns.append(mmf(pv, lhs, wo[:, k, hf * 512:(hf + 1) * 512], k == 0, k == 7))
                fw.mm(fns, [kssm, kat, kwo], [pk[pis[hf]]])
                sl4 = t % 4
                c0 = 36 + 3 * sl4
                fw.op(A, sqa(oj, pv, stat[:, c0 + hf:c0 + hf + 1]), [pk[pis[hf]]], [kst4[sl4]])
            sl4 = t % 4
            c0 = 36 + 3 * sl4
            rs4 = stat[:, c0 + 2:c0 + 3]
            fw.op(V, tt(rs4, stat[:, c0:c0 + 1], stat[:, c0 + 1:c0 + 2], ALU.add), [kst4[sl4]], [kst4[sl4]])
            fw.op(V, ts(rs4, rs4, 1.0 / D, ALU.mult, EPS, ALU.add), [kst4[sl4]], [kst4[sl4]])
            fw.op(A, act(rs4, rs4, AF.Ln), [kst4[sl4]], [kst4[sl4]])
            fw.op(A, act(rs4, rs4, AF.Exp, scale=-0.5), [kst4[sl4]], [kst4[sl4]])
            for hf in range(2):
                pv = pbanks[pis[hf]][:, :]
                sl = slice(hf * 512, (hf + 1) * 512)
                fw.op(V, stt(otmp[sl4][:, sl], pv, rs4, g2[:, sl], ALU.mult, ALU.mult),
                      [pk[pis[hf]], kst4[sl4], kC], [kot[sl4]])
            fw.op(G_, tt(X1[:, t, :], X1[:, t, :], otmp[sl4], ALU.add), [kot[sl4], kX1[t]], [kX1[t]])
        if dbg and b == 0:
            fw.dma(SY, dbg_t['x1'], X1, sem_d, kX1, [kOut])
        fw.barrier()

        if STAGE < 6:
            raise _Stop()
        f0 = Bump(86 * K, ARENA)
        h2T = f0([128, 8, 16, 128], BF16)
        fT = f0([128, NFF, 16, 128], BF16)
        hb2 = [reg(118 * K + 4096 + 2048 * i_, [128, 1024], BF16) for i_ in range(2)]
        kfj, kh2T, kfT = Tk(), Tk(), Tk()
        khb = [Tk(), Tk()]
        kst5 = [Tk(), Tk()]
        ov = out[b].rearrange("(c t) d -> c t d", t=16)
        fj1 = reg(118 * K, [128, 1024], F32)
        for t in range(16):
            i5 = t % 2
            q0, q1 = stat[:, 52 + 2 * i5:53 + 2 * i5], stat[:, 53 + 2 * i5:54 + 2 * i5]
            fw.op(A, ttr(fj1, X1[:, t, :], X1[:, t, :], q0), [kX1[t]], [kst5[i5]])
            fw.op(V, ts(q1, q0, 1.0 / D, ALU.mult, EPS, ALU.add), [kst5[i5]], [kst5[i5]])
            fw.op(A, act(q1, q1, AF.Ln), [kst5[i5]], [kst5[i5]])
            fw.op(A, act(q1, q1, AF.Exp, scale=-0.5), [kst5[i5]], [kst5[i5]])
            fw.op(A, act(hb2[i5], X1[:, t, :], AF.Copy, scale=q1), [kX1[t], kst5[i5]], [khb[i5]])
            for kk_ in range(2):
                pi = kk_ + 2 * (t % 2)
                pv = pbanks[pi][:, 0:256].bitcast(BF16).rearrange("p (f c) -> p f c", f=4)
                fns = [trf(pv[:, f, :], hb2[i5][:, (4 * kk_ + f) * 128:(4 * kk_ + f + 1) * 128], idb) for f in range(4)]
                fw.mm(fns, [khb[i5], kC], [pk[pi]])
                for f in range(4):
                    k = 4 * kk_ + f
                    if f % 2 == 0:
                        fw.op(V, ts(h2T[:, k, t, :], pv[:, f, :], g3[:, k:k + 1], ALU.mult), [pk[pi], kC], [kh2T])
                    else:
                        fw.op(A, act(h2T[:, k, t, :], pv[:, f, :], AF.Copy, scale=g3[:, k:k + 1]),
                              [pk[pi], kC], [kh2T])
        kOt = [Tk() for _ in range(16)]
        fw.dma(SY, ov, X1, sem_o, kX1, kOt)
        fw.barrier()
        f2 = Bump(64 * K, 86 * K)
        wg = [f2([128, 8, 128], BF16) for _ in range(2)]
        wu = [f2([128, 8, 128], BF16) for _ in range(2)]
        sg = [f2([128, 512], F32) for _ in range(2)]
        wd = reg(20 * K, [128, NFF, 1024], BF16)
        kwg = [Tk(), Tk()]
        kwd = Tk()
        ksg = [Tk(), Tk()]
        wg_v = w_gate.rearrange("(k p) n -> p k n", p=128)
        wu_v = w_up.rearrange("(k p) n -> p k n", p=128)
        wd_v = w_down.rearrange("(j p) n -> p j n", p=128)
        hv = h2T.rearrange("p k t c -> p k (t c)")
        pc = 0
        for j in range(NFF):
            i = j % 2
            fw.dma(G_, wg[i], wg_v[:, :, j * 128:(j + 1) * 128], sem_g[i], writes=[kwg[i]])
            fw.dma(G_, wu[i], wu_v[:, :, j * 128:(j + 1) * 128], sem_g[i], writes=[kwg[i]])
            fw.dma(G_, wd[:, j, :], wd_v[:, j, :], sem_dn, writes=[kwd])
            for n in range(4):
                pg_, pu_ = 2 * (pc % 4), 2 * (pc % 4) + 1
                si = pc % 2
                pc += 1
                pgv, puv = pbanks[pg_][:, :], pbanks[pu_][:, :]
                fns = [mmf(pgv, wg[i][:, k, :], hv[:, k, n * 512:(n + 1) * 512], k == 0, k == 7) for k in range(8)]
                fw.mm(fns, [kh2T, kwg[i]], [pk[pg_]])
                fns = [mmf(puv, wu[i][:, k, :], hv[:, k, n * 512:(n + 1) * 512], k == 0, k == 7) for k in range(8)]
                fw.mm(fns, [kh2T, kwg[i]], [pk[pu_]])
                fw.op(A, act(sg[si], pgv, AF.Silu), [pk[pg_]], [ksg[si]])
                fw.op(V, tt(fT[:, j].rearrange("p t c -> p (t c)")[:, n * 512:(n + 1) * 512], sg[si], puv, ALU.mult),
                      [ksg[si], pk[pu_]], [kfT])
        fw.barrier()
        f3 = Bump(64 * K, 118 * K)
        xr = [f3([128, 1024], F32) for _ in range(4)]
        ot2 = [f3([128, 1024], F32) for _ in range(2)]
        fj3 = f3([128, 512], F32)
        kxr = [Tk() for _ in range(4)]
        kot2 = [Tk(), Tk()]
        kst6 = [Tk(), Tk()]
        for grp in range(8):
            base = 4 * (grp % 2)
            for tl in range(2):
                t = 2 * grp + tl
                fw.dma(SY, xr[t % 4], ov[:, t, :], sem_x[t % 4], [kOt[t]], [kxr[t % 4]])
            for j in range(NFF):
                fns = []
                for tl in range(2):
                    t = 2 * grp + tl
                    for hf in range(2):
                        fns.append(mmf(pbanks[base + 2 * tl + hf][:, :], fT[:, j, t, :], wd[:, j, hf * 512:(hf + 1) * 512],
                                       j == 0, j == NFF - 1))
                fw.mm(fns, [kfT, kwd], [pk[base + q_] for q_ in range(4)])
            for tl in range(2):
                t = 2 * grp + tl
                xi = t % 4
                i6 = t % 2
                c0 = 56 + 3 * i6
                rs6 = stat[:, c0 + 2:c0 + 3]
                for hf in range(2):
                    bk = base + 2 * tl + hf
                    fw.op(A, sqa(fj3, pbanks[bk][:, :], stat[:, c0 + hf:c0 + hf + 1]), [pk[bk]], [kst6[i6]])
                fw.op(V, tt(rs6, stat[:, c0:c0 + 1], stat[:, c0 + 1:c0 + 2], ALU.add), [kst6[i6]], [kst6[i6]])
                fw.op(V, ts(rs6, rs6, 1.0 / D, ALU.mult, EPS, ALU.add), [kst6[i6]], [kst6[i6]])
                fw.op(A, act(rs6, rs6, AF.Ln), [kst6[i6]], [kst6[i6]])
                fw.op(A, act(rs6, rs6, AF.Exp, scale=-0.5), [kst6[i6]], [kst6[i6]])
                for hf in range(2):
                    bk = base + 2 * tl + hf
                    sl = slice(hf * 512, (hf + 1) * 512)
                    fw.op(V, stt(ot2[i6][:, sl], pbanks[bk][:, :], rs6, g4[:, sl], ALU.mult, ALU.mult),
                          [pk[bk], kst6[i6], kC], [kot2[i6]])
                fw.op(G_, tt(xr[xi], xr[xi], ot2[i6], ALU.add), [kot2[i6], kxr[xi]], [kxr[xi]])
                fw.dma(SY, ov[:, t, :], xr[xi], sem_st[t % 4], [kxr[xi]], [kOt[t]])
        fw.barrier()
    except _Stop:
        pass
    fw.barrier()
    fw.emit()
    return nc, es


_CACHE = {}


def _consts():
    idf = np.eye(128, dtype=np.float32)
    cb = np.zeros((128, 2, 256), np.float32)
    for r in range(2):
        kpos = r * 128 + np.arange(128)[:, None]
        qpos = np.arange(256)[None, :]
        cb[:, r, :] = np.where(kpos <= qpos, 0.0, NEG)
    tm = np.zeros((128, 2, 256), np.float32)
    dm = np.zeros((128, 2, 256), np.float32)
    for j in range(2):
        for s8 in range(8):
            for hi in range(16):
                sp = 8 * j + s8
                row = s8 * 16 + hi
                for jj in range(2):
                    pass
                for tp in range(16):
                    if tp >= sp:
                        tm[row, j, tp * 16:(tp + 1) * 16] = 1.0
                dm[row, j, sp * 16 + hi] = 1.0
    nv = np.tile(np.arange(1, 33, dtype=np.float32)[None, :], (128, 1))
    return idf, cb, tm, dm, nv


def kernel(**inp):
    f32 = np.float32
    x = np.ascontiguousarray(inp['x'], dtype=f32)

    def sq(k):
        return np.ascontiguousarray(inp[k][0], dtype=f32)

    def gp(a):
        a = a.reshape((2, 16) + a.shape[1:])
        a = np.moveaxis(a, 2, 1)
        return np.ascontiguousarray(a.reshape((128, 16) + a.shape[3:]))
    idf, cb, tm, dm, nv = _consts()
    shared = dict(
        w_in=sq('w_in'), w_glu=sq('w_glu'), w_out=sq('w_out'), w_gate=sq('w_gate'), w_up=sq('w_up'),
        w_down=sq('w_down'),
        areT=gp(sq('ssm_a_re')), aimT=gp(sq('ssm_a_im')),
        ldtT=gp(np.ascontiguousarray(np.broadcast_to(sq('ssm_log_dt')[:, None], (32, 64)))),
        breT=gp(sq('ssm_b_re')), bimT=gp(sq('ssm_b_im')),
        creT=gp(np.ascontiguousarray(sq('ssm_c_re').transpose(0, 2, 1))),
        cimT=gp(np.ascontiguousarray(sq('ssm_c_im').transpose(0, 2, 1))),
        drep=np.ascontiguousarray(np.broadcast_to(sq('ssm_d')[None], (128, 32, 16))),
        g1c=np.ascontiguousarray(sq('g_pre_mix').reshape(8, 128).T),
        g3c=np.ascontiguousarray(sq('g_pre_ffn').reshape(8, 128).T),
        g2r=np.ascontiguousarray(np.broadcast_to(sq('g_post_mix')[None], (128, 1024))),
        g4r=np.ascontiguousarray(np.broadcast_to(sq('g_post_ffn')[None], (128, 1024))),
        gsr=np.ascontiguousarray(np.broadcast_to(sq('g_ssm_out')[None], (128, 512))),
        gar=np.ascontiguousarray(np.broadcast_to(sq('g_attn_out')[None], (128, 512))),
        bgr=np.ascontiguousarray(np.broadcast_to(sq('b_glu')[None], (128, 512))),
        c_idf=idf, c_cb=cb, c_tm=tm, c_dm=dm, c_nv=nv,
    )
    if 'nc' not in _CACHE:
        _CACHE['nc'] = build(DEBUG)
    nc, _es = _CACHE['nc']
    in_maps = []
    for c in range(8):
        m = dict(shared)
        m['x'] = np.ascontiguousarray(x[2 * c:2 * c + 2])
        in_maps.append(m)
    res = run_bass_kernel_spmd(nc, in_maps, core_ids=list(range(8)))
    _CACHE['res'] = res
    return np.concatenate([np.asarray(r['out'], dtype=f32) for r in res.results], axis=0)
```

```python
import math
import numpy as np
from contextlib import ExitStack
import concourse.bass as bass
import concourse.mybir as mybir
from concourse.alu_op_type import AluOpType as ALU
from concourse.bass_utils import run_bass_kernel_spmd

F32 = mybir.dt.float32
BF16 = mybir.dt.bfloat16
U8 = mybir.dt.uint8
I32 = mybir.dt.int32
AF = mybir.ActivationFunctionType
AX = mybir.AxisListType

S = 2048
D = 1024
DFF = 2816
NFF = 22
NEG = -30000.0
EPS = 1e-6
ENGS = ['tensor', 'vector', 'scalar', 'gpsimd', 'sync']
DEBUG = False
STAGE = 99
NSTEP = 127
ABQ = 8


class _Stop(Exception):
    pass


class Tk:
    __slots__ = ('w', 'r')

    def __init__(self):
        self.w = {}
        self.r = {}


class FW:
    def __init__(self, nc, es):
        self.nc, self.es = nc, es
        self.prog = {e: [] for e in ENGS}
        self.esem = {e: es.enter_context(nc.semaphore("es_" + e)) for e in ENGS}
        self.ecnt = {e: 0 for e in ENGS}
        self.seen = {e: {} for e in ENGS}
        self.dcnt = {}

    def newsem(self, name):
        s = self.es.enter_context(self.nc.semaphore(name))
        self.dcnt[id(s)] = [s, 0]
        return s

    def _dep(self, eng, reads, writes):
        need = {}

        def add(dct):
            for k, (s, v) in dct.items():
                if need.get(k, (None, 0))[1] < v:
                    need[k] = (s, v)
        for t in reads:
            add(t.w)
        for t in writes:
            add(t.w)
            add(t.r)
        for k, (s, v) in need.items():
            if self.seen[eng].get(k, 0) >= v:
                continue
            self.seen[eng][k] = v
            self.prog[eng].append(('w', s, v))

    def _post(self, tok, reads, writes):
        s, v = tok
        k = id(s)
        for t in reads:
            if t.r.get(k, (None, 0))[1] < v:
                t.r[k] = (s, v)
        for t in writes:
            t.w = {k: (s, v)}
            t.r = {}

    def op(self, eng, fn, reads=(), writes=()):
        self._dep(eng, reads, writes)
        self.ecnt[eng] += 1
        self.prog[eng].append(('o', fn, True))
        self._post((self.esem[eng], self.ecnt[eng]), reads, writes)

    def mm(self, fns, reads=(), writes=()):
        self._dep('tensor', reads, writes)
        for f in fns[:-1]:
            self.prog['tensor'].append(('o', f, False))
        self.ecnt['tensor'] += 1
        self.prog['tensor'].append(('o', fns[-1], True))
        self._post((self.esem['tensor'], self.ecnt['tensor']), reads, writes)

    def dma(self, eng, out, in_, sem, reads=(), writes=()):
        self._dep(eng, reads, writes)
        c = self.dcnt[id(sem)]
        c[1] += 16
        self.prog[eng].append(('d', out, in_, sem))
        self._post((sem, c[1]), reads, writes)

    def barrier(self):
        for e in ENGS:
            for e2 in ENGS:
                if e2 == e or self.ecnt[e2] == 0:
                    continue
                k = id(self.esem[e2])
                if self.seen[e].get(k, 0) < self.ecnt[e2]:
                    self.seen[e][k] = self.ecnt[e2]
                    self.prog[e].append(('w', self.esem[e2], self.ecnt[e2]))
            for k, (s, v) in self.dcnt.items():
                if v > 0 and self.seen[e].get(k, 0) < v:
                    self.seen[e][k] = v
                    self.prog[e].append(('w', s, v))

    def emit(self):
        nc = self.nc
        with nc.Block() as block:
            for e in ENGS:
                prog = self.prog[e]
                sem = self.esem[e]

                def body(E, prog=prog, sem=sem):
                    for it in prog:
                        if it[0] == 'w':
                            E.wait_ge(it[1], it[2])
                        elif it[0] == 'o':
                            ins = it[1](E)
                            if it[2]:
                                ins.then_inc(sem, 1)
                        else:
                            E.dma_start(out=it[1], in_=it[2]).then_inc(it[3], 16)
                getattr(block, e)(body)


def tt(out, in0, in1, op):
    return lambda E: E.tensor_tensor(out=out, in0=in0, in1=in1, op=op)


def ts(out, in0, s1, op0, s2=None, op1=None):
    if op1 is None:
        return lambda E: E.tensor_scalar(out=out, in0=in0, scalar1=s1, scalar2=None, op0=op0)
    return lambda E: E.tensor_scalar(out=out, in0=in0, scalar1=s1, scalar2=s2, op0=op0, op1=op1)


def stt(out, in0, scalar, in1, op0, op1):
    return lambda E: E.scalar_tensor_tensor(out=out, in0=in0, scalar=scalar, in1=in1, op0=op0, op1=op1)


def ttr(out, in0, in1, accum):
    return lambda E: E.activation(out=out, in_=in0, func=AF.Square, accum_out=accum)


def act(out, in_, func, scale=None):
    if scale is None:
        return lambda E: E.activation(out=out, in_=in_, func=func)
    return lambda E: E.activation(out=out, in_=in_, func=func, scale=scale)


def sqa(out, in_, accum):
    return lambda E: E.activation(out=out, in_=in_, func=AF.Square, accum_out=accum)


def rcp(out, in_):
    return lambda E: E.reciprocal(out=out, in_=in_)


def cp(out, in_):
    return lambda E: E.tensor_copy(out=out, in_=in_)


def mmf(out, lhsT, rhs, start, stop):
    assert len(rhs.ap) == 2 and len(lhsT.ap) == 2, ("rhs", rhs.ap, lhsT.ap)
    return lambda E: E.matmul(out, lhsT, rhs, start=start, stop=stop)


def trf(out, in_, ident):
    assert len(ident.ap) == 2 and len(in_.ap) == 2, ("ident", ident.ap, in_.ap)
    return lambda E: E.transpose(out, in_, ident)


def skew(T, stages):
    ns = len(stages)
    for step in range(T + ns - 1):
        for si in range(ns):
            t = step - si
            if 0 <= t < T:
                stages[si](t)


def bc(ap, axis, shape):
    return ap.unsqueeze(axis).broadcast_to(list(shape))


def build(dbg=False):
    nc = bass.Bass("TRN2", target_bir_lowering=False)
    es = ExitStack()

    def din(name, shape):
        return nc.dram_tensor(name, list(shape), F32, kind="ExternalInput").ap()

    x = din("x", [2, S, D])
    w_in = din("w_in", [D, 2048])
    w_glu = din("w_glu", [512, 512])
    w_out = din("w_out", [1024, 1024])
    w_gate = din("w_gate", [D, DFF])
    w_up = din("w_up", [D, DFF])
    w_down = din("w_down", [DFF, D])
    areT = din("areT", [128, 16])
    aimT = din("aimT", [128, 16])
    ldtT = din("ldtT", [128, 16])
    breT = din("breT", [128, 16, 16])
    bimT = din("bimT", [128, 16, 16])
    creT = din("creT", [128, 16, 16])
    cimT = din("cimT", [128, 16, 16])
    drep = din("drep", [128, 32, 16])
    g1c = din("g1c", [128, 8])
    g3c = din("g3c", [128, 8])
    g2r = din("g2r", [128, 1024])
    g4r = din("g4r", [128, 1024])
    gsr = din("gsr", [128, 512])
    gar = din("gar", [128, 512])
    bgr = din("bgr", [128, 512])
    c_idf = din("c_idf", [128, 128])
    c_cb = din("c_cb", [128, 2, 256])
    c_tm = din("c_tm", [128, 2, 256])
    c_dm = din("c_dm", [128, 2, 256])
    c_nv = din("c_nv", [128, 32])
    out = nc.dram_tensor("out", [2, S, D], F32, kind="ExternalOutput").ap()
    GT_d = nc.dram_tensor("GT_d", [32, 128, 2, 2, 128], BF16, kind="Internal").ap()
    TOEP_d = nc.dram_tensor("TOEP_d", [32, 128, 2, 256], BF16, kind="Internal").ap()
    CT_d = nc.dram_tensor("CT_d", [16, 128, 2, 256], BF16, kind="Internal").ap()
    dbg_t = {}
    if dbg:
        dbg_t['uc'] = nc.dram_tensor("d_uc", [128, 32, 16, 16], BF16, kind="ExternalOutput").ap()
        dbg_t['qT'] = nc.dram_tensor("d_qT", [128, 4, S], BF16, kind="ExternalOutput").ap()
        dbg_t['vt'] = nc.dram_tensor("d_vt", [128, 16, 8, 65], BF16, kind="ExternalOutput").ap()
        dbg_t['y1'] = nc.dram_tensor("d_y1", [128, 16, 512], BF16, kind="ExternalOutput").ap()
        dbg_t['ssmT'] = nc.dram_tensor("d_ssmT", [128, 4, 16, 128], BF16, kind="ExternalOutput").ap()
        dbg_t['attT'] = nc.dram_tensor("d_attT", [128, 4, S], BF16, kind="ExternalOutput").ap()
        dbg_t['x1'] = nc.dram_tensor("d_x1", [128, 16, 1024], F32, kind="ExternalOutput").ap()
        dbg_t['S'] = nc.dram_tensor("d_S", [128, 2, 16, 128], F32, kind="ExternalOutput").ap()

    ARENA = 206 * 1024
    arena = es.enter_context(nc.sbuf_tensor("arena", [128, ARENA], U8))
    pbanks = [es.enter_context(nc.psum_tensor("pb%d" % i, [128, 512], F32)) for i in range(8)]
    pk = [Tk() for _ in range(8)]
    fw = FW(nc, es)

    def reg(off, shape, dt, p0=0):
        esz = 2 if dt == BF16 else 4
        n = int(np.prod(shape[1:])) * esz
        assert off % 4 == 0 and off + n <= ARENA, (off, n)
        ap = arena[p0:p0 + shape[0], off:off + n].bitcast(dt)
        if len(shape) == 3:
            ap = ap.rearrange("p (a b) -> p a b", a=shape[1])
        elif len(shape) == 4:
            ap = ap.rearrange("p (a b c) -> p a b c", a=shape[1], b=shape[2])
        elif len(shape) == 5:
            ap = ap.rearrange("p (a b c d) -> p a b c d", a=shape[1], b=shape[2], c=shape[3])
        return ap

    class Bump:
        def __init__(self, lo, hi):
            self.o, self.hi = lo, hi

        def __call__(self, shape, dt, p0=0):
            esz = 2 if dt == BF16 else 4
            n = int(np.prod(shape[1:])) * esz
            n4 = (n + 31) // 32 * 32
            a = reg(self.o, shape, dt, p0)
            self.o += n4
            assert self.o <= self.hi, (self.o, self.hi)
            return a

    K = 1024
    V, A, G_, T_, SY = 'vector', 'scalar', 'gpsimd', 'tensor', 'sync'

    cb = Bump(0, 20 * K)
    idf = cb([128, 128], F32)
    idb = cb([128, 128], BF16)
    cbias = cb([128, 2, 256], BF16)
    g1 = cb([128, 8], F32)
    g3 = cb([128, 8], F32)
    g2 = cb([128, 1024], F32)
    g4 = cb([128, 1024], F32)
    gs = cb([128, 512], F32)
    ga = cb([128, 512], F32)
    bg = cb([128, 512], F32)
    stat = cb([128, 64], F32)
    LR = cb([128, 7, 16], F32)
    LI = cb([128, 7, 16], F32)
    LIn = cb([128, 7, 16], F32)
    LAk = cb([128, 7, 2, 16], F32)
    assert cb.o <= 20 * K
    kC = Tk()
    sem_c = fw.newsem("sem_c")
    sem_c2 = fw.newsem("sem_c2")
    for dst, src in [(idf, c_idf), (g1, g1c), (g3, g3c), (g2, g2r), (g4, g4r), (gs, gsr), (ga, gar), (bg, bgr)]:
        fw.dma(SY, dst, src, sem_c, writes=[kC])

    sb = Bump(20 * K, ARENA)
    cb_off = sb.o
    CBre = sb([128, 16, 16, 16], F32)
    cbf = sb([128, 2, 256], F32)
    tmk = sb([128, 2, 256], F32)
    dmk = sb([128, 2, 256], F32)
    nv = sb([128, 32], F32)
    drp = sb([128, 32, 16], F32)
    are = sb([128, 16], F32)
    aim = sb([128, 16], F32)
    ldt = sb([128, 16], F32)
    bre = sb([128, 16, 16], F32)
    bim = sb([128, 16, 16], F32)
    cre = sb([128, 16, 16], F32)
    cim = sb([128, 16, 16], F32)
    kS = Tk()
    for dst, src in [(cbf, c_cb), (tmk, c_tm), (dmk, c_dm), (nv, c_nv), (drp, drep), (are, areT), (aim, aimT),
                     (ldt, ldtT), (bre, breT), (bim, bimT), (cre, creT), (cim, cimT)]:
        fw.dma(SY, dst, src, sem_c2, writes=[kS])
    fw.op(V, cp(idb, idf), [kC], [kC])
    fw.op(V, cp(cbias, cbf), [kS, kC], [kC])

    dtt = sb([128, 16], F32)
    adr = sb([128, 16], F32)
    adi = sb([128, 16], F32)
    ex = sb([128, 16, 32], F32)
    ang = sb([128, 16, 32], F32)
    mag = sb([128, 16, 32], F32)
    magi = sb([128, 16, 32], F32)
    r1 = sb([128, 16, 32], F32)
    r2 = sb([128, 16, 32], F32)
    sn = sb([128, 16, 32], F32)
    cs = sb([128, 16, 32], F32)
    Pre = sb([128, 16, 32], F32)
    Pim = sb([128, 16, 32], F32)
    Qre = sb([128, 16, 32], F32)
    Qim = sb([128, 16, 32], F32)
    sm = [sb([128, 16], F32) for _ in range(10)]
    bbre = sb([128, 16, 16], F32)
    bbim = sb([128, 16, 16], F32)
    tb1 = sb([128, 16, 16], F32)
    tb2 = sb([128, 16, 16], F32)
    k0 = Tk()
    fw.op(A, act(dtt, ldt, AF.Exp), [kS], [k0])
    fw.op(V, tt(adr, are, dtt, ALU.mult), [k0, kS], [k0])
    fw.op(V, tt(adi, aim, dtt, ALU.mult), [k0, kS], [k0])
    fw.op(V, tt(ex, bc(adr, 2, [128, 16, 32]), bc(nv, 1, [128, 16, 32]), ALU.mult), [k0, kS], [k0])
    fw.op(V, tt(ang, bc(adi, 2, [128, 16, 32]), bc(nv, 1, [128, 16, 32]), ALU.mult), [k0, kS], [k0])
    fw.op(A, act(mag, ex, AF.Exp), [k0], [k0])
    fw.op(A, act(magi, ex, AF.Exp, scale=-1.0), [k0], [k0])
    ki = reg(cb_off, [128, 16, 32], I32)
    kf = reg(cb_off + 2048, [128, 16, 32], F32)
    mk = reg(cb_off + 4096, [128, 16, 32], F32)

    def range_reduce(r, shift):
        fw.op(V, ts(r, ang, 1.0 / (2 * math.pi), ALU.mult, shift, ALU.add), [k0], [k0])
        fw.op(V, cp(ki, r), [k0], [k0])
        fw.op(V, cp(kf, ki), [k0], [k0])
        fw.op(V, tt(r, r, kf, ALU.subtract), [k0], [k0])
        fw.op(V, ts(mk, r, -0.5, ALU.is_lt), [k0], [k0])
        fw.op(V, tt(r, r, mk, ALU.add), [k0], [k0])
        fw.op(V, ts(mk, r, 0.5, ALU.is_gt), [k0], [k0])
        fw.op(V, tt(r, r, mk, ALU.subtract), [k0], [k0])
    range_reduce(r1, 0.0)
    range_reduce(r2, 0.25)
    fw.op(A, act(sn, r1, AF.Sin, scale=2 * math.pi), [k0], [k0])
    fw.op(A, act(cs, r2, AF.Sin, scale=2 * math.pi), [k0], [k0])
    fw.op(V, tt(Pre, mag, cs, ALU.mult), [k0], [k0])
    fw.op(V, tt(Pim, mag, sn, ALU.mult), [k0], [k0])
    fw.op(V, tt(Qre, magi, cs, ALU.mult), [k0], [k0])
    fw.op(V, stt(Qim, magi, -1.0, sn, ALU.mult, ALU.mult), [k0], [k0])
    lbre, lbim = Pre[:, :, 0], Pim[:, :, 0]
    nr, den, t0, t1_, cr, ci, rden = sm[0], sm[1], sm[2], sm[3], sm[4], sm[5], sm[6]
    fw.op(V, ts(nr, lbre, -1.0, ALU.add), [k0], [k0])
    fw.op(V, tt(den, are, are, ALU.mult), [k0, kS], [k0])
    fw.op(V, tt(t0, aim, aim, ALU.mult), [k0, kS], [k0])
    fw.op(V, tt(den, den, t0, ALU.add), [k0], [k0])
    fw.op(V, lambda E: E.reciprocal(out=rden, in_=den), [k0], [k0])
    fw.op(V, tt(t0, nr, are, ALU.mult), [k0], [k0])
    fw.op(V, tt(t1_, lbim, aim, ALU.mult), [k0], [k0])
    fw.op(V, tt(t0, t0, t1_, ALU.add), [k0], [k0])
    fw.op(V, tt(cr, t0, rden, ALU.mult), [k0], [k0])
    fw.op(V, tt(t0, lbim, are, ALU.mult), [k0], [k0])
    fw.op(V, tt(t1_, nr, aim, ALU.mult), [k0], [k0])
    fw.op(V, tt(t0, t0, t1_, ALU.subtract), [k0], [k0])
    fw.op(V, tt(ci, t0, rden, ALU.mult), [k0], [k0])
    crb, cib = bc(cr, 2, [128, 16, 16]), bc(ci, 2, [128, 16, 16])
    fw.op(V, tt(tb1, crb, bre, ALU.mult), [k0, kS], [k0])
    fw.op(V, tt(tb2, cib, bim, ALU.mult), [k0, kS], [k0])
    fw.op(V, tt(bbre, tb1, tb2, ALU.subtract), [k0], [k0])
    fw.op(V, tt(tb1, crb, bim, ALU.mult), [k0, kS], [k0])
    fw.op(V, tt(tb2, cib, bre, ALU.mult), [k0, kS], [k0])
    fw.op(V, tt(bbim, tb1, tb2, ALU.add), [k0], [k0])
    fw.op(V, cp(LR[:, 0, :], Pre[:, :, 15]), [k0], [kC])
    fw.op(V, cp(LI[:, 0, :], Pim[:, :, 15]), [k0], [kC])
    for k in range(6):
        fw.op(V, tt(sm[7], LR[:, k, :], LR[:, k, :], ALU.mult), [kC, k0], [k0])
        fw.op(V, tt(sm[8], LI[:, k, :], LI[:, k, :], ALU.mult), [kC, k0], [k0])
        fw.op(V, tt(LR[:, k + 1, :], sm[7], sm[8], ALU.subtract), [k0], [kC])
        fw.op(V, tt(sm[9], LR[:, k, :], LI[:, k, :], ALU.mult), [kC, k0], [k0])
        fw.op(V, ts(LI[:, k + 1, :], sm[9], 2.0, ALU.mult), [k0], [kC])
    fw.op(V, ts(LIn, LI, -1.0, ALU.mult), [kC], [kC])
    fw.op(V, cp(LAk[:, :, 0, :], LR), [kC], [kC])
    fw.op(V, cp(LAk[:, :, 1, :], LR), [kC], [kC])
    SH4 = [128, 16, 16, 16]
    Gre = sb(SH4, F32)
    Gim = sb(SH4, F32)
    CAre = sb(SH4, F32)
    CAim = sb(SH4, F32)
    CBim = sb(SH4, F32)
    u1_off = sb.o
    U1 = sb(SH4, F32)
    U2 = sb(SH4, F32)
    U3, U4 = U1, U2
    CTst = reg(u1_off, [128, 16, 2, 256], BF16)
    kG, kCA, kCB, kU1, kU2, kCT = [Tk() for _ in range(6)]
    kU3, kU4 = kU1, kU2

    def outer(eng, o, a, pn, rd, wr):
        fw.op(eng, tt(o, bc(a, 2, SH4), bc(pn, 3, SH4), ALU.mult), rd, wr)

    def fl(ap):
        return ap.rearrange("p a b c -> p (a b c)")
    outer(V, U1, bbre, Qre[:, :, 0:16], [k0], [kU1])
    outer(G_, U2, bbim, Qim[:, :, 0:16], [k0], [kU2])
    fw.op(V, tt(Gre, U1, U2, ALU.subtract), [kU1, kU2], [kG])
    outer(V, U3, bbre, Qim[:, :, 0:16], [k0], [kU3])
    outer(G_, U4, bbim, Qre[:, :, 0:16], [k0], [kU4])
    fw.op(V, tt(Gim, U3, U4, ALU.add), [kU3, kU4], [kG])
    outer(V, U1, cre, Pre[:, :, 0:16], [k0, kS], [kU1])
    outer(G_, U2, cim, Pim[:, :, 0:16], [k0, kS], [kU2])
    fw.op(V, tt(CAre, U1, U2, ALU.subtract), [kU1, kU2], [kCA])
    outer(V, U3, cre, Pim[:, :, 0:16], [k0, kS], [kU3])
    outer(G_, U4, cim, Pre[:, :, 0:16], [k0, kS], [kU4])
    fw.op(V, stt(fl(CAim), fl(U3), -1.0, fl(U4), ALU.mult, ALU.subtract), [kU3, kU4], [kCA])
    outer(V, U1, cre, Pre[:, :, 16:32], [k0, kS], [kU1])
    outer(G_, U2, cim, Pim[:, :, 16:32], [k0, kS], [kU2])
    fw.op(V, tt(CBre, U1, U2, ALU.subtract), [kU1, kU2], [kCB])
    outer(V, U3, cre, Pim[:, :, 16:32], [k0, kS], [kU3])
    outer(G_, U4, cim, Pre[:, :, 16:32], [k0, kS], [kU4])
    fw.op(V, stt(fl(CBim), fl(U3), -1.0, fl(U4), ALU.mult, ALU.subtract), [kU3, kU4], [kCB])
    fw.op(V, cp(CTst[:, :, 0, :], CBre.rearrange("p g t h -> p g (t h)")), [kCB], [kCT, kU1])
    fw.op(V, cp(CTst[:, :, 1, :], CBim.rearrange("p g t h -> p g (t h)")), [kCB], [kCT, kU1])
    sem_t = fw.newsem("sem_t")
    kDR = Tk()
    fw.dma(SY, CT_d.rearrange("g p r c -> p g r c"), CTst, sem_t, [kCT], [kDR])
    TPst = [sb([128, 2, 256], BF16) for _ in range(2)]
    GTst = [sb([128, 2, 2, 128], BF16) for _ in range(2)]
    TPt = [sb([128, 2, 256], F32) for _ in range(2)]
    TPd = [sb([128, 2, 256], F32) for _ in range(2)]
    kTP = [Tk() for _ in range(2)]
    kGTs = [Tk() for _ in range(2)]
    kTt = [Tk() for _ in range(2)]
    sem_tp = [fw.newsem("sem_tp%d" % i) for i in range(2)]
    sem_gt = [fw.newsem("sem_gt%d" % i) for i in range(2)]
    for gh in range(2):
        pr = slice(64 * gh, 64 * gh + 64)
        for gl in range(16):
            g = 16 * gh + gl
            i = g % 2
            pT, pG = 2 * i, 2 * i + 1
            pt_v = pbanks[pT][:, :].rearrange("p (j c) -> p j c", j=2)
            fns = []
            for j in range(2):
                l_re = Gre[pr, gl, 8 * j:8 * j + 8, :].rearrange("p s h -> p (s h)")
                l_im = Gim[pr, gl, 8 * j:8 * j + 8, :].rearrange("p s h -> p (s h)")
                fns.append(mmf(pt_v[:, j, :], l_re, CAre[pr, gl].rearrange("p t h -> p (t h)"), True, False))
                fns.append(mmf(pt_v[:, j, :], l_im, CAim[pr, gl].rearrange("p t h -> p (t h)"), False, True))
            fw.mm(fns, [kG, kCA], [pk[pT]])
            pg_v = pbanks[pG][:, :].rearrange("p (j r c) -> p j r c", j=2, r=2)
            fns = []
            for j in range(2):
                l_re = Gre[pr, gl, 8 * j:8 * j + 8, :].rearrange("p s h -> p (s h)")
                l_im = Gim[pr, gl, 8 * j:8 * j + 8, :].rearrange("p s h -> p (s h)")
                fns.append(mmf(pg_v[:, j, 0, :], l_re, idf[pr, :], True, True))
                fns.append(mmf(pg_v[:, j, 1, :], l_im, idf[pr, :], True, True))
            fw.mm(fns, [kG, kC], [pk[pG]])
            fw.op(V, tt(TPt[i], pt_v, tmk, ALU.mult), [pk[pT], kS], [kTt[i]])
            fw.op(G_, tt(TPd[i].rearrange("p j (t h) -> p j t h", t=16),
                         dmk.rearrange("p j (t h) -> p j t h", t=16),
                         drp[:, g, :].unsqueeze(1).unsqueeze(1).broadcast_to([128, 2, 16, 16]), ALU.mult),
                  [kS], [kTP[i]])
            fw.op(V, tt(TPst[i], TPt[i], TPd[i], ALU.add), [kTt[i], kTP[i]], [kTP[i]])
            fw.dma(SY, TOEP_d[g], TPst[i], sem_tp[i], [kTP[i]], [kDR])
            fw.op(A, act(GTst[i], pg_v, AF.Copy), [pk[pG]], [kGTs[i]])
            fw.dma(SY, GT_d[g], GTst[i], sem_gt[i], [kGTs[i]], [kDR])
    fw.barrier()

    X1 = reg(20 * K, [128, 16, 1024], F32)
    attT = reg(84 * K, [128, 4, S], BF16)
    ssmT = reg(100 * K, [128, 4, 16, 128], BF16)
    Uc = reg(116 * K, [128, 32, 16, 16], BF16)
    qT = reg(132 * K, [128, 4, S], BF16)
    kT = reg(148 * K, [128, 4, S], BF16)
    Vt = reg(164 * K, [128, 16, 8, 65], BF16)
    sem_x = [fw.newsem("sem_x%d" % i) for i in range(4)]
    sem_w = [fw.newsem("sem_w%d" % i) for i in range(2)]
    sem_o = fw.newsem("sem_o")
    sem_og = fw.newsem("sem_og")
    sem_wq = [fw.newsem("sem_wq%d" % i) for i in range(2)]
    sem_d = fw.newsem("sem_d")
    sem_g = [fw.newsem("sem_g%d" % i) for i in range(2)]
    sem_dn = fw.newsem("sem_dn")
    sem_st = [fw.newsem("sem_st%d" % i) for i in range(4)]
    kOut = Tk()

    try:
      if STAGE < 1:
        raise _Stop()
      for b in range(2):
        a1 = Bump(20 * K, 116 * K)
        Xst = [a1([128, 2, 1024], F32) for _ in range(2)]
        hbf = [a1([128, 2, 1024], BF16) for _ in range(2)]
        hT = a1([128, 8, S], BF16)
        wbuf = [a1([128, 8, 256], BF16) for _ in range(2)]
        junk = a1([128, 1024], F32)
        kX = [Tk(), Tk()]
        kH = [Tk(), Tk()]
        khT = [Tk() for _ in range(8)]
        kW = [Tk(), Tk()]
        kJ = Tk()
        kSt = Tk()
        kUc, kq, kk, kv = Tk(), Tk(), Tk(), Tk()
        xv = x[b].rearrange("(c t) d -> c t d", t=16)
        ss = stat[:, 0:16]
        rs = stat[:, 16:32]
        for tp in range(8):
            i = tp % 2
            fw.dma(SY, Xst[i], xv[:, 2 * tp:2 * tp + 2, :], sem_x[i], writes=[kX[i]])
            for u in range(2):
                t = 2 * tp + u
                fw.op(A, ttr(junk, Xst[i][:, u, :], Xst[i][:, u, :], ss[:, t:t + 1]), [kX[i]], [kJ, kSt])
                fw.op(V, ts(rs[:, t:t + 1], ss[:, t:t + 1], 1.0 / D, ALU.mult, EPS, ALU.add), [kSt], [kSt])
                fw.op(A, act(rs[:, t:t + 1], rs[:, t:t + 1], AF.Ln), [kSt], [kSt])
                fw.op(A, act(rs[:, t:t + 1], rs[:, t:t + 1], AF.Exp, scale=-0.5), [kSt], [kSt])
                fw.op(A, act(hbf[i][:, u, :], Xst[i][:, u, :], AF.Copy, scale=rs[:, t:t + 1]), [kX[i], kSt], [kH[i]])
            for k in range(8):
                pi = k % 4
                pv = pbanks[pi][:, 0:128].bitcast(BF16).rearrange("p (u c) -> p u c", u=2)
                fns = [trf(pv[:, u, :], hbf[i][:, u, k * 128:(k + 1) * 128], idb) for u in range(2)]
                fw.mm(fns, [kH[i], kC], [pk[pi]])
                dst = hT[:, k, :].rearrange("p (c t) -> p t c", t=16)[:, 2 * tp:2 * tp + 2, :]
                if k % 2 == 0:
                    fw.op(V, ts(dst, pv, g1[:, k:k + 1], ALU.mult), [pk[pi], kC], [khT[k]])
                else:
                    fw.op(A, act(dst, pv, AF.Copy, scale=g1[:, k:k + 1]), [pk[pi], kC], [khT[k]])
        win_v = w_in.rearrange("(k p) n -> p k n", p=128)
        pcount = 0
        for piece in range(8):
            i = piece % 2
            fw.dma(G_, wbuf[i], win_v[:, :, piece * 256:(piece + 1) * 256], sem_wq[i], writes=[kW[i]])
            if piece < 2:
                for t in range(16):
                    pi = 4 + (pcount % 4)
                    pcount += 1
                    pv = pbanks[pi][:, 0:256]
                    fns = [mmf(pv, hT[:, k, t::16], wbuf[i][:, k, :], k == 0, k == 7) for k in range(8)]
                    fw.mm(fns, khT + [kW[i]], [pk[pi]])
                    dst = Uc[:, 16 * piece:16 * piece + 16, t, :]
                    srcv = pv.rearrange("p (g h) -> p g h", g=16)
                    if t % 2 == 0:
                        fw.op(V, cp(dst, srcv), [pk[pi]], [kUc])
                    else:
                        fw.op(A, act(dst, srcv, AF.Copy), [pk[pi]], [kUc])
            elif piece < 6:
                dstT, kd, sc = (qT, kq, 0.125) if piece < 4 else (kT, kk, 1.0)
                for mm_ in range(2):
                    m = (piece % 2) * 2 + mm_
                    for n in range(4):
                        pi = 4 + (pcount % 4)
                        pcount += 1
                        pv = pbanks[pi][:, :]
                        fns = [mmf(pv, wbuf[i][:, k, mm_ * 128:(mm_ + 1) * 128], hT[:, k, n * 512:(n + 1) * 512],
                                   k == 0, k == 7) for k in range(8)]
                        fw.mm(fns, khT + [kW[i]], [pk[pi]])
                        dst = dstT[:, m, n * 512:(n + 1) * 512]
                        if n % 2 == 0:
                            fw.op(V, ts(dst, pv, sc, ALU.mult), [pk[pi]], [kd])
                        else:
                            fw.op(A, act(dst, pv, AF.Copy, scale=sc), [pk[pi]], [kd])
            else:
                hh = piece - 6
                for it in range(16):
                    pi = 4 + (pcount % 4)
                    pcount += 1
                    pv = pbanks[pi][:, 0:256]
                    fns = [mmf(pv, hT[:, k, it * 128:(it + 1) * 128], wbuf[i][:, k, :], k == 0, k == 7)
                           for k in range(8)]
                    fw.mm(fns, khT + [kW[i]], [pk[pi]])
                    dst = Vt[:, it, 4 * hh:4 * hh + 4, 0:64]
                    src = pv.rearrange("p (h d) -> p h d", h=4)
                    if it % 2 == 0:
                        fw.op(V, cp(dst, src), [pk[pi]], [kv])
                    else:
                        fw.op(A, act(dst, src, AF.Copy), [pk[pi]], [kv])
        fw.op(G_, lambda E: E.memset(Vt[:, :, :, 64:65], 1.0), [kv], [kv])
        if dbg and b == 0:
            fw.dma(SY, dbg_t['uc'], Uc, sem_d, [kUc], [kOut])
            fw.dma(SY, dbg_t['qT'], qT, sem_d, [kq], [kOut])
            fw.dma(SY, dbg_t['vt'], Vt, sem_d, [kv], [kOut])
        fw.barrier()

        if STAGE < 2:
            raise _Stop()
        a2 = Bump(181 * K, ARENA)
        Sbf = a2([128, 2, 16, 130], BF16)
        a2b = Bump(20 * K, 84 * K)
        UT = a2b([128, 32, 2, 128], BF16)
        Wf = a2b([128, 2, 16, 128], F32)
        GTb = [a2b([128, 2, 2, 2, 128], BF16) for _ in range(2)]
        T1 = a2b([128, 2, 16], F32)
        T2 = a2b([128, 2, 16], F32)
        kUT, kWf, kSb, kT1, kT2 = Tk(), Tk(), Tk(), Tk(), Tk()
        kGTb = [Tk(), Tk()]
        for g in range(32):
            pi = g % 4
            pv = pbanks[pi][:, 0:128].bitcast(BF16).rearrange("p (j c) -> p j c", j=2)
            fns = [trf(pv[:, j, :], Uc[:, g, 8 * j:8 * j + 8, :].rearrange("p s h -> p (s h)"), idb) for j in range(2)]
            fw.mm(fns, [kUc, kC], [pk[pi]])
            if g % 2 == 0:
                fw.op(V, cp(UT[:, g], pv), [pk[pi]], [kUT])
            else:
                fw.op(A, act(UT[:, g], pv, AF.Copy), [pk[pi]], [kUT])
        if STAGE < 2.2:
            raise _Stop()
        for gl in range(16):
            i = gl % 2
            for gh in range(2):
                fw.dma(SY, GTb[i][:, gh], GT_d[16 * gh + gl], sem_w[i], writes=[kGTb[i]])
            pi = 4 + gl % 4
            pv = pbanks[pi][:, 0:256].rearrange("p (r c) -> p r c", r=2)
            fns = []
            for r in range(2):
                n = 0
                for gh in range(2):
                    for j in range(2):
                        fns.append(mmf(pv[:, r, :], GTb[i][:, gh, j, r, :], UT[:, 16 * gh + gl, j, :], n == 0, n == 3))
                        n += 1
            fw.mm(fns, [kUT, kGTb[i]], [pk[pi]])
            fw.op(V, cp(Wf[:, :, gl, :], pv), [pk[pi]], [kWf])
        if STAGE < 2.5:
            raise _Stop()
        WfB = reg(116 * K, [128, 2, 16, 128], F32)
        TT = reg(190 * K, [128, 2, 16, 128], F32)
        kWB, kTT = kUc, Tk()
        ks = {'cur': Wf, 'nxt': WfB, 'kcur': kWf, 'knxt': kWB}

        def ks_level(k):
            d = 1 << k
            n = 128 - d
            cur, nxt, kcur, knxt = ks['cur'], ks['nxt'], ks['kcur'], ks['knxt']
            fw.op(V, tt(TT[:, :, :, 0:n], cur[:, :, :, 0:n], bc(LAk[:, k], 3, [128, 2, 16, n]), ALU.mult),
                  [kcur, kC], [kTT])
            fw.op(V, tt(nxt[:, :, :, d:128], cur[:, :, :, d:128], TT[:, :, :, 0:n], ALU.add), [kcur, kTT], [knxt])
            fw.op(V, tt(TT[:, 0, :, 0:n], cur[:, 1, :, 0:n], bc(LIn[:, k, :], 2, [128, 16, n]), ALU.mult),
                  [kcur, kC], [kTT])
            fw.op(V, tt(TT[:, 1, :, 0:n], cur[:, 0, :, 0:n], bc(LI[:, k, :], 2, [128, 16, n]), ALU.mult),
                  [kcur, kC], [kTT])
            fw.op(V, tt(nxt[:, :, :, d:128], nxt[:, :, :, d:128], TT[:, :, :, 0:n], ALU.add), [knxt, kTT], [knxt])
            fw.op(V, cp(nxt[:, :, :, 0:d], cur[:, :, :, 0:d]), [kcur], [knxt])
            ks['cur'], ks['nxt'], ks['kcur'], ks['knxt'] = nxt, cur, knxt, kcur

        def ks_finish():
            Wfin, kWfin = ks['cur'], ks['kcur']
            fw.op(V, lambda E: E.memset(Sbf[:, :, :, 0:2], 0.0), [], [kSb])
            for r in range(2):
                fw.op(A, act(Sbf[:, r, :, 2:130], Wfin[:, r], AF.Copy), [kWfin], [kSb])
            if dbg and b == 0:
                fw.dma(SY, dbg_t['S'], Wfin, sem_d, [kWfin], [kOut])

        if STAGE < 3:
            raise _Stop()
        a3 = Bump(52 * K, 84 * K)
        MBT = a3([128, S], BF16)
        kmT = a3([128, 4, 8], BF16)
        kmf = a3([128, 4, 8], F32)
        gt = a3([128, 8, 8], F32)
        top8 = a3([128, 8, 8], F32)
        mbt = a3([128, 2, 64], BF16)
        PT = [a3([128, 256], BF16) for _ in range(3)]
        Ao = a3([128, 2, 512], F32)
        Aj = a3([128, 512], F32)
        Ab = a3([128, 2, 512], BF16)
        rden_a = a3([128, 4], F32)
        kMB, kkm, kgt, kmb = Tk(), Tk(), Tk(), Tk()
        Esel = reg(100 * K, [128, 64, 128], BF16)
        kEs = Tk()
        fw.op(V, cp(Esel[0:64], bc(idb[0:64, 0:64], 2, [64, 64, 128])), [kC], [kEs])
        fw.op(V, cp(Esel[64:128], bc(idb[64:128, 64:128], 2, [64, 64, 128])), [kC], [kEs])
        kPT = [Tk() for _ in range(3)]
        kAo, kAj, kAb, kat, krd = Tk(), Tk(), Tk(), Tk(), Tk()
        if STAGE < 3.05:
            raise _Stop()
        kjunk = a3([128, 256], BF16)
        kkj = Tk()
        for m in range(4):
            for n in range(8):
                fw.op(A, (lambda m=m, n=n: (lambda E: E.activation(out=kjunk, in_=kT[:, m, n * 256:(n + 1) * 256],
                                                                   func=AF.Copy, accum_out=kmf[:, m, n:n + 1])))(),
                      [kk], [kkj, kkm])
        if STAGE < 3.08:
            raise _Stop()
        fw.op(A, act(kmT, kmf, AF.Copy, scale=1.0 / 256), [kkm], [kkm])
        if STAGE < 3.1:
            raise _Stop()
        fw.op(G_, lambda E: E.memset(MBT, 0.0), [], [kMB, kGTb[0], kGTb[1]])
        for qt in range(8, 16):
            bq = qt // 2
            pve = pbanks[0][:, 0:32].rearrange("p (h n) -> p h n", h=4)
            pvo = pbanks[1][:, 0:32].rearrange("p (h n) -> p h n", h=4)
            fe = [mmf(pve[:, h2, :], qT[0:64, h2, qt * 128:(qt + 1) * 128], kmT[0:64, h2, :], True, True)
                  for h2 in range(4)]
            fw.mm(fe, [kq, kkm], [pk[0]])
            fo = [mmf(pvo[:, h2, :], qT[64:128, h2, qt * 128:(qt + 1) * 128], kmT[64:128, h2, :], True, True)
                  for h2 in range(4)]
            fw.mm(fo, [kq, kkm], [pk[1]])
            fw.op(G_, lambda E: E.memset(gt, NEG), [kgt], [kgt])
            gtv = gt.rearrange("p (h2 e) n -> p h2 e n", e=2)
            fw.op(V, cp(gtv[:, :, 0, 0:bq], pve[:, :, 0:bq]), [pk[0], kgt], [kgt])
            fw.op(V, cp(gtv[:, :, 1, 0:bq], pvo[:, :, 0:bq]), [pk[1], kgt], [kgt])
            for h in range(8):
                fw.op(V, (lambda hh: (lambda E: E.max(out=top8[:, hh, :], in_=gt[:, hh, :])))(h), [kgt], [kmb])
            for dup in range(2):
                fw.op(V, tt(mbt[:, dup, :].rearrange("p (h n) -> p h n", h=8), gt,
                            top8[:, :, 2:3].broadcast_to([128, 8, 8]), ALU.is_lt), [kgt, kmb], [kmb])
            pi2 = 2 + qt % 2
            pv2 = pbanks[pi2][:, 0:64].bitcast(BF16)
            fw.mm([trf(pv2, mbt.rearrange("p d c -> p (d c)"), idb)], [kmb, kC], [pk[pi2]])
            fw.op(V, ts(MBT[:, qt * 128:(qt + 1) * 128], pv2, NEG, ALU.mult), [pk[pi2]], [kMB])
        if STAGE < 3.2:
            raise _Stop()
        ucount = 0
        LAG = 2
        for bq in range(ABQ):
            nkt = 2 * bq + 2

            def stage_a(h, kt, pi, bq=bq):
                pr = slice(64 * (h % 2), 64 * (h % 2) + 64)
                m = h // 2
                n = kt // 2
                pv = pbanks[pi][:, 0:256]
                fns = [mmf(pv, kT[pr, m, kt * 128:(kt + 1) * 128], qT[pr, m, bq * 256:(bq + 1) * 256], True, False)]
                rd = [kq, kk]
                if n == bq:
                    fns.append(mmf(pv, idb, cbias[:, kt - 2 * bq, :], False, True))
                    rd.append(kC)
                elif bq >= 4:
                    r = h * 8 + n
                    fns.append(mmf(pv, Esel[pr, r, :], MBT[pr, bq * 256:(bq + 1) * 256], False, True))
                    rd += [kEs, kMB]
                else:
                    fns[0] = mmf(pv, kT[pr, m, kt * 128:(kt + 1) * 128], qT[pr, m, bq * 256:(bq + 1) * 256], True, True)
                fw.mm(fns, rd, [pk[pi]])
                fw.op(A, act(PT[pi], pv, AF.Exp), [pk[pi]], [kPT[pi]])

            def stage_b(h, kt, pi, nkt=nkt):
                pos = [4 + 2 * (h % 2), 5 + 2 * (h % 2)]
                povs = [pbanks[pos[0]][:, 0:65], pbanks[pos[1]][:, 0:65]]
                fns = [mmf(povs[u], PT[pi][:, u * 128:(u + 1) * 128], Vt[:, kt, h, :], kt == 0, kt == nkt - 1)
                       for u in range(2)]
                fw.mm(fns, [kPT[pi], kv], [pk[pos[0]], pk[pos[1]]])
                if kt == nkt - 1:
                    for u in range(2):
                        fw.op(V, rcp(rden_a[:, u:u + 1], povs[u][:, 64:65]), [pk[pos[u]]], [krd])
                        fw.op(V, ts(Ao[:, u, h * 64:(h + 1) * 64], povs[u][:, 0:64], rden_a[:, u:u + 1], ALU.mult),
                              [pk[pos[u]], krd], [kAo])

            pend = []
            for h in range(8):
                for kt in range(nkt):
                    pi = ucount % 3
                    ucount += 1
                    stage_a(h, kt, pi)
                    pend.append((h, kt, pi))
                    if len(pend) > LAG:
                        stage_b(*pend.pop(0))
            while pend:
                stage_b(*pend.pop(0))
            if bq < 7:
                ks_level(bq)
            else:
                ks_finish()
            for u in range(2):
                fw.op(A, ttr(Aj, Ao[:, u, :], Ao[:, u, :], rden_a[:, 2:3]), [kAo], [kAj, krd])
                fw.op(V, ts(rden_a[:, 3:4], rden_a[:, 2:3], 1.0 / 512, ALU.mult, EPS, ALU.add), [krd], [krd])
                fw.op(A, act(rden_a[:, 3:4], rden_a[:, 3:4], AF.Ln), [krd], [krd])
                fw.op(A, act(rden_a[:, 3:4], rden_a[:, 3:4], AF.Exp, scale=-0.5), [krd], [krd])
                fw.op(V, stt(Ab[:, u, :], Ao[:, u, :], rden_a[:, 3:4], ga, ALU.mult, ALU.mult), [kAo, krd, kC], [kAb])
            for u in range(2):
                pi = 3
                pv = pbanks[pi][:, 0:256].bitcast(BF16).rearrange("p (f c) -> p f c", f=4)
                fns = [trf(pv[:, f, :], Ab[:, u, f * 128:(f + 1) * 128], idb) for f in range(4)]
                fw.mm(fns, [kAb, kC], [pk[pi]])
                tok0 = bq * 256 + u * 128
                fw.op(A, act(attT[:, :, tok0:tok0 + 128], pv, AF.Copy), [pk[pi]], [kat])
        if dbg and b == 0:
            fw.dma(SY, dbg_t['attT'], attT, sem_d, [kat], [kOut])
        fw.barrier()

        if STAGE < 4:
            raise _Stop()
        a4 = Bump(132 * K, 181 * K)
        Y1 = a4([128, 16, 512], BF16)
        TCb = [(a4([128, 2, 2, 256], BF16), a4([128, 2, 256], BF16)) for _ in range(2)]
        wgl = a4([128, 4, 512], BF16)
        gb = Bump(36 * K, 84 * K)
        NB = 4
        Y1T = [gb([128, 4, 128], BF16) for _ in range(NB)]
        zt = [gb([128, 512], F32) for _ in range(NB)]
        zs = [gb([128, 512], F32) for _ in range(NB)]
        y2 = [gb([128, 512], F32) for _ in range(NB)]
        y2b = [gb([128, 512], BF16) for _ in range(NB)]
        kY1, kwg, kssm = Tk(), Tk(), Tk()
        kzt, kzs, ky2, ky2b, kss = [[Tk() for _ in range(NB)] for _ in range(5)]
        kTC = [Tk(), Tk()]
        kY1T = [Tk() for _ in range(NB)]
        fw.dma(G_, wgl, w_glu.rearrange("(k p) n -> p k n", p=128), sem_og, writes=[kwg])
        C1 = 0.7978845608028654 * 2.0
        for gl in range(16):
            i = gl % 2
            for gh in range(2):
                fw.dma(SY, TCb[i][0][:, gh], TOEP_d[16 * gh + gl], sem_w[i], writes=[kTC[i]])
            fw.dma(SY, TCb[i][1], CT_d[gl], sem_w[i], writes=[kTC[i]])
            for gh in range(2):
                g = 16 * gh + gl
                pr = slice(64 * gh, 64 * gh + 64)
                pi = (2 * gl + gh) % 4
                pv = pbanks[pi][:, 0:256]
                fns = [mmf(pv, UT[:, g, 0, :], TCb[i][0][:, gh, 0, :], True, False),
                       mmf(pv, UT[:, g, 1, :], TCb[i][0][:, gh, 1, :], False, False),
                       mmf(pv, Sbf[pr, 0, gl, 1:129], TCb[i][1][pr, 0, :], False, False),
                       mmf(pv, Sbf[pr, 1, gl, 1:129], TCb[i][1][pr, 1, :], False, True)]
                fw.mm(fns, [kUT, kSb, kTC[i]], [pk[pi]])
                fw.op(A, act(Y1[:, :, 16 * g:16 * g + 16], pv.rearrange("p (t h) -> p t h", t=16), AF.Gelu_apprx_tanh),
                      [pk[pi]], [kY1])
        if dbg and b == 0:
            fw.dma(SY, dbg_t['y1'], Y1, sem_d, [kY1], [kOut])
        def glu_s0(t):
            i = t % NB
            pi = t % 2
            pv = pbanks[pi][:, 0:256].bitcast(BF16).rearrange("p (f c) -> p f c", f=4)
            fns = [trf(pv[:, f, :], Y1[:, t, f * 128:(f + 1) * 128], idb) for f in range(4)]
            fw.mm(fns, [kY1, kC], [pk[pi]])
            fw.op(A, act(Y1T[i], pv, AF.Copy), [pk[pi]], [kY1T[i]])
            pz = 2 + t % 4
            pzv = pbanks[pz][:, :]
            fns = [mmf(pzv, Y1T[i][:, f, :], wgl[:, f, :], f == 0, f == 3) for f in range(4)]
            fw.mm(fns, [kY1T[i], kwg], [pk[pz]])

        def glu_s1(t):
            i = t % NB
            pz = 2 + t % 4
            pzv = pbanks[pz][:, :]
            s0 = stat[:, 32 + 2 * i:33 + 2 * i]
            fw.op(V, tt(zt[i], pzv, bg, ALU.add), [pk[pz], kC], [kzt[i]])
            fw.op(A, act(zs[i], zt[i], AF.Exp, scale=-1.0), [kzt[i]], [kzs[i]])
            fw.op(V, ts(zs[i], zs[i], 1.0, ALU.add), [kzs[i]], [kzs[i]])
            fw.op(V, rcp(zs[i], zs[i]), [kzs[i]], [kzs[i]])
            fw.op(V, tt(y2[i], zs[i], Y1[:, t, :], ALU.mult), [kzs[i], kY1], [ky2[i]])
            fw.op(A, ttr(zt[i], y2[i], y2[i], s0), [ky2[i], kzt[i]], [kzt[i], kss[i]])

        def glu_s2(t):
            i = t % NB
            s0, s1 = stat[:, 32 + 2 * i:33 + 2 * i], stat[:, 33 + 2 * i:34 + 2 * i]
            fw.op(V, ts(s1, s0, 1.0 / 512, ALU.mult, EPS, ALU.add), [kss[i]], [kss[i]])
            fw.op(A, act(s1, s1, AF.Ln), [kss[i]], [kss[i]])
            fw.op(A, act(s1, s1, AF.Exp, scale=-0.5), [kss[i]], [kss[i]])
            fw.op(V, stt(y2b[i], y2[i], s1, gs, ALU.mult, ALU.mult), [ky2[i], kss[i], kC], [ky2b[i]])

        def glu_s3(t):
            i = t % NB
            pi2 = 6 + t % 2
            pv2 = pbanks[pi2][:, 0:256].bitcast(BF16).rearrange("p (f c) -> p f c", f=4)
            fns = [trf(pv2[:, f, :], y2b[i][:, f * 128:(f + 1) * 128], idb) for f in range(4)]
            fw.mm(fns, [ky2b[i], kC], [pk[pi2]])
            fw.op(A, act(ssmT[:, :, t, :], pv2, AF.Copy), [pk[pi2]], [kssm])

        skew(16, [glu_s0, glu_s1, glu_s2, glu_s3])
        if dbg and b == 0:
            fw.dma(SY, dbg_t['ssmT'], ssmT, sem_d, [kssm], [kOut])
        fw.barrier()

        if STAGE < 5:
            raise _Stop()
        a5 = Bump(116 * K, ARENA)
        wo = a5([128, 8, 1024], BF16)
        otmp = [a5([128, 1024], F32) for _ in range(4)]
        oj = a5([128, 512], F32)
        kwo, koj = Tk(), Tk()
        kot = [Tk() for _ in range(4)]
        kst4 = [Tk() for _ in range(4)]
        kX1 = [Tk() for _ in range(16)]
        fw.dma(G_, wo, w_out.rearrange("(k p) n -> p k n", p=128), sem_og, writes=[kwo])
        for q4 in range(4):
            fw.dma(SY, X1[:, 4 * q4:4 * q4 + 4, :], xv[:, 4 * q4:4 * q4 + 4, :], sem_x[q4],
                   writes=[kX1[4 * q4 + j] for j in range(4)])
        def a4_s0(t):
            pis = [2 * (t % 4), 2 * (t % 4) + 1]
            for hf in range(2):
                pv = pbanks[pis[hf]][:, :]
                fns = []
                for k in range(8):
                    lhs = ssmT[:, k, t, :] if k < 4 else attT[:, k - 4, t::16]
                    fns.append(mmf(pv, lhs, wo[:, k, hf * 512:(hf + 1) * 512], k == 0, k == 7))
                fw.mm(fns, [kssm, kat, kwo], [pk[pis[hf]]])
                sl4 = t % 4
                c0 = 36 + 3 * sl4
                fw.op(A, sqa(oj, pv, stat[:, c0 + hf:c0 + hf + 1]), [pk[pis[hf]]], [kst4[sl4], koj])

        def a4_s1(t):
            pis = [2 * (t % 4), 2 * (t % 4) + 1]
            sl4 = t % 4
            c0 = 36 + 3 * sl4
            rs4 = stat[:, c0 + 2:c0 + 3]
            fw.op(V, tt(rs4, stat[:, c0:c0 + 1], stat[:, c0 + 1:c0 + 2], ALU.add), [kst4[sl4]], [kst4[sl4]])
            fw.op(V, ts(rs4, rs4, 1.0 / D, ALU.mult, EPS, ALU.add), [kst4[sl4]], [kst4[sl4]])
            fw.op(A, act(rs4, rs4, AF.Ln), [kst4[sl4]], [kst4[sl4]])
            fw.op(A, act(rs4, rs4, AF.Exp, scale=-0.5), [kst4[sl4]], [kst4[sl4]])
            for hf in range(2):
                pv = pbanks[pis[hf]][:, :]
                sl = slice(hf * 512, (hf + 1) * 512)
                fw.op(V, stt(otmp[sl4][:, sl], pv, rs4, g2[:, sl], ALU.mult, ALU.mult),
                      [pk[pis[hf]], kst4[sl4], kC], [kot[sl4]])
            fw.op(G_, tt(X1[:, t, :], X1[:, t, :], otmp[sl4], ALU.add), [kot[sl4], kX1[t]], [kX1[t]])

        skew(16, [a4_s0, a4_s1])
        if dbg and b == 0:
            fw.dma(SY, dbg_t['x1'], X1, sem_d, kX1, [kOut])
        fw.barrier()

        if STAGE < 6:
            raise _Stop()
        f0 = Bump(86 * K, ARENA)
        h2T = f0([128, 8, 16, 128], BF16)
        fT = f0([128, NFF, 16, 128], BF16)
        hb2 = [reg(118 * K + 4096 + 2048 * i_, [128, 1024], BF16) for i_ in range(2)]
        kfj, kh2T, kfT = Tk(), Tk(), Tk()
        khb = [Tk(), Tk()]
        kst5 = [Tk(), Tk()]
        ov = out[b].rearrange("(c t) d -> c t d", t=16)
        fj1 = reg(118 * K, [128, 1024], F32)
        def f1_s0(t):
            i5 = t % 2
            q0, q1 = stat[:, 52 + 2 * i5:53 + 2 * i5], stat[:, 53 + 2 * i5:54 + 2 * i5]
            fw.op(A, ttr(fj1, X1[:, t, :], X1[:, t, :], q0), [kX1[t]], [kst5[i5], kfj])
            fw.op(V, ts(q1, q0, 1.0 / D, ALU.mult, EPS, ALU.add), [kst5[i5]], [kst5[i5]])
            fw.op(A, act(q1, q1, AF.Ln), [kst5[i5]], [kst5[i5]])
            fw.op(A, act(q1, q1, AF.Exp, scale=-0.5), [kst5[i5]], [kst5[i5]])
            fw.op(A, act(hb2[i5], X1[:, t, :], AF.Copy, scale=q1), [kX1[t], kst5[i5]], [khb[i5]])

        def f1_s1(t):
            i5 = t % 2
            for kk_ in range(2):
                pi = kk_ + 2 * (t % 2)
                pv = pbanks[pi][:, 0:256].bitcast(BF16).rearrange("p (f c) -> p f c", f=4)
                fns = [trf(pv[:, f, :], hb2[i5][:, (4 * kk_ + f) * 128:(4 * kk_ + f + 1) * 128], idb) for f in range(4)]
                fw.mm(fns, [khb[i5], kC], [pk[pi]])
                for f in range(4):
                    k = 4 * kk_ + f
                    if f % 2 == 0:
                        fw.op(V, ts(h2T[:, k, t, :], pv[:, f, :], g3[:, k:k + 1], ALU.mult), [pk[pi], kC], [kh2T])
                    else:
                        fw.op(A, act(h2T[:, k, t, :], pv[:, f, :], AF.Copy, scale=g3[:, k:k + 1]),
                              [pk[pi], kC], [kh2T])

        skew(16, [f1_s0, f1_s1])
        kOt = [Tk() for _ in range(16)]
        fw.dma(SY, ov, X1, sem_o, kX1, kOt)
        fw.barrier()
        f2 = Bump(64 * K, 86 * K)
        wg = [f2([128, 8, 128], BF16) for _ in range(2)]
        wu = [f2([128, 8, 128], BF16) for _ in range(2)]
        sg = [f2([128, 512], F32) for _ in range(2)]
        wd = reg(20 * K, [128, NFF, 1024], BF16)
        kwg = [Tk(), Tk()]
        kwd = Tk()
        ksg = [Tk(), Tk()]
        wg_v = w_gate.rearrange("(k p) n -> p k n", p=128)
        wu_v = w_up.rearrange("(k p) n -> p k n", p=128)
        wd_v = w_down.rearrange("(j p) n -> p j n", p=128)
        hv = h2T.rearrange("p k t c -> p k (t c)")
        pc = 0
        for j in range(NFF):
            i = j % 2
            fw.dma(G_, wg[i], wg_v[:, :, j * 128:(j + 1) * 128], sem_g[i], writes=[kwg[i]])
            fw.dma(G_, wu[i], wu_v[:, :, j * 128:(j + 1) * 128], sem_g[i], writes=[kwg[i]])
            fw.dma(G_, wd[:, j, :], wd_v[:, j, :], sem_dn, writes=[kwd])
            for n in range(4):
                pg_, pu_ = 2 * (pc % 4), 2 * (pc % 4) + 1
                si = pc % 2
                pc += 1
                pgv, puv = pbanks[pg_][:, :], pbanks[pu_][:, :]
                fns = [mmf(pgv, wg[i][:, k, :], hv[:, k, n * 512:(n + 1) * 512], k == 0, k == 7) for k in range(8)]
                fw.mm(fns, [kh2T, kwg[i]], [pk[pg_]])
                fns = [mmf(puv, wu[i][:, k, :], hv[:, k, n * 512:(n + 1) * 512], k == 0, k == 7) for k in range(8)]
                fw.mm(fns, [kh2T, kwg[i]], [pk[pu_]])
                fw.op(A, act(sg[si], pgv, AF.Silu), [pk[pg_]], [ksg[si]])
                fw.op(V, tt(fT[:, j].rearrange("p t c -> p (t c)")[:, n * 512:(n + 1) * 512], sg[si], puv, ALU.mult),
                      [ksg[si], pk[pu_]], [kfT])
        fw.barrier()
        f3 = Bump(64 * K, 118 * K)
        xr = [f3([128, 1024], F32) for _ in range(4)]
        ot2 = [f3([128, 1024], F32) for _ in range(2)]
        fj3 = f3([128, 512], F32)
        kxr = [Tk() for _ in range(4)]
        kot2 = [Tk(), Tk()]
        kst6 = [Tk(), Tk()]
        for grp in range(8):
            base = 4 * (grp % 2)
            for tl in range(2):
                t = 2 * grp + tl
                fw.dma(SY, xr[t % 4], ov[:, t, :], sem_x[t % 4], [kOt[t]], [kxr[t % 4]])
            for j in range(NFF):
                fns = []
                for tl in range(2):
                    t = 2 * grp + tl
                    for hf in range(2):
                        fns.append(mmf(pbanks[base + 2 * tl + hf][:, :], fT[:, j, t, :], wd[:, j, hf * 512:(hf + 1) * 512],
                                       j == 0, j == NFF - 1))
                fw.mm(fns, [kfT, kwd], [pk[base + q_] for q_ in range(4)])
            for tl in range(2):
                t = 2 * grp + tl
                xi = t % 4
                i6 = t % 2
                c0 = 56 + 3 * i6
                rs6 = stat[:, c0 + 2:c0 + 3]
                for hf in range(2):
                    bk = base + 2 * tl + hf
                    fw.op(A, sqa(fj3, pbanks[bk][:, :], stat[:, c0 + hf:c0 + hf + 1]), [pk[bk]], [kst6[i6], kfj])
                fw.op(V, tt(rs6, stat[:, c0:c0 + 1], stat[:, c0 + 1:c0 + 2], ALU.add), [kst6[i6]], [kst6[i6]])
                fw.op(V, ts(rs6, rs6, 1.0 / D, ALU.mult, EPS, ALU.add), [kst6[i6]], [kst6[i6]])
                fw.op(A, act(rs6, rs6, AF.Ln), [kst6[i6]], [kst6[i6]])
                fw.op(A, act(rs6, rs6, AF.Exp, scale=-0.5), [kst6[i6]], [kst6[i6]])
                for hf in range(2):
                    bk = base + 2 * tl + hf
                    sl = slice(hf * 512, (hf + 1) * 512)
                    fw.op(V, stt(ot2[i6][:, sl], pbanks[bk][:, :], rs6, g4[:, sl], ALU.mult, ALU.mult),
                          [pk[bk], kst6[i6], kC], [kot2[i6]])
                fw.op(G_, tt(xr[xi], xr[xi], ot2[i6], ALU.add), [kot2[i6], kxr[xi]], [kxr[xi]])
                fw.dma(SY, ov[:, t, :], xr[xi], sem_st[t % 4], [kxr[xi]], [kOt[t]])
        fw.barrier()
    except _Stop:
        pass
    fw.barrier()
    fw.emit()
    return nc, es


_CACHE = {}


def _consts():
    idf = np.eye(128, dtype=np.float32)
    cb = np.zeros((128, 2, 256), np.float32)
    for r in range(2):
        kpos = r * 128 + np.arange(128)[:, None]
        qpos = np.arange(256)[None, :]
        cb[:, r, :] = np.where(kpos <= qpos, 0.0, NEG)
    tm = np.zeros((128, 2, 256), np.float32)
    dm = np.zeros((128, 2, 256), np.float32)
    for j in range(2):
        for s8 in range(8):
            for hi in range(16):
                sp = 8 * j + s8
                row = s8 * 16 + hi
                for jj in range(2):
                    pass
                for tp in range(16):
                    if tp >= sp:
                        tm[row, j, tp * 16:(tp + 1) * 16] = 1.0
                dm[row, j, sp * 16 + hi] = 1.0
    nv = np.tile(np.arange(1, 33, dtype=np.float32)[None, :], (128, 1))
    return idf, cb, tm, dm, nv


def kernel(**inp):
    f32 = np.float32
    x = np.ascontiguousarray(inp['x'], dtype=f32)

    def sq(k):
        return np.ascontiguousarray(inp[k][0], dtype=f32)

    def gp(a):
        a = a.reshape((2, 16) + a.shape[1:])
        a = np.moveaxis(a, 2, 1)
        return np.ascontiguousarray(a.reshape((128, 16) + a.shape[3:]))
    idf, cb, tm, dm, nv = _consts()
    shared = dict(
        w_in=sq('w_in'), w_glu=sq('w_glu'), w_out=sq('w_out'), w_gate=sq('w_gate'), w_up=sq('w_up'),
        w_down=sq('w_down'),
        areT=gp(sq('ssm_a_re')), aimT=gp(sq('ssm_a_im')),
        ldtT=gp(np.ascontiguousarray(np.broadcast_to(sq('ssm_log_dt')[:, None], (32, 64)))),
        breT=gp(sq('ssm_b_re')), bimT=gp(sq('ssm_b_im')),
        creT=gp(np.ascontiguousarray(sq('ssm_c_re').transpose(0, 2, 1))),
        cimT=gp(np.ascontiguousarray(sq('ssm_c_im').transpose(0, 2, 1))),
        drep=np.ascontiguousarray(np.broadcast_to(sq('ssm_d')[None], (128, 32, 16))),
        g1c=np.ascontiguousarray(sq('g_pre_mix').reshape(8, 128).T),
        g3c=np.ascontiguousarray(sq('g_pre_ffn').reshape(8, 128).T),
        g2r=np.ascontiguousarray(np.broadcast_to(sq('g_post_mix')[None], (128, 1024))),
        g4r=np.ascontiguousarray(np.broadcast_to(sq('g_post_ffn')[None], (128, 1024))),
        gsr=np.ascontiguousarray(np.broadcast_to(sq('g_ssm_out')[None], (128, 512))),
        gar=np.ascontiguousarray(np.broadcast_to(sq('g_attn_out')[None], (128, 512))),
        bgr=np.ascontiguousarray(np.broadcast_to(sq('b_glu')[None], (128, 512))),
        c_idf=idf, c_cb=cb, c_tm=tm, c_dm=dm, c_nv=nv,
    )
    if 'nc' not in _CACHE:
        _CACHE['nc'] = build(DEBUG)
    nc, _es = _CACHE['nc']
    in_maps = []
    for c in range(8):
        m = dict(shared)
        m['x'] = np.ascontiguousarray(x[2 * c:2 * c + 2])
        in_maps.append(m)
    res = run_bass_kernel_spmd(nc, in_maps, core_ids=list(range(8)))
    _CACHE['res'] = res
    return np.concatenate([np.asarray(r['out'], dtype=f32) for r in res.results], axis=0)
```

```python
import math
import numpy as np
from contextlib import ExitStack
import concourse.bass as bass
import concourse.mybir as mybir
from concourse.alu_op_type import AluOpType as ALU
from concourse.bass_utils import run_bass_kernel_spmd

F32 = mybir.dt.float32
BF16 = mybir.dt.bfloat16
U8 = mybir.dt.uint8
I32 = mybir.dt.int32
AF = mybir.ActivationFunctionType
AX = mybir.AxisListType

S = 2048
D = 1024
DFF = 2816
NFF = 22
NEG = -30000.0
EPS = 1e-6
ENGS = ['tensor', 'vector', 'scalar', 'gpsimd', 'sync']
DEBUG = False
STAGE = 99
NSTEP = 127
ABQ = 8


class _Stop(Exception):
    pass


class Tk:
    __slots__ = ('w', 'r')

    def __init__(self):
        self.w = {}
        self.r = {}


class FW:
    def __init__(self, nc, es):
        self.nc, self.es = nc, es
        self.prog = {e: [] for e in ENGS}
        self.esem = {e: es.enter_context(nc.semaphore("es_" + e)) for e in ENGS}
        self.ecnt = {e: 0 for e in ENGS}
        self.seen = {e: {} for e in ENGS}
        self.dcnt = {}

    def newsem(self, name):
        s = self.es.enter_context(self.nc.semaphore(name))
        self.dcnt[id(s)] = [s, 0]
        return s

    def _dep(self, eng, reads, writes):
        need = {}

        def add(dct):
            for k, (s, v) in dct.items():
                if need.get(k, (None, 0))[1] < v:
                    need[k] = (s, v)
        for t in reads:
            add(t.w)
        for t in writes:
            add(t.w)
            add(t.r)
        for k, (s, v) in need.items():
            if self.seen[eng].get(k, 0) >= v:
                continue
            self.seen[eng][k] = v
            self.prog[eng].append(('w', s, v))

    def _post(self, tok, reads, writes):
        s, v = tok
        k = id(s)
        for t in reads:
            if t.r.get(k, (None, 0))[1] < v:
                t.r[k] = (s, v)
        for t in writes:
            t.w = {k: (s, v)}
            t.r = {}

    def op(self, eng, fn, reads=(), writes=()):
        self._dep(eng, reads, writes)
        self.ecnt[eng] += 1
        self.prog[eng].append(('o', fn, True))
        self._post((self.esem[eng], self.ecnt[eng]), reads, writes)

    def mm(self, fns, reads=(), writes=()):
        self._dep('tensor', reads, writes)
        for f in fns[:-1]:
            self.prog['tensor'].append(('o', f, False))
        self.ecnt['tensor'] += 1
        self.prog['tensor'].append(('o', fns[-1], True))
        self._post((self.esem['tensor'], self.ecnt['tensor']), reads, writes)

    def dma(self, eng, out, in_, sem, reads=(), writes=()):
        self._dep(eng, reads, writes)
        c = self.dcnt[id(sem)]
        c[1] += 16
        self.prog[eng].append(('d', out, in_, sem))
        self._post((sem, c[1]), reads, writes)

    def barrier(self):
        for e in ENGS:
            for e2 in ENGS:
                if e2 == e or self.ecnt[e2] == 0:
                    continue
                k = id(self.esem[e2])
                if self.seen[e].get(k, 0) < self.ecnt[e2]:
                    self.seen[e][k] = self.ecnt[e2]
                    self.prog[e].append(('w', self.esem[e2], self.ecnt[e2]))
            for k, (s, v) in self.dcnt.items():
                if v > 0 and self.seen[e].get(k, 0) < v:
                    self.seen[e][k] = v
                    self.prog[e].append(('w', s, v))

    def emit(self):
        nc = self.nc
        with nc.Block() as block:
            for e in ENGS:
                prog = self.prog[e]
                sem = self.esem[e]

                def body(E, prog=prog, sem=sem):
                    for it in prog:
                        if it[0] == 'w':
                            E.wait_ge(it[1], it[2])
                        elif it[0] == 'o':
                            ins = it[1](E)
                            if it[2]:
                                ins.then_inc(sem, 1)
                        else:
                            E.dma_start(out=it[1], in_=it[2]).then_inc(it[3], 16)
                getattr(block, e)(body)


def tt(out, in0, in1, op):
    return lambda E: E.tensor_tensor(out=out, in0=in0, in1=in1, op=op)


def ts(out, in0, s1, op0, s2=None, op1=None):
    if op1 is None:
        return lambda E: E.tensor_scalar(out=out, in0=in0, scalar1=s1, scalar2=None, op0=op0)
    return lambda E: E.tensor_scalar(out=out, in0=in0, scalar1=s1, scalar2=s2, op0=op0, op1=op1)


def stt(out, in0, scalar, in1, op0, op1):
    return lambda E: E.scalar_tensor_tensor(out=out, in0=in0, scalar=scalar, in1=in1, op0=op0, op1=op1)


def ttr(out, in0, in1, accum):
    return lambda E: E.activation(out=out, in_=in0, func=AF.Square, accum_out=accum)


def act(out, in_, func, scale=None):
    if scale is None:
        return lambda E: E.activation(out=out, in_=in_, func=func)
    return lambda E: E.activation(out=out, in_=in_, func=func, scale=scale)


def sqa(out, in_, accum):
    return lambda E: E.activation(out=out, in_=in_, func=AF.Square, accum_out=accum)


def rcp(out, in_):
    return lambda E: E.reciprocal(out=out, in_=in_)


def cp(out, in_):
    return lambda E: E.tensor_copy(out=out, in_=in_)


def mmf(out, lhsT, rhs, start, stop):
    assert len(rhs.ap) == 2 and len(lhsT.ap) == 2, ("rhs", rhs.ap, lhsT.ap)
    return lambda E: E.matmul(out, lhsT, rhs, start=start, stop=stop)


def trf(out, in_, ident):
    assert len(ident.ap) == 2 and len(in_.ap) == 2, ("ident", ident.ap, in_.ap)
    return lambda E: E.transpose(out, in_, ident)


def skew(T, stages):
    ns = len(stages)
    for step in range(T + ns - 1):
        for si in range(ns):
            t = step - si
            if 0 <= t < T:
                stages[si](t)


def bc(ap, axis, shape):
    return ap.unsqueeze(axis).broadcast_to(list(shape))


def build(dbg=False):
    nc = bass.Bass("TRN2", target_bir_lowering=False)
    es = ExitStack()

    def din(name, shape):
        return nc.dram_tensor(name, list(shape), F32, kind="ExternalInput").ap()

    x = din("x", [2, S, D])
    w_in = din("w_in", [D, 2048])
    w_glu = din("w_glu", [512, 512])
    w_out = din("w_out", [1024, 1024])
    w_gate = din("w_gate", [D, DFF])
    w_up = din("w_up", [D, DFF])
    w_down = din("w_down", [DFF, D])
    areT = din("areT", [128, 16])
    aimT = din("aimT", [128, 16])
    ldtT = din("ldtT", [128, 16])
    breT = din("breT", [128, 16, 16])
    bimT = din("bimT", [128, 16, 16])
    creT = din("creT", [128, 16, 16])
    cimT = din("cimT", [128, 16, 16])
    drep = din("drep", [128, 32, 16])
    g1c = din("g1c", [128, 8])
    g3c = din("g3c", [128, 8])
    g2r = din("g2r", [128, 1024])
    g4r = din("g4r", [128, 1024])
    gsr = din("gsr", [128, 512])
    gar = din("gar", [128, 512])
    bgr = din("bgr", [128, 512])
    c_idf = din("c_idf", [128, 128])
    c_cb = din("c_cb", [128, 2, 256])
    c_tm = din("c_tm", [128, 2, 256])
    c_dm = din("c_dm", [128, 2, 256])
    c_nv = din("c_nv", [128, 32])
    out = nc.dram_tensor("out", [2, S, D], F32, kind="ExternalOutput").ap()
    GT_d = nc.dram_tensor("GT_d", [32, 128, 2, 2, 128], BF16, kind="Internal").ap()
    TOEP_d = nc.dram_tensor("TOEP_d", [32, 128, 2, 256], BF16, kind="Internal").ap()
    CT_d = nc.dram_tensor("CT_d", [16, 128, 2, 256], BF16, kind="Internal").ap()
    dbg_t = {}
    if dbg:
        dbg_t['uc'] = nc.dram_tensor("d_uc", [128, 32, 16, 16], BF16, kind="ExternalOutput").ap()
        dbg_t['qT'] = nc.dram_tensor("d_qT", [128, 4, S], BF16, kind="ExternalOutput").ap()
        dbg_t['vt'] = nc.dram_tensor("d_vt", [128, 16, 8, 65], BF16, kind="ExternalOutput").ap()
        dbg_t['y1'] = nc.dram_tensor("d_y1", [128, 16, 512], BF16, kind="ExternalOutput").ap()
        dbg_t['ssmT'] = nc.dram_tensor("d_ssmT", [128, 4, 16, 128], BF16, kind="ExternalOutput").ap()
        dbg_t['attT'] = nc.dram_tensor("d_attT", [128, 4, S], BF16, kind="ExternalOutput").ap()
        dbg_t['x1'] = nc.dram_tensor("d_x1", [128, 16, 1024], F32, kind="ExternalOutput").ap()
        dbg_t['S'] = nc.dram_tensor("d_S", [128, 2, 16, 128], F32, kind="ExternalOutput").ap()

    ARENA = 206 * 1024
    arena = es.enter_context(nc.sbuf_tensor("arena", [128, ARENA], U8))
    pbanks = [es.enter_context(nc.psum_tensor("pb%d" % i, [128, 512], F32)) for i in range(8)]
    pk = [Tk() for _ in range(8)]
    fw = FW(nc, es)

    def reg(off, shape, dt, p0=0):
        esz = 2 if dt == BF16 else 4
        n = int(np.prod(shape[1:])) * esz
        assert off % 4 == 0 and off + n <= ARENA, (off, n)
        ap = arena[p0:p0 + shape[0], off:off + n].bitcast(dt)
        if len(shape) == 3:
            ap = ap.rearrange("p (a b) -> p a b", a=shape[1])
        elif len(shape) == 4:
            ap = ap.rearrange("p (a b c) -> p a b c", a=shape[1], b=shape[2])
        elif len(shape) == 5:
            ap = ap.rearrange("p (a b c d) -> p a b c d", a=shape[1], b=shape[2], c=shape[3])
        return ap

    class Bump:
        def __init__(self, lo, hi):
            self.o, self.hi = lo, hi

        def __call__(self, shape, dt, p0=0):
            esz = 2 if dt == BF16 else 4
            n = int(np.prod(shape[1:])) * esz
            n4 = (n + 31) // 32 * 32
            a = reg(self.o, shape, dt, p0)
            self.o += n4
            assert self.o <= self.hi, (self.o, self.hi)
            return a

    K = 1024
    V, A, G_, T_, SY = 'vector', 'scalar', 'gpsimd', 'tensor', 'sync'

    cb = Bump(0, 20 * K)
    idf = cb([128, 128], F32)
    idb = cb([128, 128], BF16)
    cbias = cb([128, 2, 256], BF16)
    g1 = cb([128, 8], F32)
    g3 = cb([128, 8], F32)
    g2 = cb([128, 1024], F32)
    g4 = cb([128, 1024], F32)
    gs = cb([128, 512], F32)
    ga = cb([128, 512], F32)
    bg = cb([128, 512], F32)
    stat = cb([128, 64], F32)
    LR = cb([128, 7, 16], F32)
    LI = cb([128, 7, 16], F32)
    LIn = cb([128, 7, 16], F32)
    LAk = cb([128, 7, 2, 16], F32)
    assert cb.o <= 20 * K
    kC = Tk()
    sem_c = fw.newsem("sem_c")
    sem_c2 = fw.newsem("sem_c2")
    for dst, src in [(idf, c_idf), (g1, g1c), (g3, g3c), (g2, g2r), (g4, g4r), (gs, gsr), (ga, gar), (bg, bgr)]:
        fw.dma(SY, dst, src, sem_c, writes=[kC])

    sb = Bump(20 * K, ARENA)
    cb_off = sb.o
    CBre = sb([128, 16, 16, 16], F32)
    cbf = sb([128, 2, 256], F32)
    tmk = sb([128, 2, 256], F32)
    dmk = sb([128, 2, 256], F32)
    nv = sb([128, 32], F32)
    drp = sb([128, 32, 16], F32)
    are = sb([128, 16], F32)
    aim = sb([128, 16], F32)
    ldt = sb([128, 16], F32)
    bre = sb([128, 16, 16], F32)
    bim = sb([128, 16, 16], F32)
    cre = sb([128, 16, 16], F32)
    cim = sb([128, 16, 16], F32)
    kS = Tk()
    for dst, src in [(cbf, c_cb), (tmk, c_tm), (dmk, c_dm), (nv, c_nv), (drp, drep), (are, areT), (aim, aimT),
                     (ldt, ldtT), (bre, breT), (bim, bimT), (cre, creT), (cim, cimT)]:
        fw.dma(SY, dst, src, sem_c2, writes=[kS])
    fw.op(V, cp(idb, idf), [kC], [kC])
    fw.op(V, cp(cbias, cbf), [kS, kC], [kC])

    dtt = sb([128, 16], F32)
    adr = sb([128, 16], F32)
    adi = sb([128, 16], F32)
    ex = sb([128, 16, 32], F32)
    ang = sb([128, 16, 32], F32)
    mag = sb([128, 16, 32], F32)
    magi = sb([128, 16, 32], F32)
    r1 = sb([128, 16, 32], F32)
    r2 = sb([128, 16, 32], F32)
    sn = sb([128, 16, 32], F32)
    cs = sb([128, 16, 32], F32)
    Pre = sb([128, 16, 32], F32)
    Pim = sb([128, 16, 32], F32)
    Qre = sb([128, 16, 32], F32)
    Qim = sb([128, 16, 32], F32)
    sm = [sb([128, 16], F32) for _ in range(10)]
    bbre = sb([128, 16, 16], F32)
    bbim = sb([128, 16, 16], F32)
    tb1 = sb([128, 16, 16], F32)
    tb2 = sb([128, 16, 16], F32)
    k0 = Tk()
    fw.op(A, act(dtt, ldt, AF.Exp), [kS], [k0])
    fw.op(V, tt(adr, are, dtt, ALU.mult), [k0, kS], [k0])
    fw.op(V, tt(adi, aim, dtt, ALU.mult), [k0, kS], [k0])
    fw.op(V, tt(ex, bc(adr, 2, [128, 16, 32]), bc(nv, 1, [128, 16, 32]), ALU.mult), [k0, kS], [k0])
    fw.op(V, tt(ang, bc(adi, 2, [128, 16, 32]), bc(nv, 1, [128, 16, 32]), ALU.mult), [k0, kS], [k0])
    fw.op(A, act(mag, ex, AF.Exp), [k0], [k0])
    fw.op(A, act(magi, ex, AF.Exp, scale=-1.0), [k0], [k0])
    ki = reg(cb_off, [128, 16, 32], I32)
    kf = reg(cb_off + 2048, [128, 16, 32], F32)
    mk = reg(cb_off + 4096, [128, 16, 32], F32)

    def range_reduce(r, shift):
        fw.op(V, ts(r, ang, 1.0 / (2 * math.pi), ALU.mult, shift, ALU.add), [k0], [k0])
        fw.op(V, cp(ki, r), [k0], [k0])
        fw.op(V, cp(kf, ki), [k0], [k0])
        fw.op(V, tt(r, r, kf, ALU.subtract), [k0], [k0])
        fw.op(V, ts(mk, r, -0.5, ALU.is_lt), [k0], [k0])
        fw.op(V, tt(r, r, mk, ALU.add), [k0], [k0])
        fw.op(V, ts(mk, r, 0.5, ALU.is_gt), [k0], [k0])
        fw.op(V, tt(r, r, mk, ALU.subtract), [k0], [k0])
    range_reduce(r1, 0.0)
    range_reduce(r2, 0.25)
    fw.op(A, act(sn, r1, AF.Sin, scale=2 * math.pi), [k0], [k0])
    fw.op(A, act(cs, r2, AF.Sin, scale=2 * math.pi), [k0], [k0])
    fw.op(V, tt(Pre, mag, cs, ALU.mult), [k0], [k0])
    fw.op(V, tt(Pim, mag, sn, ALU.mult), [k0], [k0])
    fw.op(V, tt(Qre, magi, cs, ALU.mult), [k0], [k0])
    fw.op(V, stt(Qim, magi, -1.0, sn, ALU.mult, ALU.mult), [k0], [k0])
    lbre, lbim = Pre[:, :, 0], Pim[:, :, 0]
    nr, den, t0, t1_, cr, ci, rden = sm[0], sm[1], sm[2], sm[3], sm[4], sm[5], sm[6]
    fw.op(V, ts(nr, lbre, -1.0, ALU.add), [k0], [k0])
    fw.op(V, tt(den, are, are, ALU.mult), [k0, kS], [k0])
    fw.op(V, tt(t0, aim, aim, ALU.mult), [k0, kS], [k0])
    fw.op(V, tt(den, den, t0, ALU.add), [k0], [k0])
    fw.op(V, lambda E: E.reciprocal(out=rden, in_=den), [k0], [k0])
    fw.op(V, tt(t0, nr, are, ALU.mult), [k0], [k0])
    fw.op(V, tt(t1_, lbim, aim, ALU.mult), [k0], [k0])
    fw.op(V, tt(t0, t0, t1_, ALU.add), [k0], [k0])
    fw.op(V, tt(cr, t0, rden, ALU.mult), [k0], [k0])
    fw.op(V, tt(t0, lbim, are, ALU.mult), [k0], [k0])
    fw.op(V, tt(t1_, nr, aim, ALU.mult), [k0], [k0])
    fw.op(V, tt(t0, t0, t1_, ALU.subtract), [k0], [k0])
    fw.op(V, tt(ci, t0, rden, ALU.mult), [k0], [k0])
    crb, cib = bc(cr, 2, [128, 16, 16]), bc(ci, 2, [128, 16, 16])
    fw.op(V, tt(tb1, crb, bre, ALU.mult), [k0, kS], [k0])
    fw.op(V, tt(tb2, cib, bim, ALU.mult), [k0, kS], [k0])
    fw.op(V, tt(bbre, tb1, tb2, ALU.subtract), [k0], [k0])
    fw.op(V, tt(tb1, crb, bim, ALU.mult), [k0, kS], [k0])
    fw.op(V, tt(tb2, cib, bre, ALU.mult), [k0, kS], [k0])
    fw.op(V, tt(bbim, tb1, tb2, ALU.add), [k0], [k0])
    fw.op(V, cp(LR[:, 0, :], Pre[:, :, 15]), [k0], [kC])
    fw.op(V, cp(LI[:, 0, :], Pim[:, :, 15]), [k0], [kC])
    for k in range(6):
        fw.op(V, tt(sm[7], LR[:, k, :], LR[:, k, :], ALU.mult), [kC, k0], [k0])
        fw.op(V, tt(sm[8], LI[:, k, :], LI[:, k, :], ALU.mult), [kC, k0], [k0])
        fw.op(V, tt(LR[:, k + 1, :], sm[7], sm[8], ALU.subtract), [k0], [kC])
        fw.op(V, tt(sm[9], LR[:, k, :], LI[:, k, :], ALU.mult), [kC, k0], [k0])
        fw.op(V, ts(LI[:, k + 1, :], sm[9], 2.0, ALU.mult), [k0], [kC])
    fw.op(V, ts(LIn, LI, -1.0, ALU.mult), [kC], [kC])
    fw.op(V, cp(LAk[:, :, 0, :], LR), [kC], [kC])
    fw.op(V, cp(LAk[:, :, 1, :], LR), [kC], [kC])
    SH4 = [128, 16, 16, 16]
    Gre = sb(SH4, F32)
    Gim = sb(SH4, F32)
    CAre = sb(SH4, F32)
    CAim = sb(SH4, F32)
    CBim = sb(SH4, F32)
    u1_off = sb.o
    U1 = sb(SH4, F32)
    U2 = sb(SH4, F32)
    U3, U4 = U1, U2
    CTst = reg(u1_off, [128, 16, 2, 256], BF16)
    kG, kCA, kCB, kU1, kU2, kCT = [Tk() for _ in range(6)]
    kU3, kU4 = kU1, kU2

    def outer(eng, o, a, pn, rd, wr):
        fw.op(eng, tt(o, bc(a, 2, SH4), bc(pn, 3, SH4), ALU.mult), rd, wr)

    def fl(ap):
        return ap.rearrange("p a b c -> p (a b c)")
    outer(V, U1, bbre, Qre[:, :, 0:16], [k0], [kU1])
    outer(G_, U2, bbim, Qim[:, :, 0:16], [k0], [kU2])
    fw.op(V, tt(Gre, U1, U2, ALU.subtract), [kU1, kU2], [kG])
    outer(V, U3, bbre, Qim[:, :, 0:16], [k0], [kU3])
    outer(G_, U4, bbim, Qre[:, :, 0:16], [k0], [kU4])
    fw.op(V, tt(Gim, U3, U4, ALU.add), [kU3, kU4], [kG])
    outer(V, U1, cre, Pre[:, :, 0:16], [k0, kS], [kU1])
    outer(G_, U2, cim, Pim[:, :, 0:16], [k0, kS], [kU2])
    fw.op(V, tt(CAre, U1, U2, ALU.subtract), [kU1, kU2], [kCA])
    outer(V, U3, cre, Pim[:, :, 0:16], [k0, kS], [kU3])
    outer(G_, U4, cim, Pre[:, :, 0:16], [k0, kS], [kU4])
    fw.op(V, stt(fl(CAim), fl(U3), -1.0, fl(U4), ALU.mult, ALU.subtract), [kU3, kU4], [kCA])
    outer(V, U1, cre, Pre[:, :, 16:32], [k0, kS], [kU1])
    outer(G_, U2, cim, Pim[:, :, 16:32], [k0, kS], [kU2])
    fw.op(V, tt(CBre, U1, U2, ALU.subtract), [kU1, kU2], [kCB])
    outer(V, U3, cre, Pim[:, :, 16:32], [k0, kS], [kU3])
    outer(G_, U4, cim, Pre[:, :, 16:32], [k0, kS], [kU4])
    fw.op(V, stt(fl(CBim), fl(U3), -1.0, fl(U4), ALU.mult, ALU.subtract), [kU3, kU4], [kCB])
    fw.op(V, cp(CTst[:, :, 0, :], CBre.rearrange("p g t h -> p g (t h)")), [kCB], [kCT, kU1])
    fw.op(V, cp(CTst[:, :, 1, :], CBim.rearrange("p g t h -> p g (t h)")), [kCB], [kCT, kU1])
    sem_t = fw.newsem("sem_t")
    kDR = Tk()
    fw.dma(SY, CT_d.rearrange("g p r c -> p g r c"), CTst, sem_t, [kCT], [kDR])
    TPst = [sb([128, 2, 256], BF16) for _ in range(2)]
    GTst = [sb([128, 2, 2, 128], BF16) for _ in range(2)]
    TPt = [sb([128, 2, 256], F32) for _ in range(2)]
    TPd = [sb([128, 2, 256], F32) for _ in range(2)]
    kTP = [Tk() for _ in range(2)]
    kGTs = [Tk() for _ in range(2)]
    kTt = [Tk() for _ in range(2)]
    sem_tp = [fw.newsem("sem_tp%d" % i) for i in range(2)]
    sem_gt = [fw.newsem("sem_gt%d" % i) for i in range(2)]
    for gh in range(2):
        pr = slice(64 * gh, 64 * gh + 64)
        for gl in range(16):
            g = 16 * gh + gl
            i = g % 2
            pT, pG = 2 * i, 2 * i + 1
            pt_v = pbanks[pT][:, :].rearrange("p (j c) -> p j c", j=2)
            fns = []
            for j in range(2):
                l_re = Gre[pr, gl, 8 * j:8 * j + 8, :].rearrange("p s h -> p (s h)")
                l_im = Gim[pr, gl, 8 * j:8 * j + 8, :].rearrange("p s h -> p (s h)")
                fns.append(mmf(pt_v[:, j, :], l_re, CAre[pr, gl].rearrange("p t h -> p (t h)"), True, False))
                fns.append(mmf(pt_v[:, j, :], l_im, CAim[pr, gl].rearrange("p t h -> p (t h)"), False, True))
            fw.mm(fns, [kG, kCA], [pk[pT]])
            pg_v = pbanks[pG][:, :].rearrange("p (j r c) -> p j r c", j=2, r=2)
            fns = []
            for j in range(2):
                l_re = Gre[pr, gl, 8 * j:8 * j + 8, :].rearrange("p s h -> p (s h)")
                l_im = Gim[pr, gl, 8 * j:8 * j + 8, :].rearrange("p s h -> p (s h)")
                fns.append(mmf(pg_v[:, j, 0, :], l_re, idf[pr, :], True, True))
                fns.append(mmf(pg_v[:, j, 1, :], l_im, idf[pr, :], True, True))
            fw.mm(fns, [kG, kC], [pk[pG]])
            fw.op(V, tt(TPt[i], pt_v, tmk, ALU.mult), [pk[pT], kS], [kTt[i]])
            fw.op(G_, tt(TPd[i].rearrange("p j (t h) -> p j t h", t=16),
                         dmk.rearrange("p j (t h) -> p j t h", t=16),
                         drp[:, g, :].unsqueeze(1).unsqueeze(1).broadcast_to([128, 2, 16, 16]), ALU.mult),
                  [kS], [kTP[i]])
            fw.op(V, tt(TPst[i], TPt[i], TPd[i], ALU.add), [kTt[i], kTP[i]], [kTP[i]])
            fw.dma(SY, TOEP_d[g], TPst[i], sem_tp[i], [kTP[i]], [kDR])
            fw.op(A, act(GTst[i], pg_v, AF.Copy), [pk[pG]], [kGTs[i]])
            fw.dma(SY, GT_d[g], GTst[i], sem_gt[i], [kGTs[i]], [kDR])
    fw.barrier()

    X1 = reg(20 * K, [128, 16, 1024], F32)
    attT = reg(84 * K, [128, 4, S], BF16)
    ssmT = reg(100 * K, [128, 4, 16, 128], BF16)
    Uc = reg(116 * K, [128, 32, 16, 16], BF16)
    qT = reg(132 * K, [128, 4, S], BF16)
    kT = reg(148 * K, [128, 4, S], BF16)
    Vt = reg(164 * K, [128, 16, 8, 65], BF16)
    sem_x = [fw.newsem("sem_x%d" % i) for i in range(4)]
    sem_w = [fw.newsem("sem_w%d" % i) for i in range(2)]
    sem_o = fw.newsem("sem_o")
    sem_og = fw.newsem("sem_og")
    sem_wq = [fw.newsem("sem_wq%d" % i) for i in range(2)]
    sem_d = fw.newsem("sem_d")
    sem_g = [fw.newsem("sem_g%d" % i) for i in range(2)]
    sem_dn = fw.newsem("sem_dn")
    sem_st = [fw.newsem("sem_st%d" % i) for i in range(4)]
    kOut = Tk()

    try:
      if STAGE < 1:
        raise _Stop()
      for b in range(2):
        a1 = Bump(20 * K, 116 * K)
        Xst = [a1([128, 2, 1024], F32) for _ in range(2)]
        hbf = [a1([128, 2, 1024], BF16) for _ in range(2)]
        hT = a1([128, 8, S], BF16)
        wbuf = [a1([128, 8, 256], BF16) for _ in range(2)]
        junk = a1([128, 1024], F32)
        kX = [Tk(), Tk()]
        kH = [Tk(), Tk()]
        khT = [Tk() for _ in range(8)]
        kW = [Tk(), Tk()]
        kJ = Tk()
        kSt = Tk()
        kUc, kq, kk, kv = Tk(), Tk(), Tk(), Tk()
        xv = x[b].rearrange("(c t) d -> c t d", t=16)
        ss = stat[:, 0:16]
        rs = stat[:, 16:32]
        def a1_s0(tp):
            i = tp % 2
            fw.dma(SY, Xst[i], xv[:, 2 * tp:2 * tp + 2, :], sem_x[i], writes=[kX[i]])
            for u in range(2):
                t = 2 * tp + u
                fw.op(A, ttr(junk, Xst[i][:, u, :], Xst[i][:, u, :], ss[:, t:t + 1]), [kX[i]], [kJ, kSt])
                fw.op(V, ts(rs[:, t:t + 1], ss[:, t:t + 1], 1.0 / D, ALU.mult, EPS, ALU.add), [kSt], [kSt])
                fw.op(A, act(rs[:, t:t + 1], rs[:, t:t + 1], AF.Ln), [kSt], [kSt])
                fw.op(A, act(rs[:, t:t + 1], rs[:, t:t + 1], AF.Exp, scale=-0.5), [kSt], [kSt])
                fw.op(A, act(hbf[i][:, u, :], Xst[i][:, u, :], AF.Copy, scale=rs[:, t:t + 1]), [kX[i], kSt], [kH[i]])

        def a1_s1(tp):
            i = tp % 2
            for k in range(8):
                pi = k % 4
                pv = pbanks[pi][:, 0:128].bitcast(BF16).rearrange("p (u c) -> p u c", u=2)
                fns = [trf(pv[:, u, :], hbf[i][:, u, k * 128:(k + 1) * 128], idb) for u in range(2)]
                fw.mm(fns, [kH[i], kC], [pk[pi]])
                dst = hT[:, k, :].rearrange("p (c t) -> p t c", t=16)[:, 2 * tp:2 * tp + 2, :]
                if k % 2 == 0:
                    fw.op(V, ts(dst, pv, g1[:, k:k + 1], ALU.mult), [pk[pi], kC], [khT[k]])
                else:
                    fw.op(A, act(dst, pv, AF.Copy, scale=g1[:, k:k + 1]), [pk[pi], kC], [khT[k]])

        skew(8, [a1_s0, a1_s1])
        win_v = w_in.rearrange("(k p) n -> p k n", p=128)
        pcount = 0
        for piece in range(8):
            i = piece % 2
            fw.dma(G_, wbuf[i], win_v[:, :, piece * 256:(piece + 1) * 256], sem_wq[i], writes=[kW[i]])
            if piece < 2:
                for t in range(16):
                    pi = 4 + (pcount % 4)
                    pcount += 1
                    pv = pbanks[pi][:, 0:256]
                    fns = [mmf(pv, hT[:, k, t::16], wbuf[i][:, k, :], k == 0, k == 7) for k in range(8)]
                    fw.mm(fns, khT + [kW[i]], [pk[pi]])
                    dst = Uc[:, 16 * piece:16 * piece + 16, t, :]
                    srcv = pv.rearrange("p (g h) -> p g h", g=16)
                    if t % 2 == 0:
                        fw.op(V, cp(dst, srcv), [pk[pi]], [kUc])
                    else:
                        fw.op(A, act(dst, srcv, AF.Copy), [pk[pi]], [kUc])
            elif piece < 6:
                dstT, kd, sc = (qT, kq, 0.125) if piece < 4 else (kT, kk, 1.0)
                for mm_ in range(2):
                    m = (piece % 2) * 2 + mm_
                    for n in range(4):
                        pi = 4 + (pcount % 4)
                        pcount += 1
                        pv = pbanks[pi][:, :]
                        fns = [mmf(pv, wbuf[i][:, k, mm_ * 128:(mm_ + 1) * 128], hT[:, k, n * 512:(n + 1) * 512],
                                   k == 0, k == 7) for k in range(8)]
                        fw.mm(fns, khT + [kW[i]], [pk[pi]])
                        dst = dstT[:, m, n * 512:(n + 1) * 512]
                        if n % 2 == 0:
                            fw.op(V, ts(dst, pv, sc, ALU.mult), [pk[pi]], [kd])
                        else:
                            fw.op(A, act(dst, pv, AF.Copy, scale=sc), [pk[pi]], [kd])
            else:
                hh = piece - 6
                for it in range(16):
                    pi = 4 + (pcount % 4)
                    pcount += 1
                    pv = pbanks[pi][:, 0:256]
                    fns = [mmf(pv, hT[:, k, it * 128:(it + 1) * 128], wbuf[i][:, k, :], k == 0, k == 7)
                           for k in range(8)]
                    fw.mm(fns, khT + [kW[i]], [pk[pi]])
                    dst = Vt[:, it, 4 * hh:4 * hh + 4, 0:64]
                    src = pv.rearrange("p (h d) -> p h d", h=4)
                    if it % 2 == 0:
                        fw.op(V, cp(dst, src), [pk[pi]], [kv])
                    else:
                        fw.op(A, act(dst, src, AF.Copy), [pk[pi]], [kv])
        fw.op(G_, lambda E: E.memset(Vt[:, :, :, 64:65], 1.0), [kv], [kv])
        if dbg and b == 0:
            fw.dma(SY, dbg_t['uc'], Uc, sem_d, [kUc], [kOut])
            fw.dma(SY, dbg_t['qT'], qT, sem_d, [kq], [kOut])
            fw.dma(SY, dbg_t['vt'], Vt, sem_d, [kv], [kOut])
        fw.barrier()

        if STAGE < 2:
            raise _Stop()
        a2 = Bump(181 * K, ARENA)
        Sbf = a2([128, 2, 16, 130], BF16)
        a2b = Bump(20 * K, 84 * K)
        UT = a2b([128, 32, 2, 128], BF16)
        Wf = a2b([128, 2, 16, 128], F32)
        GTb = [a2b([128, 2, 2, 2, 128], BF16) for _ in range(2)]
        T1 = a2b([128, 2, 16], F32)
        T2 = a2b([128, 2, 16], F32)
        kUT, kWf, kSb, kT1, kT2 = Tk(), Tk(), Tk(), Tk(), Tk()
        kGTb = [Tk(), Tk()]
        for g in range(32):
            pi = g % 4
            pv = pbanks[pi][:, 0:128].bitcast(BF16).rearrange("p (j c) -> p j c", j=2)
            fns = [trf(pv[:, j, :], Uc[:, g, 8 * j:8 * j + 8, :].rearrange("p s h -> p (s h)"), idb) for j in range(2)]
            fw.mm(fns, [kUc, kC], [pk[pi]])
            if g % 2 == 0:
                fw.op(V, cp(UT[:, g], pv), [pk[pi]], [kUT])
            else:
                fw.op(A, act(UT[:, g], pv, AF.Copy), [pk[pi]], [kUT])
        if STAGE < 2.2:
            raise _Stop()
        for gl in range(16):
            i = gl % 2
            for gh in range(2):
                fw.dma(SY, GTb[i][:, gh], GT_d[16 * gh + gl], sem_w[i], writes=[kGTb[i]])
            pi = 4 + gl % 4
            pv = pbanks[pi][:, 0:256].rearrange("p (r c) -> p r c", r=2)
            fns = []
            for r in range(2):
                n = 0
                for gh in range(2):
                    for j in range(2):
                        fns.append(mmf(pv[:, r, :], GTb[i][:, gh, j, r, :], UT[:, 16 * gh + gl, j, :], n == 0, n == 3))
                        n += 1
            fw.mm(fns, [kUT, kGTb[i]], [pk[pi]])
            fw.op(V, cp(Wf[:, :, gl, :], pv), [pk[pi]], [kWf])
        if STAGE < 2.5:
            raise _Stop()
        WfB = reg(116 * K, [128, 2, 16, 128], F32)
        TT = reg(190 * K, [128, 2, 16, 128], F32)
        kWB, kTT = kUc, Tk()
        ks = {'cur': Wf, 'nxt': WfB, 'kcur': kWf, 'knxt': kWB}

        def ks_ops():
            for k in range(7):
                d = 1 << k
                n = 128 - d
                cur, nxt, kcur, knxt = ks['cur'], ks['nxt'], ks['kcur'], ks['knxt']
                fw.op(V, tt(TT[:, :, :, 0:n], cur[:, :, :, 0:n], bc(LAk[:, k], 3, [128, 2, 16, n]), ALU.mult),
                      [kcur, kC], [kTT])
                yield
                fw.op(V, tt(nxt[:, :, :, d:128], cur[:, :, :, d:128], TT[:, :, :, 0:n], ALU.add), [kcur, kTT], [knxt])
                yield
                fw.op(V, tt(TT[:, 0, :, 0:n], cur[:, 1, :, 0:n], bc(LIn[:, k, :], 2, [128, 16, n]), ALU.mult),
                      [kcur, kC], [kTT])
                yield
                fw.op(V, tt(TT[:, 1, :, 0:n], cur[:, 0, :, 0:n], bc(LI[:, k, :], 2, [128, 16, n]), ALU.mult),
                      [kcur, kC], [kTT])
                yield
                fw.op(V, tt(nxt[:, :, :, d:128], nxt[:, :, :, d:128], TT[:, :, :, 0:n], ALU.add), [knxt, kTT], [knxt])
                yield
                fw.op(V, cp(nxt[:, :, :, 0:d], cur[:, :, :, 0:d]), [kcur], [knxt])
                ks['cur'], ks['nxt'], ks['kcur'], ks['knxt'] = nxt, cur, knxt, kcur
                yield

        ks_gen = ks_ops()

        def ks_finish():
            Wfin, kWfin = ks['cur'], ks['kcur']
            fw.op(V, lambda E: E.memset(Sbf[:, :, :, 0:2], 0.0), [], [kSb])
            for r in range(2):
                fw.op(A, act(Sbf[:, r, :, 2:130], Wfin[:, r], AF.Copy), [kWfin], [kSb])
            if dbg and b == 0:
                fw.dma(SY, dbg_t['S'], Wfin, sem_d, [kWfin], [kOut])

        if STAGE < 3:
            raise _Stop()
        a3 = Bump(52 * K, 84 * K)
        MBT = a3([128, S], BF16)
        kmT = a3([128, 4, 8], BF16)
        kmf = a3([128, 4, 8], F32)
        gt = a3([128, 8, 8], F32)
        top8 = a3([128, 8, 8], F32)
        mbt = a3([128, 2, 64], BF16)
        PT = [a3([128, 256], BF16) for _ in range(3)]
        Ao = a3([128, 2, 512], F32)
        Aj = a3([128, 512], F32)
        Ab = a3([128, 2, 512], BF16)
        rden_a = a3([128, 4], F32)
        kMB, kkm, kgt, kmb = Tk(), Tk(), Tk(), Tk()
        Esel = reg(100 * K, [128, 64, 128], BF16)
        kEs = Tk()
        fw.op(V, cp(Esel[0:64], bc(idb[0:64, 0:64], 2, [64, 64, 128])), [kC], [kEs])
        fw.op(V, cp(Esel[64:128], bc(idb[64:128, 64:128], 2, [64, 64, 128])), [kC], [kEs])
        kPT = [Tk() for _ in range(3)]
        kAo, kAj, kAb, kat, krd = Tk(), Tk(), Tk(), Tk(), Tk()
        if STAGE < 3.05:
            raise _Stop()
        kjunk = a3([128, 256], BF16)
        kkj = Tk()
        for m in range(4):
            for n in range(8):
                fw.op(A, (lambda m=m, n=n: (lambda E: E.activation(out=kjunk, in_=kT[:, m, n * 256:(n + 1) * 256],
                                                                   func=AF.Copy, accum_out=kmf[:, m, n:n + 1])))(),
                      [kk], [kkj, kkm])
        if STAGE < 3.08:
            raise _Stop()
        fw.op(A, act(kmT, kmf, AF.Copy, scale=1.0 / 256), [kkm], [kkm])
        if STAGE < 3.1:
            raise _Stop()
        fw.op(G_, lambda E: E.memset(MBT, 0.0), [], [kMB, kGTb[0], kGTb[1]])
        for qt in range(8, 16):
            bq = qt // 2
            pve = pbanks[0][:, 0:32].rearrange("p (h n) -> p h n", h=4)
            pvo = pbanks[1][:, 0:32].rearrange("p (h n) -> p h n", h=4)
            fe = [mmf(pve[:, h2, :], qT[0:64, h2, qt * 128:(qt + 1) * 128], kmT[0:64, h2, :], True, True)
                  for h2 in range(4)]
            fw.mm(fe, [kq, kkm], [pk[0]])
            fo = [mmf(pvo[:, h2, :], qT[64:128, h2, qt * 128:(qt + 1) * 128], kmT[64:128, h2, :], True, True)
                  for h2 in range(4)]
            fw.mm(fo, [kq, kkm], [pk[1]])
            fw.op(G_, lambda E: E.memset(gt, NEG), [kgt], [kgt])
            gtv = gt.rearrange("p (h2 e) n -> p h2 e n", e=2)
            fw.op(V, cp(gtv[:, :, 0, 0:bq], pve[:, :, 0:bq]), [pk[0], kgt], [kgt])
            fw.op(V, cp(gtv[:, :, 1, 0:bq], pvo[:, :, 0:bq]), [pk[1], kgt], [kgt])
            for h in range(8):
                fw.op(V, (lambda hh: (lambda E: E.max(out=top8[:, hh, :], in_=gt[:, hh, :])))(h), [kgt], [kmb])
            for dup in range(2):
                fw.op(V, tt(mbt[:, dup, :].rearrange("p (h n) -> p h n", h=8), gt,
                            top8[:, :, 2:3].broadcast_to([128, 8, 8]), ALU.is_lt), [kgt, kmb], [kmb])
            pi2 = 2 + qt % 2
            pv2 = pbanks[pi2][:, 0:64].bitcast(BF16)
            fw.mm([trf(pv2, mbt.rearrange("p d c -> p (d c)"), idb)], [kmb, kC], [pk[pi2]])
            fw.op(V, ts(MBT[:, qt * 128:(qt + 1) * 128], pv2, NEG, ALU.mult), [pk[pi2]], [kMB])
        if STAGE < 3.2:
            raise _Stop()
        ucount = 0
        LAG = 2
        for bq in range(ABQ):
            nkt = 2 * bq + 2

            def stage_a(h, kt, pi, bq=bq):
                pr = slice(64 * (h % 2), 64 * (h % 2) + 64)
                m = h // 2
                n = kt // 2
                pv = pbanks[pi][:, 0:256]
                fns = [mmf(pv, kT[pr, m, kt * 128:(kt + 1) * 128], qT[pr, m, bq * 256:(bq + 1) * 256], True, False)]
                rd = [kq, kk]
                if n == bq:
                    fns.append(mmf(pv, idb, cbias[:, kt - 2 * bq, :], False, True))
                    rd.append(kC)
                elif bq >= 4:
                    r = h * 8 + n
                    fns.append(mmf(pv, Esel[pr, r, :], MBT[pr, bq * 256:(bq + 1) * 256], False, True))
                    rd += [kEs, kMB]
                else:
                    fns[0] = mmf(pv, kT[pr, m, kt * 128:(kt + 1) * 128], qT[pr, m, bq * 256:(bq + 1) * 256], True, True)
                fw.mm(fns, rd, [pk[pi]])
                fw.op(A, act(PT[pi], pv, AF.Exp), [pk[pi]], [kPT[pi]])

            def stage_b(h, kt, pi, nkt=nkt):
                pos = [4 + 2 * (h % 2), 5 + 2 * (h % 2)]
                povs = [pbanks[pos[0]][:, 0:65], pbanks[pos[1]][:, 0:65]]
                fns = [mmf(povs[u], PT[pi][:, u * 128:(u + 1) * 128], Vt[:, kt, h, :], kt == 0, kt == nkt - 1)
                       for u in range(2)]
                fw.mm(fns, [kPT[pi], kv], [pk[pos[0]], pk[pos[1]]])
                if kt == nkt - 1:
                    for u in range(2):
                        fw.op(V, rcp(rden_a[:, u:u + 1], povs[u][:, 64:65]), [pk[pos[u]]], [krd])
                        fw.op(V, ts(Ao[:, u, h * 64:(h + 1) * 64], povs[u][:, 0:64], rden_a[:, u:u + 1], ALU.mult),
                              [pk[pos[u]], krd], [kAo])

            pend = []
            for h in range(8):
                for kt in range(nkt):
                    pi = ucount % 3
                    ucount += 1
                    stage_a(h, kt, pi)
                    pend.append((h, kt, pi))
                    if ucount % 12 == 0:
                        next(ks_gen, None)
                    if len(pend) > LAG:
                        stage_b(*pend.pop(0))
            while pend:
                stage_b(*pend.pop(0))
            if bq == 7:
                for _ in ks_gen:
                    pass
                ks_finish()
            for u in range(2):
                fw.op(A, ttr(Aj, Ao[:, u, :], Ao[:, u, :], rden_a[:, 2:3]), [kAo], [kAj, krd])
                fw.op(V, ts(rden_a[:, 3:4], rden_a[:, 2:3], 1.0 / 512, ALU.mult, EPS, ALU.add), [krd], [krd])
                fw.op(A, act(rden_a[:, 3:4], rden_a[:, 3:4], AF.Ln), [krd], [krd])
                fw.op(A, act(rden_a[:, 3:4], rden_a[:, 3:4], AF.Exp, scale=-0.5), [krd], [krd])
                fw.op(V, stt(Ab[:, u, :], Ao[:, u, :], rden_a[:, 3:4], ga, ALU.mult, ALU.mult), [kAo, krd, kC], [kAb])
            for u in range(2):
                pi = 3
                pv = pbanks[pi][:, 0:256].bitcast(BF16).rearrange("p (f c) -> p f c", f=4)
                fns = [trf(pv[:, f, :], Ab[:, u, f * 128:(f + 1) * 128], idb) for f in range(4)]
                fw.mm(fns, [kAb, kC], [pk[pi]])
                tok0 = bq * 256 + u * 128
                fw.op(A, act(attT[:, :, tok0:tok0 + 128], pv, AF.Copy), [pk[pi]], [kat])
        if dbg and b == 0:
            fw.dma(SY, dbg_t['attT'], attT, sem_d, [kat], [kOut])
        fw.barrier()

        if STAGE < 4:
            raise _Stop()
        a4 = Bump(132 * K, 181 * K)
        Y1 = a4([128, 16, 512], BF16)
        TCb = [(a4([128, 2, 2, 256], BF16), a4([128, 2, 256], BF16)) for _ in range(2)]
        wgl = a4([128, 4, 512], BF16)
        gb = Bump(36 * K, 84 * K)
        NB = 4
        Y1T = [gb([128, 4, 128], BF16) for _ in range(NB)]
        zt = [gb([128, 512], F32) for _ in range(NB)]
        zs = [gb([128, 512], F32) for _ in range(NB)]
        y2 = [gb([128, 512], F32) for _ in range(NB)]
        y2b = [gb([128, 512], BF16) for _ in range(NB)]
        kY1, kwg, kssm = Tk(), Tk(), Tk()
        kzt, kzs, ky2, ky2b, kss = [[Tk() for _ in range(NB)] for _ in range(5)]
        kTC = [Tk(), Tk()]
        kY1T = [Tk() for _ in range(NB)]
        fw.dma(G_, wgl, w_glu.rearrange("(k p) n -> p k n", p=128), sem_og, writes=[kwg])
        C1 = 0.7978845608028654 * 2.0
        for gl in range(16):
            i = gl % 2
            for gh in range(2):
                fw.dma(SY, TCb[i][0][:, gh], TOEP_d[16 * gh + gl], sem_w[i], writes=[kTC[i]])
            fw.dma(SY, TCb[i][1], CT_d[gl], sem_w[i], writes=[kTC[i]])
            for gh in range(2):
                g = 16 * gh + gl
                pr = slice(64 * gh, 64 * gh + 64)
                pi = (2 * gl + gh) % 4
                pv = pbanks[pi][:, 0:256]
                fns = [mmf(pv, UT[:, g, 0, :], TCb[i][0][:, gh, 0, :], True, False),
                       mmf(pv, UT[:, g, 1, :], TCb[i][0][:, gh, 1, :], False, False),
                       mmf(pv, Sbf[pr, 0, gl, 1:129], TCb[i][1][pr, 0, :], False, False),
                       mmf(pv, Sbf[pr, 1, gl, 1:129], TCb[i][1][pr, 1, :], False, True)]
                fw.mm(fns, [kUT, kSb, kTC[i]], [pk[pi]])
                fw.op(A, act(Y1[:, :, 16 * g:16 * g + 16], pv.rearrange("p (t h) -> p t h", t=16), AF.Gelu_apprx_tanh),
                      [pk[pi]], [kY1])
        if dbg and b == 0:
            fw.dma(SY, dbg_t['y1'], Y1, sem_d, [kY1], [kOut])
        def glu_s0(t):
            i = t % NB
            pi = t % 2
            pv = pbanks[pi][:, 0:256].bitcast(BF16).rearrange("p (f c) -> p f c", f=4)
            fns = [trf(pv[:, f, :], Y1[:, t, f * 128:(f + 1) * 128], idb) for f in range(4)]
            fw.mm(fns, [kY1, kC], [pk[pi]])
            fw.op(A, act(Y1T[i], pv, AF.Copy), [pk[pi]], [kY1T[i]])
            pz = 2 + t % 4
            pzv = pbanks[pz][:, :]
            fns = [mmf(pzv, Y1T[i][:, f, :], wgl[:, f, :], f == 0, f == 3) for f in range(4)]
            fw.mm(fns, [kY1T[i], kwg], [pk[pz]])

        def glu_s1(t):
            i = t % NB
            pz = 2 + t % 4
            pzv = pbanks[pz][:, :]
            s0 = stat[:, 32 + 2 * i:33 + 2 * i]
            fw.op(V, tt(zt[i], pzv, bg, ALU.add), [pk[pz], kC], [kzt[i]])
            fw.op(A, act(zs[i], zt[i], AF.Exp, scale=-1.0), [kzt[i]], [kzs[i]])
            fw.op(V, ts(zs[i], zs[i], 1.0, ALU.add), [kzs[i]], [kzs[i]])
            fw.op(V, rcp(zs[i], zs[i]), [kzs[i]], [kzs[i]])
            fw.op(V, tt(y2[i], zs[i], Y1[:, t, :], ALU.mult), [kzs[i], kY1], [ky2[i]])
            fw.op(A, ttr(zt[i], y2[i], y2[i], s0), [ky2[i], kzt[i]], [kzt[i], kss[i]])

        def glu_s2(t):
            i = t % NB
            s0, s1 = stat[:, 32 + 2 * i:33 + 2 * i], stat[:, 33 + 2 * i:34 + 2 * i]
            fw.op(V, ts(s1, s0, 1.0 / 512, ALU.mult, EPS, ALU.add), [kss[i]], [kss[i]])
            fw.op(A, act(s1, s1, AF.Ln), [kss[i]], [kss[i]])
            fw.op(A, act(s1, s1, AF.Exp, scale=-0.5), [kss[i]], [kss[i]])
            fw.op(V, stt(y2b[i], y2[i], s1, gs, ALU.mult, ALU.mult), [ky2[i], kss[i], kC], [ky2b[i]])

        def glu_s3(t):
            i = t % NB
            pi2 = 6 + t % 2
            pv2 = pbanks[pi2][:, 0:256].bitcast(BF16).rearrange("p (f c) -> p f c", f=4)
            fns = [trf(pv2[:, f, :], y2b[i][:, f * 128:(f + 1) * 128], idb) for f in range(4)]
            fw.mm(fns, [ky2b[i], kC], [pk[pi2]])
            fw.op(A, act(ssmT[:, :, t, :], pv2, AF.Copy), [pk[pi2]], [kssm])

        skew(16, [glu_s0, glu_s1, glu_s2, glu_s3])
        if dbg and b == 0:
            fw.dma(SY, dbg_t['ssmT'], ssmT, sem_d, [kssm], [kOut])
        fw.barrier()

        if STAGE < 5:
            raise _Stop()
        a5 = Bump(116 * K, ARENA)
        wo = a5([128, 8, 1024], BF16)
        otmp = [a5([128, 1024], F32) for _ in range(4)]
        oj = a5([128, 512], F32)
        kwo, koj = Tk(), Tk()
        kot = [Tk() for _ in range(4)]
        kst4 = [Tk() for _ in range(4)]
        kX1 = [Tk() for _ in range(16)]
        fw.dma(G_, wo, w_out.rearrange("(k p) n -> p k n", p=128), sem_og, writes=[kwo])
        for q4 in range(4):
            fw.dma(SY, X1[:, 4 * q4:4 * q4 + 4, :], xv[:, 4 * q4:4 * q4 + 4, :], sem_x[q4],
                   writes=[kX1[4 * q4 + j] for j in range(4)])
        def a4_s0(t):
            pis = [2 * (t % 4), 2 * (t % 4) + 1]
            for hf in range(2):
                pv = pbanks[pis[hf]][:, :]
                fns = []
                for k in range(8):
                    lhs = ssmT[:, k, t, :] if k < 4 else attT[:, k - 4, t::16]
                    fns.append(mmf(pv, lhs, wo[:, k, hf * 512:(hf + 1) * 512], k == 0, k == 7))
                fw.mm(fns, [kssm, kat, kwo], [pk[pis[hf]]])
                sl4 = t % 4
                c0 = 36 + 3 * sl4
                fw.op(A, sqa(oj, pv, stat[:, c0 + hf:c0 + hf + 1]), [pk[pis[hf]]], [kst4[sl4], koj])

        def a4_s1(t):
            pis = [2 * (t % 4), 2 * (t % 4) + 1]
            sl4 = t % 4
            c0 = 36 + 3 * sl4
            rs4 = stat[:, c0 + 2:c0 + 3]
            fw.op(V, tt(rs4, stat[:, c0:c0 + 1], stat[:, c0 + 1:c0 + 2], ALU.add), [kst4[sl4]], [kst4[sl4]])
            fw.op(V, ts(rs4, rs4, 1.0 / D, ALU.mult, EPS, ALU.add), [kst4[sl4]], [kst4[sl4]])
            fw.op(A, act(rs4, rs4, AF.Ln), [kst4[sl4]], [kst4[sl4]])
            fw.op(A, act(rs4, rs4, AF.Exp, scale=-0.5), [kst4[sl4]], [kst4[sl4]])
            for hf in range(2):
                pv = pbanks[pis[hf]][:, :]
                sl = slice(hf * 512, (hf + 1) * 512)
                fw.op(V, stt(otmp[sl4][:, sl], pv, rs4, g2[:, sl], ALU.mult, ALU.mult),
                      [pk[pis[hf]], kst4[sl4], kC], [kot[sl4]])
            fw.op(G_, tt(X1[:, t, :], X1[:, t, :], otmp[sl4], ALU.add), [kot[sl4], kX1[t]], [kX1[t]])

        skew(16, [a4_s0, a4_s1])
        if dbg and b == 0:
            fw.dma(SY, dbg_t['x1'], X1, sem_d, kX1, [kOut])
        fw.barrier()

        if STAGE < 6:
            raise _Stop()
        f0 = Bump(86 * K, ARENA)
        h2T = f0([128, 8, 16, 128], BF16)
        fT = f0([128, NFF, 16, 128], BF16)
        hb2 = [reg(118 * K + 4096 + 2048 * i_, [128, 1024], BF16) for i_ in range(2)]
        kfj, kh2T, kfT = Tk(), Tk(), Tk()
        khb = [Tk(), Tk()]
        kst5 = [Tk(), Tk()]
        ov = out[b].rearrange("(c t) d -> c t d", t=16)
        fj1 = reg(118 * K, [128, 1024], F32)
        def f1_s0(t):
            i5 = t % 2
            q0, q1 = stat[:, 52 + 2 * i5:53 + 2 * i5], stat[:, 53 + 2 * i5:54 + 2 * i5]
            fw.op(A, ttr(fj1, X1[:, t, :], X1[:, t, :], q0), [kX1[t]], [kst5[i5], kfj])
            fw.op(V, ts(q1, q0, 1.0 / D, ALU.mult, EPS, ALU.add), [kst5[i5]], [kst5[i5]])
            fw.op(A, act(q1, q1, AF.Ln), [kst5[i5]], [kst5[i5]])
            fw.op(A, act(q1, q1, AF.Exp, scale=-0.5), [kst5[i5]], [kst5[i5]])
            fw.op(A, act(hb2[i5], X1[:, t, :], AF.Copy, scale=q1), [kX1[t], kst5[i5]], [khb[i5]])

        def f1_s1(t):
            i5 = t % 2
            for kk_ in range(2):
                pi = kk_ + 2 * (t % 2)
                pv = pbanks[pi][:, 0:256].bitcast(BF16).rearrange("p (f c) -> p f c", f=4)
                fns = [trf(pv[:, f, :], hb2[i5][:, (4 * kk_ + f) * 128:(4 * kk_ + f + 1) * 128], idb) for f in range(4)]
                fw.mm(fns, [khb[i5], kC], [pk[pi]])
                for f in range(4):
                    k = 4 * kk_ + f
                    if f % 2 == 0:
                        fw.op(V, ts(h2T[:, k, t, :], pv[:, f, :], g3[:, k:k + 1], ALU.mult), [pk[pi], kC], [kh2T])
                    else:
                        fw.op(A, act(h2T[:, k, t, :], pv[:, f, :], AF.Copy, scale=g3[:, k:k + 1]),
                              [pk[pi], kC], [kh2T])

        skew(16, [f1_s0, f1_s1])
        kOt = [Tk() for _ in range(16)]
        fw.dma(SY, ov, X1, sem_o, kX1, kOt)
        fw.barrier()
        f2 = Bump(64 * K, 86 * K)
        wg = [f2([128, 8, 128], BF16) for _ in range(2)]
        wu = [f2([128, 8, 128], BF16) for _ in range(2)]
        sg = [f2([128, 512], F32) for _ in range(2)]
        wd = reg(20 * K, [128, NFF, 1024], BF16)
        kwg = [Tk(), Tk()]
        kwd = Tk()
        ksg = [Tk(), Tk()]
        wg_v = w_gate.rearrange("(k p) n -> p k n", p=128)
        wu_v = w_up.rearrange("(k p) n -> p k n", p=128)
        wd_v = w_down.rearrange("(j p) n -> p j n", p=128)
        hv = h2T.rearrange("p k t c -> p k (t c)")
        pc = 0
        for j in range(NFF):
            i = j % 2
            fw.dma(G_, wg[i], wg_v[:, :, j * 128:(j + 1) * 128], sem_g[i], writes=[kwg[i]])
            fw.dma(G_, wu[i], wu_v[:, :, j * 128:(j + 1) * 128], sem_g[i], writes=[kwg[i]])
            fw.dma(G_, wd[:, j, :], wd_v[:, j, :], sem_dn, writes=[kwd])
            for n in range(4):
                pg_, pu_ = 2 * (pc % 4), 2 * (pc % 4) + 1
                si = pc % 2
                pc += 1
                pgv, puv = pbanks[pg_][:, :], pbanks[pu_][:, :]
                fns = [mmf(pgv, wg[i][:, k, :], hv[:, k, n * 512:(n + 1) * 512], k == 0, k == 7) for k in range(8)]
                fw.mm(fns, [kh2T, kwg[i]], [pk[pg_]])
                fns = [mmf(puv, wu[i][:, k, :], hv[:, k, n * 512:(n + 1) * 512], k == 0, k == 7) for k in range(8)]
                fw.mm(fns, [kh2T, kwg[i]], [pk[pu_]])
                fw.op(A, act(sg[si], pgv, AF.Silu), [pk[pg_]], [ksg[si]])
                fw.op(V, tt(fT[:, j].rearrange("p t c -> p (t c)")[:, n * 512:(n + 1) * 512], sg[si], puv, ALU.mult),
                      [ksg[si], pk[pu_]], [kfT])
        fw.barrier()
        f3 = Bump(64 * K, 118 * K)
        xr = [f3([128, 1024], F32) for _ in range(4)]
        ot2 = [f3([128, 1024], F32) for _ in range(2)]
        fj3 = f3([128, 512], F32)
        kxr = [Tk() for _ in range(4)]
        kot2 = [Tk(), Tk()]
        kst6 = [Tk(), Tk()]
        for grp in range(8):
            base = 4 * (grp % 2)
            for tl in range(2):
                t = 2 * grp + tl
                fw.dma(SY, xr[t % 4], ov[:, t, :], sem_x[t % 4], [kOt[t]], [kxr[t % 4]])
            for j in range(NFF):
                fns = []
                for tl in range(2):
                    t = 2 * grp + tl
                    for hf in range(2):
                        fns.append(mmf(pbanks[base + 2 * tl + hf][:, :], fT[:, j, t, :], wd[:, j, hf * 512:(hf + 1) * 512],
                                       j == 0, j == NFF - 1))
                fw.mm(fns, [kfT, kwd], [pk[base + q_] for q_ in range(4)])
            for tl in range(2):
                t = 2 * grp + tl
                xi = t % 4
                i6 = t % 2
                c0 = 56 + 3 * i6
                rs6 = stat[:, c0 + 2:c0 + 3]
                for hf in range(2):
                    bk = base + 2 * tl + hf
                    fw.op(A, sqa(fj3, pbanks[bk][:, :], stat[:, c0 + hf:c0 + hf + 1]), [pk[bk]], [kst6[i6], kfj])
                fw.op(V, tt(rs6, stat[:, c0:c0 + 1], stat[:, c0 + 1:c0 + 2], ALU.add), [kst6[i6]], [kst6[i6]])
                fw.op(V, ts(rs6, rs6, 1.0 / D, ALU.mult, EPS, ALU.add), [kst6[i6]], [kst6[i6]])
                fw.op(A, act(rs6, rs6, AF.Ln), [kst6[i6]], [kst6[i6]])
                fw.op(A, act(rs6, rs6, AF.Exp, scale=-0.5), [kst6[i6]], [kst6[i6]])
                for hf in range(2):
                    bk = base + 2 * tl + hf
                    sl = slice(hf * 512, (hf + 1) * 512)
                    fw.op(V, stt(ot2[i6][:, sl], pbanks[bk][:, :], rs6, g4[:, sl], ALU.mult, ALU.mult),
                          [pk[bk], kst6[i6], kC], [kot2[i6]])
                fw.op(G_, tt(xr[xi], xr[xi], ot2[i6], ALU.add), [kot2[i6], kxr[xi]], [kxr[xi]])
                fw.dma(SY, ov[:, t, :], xr[xi], sem_st[t % 4], [kxr[xi]], [kOt[t]])
        fw.barrier()
    except _Stop:
        pass
    fw.barrier()
    fw.emit()
    return nc, es


_CACHE = {}


def _consts():
    idf = np.eye(128, dtype=np.float32)
    cb = np.zeros((128, 2, 256), np.float32)
    for r in range(2):
        kpos = r * 128 + np.arange(128)[:, None]
        qpos = np.arange(256)[None, :]
        cb[:, r, :] = np.where(kpos <= qpos, 0.0, NEG)
    tm = np.zeros((128, 2, 256), np.float32)
    dm = np.zeros((128, 2, 256), np.float32)
    for j in range(2):
        for s8 in range(8):
            for hi in range(16):
                sp = 8 * j + s8
                row = s8 * 16 + hi
                for jj in range(2):
                    pass
                for tp in range(16):
                    if tp >= sp:
                        tm[row, j, tp * 16:(tp + 1) * 16] = 1.0
                dm[row, j, sp * 16 + hi] = 1.0
    nv = np.tile(np.arange(1, 33, dtype=np.float32)[None, :], (128, 1))
    return idf, cb, tm, dm, nv


def kernel(**inp):
    f32 = np.float32
    x = np.ascontiguousarray(inp['x'], dtype=f32)

    def sq(k):
        return np.ascontiguousarray(inp[k][0], dtype=f32)

    def gp(a):
        a = a.reshape((2, 16) + a.shape[1:])
        a = np.moveaxis(a, 2, 1)
        return np.ascontiguousarray(a.reshape((128, 16) + a.shape[3:]))
    idf, cb, tm, dm, nv = _consts()
    shared = dict(
        w_in=sq('w_in'), w_glu=sq('w_glu'), w_out=sq('w_out'), w_gate=sq('w_gate'), w_up=sq('w_up'),
        w_down=sq('w_down'),
        areT=gp(sq('ssm_a_re')), aimT=gp(sq('ssm_a_im')),
        ldtT=gp(np.ascontiguousarray(np.broadcast_to(sq('ssm_log_dt')[:, None], (32, 64)))),
        breT=gp(sq('ssm_b_re')), bimT=gp(sq('ssm_b_im')),
        creT=gp(np.ascontiguousarray(sq('ssm_c_re').transpose(0, 2, 1))),
        cimT=gp(np.ascontiguousarray(sq('ssm_c_im').transpose(0, 2, 1))),
        drep=np.ascontiguousarray(np.broadcast_to(sq('ssm_d')[None], (128, 32, 16))),
        g1c=np.ascontiguousarray(sq('g_pre_mix').reshape(8, 128).T),
        g3c=np.ascontiguousarray(sq('g_pre_ffn').reshape(8, 128).T),
        g2r=np.ascontiguousarray(np.broadcast_to(sq('g_post_mix')[None], (128, 1024))),
        g4r=np.ascontiguousarray(np.broadcast_to(sq('g_post_ffn')[None], (128, 1024))),
        gsr=np.ascontiguousarray(np.broadcast_to(sq('g_ssm_out')[None], (128, 512))),
        gar=np.ascontiguousarray(np.broadcast_to(sq('g_attn_out')[None], (128, 512))),
        bgr=np.ascontiguousarray(np.broadcast_to(sq('b_glu')[None], (128, 512))),
        c_idf=idf, c_cb=cb, c_tm=tm, c_dm=dm, c_nv=nv,
    )
    if 'nc' not in _CACHE:
        _CACHE['nc'] = build(DEBUG)
    nc, _es = _CACHE['nc']
    in_maps = []
    for c in range(8):
        m = dict(shared)
        m['x'] = np.ascontiguousarray(x[2 * c:2 * c + 2])
        in_maps.append(m)
    res = run_bass_kernel_spmd(nc, in_maps, core_ids=list(range(8)))
    _CACHE['res'] = res
    return np.concatenate([np.asarray(r['out'], dtype=f32) for r in res.results], axis=0)
```

```python
import math
import numpy as np
from contextlib import ExitStack
import concourse.bass as bass
import concourse.mybir as mybir
from concourse.alu_op_type import AluOpType as ALU
from concourse.bass_utils import run_bass_kernel_spmd

F32 = mybir.dt.float32
BF16 = mybir.dt.bfloat16
U8 = mybir.dt.uint8
I32 = mybir.dt.int32
AF = mybir.ActivationFunctionType
AX = mybir.AxisListType

S = 2048
D = 1024
DFF = 2816
NFF = 22
NEG = -30000.0
EPS = 1e-6
ENGS = ['tensor', 'vector', 'scalar', 'gpsimd', 'sync']
DEBUG = False
STAGE = 99
NSTEP = 127
ABQ = 8


class _Stop(Exception):
    pass


class Tk:
    __slots__ = ('w', 'r')

    def __init__(self):
        self.w = {}
        self.r = {}


class FW:
    def __init__(self, nc, es):
        self.nc, self.es = nc, es
        self.prog = {e: [] for e in ENGS}
        self.esem = {e: es.enter_context(nc.semaphore("es_" + e)) for e in ENGS}
        self.ecnt = {e: 0 for e in ENGS}
        self.seen = {e: {} for e in ENGS}
        self.dcnt = {}

    def newsem(self, name):
        s = self.es.enter_context(self.nc.semaphore(name))
        self.dcnt[id(s)] = [s, 0]
        return s

    def _dep(self, eng, reads, writes):
        need = {}

        def add(dct):
            for k, (s, v) in dct.items():
                if need.get(k, (None, 0))[1] < v:
                    need[k] = (s, v)
        for t in reads:
            add(t.w)
        for t in writes:
            add(t.w)
            add(t.r)
        for k, (s, v) in need.items():
            if self.seen[eng].get(k, 0) >= v:
                continue
            self.seen[eng][k] = v
            self.prog[eng].append(('w', s, v))

    def _post(self, tok, reads, writes):
        s, v = tok
        k = id(s)
        for t in reads:
            if t.r.get(k, (None, 0))[1] < v:
                t.r[k] = (s, v)
        for t in writes:
            t.w = {k: (s, v)}
            t.r = {}

    def op(self, eng, fn, reads=(), writes=()):
        self._dep(eng, reads, writes)
        self.ecnt[eng] += 1
        self.prog[eng].append(('o', fn, True))
        self._post((self.esem[eng], self.ecnt[eng]), reads, writes)

    def mm(self, fns, reads=(), writes=()):
        self._dep('tensor', reads, writes)
        for f in fns[:-1]:
            self.prog['tensor'].append(('o', f, False))
        self.ecnt['tensor'] += 1
        self.prog['tensor'].append(('o', fns[-1], True))
        self._post((self.esem['tensor'], self.ecnt['tensor']), reads, writes)

    def dma(self, eng, out, in_, sem, reads=(), writes=()):
        self._dep(eng, reads, writes)
        c = self.dcnt[id(sem)]
        c[1] += 16
        self.prog[eng].append(('d', out, in_, sem))
        self._post((sem, c[1]), reads, writes)

    def barrier(self):
        for e in ENGS:
            for e2 in ENGS:
                if e2 == e or self.ecnt[e2] == 0:
                    continue
                k = id(self.esem[e2])
                if self.seen[e].get(k, 0) < self.ecnt[e2]:
                    self.seen[e][k] = self.ecnt[e2]
                    self.prog[e].append(('w', self.esem[e2], self.ecnt[e2]))
            for k, (s, v) in self.dcnt.items():
                if v > 0 and self.seen[e].get(k, 0) < v:
                    self.seen[e][k] = v
                    self.prog[e].append(('w', s, v))

    def emit(self):
        nc = self.nc
        with nc.Block() as block:
            for e in ENGS:
                prog = self.prog[e]
                sem = self.esem[e]

                def body(E, prog=prog, sem=sem):
                    for it in prog:
                        if it[0] == 'w':
                            E.wait_ge(it[1], it[2])
                        elif it[0] == 'o':
                            ins = it[1](E)
                            if it[2]:
                                ins.then_inc(sem, 1)
                        else:
                            E.dma_start(out=it[1], in_=it[2]).then_inc(it[3], 16)
                getattr(block, e)(body)


def tt(out, in0, in1, op):
    return lambda E: E.tensor_tensor(out=out, in0=in0, in1=in1, op=op)


def ts(out, in0, s1, op0, s2=None, op1=None):
    if op1 is None:
        return lambda E: E.tensor_scalar(out=out, in0=in0, scalar1=s1, scalar2=None, op0=op0)
    return lambda E: E.tensor_scalar(out=out, in0=in0, scalar1=s1, scalar2=s2, op0=op0, op1=op1)


def stt(out, in0, scalar, in1, op0, op1):
    return lambda E: E.scalar_tensor_tensor(out=out, in0=in0, scalar=scalar, in1=in1, op0=op0, op1=op1)


def ttr(out, in0, in1, accum):
    return lambda E: E.activation(out=out, in_=in0, func=AF.Square, accum_out=accum)


def act(out, in_, func, scale=None):
    if scale is None:
        return lambda E: E.activation(out=out, in_=in_, func=func)
    return lambda E: E.activation(out=out, in_=in_, func=func, scale=scale)


def sqa(out, in_, accum):
    return lambda E: E.activation(out=out, in_=in_, func=AF.Square, accum_out=accum)


def rcp(out, in_):
    return lambda E: E.reciprocal(out=out, in_=in_)


def cp(out, in_):
    return lambda E: E.tensor_copy(out=out, in_=in_)


def mmf(out, lhsT, rhs, start, stop):
    assert len(rhs.ap) == 2 and len(lhsT.ap) == 2, ("rhs", rhs.ap, lhsT.ap)
    return lambda E: E.matmul(out, lhsT, rhs, start=start, stop=stop)


def trf(out, in_, ident):
    assert len(ident.ap) == 2 and len(in_.ap) == 2, ("ident", ident.ap, in_.ap)
    return lambda E: E.transpose(out, in_, ident)


def skew(T, stages):
    ns = len(stages)
    for step in range(T + ns - 1):
        for si in range(ns):
            t = step - si
            if 0 <= t < T:
                stages[si](t)


def bc(ap, axis, shape):
    return ap.unsqueeze(axis).broadcast_to(list(shape))


def build(dbg=False):
    nc = bass.Bass("TRN2", target_bir_lowering=False)
    es = ExitStack()

    def din(name, shape):
        return nc.dram_tensor(name, list(shape), F32, kind="ExternalInput").ap()

    x = din("x", [2, S, D])
    w_in = din("w_in", [D, 2048])
    w_glu = din("w_glu", [512, 512])
    w_out = din("w_out", [1024, 1024])
    w_gate = din("w_gate", [D, DFF])
    w_up = din("w_up", [D, DFF])
    w_down = din("w_down", [DFF, D])
    areT = din("areT", [128, 16])
    aimT = din("aimT", [128, 16])
    ldtT = din("ldtT", [128, 16])
    breT = din("breT", [128, 16, 16])
    bimT = din("bimT", [128, 16, 16])
    creT = din("creT", [128, 16, 16])
    cimT = din("cimT", [128, 16, 16])
    drep = din("drep", [128, 32, 16])
    g1c = din("g1c", [128, 8])
    g3c = din("g3c", [128, 8])
    g2r = din("g2r", [128, 1024])
    g4r = din("g4r", [128, 1024])
    gsr = din("gsr", [128, 512])
    gar = din("gar", [128, 512])
    bgr = din("bgr", [128, 512])
    c_idf = din("c_idf", [128, 128])
    c_cb = din("c_cb", [128, 2, 256])
    c_tm = din("c_tm", [128, 2, 256])
    c_dm = din("c_dm", [128, 2, 256])
    c_nv = din("c_nv", [128, 32])
    out = nc.dram_tensor("out", [2, S, D], F32, kind="ExternalOutput").ap()
    GT_d = nc.dram_tensor("GT_d", [32, 128, 2, 2, 128], BF16, kind="Internal").ap()
    TOEP_d = nc.dram_tensor("TOEP_d", [32, 128, 2, 256], BF16, kind="Internal").ap()
    CT_d = nc.dram_tensor("CT_d", [16, 128, 2, 256], BF16, kind="Internal").ap()
    dbg_t = {}
    if dbg:
        dbg_t['uc'] = nc.dram_tensor("d_uc", [128, 32, 16, 16], BF16, kind="ExternalOutput").ap()
        dbg_t['qT'] = nc.dram_tensor("d_qT", [128, 4, S], BF16, kind="ExternalOutput").ap()
        dbg_t['vt'] = nc.dram_tensor("d_vt", [128, 16, 8, 65], BF16, kind="ExternalOutput").ap()
        dbg_t['y1'] = nc.dram_tensor("d_y1", [128, 16, 512], BF16, kind="ExternalOutput").ap()
        dbg_t['ssmT'] = nc.dram_tensor("d_ssmT", [128, 4, 16, 128], BF16, kind="ExternalOutput").ap()
        dbg_t['attT'] = nc.dram_tensor("d_attT", [128, 4, S], BF16, kind="ExternalOutput").ap()
        dbg_t['x1'] = nc.dram_tensor("d_x1", [128, 16, 1024], F32, kind="ExternalOutput").ap()
        dbg_t['S'] = nc.dram_tensor("d_S", [128, 2, 16, 128], F32, kind="ExternalOutput").ap()

    ARENA = 206 * 1024
    arena = es.enter_context(nc.sbuf_tensor("arena", [128, ARENA], U8))
    pbanks = [es.enter_context(nc.psum_tensor("pb%d" % i, [128, 512], F32)) for i in range(8)]
    pk = [Tk() for _ in range(8)]
    fw = FW(nc, es)

    def reg(off, shape, dt, p0=0):
        esz = 2 if dt == BF16 else 4
        n = int(np.prod(shape[1:])) * esz
        assert off % 4 == 0 and off + n <= ARENA, (off, n)
        ap = arena[p0:p0 + shape[0], off:off + n].bitcast(dt)
        if len(shape) == 3:
            ap = ap.rearrange("p (a b) -> p a b", a=shape[1])
        elif len(shape) == 4:
            ap = ap.rearrange("p (a b c) -> p a b c", a=shape[1], b=shape[2])
        elif len(shape) == 5:
            ap = ap.rearrange("p (a b c d) -> p a b c d", a=shape[1], b=shape[2], c=shape[3])
        return ap

    class Bump:
        def __init__(self, lo, hi):
            self.o, self.hi = lo, hi

        def __call__(self, shape, dt, p0=0):
            esz = 2 if dt == BF16 else 4
            n = int(np.prod(shape[1:])) * esz
            n4 = (n + 31) // 32 * 32
            a = reg(self.o, shape, dt, p0)
            self.o += n4
            assert self.o <= self.hi, (self.o, self.hi)
            return a

    K = 1024
    V, A, G_, T_, SY = 'vector', 'scalar', 'gpsimd', 'tensor', 'sync'

    cb = Bump(0, 20 * K)
    idf = cb([128, 128], F32)
    idb = cb([128, 128], BF16)
    cbias = cb([128, 2, 256], BF16)
    g1 = cb([128, 8], F32)
    g3 = cb([128, 8], F32)
    g2 = cb([128, 1024], F32)
    g4 = cb([128, 1024], F32)
    gs = cb([128, 512], F32)
    ga = cb([128, 512], F32)
    bg = cb([128, 512], F32)
    stat = cb([128, 64], F32)
    LR = cb([128, 7, 16], F32)
    LI = cb([128, 7, 16], F32)
    LIn = cb([128, 7, 16], F32)
    LAk = cb([128, 7, 2, 16], F32)
    assert cb.o <= 20 * K
    kC = Tk()
    sem_c = fw.newsem("sem_c")
    sem_c2 = fw.newsem("sem_c2")
    for dst, src in [(idf, c_idf), (g1, g1c), (g3, g3c), (g2, g2r), (g4, g4r), (gs, gsr), (ga, gar), (bg, bgr)]:
        fw.dma(SY, dst, src, sem_c, writes=[kC])

    sb = Bump(20 * K, ARENA)
    cb_off = sb.o
    CBre = sb([128, 16, 16, 16], F32)
    cbf = sb([128, 2, 256], F32)
    tmk = sb([128, 2, 256], F32)
    dmk = sb([128, 2, 256], F32)
    nv = sb([128, 32], F32)
    drp = sb([128, 32, 16], F32)
    are = sb([128, 16], F32)
    aim = sb([128, 16], F32)
    ldt = sb([128, 16], F32)
    bre = sb([128, 16, 16], F32)
    bim = sb([128, 16, 16], F32)
    cre = sb([128, 16, 16], F32)
    cim = sb([128, 16, 16], F32)
    kS = Tk()
    for dst, src in [(cbf, c_cb), (tmk, c_tm), (dmk, c_dm), (nv, c_nv), (drp, drep), (are, areT), (aim, aimT),
                     (ldt, ldtT), (bre, breT), (bim, bimT), (cre, creT), (cim, cimT)]:
        fw.dma(SY, dst, src, sem_c2, writes=[kS])
    fw.op(V, cp(idb, idf), [kC], [kC])
    fw.op(V, cp(cbias, cbf), [kS, kC], [kC])

    dtt = sb([128, 16], F32)
    adr = sb([128, 16], F32)
    adi = sb([128, 16], F32)
    ex = sb([128, 16, 32], F32)
    ang = sb([128, 16, 32], F32)
    mag = sb([128, 16, 32], F32)
    magi = sb([128, 16, 32], F32)
    r1 = sb([128, 16, 32], F32)
    r2 = sb([128, 16, 32], F32)
    sn = sb([128, 16, 32], F32)
    cs = sb([128, 16, 32], F32)
    Pre = sb([128, 16, 32], F32)
    Pim = sb([128, 16, 32], F32)
    Qre = sb([128, 16, 32], F32)
    Qim = sb([128, 16, 32], F32)
    sm = [sb([128, 16], F32) for _ in range(10)]
    bbre = sb([128, 16, 16], F32)
    bbim = sb([128, 16, 16], F32)
    tb1 = sb([128, 16, 16], F32)
    tb2 = sb([128, 16, 16], F32)
    k0 = Tk()
    fw.op(A, act(dtt, ldt, AF.Exp), [kS], [k0])
    fw.op(V, tt(adr, are, dtt, ALU.mult), [k0, kS], [k0])
    fw.op(V, tt(adi, aim, dtt, ALU.mult), [k0, kS], [k0])
    fw.op(V, tt(ex, bc(adr, 2, [128, 16, 32]), bc(nv, 1, [128, 16, 32]), ALU.mult), [k0, kS], [k0])
    fw.op(V, tt(ang, bc(adi, 2, [128, 16, 32]), bc(nv, 1, [128, 16, 32]), ALU.mult), [k0, kS], [k0])
    fw.op(A, act(mag, ex, AF.Exp), [k0], [k0])
    fw.op(A, act(magi, ex, AF.Exp, scale=-1.0), [k0], [k0])
    ki = reg(cb_off, [128, 16, 32], I32)
    kf = reg(cb_off + 2048, [128, 16, 32], F32)
    mk = reg(cb_off + 4096, [128, 16, 32], F32)

    def range_reduce(r, shift):
        fw.op(V, ts(r, ang, 1.0 / (2 * math.pi), ALU.mult, shift, ALU.add), [k0], [k0])
        fw.op(V, cp(ki, r), [k0], [k0])
        fw.op(V, cp(kf, ki), [k0], [k0])
        fw.op(V, tt(r, r, kf, ALU.subtract), [k0], [k0])
        fw.op(V, ts(mk, r, -0.5, ALU.is_lt), [k0], [k0])
        fw.op(V, tt(r, r, mk, ALU.add), [k0], [k0])
        fw.op(V, ts(mk, r, 0.5, ALU.is_gt), [k0], [k0])
        fw.op(V, tt(r, r, mk, ALU.subtract), [k0], [k0])
    range_reduce(r1, 0.0)
    range_reduce(r2, 0.25)
    fw.op(A, act(sn, r1, AF.Sin, scale=2 * math.pi), [k0], [k0])
    fw.op(A, act(cs, r2, AF.Sin, scale=2 * math.pi), [k0], [k0])
    fw.op(V, tt(Pre, mag, cs, ALU.mult), [k0], [k0])
    fw.op(V, tt(Pim, mag, sn, ALU.mult), [k0], [k0])
    fw.op(V, tt(Qre, magi, cs, ALU.mult), [k0], [k0])
    fw.op(V, stt(Qim, magi, -1.0, sn, ALU.mult, ALU.mult), [k0], [k0])
    lbre, lbim = Pre[:, :, 0], Pim[:, :, 0]
    nr, den, t0, t1_, cr, ci, rden = sm[0], sm[1], sm[2], sm[3], sm[4], sm[5], sm[6]
    fw.op(V, ts(nr, lbre, -1.0, ALU.add), [k0], [k0])
    fw.op(V, tt(den, are, are, ALU.mult), [k0, kS], [k0])
    fw.op(V, tt(t0, aim, aim, ALU.mult), [k0, kS], [k0])
    fw.op(V, tt(den, den, t0, ALU.add), [k0], [k0])
    fw.op(V, lambda E: E.reciprocal(out=rden, in_=den), [k0], [k0])
    fw.op(V, tt(t0, nr, are, ALU.mult), [k0], [k0])
    fw.op(V, tt(t1_, lbim, aim, ALU.mult), [k0], [k0])
    fw.op(V, tt(t0, t0, t1_, ALU.add), [k0], [k0])
    fw.op(V, tt(cr, t0, rden, ALU.mult), [k0], [k0])
    fw.op(V, tt(t0, lbim, are, ALU.mult), [k0], [k0])
    fw.op(V, tt(t1_, nr, aim, ALU.mult), [k0], [k0])
    fw.op(V, tt(t0, t0, t1_, ALU.subtract), [k0], [k0])
    fw.op(V, tt(ci, t0, rden, ALU.mult), [k0], [k0])
    crb, cib = bc(cr, 2, [128, 16, 16]), bc(ci, 2, [128, 16, 16])
    fw.op(V, tt(tb1, crb, bre, ALU.mult), [k0, kS], [k0])
    fw.op(V, tt(tb2, cib, bim, ALU.mult), [k0, kS], [k0])
    fw.op(V, tt(bbre, tb1, tb2, ALU.subtract), [k0], [k0])
    fw.op(V, tt(tb1, crb, bim, ALU.mult), [k0, kS], [k0])
    fw.op(V, tt(tb2, cib, bre, ALU.mult), [k0, kS], [k0])
    fw.op(V, tt(bbim, tb1, tb2, ALU.add), [k0], [k0])
    fw.op(V, cp(LR[:, 0, :], Pre[:, :, 15]), [k0], [kC])
    fw.op(V, cp(LI[:, 0, :], Pim[:, :, 15]), [k0], [kC])
    for k in range(6):
        fw.op(V, tt(sm[7], LR[:, k, :], LR[:, k, :], ALU.mult), [kC, k0], [k0])
        fw.op(V, tt(sm[8], LI[:, k, :], LI[:, k, :], ALU.mult), [kC, k0], [k0])
        fw.op(V, tt(LR[:, k + 1, :], sm[7], sm[8], ALU.subtract), [k0], [kC])
        fw.op(V, tt(sm[9], LR[:, k, :], LI[:, k, :], ALU.mult), [kC, k0], [k0])
        fw.op(V, ts(LI[:, k + 1, :], sm[9], 2.0, ALU.mult), [k0], [kC])
    fw.op(V, ts(LIn, LI, -1.0, ALU.mult), [kC], [kC])
    fw.op(V, cp(LAk[:, :, 0, :], LR), [kC], [kC])
    fw.op(V, cp(LAk[:, :, 1, :], LR), [kC], [kC])
    SH4 = [128, 16, 16, 16]
    Gre = sb(SH4, F32)
    Gim = sb(SH4, F32)
    CAre = sb(SH4, F32)
    CAim = sb(SH4, F32)
    CBim = sb(SH4, F32)
    u1_off = sb.o
    U1 = sb(SH4, F32)
    U2 = sb(SH4, F32)
    U3, U4 = U1, U2
    CTst = reg(u1_off, [128, 16, 2, 256], BF16)
    kG, kCA, kCB, kU1, kU2, kCT = [Tk() for _ in range(6)]
    kU3, kU4 = kU1, kU2

    def outer(eng, o, a, pn, rd, wr):
        fw.op(eng, tt(o, bc(a, 2, SH4), bc(pn, 3, SH4), ALU.mult), rd, wr)

    def fl(ap):
        return ap.rearrange("p a b c -> p (a b c)")
    outer(V, U1, bbre, Qre[:, :, 0:16], [k0], [kU1])
    outer(G_, U2, bbim, Qim[:, :, 0:16], [k0], [kU2])
    fw.op(V, tt(Gre, U1, U2, ALU.subtract), [kU1, kU2], [kG])
    outer(V, U3, bbre, Qim[:, :, 0:16], [k0], [kU3])
    outer(G_, U4, bbim, Qre[:, :, 0:16], [k0], [kU4])
    fw.op(V, tt(Gim, U3, U4, ALU.add), [kU3, kU4], [kG])
    outer(V, U1, cre, Pre[:, :, 0:16], [k0, kS], [kU1])
    outer(G_, U2, cim, Pim[:, :, 0:16], [k0, kS], [kU2])
    fw.op(V, tt(CAre, U1, U2, ALU.subtract), [kU1, kU2], [kCA])
    outer(V, U3, cre, Pim[:, :, 0:16], [k0, kS], [kU3])
    outer(G_, U4, cim, Pre[:, :, 0:16], [k0, kS], [kU4])
    fw.op(V, stt(fl(CAim), fl(U3), -1.0, fl(U4), ALU.mult, ALU.subtract), [kU3, kU4], [kCA])
    outer(V, U1, cre, Pre[:, :, 16:32], [k0, kS], [kU1])
    outer(G_, U2, cim, Pim[:, :, 16:32], [k0, kS], [kU2])
    fw.op(V, tt(CBre, U1, U2, ALU.subtract), [kU1, kU2], [kCB])
    outer(V, U3, cre, Pim[:, :, 16:32], [k0, kS], [kU3])
    outer(G_, U4, cim, Pre[:, :, 16:32], [k0, kS], [kU4])
    fw.op(V, stt(fl(CBim), fl(U3), -1.0, fl(U4), ALU.mult, ALU.subtract), [kU3, kU4], [kCB])
    fw.op(V, cp(CTst[:, :, 0, :], CBre.rearrange("p g t h -> p g (t h)")), [kCB], [kCT, kU1])
    fw.op(V, cp(CTst[:, :, 1, :], CBim.rearrange("p g t h -> p g (t h)")), [kCB], [kCT, kU1])
    sem_t = fw.newsem("sem_t")
    kDR = Tk()
    fw.dma(SY, CT_d.rearrange("g p r c -> p g r c"), CTst, sem_t, [kCT], [kDR])
    TPst = [sb([128, 2, 256], BF16) for _ in range(2)]
    GTst = [sb([128, 2, 2, 128], BF16) for _ in range(2)]
    TPt = [sb([128, 2, 256], F32) for _ in range(2)]
    TPd = [sb([128, 2, 256], F32) for _ in range(2)]
    kTP = [Tk() for _ in range(2)]
    kGTs = [Tk() for _ in range(2)]
    kTt = [Tk() for _ in range(2)]
    sem_tp = [fw.newsem("sem_tp%d" % i) for i in range(2)]
    sem_gt = [fw.newsem("sem_gt%d" % i) for i in range(2)]
    for gh in range(2):
        pr = slice(64 * gh, 64 * gh + 64)
        for gl in range(16):
            g = 16 * gh + gl
            i = g % 2
            pT, pG = 2 * i, 2 * i + 1
            pt_v = pbanks[pT][:, :].rearrange("p (j c) -> p j c", j=2)
            fns = []
            for j in range(2):
                l_re = Gre[pr, gl, 8 * j:8 * j + 8, :].rearrange("p s h -> p (s h)")
                l_im = Gim[pr, gl, 8 * j:8 * j + 8, :].rearrange("p s h -> p (s h)")
                fns.append(mmf(pt_v[:, j, :], l_re, CAre[pr, gl].rearrange("p t h -> p (t h)"), True, False))
                fns.append(mmf(pt_v[:, j, :], l_im, CAim[pr, gl].rearrange("p t h -> p (t h)"), False, True))
            fw.mm(fns, [kG, kCA], [pk[pT]])
            pg_v = pbanks[pG][:, :].rearrange("p (j r c) -> p j r c", j=2, r=2)
            fns = []
            for j in range(2):
                l_re = Gre[pr, gl, 8 * j:8 * j + 8, :].rearrange("p s h -> p (s h)")
                l_im = Gim[pr, gl, 8 * j:8 * j + 8, :].rearrange("p s h -> p (s h)")
                fns.append(mmf(pg_v[:, j, 0, :], l_re, idf[pr, :], True, True))
                fns.append(mmf(pg_v[:, j, 1, :], l_im, idf[pr, :], True, True))
            fw.mm(fns, [kG, kC], [pk[pG]])
            fw.op(V, tt(TPt[i], pt_v, tmk, ALU.mult), [pk[pT], kS], [kTt[i]])
            fw.op(G_, tt(TPd[i].rearrange("p j (t h) -> p j t h", t=16),
                         dmk.rearrange("p j (t h) -> p j t h", t=16),
                         drp[:, g, :].unsqueeze(1).unsqueeze(1).broadcast_to([128, 2, 16, 16]), ALU.mult),
                  [kS], [kTP[i]])
            fw.op(V, tt(TPst[i], TPt[i], TPd[i], ALU.add), [kTt[i], kTP[i]], [kTP[i]])
            fw.dma(SY, TOEP_d[g], TPst[i], sem_tp[i], [kTP[i]], [kDR])
            fw.op(A, act(GTst[i], pg_v, AF.Copy), [pk[pG]], [kGTs[i]])
            fw.dma(SY, GT_d[g], GTst[i], sem_gt[i], [kGTs[i]], [kDR])
    fw.barrier()

    X1 = reg(20 * K, [128, 16, 1024], F32)
    attT = reg(84 * K, [128, 4, S], BF16)
    ssmT = reg(100 * K, [128, 4, 16, 128], BF16)
    Uc = reg(116 * K, [128, 32, 16, 16], BF16)
    qT = reg(132 * K, [128, 4, S], BF16)
    kT = reg(148 * K, [128, 4, S], BF16)
    Vt = reg(164 * K, [128, 16, 8, 65], BF16)
    sem_x = [fw.newsem("sem_x%d" % i) for i in range(4)]
    sem_w = [fw.newsem("sem_w%d" % i) for i in range(2)]
    sem_o = fw.newsem("sem_o")
    sem_og = fw.newsem("sem_og")
    sem_wq = [fw.newsem("sem_wq%d" % i) for i in range(2)]
    sem_d = fw.newsem("sem_d")
    sem_g = [fw.newsem("sem_g%d" % i) for i in range(2)]
    sem_dn = fw.newsem("sem_dn")
    sem_st = [fw.newsem("sem_st%d" % i) for i in range(4)]
    kOut = Tk()

    try:
      if STAGE < 1:
        raise _Stop()
      for b in range(2):
        a1 = Bump(20 * K, 116 * K)
        Xst = [a1([128, 2, 1024], F32) for _ in range(2)]
        hbf = [a1([128, 2, 1024], BF16) for _ in range(2)]
        hT = a1([128, 8, S], BF16)
        wbuf = [a1([128, 8, 256], BF16) for _ in range(2)]
        junk = a1([128, 1024], F32)
        kX = [Tk(), Tk()]
        kH = [Tk(), Tk()]
        khT = [Tk() for _ in range(8)]
        kW = [Tk(), Tk()]
        kJ = Tk()
        kSt = Tk()
        kUc, kq, kk, kv = Tk(), Tk(), Tk(), Tk()
        xv = x[b].rearrange("(c t) d -> c t d", t=16)
        ss = stat[:, 0:16]
        rs = stat[:, 16:32]
        def a1_s0(tp):
            i = tp % 2
            fw.dma(SY, Xst[i], xv[:, 2 * tp:2 * tp + 2, :], sem_x[i], writes=[kX[i]])
            for u in range(2):
                t = 2 * tp + u
                fw.op(A, ttr(junk, Xst[i][:, u, :], Xst[i][:, u, :], ss[:, t:t + 1]), [kX[i]], [kJ, kSt])
                fw.op(V, ts(rs[:, t:t + 1], ss[:, t:t + 1], 1.0 / D, ALU.mult, EPS, ALU.add), [kSt], [kSt])
                fw.op(A, act(rs[:, t:t + 1], rs[:, t:t + 1], AF.Ln), [kSt], [kSt])
                fw.op(A, act(rs[:, t:t + 1], rs[:, t:t + 1], AF.Exp, scale=-0.5), [kSt], [kSt])
                fw.op(A, act(hbf[i][:, u, :], Xst[i][:, u, :], AF.Copy, scale=rs[:, t:t + 1]), [kX[i], kSt], [kH[i]])

        def a1_s1(tp):
            i = tp % 2
            for k in range(8):
                pi = k % 4
                pv = pbanks[pi][:, 0:128].bitcast(BF16).rearrange("p (u c) -> p u c", u=2)
                fns = [trf(pv[:, u, :], hbf[i][:, u, k * 128:(k + 1) * 128], idb) for u in range(2)]
                fw.mm(fns, [kH[i], kC], [pk[pi]])
                dst = hT[:, k, :].rearrange("p (c t) -> p t c", t=16)[:, 2 * tp:2 * tp + 2, :]
                if k % 2 == 0:
                    fw.op(V, ts(dst, pv, g1[:, k:k + 1], ALU.mult), [pk[pi], kC], [khT[k]])
                else:
                    fw.op(A, act(dst, pv, AF.Copy, scale=g1[:, k:k + 1]), [pk[pi], kC], [khT[k]])

        skew(8, [a1_s0, a1_s1])
        win_v = w_in.rearrange("(k p) n -> p k n", p=128)
        pcount = 0
        for piece in range(8):
            i = piece % 2
            fw.dma(G_, wbuf[i], win_v[:, :, piece * 256:(piece + 1) * 256], sem_wq[i], writes=[kW[i]])
            if piece < 2:
                for t in range(16):
                    pi = 4 + (pcount % 4)
                    pcount += 1
                    pv = pbanks[pi][:, 0:256]
                    fns = [mmf(pv, hT[:, k, t::16], wbuf[i][:, k, :], k == 0, k == 7) for k in range(8)]
                    fw.mm(fns, khT + [kW[i]], [pk[pi]])
                    dst = Uc[:, 16 * piece:16 * piece + 16, t, :]
                    srcv = pv.rearrange("p (g h) -> p g h", g=16)
                    if t % 2 == 0:
                        fw.op(V, cp(dst, srcv), [pk[pi]], [kUc])
                    else:
                        fw.op(A, act(dst, srcv, AF.Copy), [pk[pi]], [kUc])
            elif piece < 6:
                dstT, kd, sc = (qT, kq, 0.125) if piece < 4 else (kT, kk, 1.0)
                for mm_ in range(2):
                    m = (piece % 2) * 2 + mm_
                    for n in range(4):
                        pi = 4 + (pcount % 4)
                        pcount += 1
                        pv = pbanks[pi][:, :]
                        fns = [mmf(pv, wbuf[i][:, k, mm_ * 128:(mm_ + 1) * 128], hT[:, k, n * 512:(n + 1) * 512],
                                   k == 0, k == 7) for k in range(8)]
                        fw.mm(fns, khT + [kW[i]], [pk[pi]])
                        dst = dstT[:, m, n * 512:(n + 1) * 512]
                        if n % 2 == 0:
                            fw.op(V, ts(dst, pv, sc, ALU.mult), [pk[pi]], [kd])
                        else:
                            fw.op(A, act(dst, pv, AF.Copy, scale=sc), [pk[pi]], [kd])
            else:
                hh = piece - 6
                for it in range(16):
                    pi = 4 + (pcount % 4)
                    pcount += 1
                    pv = pbanks[pi][:, 0:256]
                    fns = [mmf(pv, hT[:, k, it * 128:(it + 1) * 128], wbuf[i][:, k, :], k == 0, k == 7)
                           for k in range(8)]
                    fw.mm(fns, khT + [kW[i]], [pk[pi]])
                    dst = Vt[:, it, 4 * hh:4 * hh + 4, 0:64]
                    src = pv.rearrange("p (h d) -> p h d", h=4)
                    if it % 2 == 0:
                        fw.op(V, cp(dst, src), [pk[pi]], [kv])
                    else:
                        fw.op(A, act(dst, src, AF.Copy), [pk[pi]], [kv])
        fw.op(G_, lambda E: E.memset(Vt[:, :, :, 64:65], 1.0), [kv], [kv])
        if dbg and b == 0:
            fw.dma(SY, dbg_t['uc'], Uc, sem_d, [kUc], [kOut])
            fw.dma(SY, dbg_t['qT'], qT, sem_d, [kq], [kOut])
            fw.dma(SY, dbg_t['vt'], Vt, sem_d, [kv], [kOut])
        fw.barrier()

        if STAGE < 2:
            raise _Stop()
        a2 = Bump(181 * K, ARENA)
        Sbf = a2([128, 2, 16, 130], BF16)
        a2b = Bump(20 * K, 84 * K)
        UT = a2b([128, 32, 2, 128], BF16)
        Wf = a2b([128, 2, 16, 128], F32)
        GTb = [a2b([128, 2, 2, 2, 128], BF16) for _ in range(2)]
        T1 = a2b([128, 2, 16], F32)
        T2 = a2b([128, 2, 16], F32)
        kUT, kWf, kSb, kT1, kT2 = Tk(), Tk(), Tk(), Tk(), Tk()
        kGTb = [Tk(), Tk()]
        for g in range(32):
            pi = g % 4
            pv = pbanks[pi][:, 0:128].bitcast(BF16).rearrange("p (j c) -> p j c", j=2)
            fns = [trf(pv[:, j, :], Uc[:, g, 8 * j:8 * j + 8, :].rearrange("p s h -> p (s h)"), idb) for j in range(2)]
            fw.mm(fns, [kUc, kC], [pk[pi]])
            if g % 2 == 0:
                fw.op(V, cp(UT[:, g], pv), [pk[pi]], [kUT])
            else:
                fw.op(A, act(UT[:, g], pv, AF.Copy), [pk[pi]], [kUT])
        if STAGE < 2.2:
            raise _Stop()
        for gl in range(16):
            i = gl % 2
            for gh in range(2):
                fw.dma(SY, GTb[i][:, gh], GT_d[16 * gh + gl], sem_w[i], writes=[kGTb[i]])
            pi = 4 + gl % 4
            pv = pbanks[pi][:, 0:256].rearrange("p (r c) -> p r c", r=2)
            fns = []
            for r in range(2):
                n = 0
                for gh in range(2):
                    for j in range(2):
                        fns.append(mmf(pv[:, r, :], GTb[i][:, gh, j, r, :], UT[:, 16 * gh + gl, j, :], n == 0, n == 3))
                        n += 1
            fw.mm(fns, [kUT, kGTb[i]], [pk[pi]])
            fw.op(V, cp(Wf[:, :, gl, :], pv), [pk[pi]], [kWf])
        if STAGE < 2.5:
            raise _Stop()
        WfB = reg(116 * K, [128, 2, 16, 128], F32)
        TT = reg(190 * K, [128, 2, 16, 128], F32)
        kWB, kTT = kUc, Tk()
        ks = {'cur': Wf, 'nxt': WfB, 'kcur': kWf, 'knxt': kWB}

        def ks_ops():
            for k in range(7):
                d = 1 << k
                n = 128 - d
                cur, nxt, kcur, knxt = ks['cur'], ks['nxt'], ks['kcur'], ks['knxt']
                fw.op(V, tt(TT[:, :, :, 0:n], cur[:, :, :, 0:n], bc(LAk[:, k], 3, [128, 2, 16, n]), ALU.mult),
                      [kcur, kC], [kTT])
                yield
                fw.op(V, tt(nxt[:, :, :, d:128], cur[:, :, :, d:128], TT[:, :, :, 0:n], ALU.add), [kcur, kTT], [knxt])
                yield
                fw.op(V, tt(TT[:, 0, :, 0:n], cur[:, 1, :, 0:n], bc(LIn[:, k, :], 2, [128, 16, n]), ALU.mult),
                      [kcur, kC], [kTT])
                yield
                fw.op(V, tt(TT[:, 1, :, 0:n], cur[:, 0, :, 0:n], bc(LI[:, k, :], 2, [128, 16, n]), ALU.mult),
                      [kcur, kC], [kTT])
                yield
                fw.op(V, tt(nxt[:, :, :, d:128], nxt[:, :, :, d:128], TT[:, :, :, 0:n], ALU.add), [knxt, kTT], [knxt])
                yield
                fw.op(V, cp(nxt[:, :, :, 0:d], cur[:, :, :, 0:d]), [kcur], [knxt])
                ks['cur'], ks['nxt'], ks['kcur'], ks['knxt'] = nxt, cur, knxt, kcur
                yield

        ks_gen = ks_ops()

        def ks_finish():
            Wfin, kWfin = ks['cur'], ks['kcur']
            fw.op(V, lambda E: E.memset(Sbf[:, :, :, 0:2], 0.0), [], [kSb])
            for r in range(2):
                fw.op(A, act(Sbf[:, r, :, 2:130], Wfin[:, r], AF.Copy), [kWfin], [kSb])
            if dbg and b == 0:
                fw.dma(SY, dbg_t['S'], Wfin, sem_d, [kWfin], [kOut])

        if STAGE < 3:
            raise _Stop()
        a3 = Bump(52 * K, 84 * K)
        MBT = a3([128, S], BF16)
        kmT = a3([128, 4, 8], BF16)
        kmf = a3([128, 4, 8], F32)
        gtL = [a3([128, 8, 8], F32) for _ in range(3)]
        top8L = [a3([128, 8, 8], F32) for _ in range(3)]
        mbtL = [a3([128, 2, 64], BF16) for _ in range(3)]
        kgtL = [Tk() for _ in range(3)]
        kmbL = [Tk() for _ in range(3)]
        PT = [a3([128, 256], BF16) for _ in range(3)]
        Ao = a3([128, 2, 512], F32)
        Aj = a3([128, 512], F32)
        Ab = a3([128, 2, 512], BF16)
        rden_a = a3([128, 4], F32)
        kMB, kkm, kgt, kmb = Tk(), Tk(), Tk(), Tk()
        Esel = reg(100 * K, [128, 64, 128], BF16)
        kEs = Tk()
        fw.op(V, cp(Esel[0:64], bc(idb[0:64, 0:64], 2, [64, 64, 128])), [kC], [kEs])
        fw.op(V, cp(Esel[64:128], bc(idb[64:128, 64:128], 2, [64, 64, 128])), [kC], [kEs])
        kPT = [Tk() for _ in range(3)]
        kAo, kAj, kAb, kat, krd = Tk(), Tk(), Tk(), Tk(), Tk()
        if STAGE < 3.05:
            raise _Stop()
        kjunk = a3([128, 256], BF16)
        kkj = Tk()
        for m in range(4):
            for n in range(8):
                fw.op(A, (lambda m=m, n=n: (lambda E: E.activation(out=kjunk, in_=kT[:, m, n * 256:(n + 1) * 256],
                                                                   func=AF.Copy, accum_out=kmf[:, m, n:n + 1])))(),
                      [kk], [kkj, kkm])
        if STAGE < 3.08:
            raise _Stop()
        fw.op(A, act(kmT, kmf, AF.Copy, scale=1.0 / 256), [kkm], [kkm])
        if STAGE < 3.1:
            raise _Stop()
        fw.op(G_, lambda E: E.memset(MBT, 0.0), [], [kMB, kGTb[0], kGTb[1]])
        def gate_s0(q8):
            qt = 8 + q8
            bq = qt // 2
            i3 = q8 % 3
            gt, kgt = gtL[i3], kgtL[i3]
            be, bo = (0, 1) if q8 % 2 == 0 else (4, 5)
            pve = pbanks[be][:, 0:32].rearrange("p (h n) -> p h n", h=4)
            pvo = pbanks[bo][:, 0:32].rearrange("p (h n) -> p h n", h=4)
            fe = [mmf(pve[:, h2, :], qT[0:64, h2, qt * 128:(qt + 1) * 128], kmT[0:64, h2, :], True, True)
                  for h2 in range(4)]
            fw.mm(fe, [kq, kkm], [pk[be]])
            fo = [mmf(pvo[:, h2, :], qT[64:128, h2, qt * 128:(qt + 1) * 128], kmT[64:128, h2, :], True, True)
                  for h2 in range(4)]
            fw.mm(fo, [kq, kkm], [pk[bo]])
            fw.op(G_, lambda E: E.memset(gt, NEG), [kgt], [kgt])
            gtv = gt.rearrange("p (h2 e) n -> p h2 e n", e=2)
            fw.op(V, cp(gtv[:, :, 0, 0:bq], pve[:, :, 0:bq]), [pk[be], kgt], [kgt])
            fw.op(V, cp(gtv[:, :, 1, 0:bq], pvo[:, :, 0:bq]), [pk[bo], kgt], [kgt])

        def gate_s1(q8):
            i3 = q8 % 3
            gt, kgt, top8, mbt, kmb = gtL[i3], kgtL[i3], top8L[i3], mbtL[i3], kmbL[i3]
            for h in range(8):
                fw.op(V, (lambda hh: (lambda E: E.max(out=top8[:, hh, :], in_=gt[:, hh, :])))(h), [kgt], [kmb])
            for dup in range(2):
                fw.op(V, tt(mbt[:, dup, :].rearrange("p (h n) -> p h n", h=8), gt,
                            top8[:, :, 2:3].broadcast_to([128, 8, 8]), ALU.is_lt), [kgt, kmb], [kmb])

        def gate_s2(q8):
            qt = 8 + q8
            i3 = q8 % 3
            mbt, kmb = mbtL[i3], kmbL[i3]
            pi2 = 2 + qt % 2
            pv2 = pbanks[pi2][:, 0:64].bitcast(BF16)
            fw.mm([trf(pv2, mbt.rearrange("p d c -> p (d c)"), idb)], [kmb, kC], [pk[pi2]])
            fw.op(V, ts(MBT[:, qt * 128:(qt + 1) * 128], pv2, NEG, ALU.mult), [pk[pi2]], [kMB])

        skew(8, [gate_s0, gate_s1, gate_s2])
        if STAGE < 3.2:
            raise _Stop()
        ucount = 0
        LAG = 2
        for bq in range(ABQ):
            nkt = 2 * bq + 2

            def stage_a(h, kt, pi, bq=bq):
                pr = slice(64 * (h % 2), 64 * (h % 2) + 64)
                m = h // 2
                n = kt // 2
                pv = pbanks[pi][:, 0:256]
                fns = [mmf(pv, kT[pr, m, kt * 128:(kt + 1) * 128], qT[pr, m, bq * 256:(bq + 1) * 256], True, False)]
                rd = [kq, kk]
                if n == bq:
                    fns.append(mmf(pv, idb, cbias[:, kt - 2 * bq, :], False, True))
                    rd.append(kC)
                elif bq >= 4:
                    r = h * 8 + n
                    fns.append(mmf(pv, Esel[pr, r, :], MBT[pr, bq * 256:(bq + 1) * 256], False, True))
                    rd += [kEs, kMB]
                else:
                    fns[0] = mmf(pv, kT[pr, m, kt * 128:(kt + 1) * 128], qT[pr, m, bq * 256:(bq + 1) * 256], True, True)
                fw.mm(fns, rd, [pk[pi]])
                fw.op(A, act(PT[pi], pv, AF.Exp), [pk[pi]], [kPT[pi]])

            def stage_b(h, kt, pi, nkt=nkt):
                pos = [4 + 2 * (h % 2), 5 + 2 * (h % 2)]
                povs = [pbanks[pos[0]][:, 0:65], pbanks[pos[1]][:, 0:65]]
                fns = [mmf(povs[u], PT[pi][:, u * 128:(u + 1) * 128], Vt[:, kt, h, :], kt == 0, kt == nkt - 1)
                       for u in range(2)]
                fw.mm(fns, [kPT[pi], kv], [pk[pos[0]], pk[pos[1]]])
                if kt == nkt - 1:
                    for u in range(2):
                        fw.op(V, rcp(rden_a[:, u:u + 1], povs[u][:, 64:65]), [pk[pos[u]]], [krd])
                        fw.op(V, ts(Ao[:, u, h * 64:(h + 1) * 64], povs[u][:, 0:64], rden_a[:, u:u + 1], ALU.mult),
                              [pk[pos[u]], krd], [kAo])

            pend = []
            for h in range(8):
                for kt in range(nkt):
                    pi = ucount % 3
                    ucount += 1
                    stage_a(h, kt, pi)
                    pend.append((h, kt, pi))
                    if ucount % 12 == 0:
                        next(ks_gen, None)
                    if len(pend) > LAG:
                        stage_b(*pend.pop(0))
            while pend:
                stage_b(*pend.pop(0))
            if bq == 7:
                for _ in ks_gen:
                    pass
                ks_finish()
            for u in range(2):
                fw.op(A, ttr(Aj, Ao[:, u, :], Ao[:, u, :], rden_a[:, 2:3]), [kAo], [kAj, krd])
                fw.op(V, ts(rden_a[:, 3:4], rden_a[:, 2:3], 1.0 / 512, ALU.mult, EPS, ALU.add), [krd], [krd])
                fw.op(A, act(rden_a[:, 3:4], rden_a[:, 3:4], AF.Ln), [krd], [krd])
                fw.op(A, act(rden_a[:, 3:4], rden_a[:, 3:4], AF.Exp, scale=-0.5), [krd], [krd])
                fw.op(V, stt(Ab[:, u, :], Ao[:, u, :], rden_a[:, 3:4], ga, ALU.mult, ALU.mult), [kAo, krd, kC], [kAb])
            for u in range(2):
                pi = 3
                pv = pbanks[pi][:, 0:256].bitcast(BF16).rearrange("p (f c) -> p f c", f=4)
                fns = [trf(pv[:, f, :], Ab[:, u, f * 128:(f + 1) * 128], idb) for f in range(4)]
                fw.mm(fns, [kAb, kC], [pk[pi]])
                tok0 = bq * 256 + u * 128
                fw.op(A, act(attT[:, :, tok0:tok0 + 128], pv, AF.Copy), [pk[pi]], [kat])
        if dbg and b == 0:
            fw.dma(SY, dbg_t['attT'], attT, sem_d, [kat], [kOut])
        fw.barrier()

        if STAGE < 4:
            raise _Stop()
        a4 = Bump(132 * K, 181 * K)
        Y1 = a4([128, 16, 512], BF16)
        TCb = [(a4([128, 2, 2, 256], BF16), a4([128, 2, 256], BF16)) for _ in range(2)]
        wgl = a4([128, 4, 512], BF16)
        gb = Bump(36 * K, 84 * K)
        NB = 4
        Y1T = [gb([128, 4, 128], BF16) for _ in range(NB)]
        zt = [gb([128, 512], F32) for _ in range(NB)]
        zs = [gb([128, 512], F32) for _ in range(NB)]
        y2 = [gb([128, 512], F32) for _ in range(NB)]
        y2b = [gb([128, 512], BF16) for _ in range(NB)]
        kY1, kwg, kssm = Tk(), Tk(), Tk()
        kzt, kzs, ky2, ky2b, kss = [[Tk() for _ in range(NB)] for _ in range(5)]
        kTC = [Tk(), Tk()]
        kY1T = [Tk() for _ in range(NB)]
        fw.dma(G_, wgl, w_glu.rearrange("(k p) n -> p k n", p=128), sem_og, writes=[kwg])
        C1 = 0.7978845608028654 * 2.0
        for gl in range(16):
            i = gl % 2
            for gh in range(2):
                fw.dma(SY, TCb[i][0][:, gh], TOEP_d[16 * gh + gl], sem_w[i], writes=[kTC[i]])
            fw.dma(SY, TCb[i][1], CT_d[gl], sem_w[i], writes=[kTC[i]])
            for gh in range(2):
                g = 16 * gh + gl
                pr = slice(64 * gh, 64 * gh + 64)
                pi = (2 * gl + gh) % 4
                pv = pbanks[pi][:, 0:256]
                fns = [mmf(pv, UT[:, g, 0, :], TCb[i][0][:, gh, 0, :], True, False),
                       mmf(pv, UT[:, g, 1, :], TCb[i][0][:, gh, 1, :], False, False),
                       mmf(pv, Sbf[pr, 0, gl, 1:129], TCb[i][1][pr, 0, :], False, False),
                       mmf(pv, Sbf[pr, 1, gl, 1:129], TCb[i][1][pr, 1, :], False, True)]
                fw.mm(fns, [kUT, kSb, kTC[i]], [pk[pi]])
                fw.op(A, act(Y1[:, :, 16 * g:16 * g + 16], pv.rearrange("p (t h) -> p t h", t=16), AF.Gelu_apprx_tanh),
                      [pk[pi]], [kY1])
        if dbg and b == 0:
            fw.dma(SY, dbg_t['y1'], Y1, sem_d, [kY1], [kOut])
        def glu_s0(t):
            i = t % NB
            pi = t % 2
            pv = pbanks[pi][:, 0:256].bitcast(BF16).rearrange("p (f c) -> p f c", f=4)
            fns = [trf(pv[:, f, :], Y1[:, t, f * 128:(f + 1) * 128], idb) for f in range(4)]
            fw.mm(fns, [kY1, kC], [pk[pi]])
            fw.op(A, act(Y1T[i], pv, AF.Copy), [pk[pi]], [kY1T[i]])
            pz = 2 + t % 4
            pzv = pbanks[pz][:, :]
            fns = [mmf(pzv, Y1T[i][:, f, :], wgl[:, f, :], f == 0, f == 3) for f in range(4)]
            fw.mm(fns, [kY1T[i], kwg], [pk[pz]])

        def glu_s1(t):
            i = t % NB
            pz = 2 + t % 4
            pzv = pbanks[pz][:, :]
            s0 = stat[:, 32 + 2 * i:33 + 2 * i]
            fw.op(V, tt(zt[i], pzv, bg, ALU.add), [pk[pz], kC], [kzt[i]])
            fw.op(A, act(zs[i], zt[i], AF.Exp, scale=-1.0), [kzt[i]], [kzs[i]])
            fw.op(V, ts(zs[i], zs[i], 1.0, ALU.add), [kzs[i]], [kzs[i]])
            fw.op(V, rcp(zs[i], zs[i]), [kzs[i]], [kzs[i]])
            fw.op(V, tt(y2[i], zs[i], Y1[:, t, :], ALU.mult), [kzs[i], kY1], [ky2[i]])
            fw.op(A, ttr(zt[i], y2[i], y2[i], s0), [ky2[i], kzt[i]], [kzt[i], kss[i]])

        def glu_s2(t):
            i = t % NB
            s0, s1 = stat[:, 32 + 2 * i:33 + 2 * i], stat[:, 33 + 2 * i:34 + 2 * i]
            fw.op(V, ts(s1, s0, 1.0 / 512, ALU.mult, EPS, ALU.add), [kss[i]], [kss[i]])
            fw.op(A, act(s1, s1, AF.Ln), [kss[i]], [kss[i]])
            fw.op(A, act(s1, s1, AF.Exp, scale=-0.5), [kss[i]], [kss[i]])
            fw.op(V, stt(y2b[i], y2[i], s1, gs, ALU.mult, ALU.mult), [ky2[i], kss[i], kC], [ky2b[i]])

        def glu_s3(t):
            i = t % NB
            pi2 = 6 + t % 2
            pv2 = pbanks[pi2][:, 0:256].bitcast(BF16).rearrange("p (f c) -> p f c", f=4)
            fns = [trf(pv2[:, f, :], y2b[i][:, f * 128:(f + 1) * 128], idb) for f in range(4)]
            fw.mm(fns, [ky2b[i], kC], [pk[pi2]])
            fw.op(A, act(ssmT[:, :, t, :], pv2, AF.Copy), [pk[pi2]], [kssm])

        skew(16, [glu_s0, glu_s1, glu_s2, glu_s3])
        if dbg and b == 0:
            fw.dma(SY, dbg_t['ssmT'], ssmT, sem_d, [kssm], [kOut])
        fw.barrier()

        if STAGE < 5:
            raise _Stop()
        a5 = Bump(116 * K, ARENA)
        wo = a5([128, 8, 1024], BF16)
        otmp = [a5([128, 1024], F32) for _ in range(4)]
        oj = a5([128, 512], F32)
        kwo, koj = Tk(), Tk()
        kot = [Tk() for _ in range(4)]
        kst4 = [Tk() for _ in range(4)]
        kX1 = [Tk() for _ in range(16)]
        fw.dma(G_, wo, w_out.rearrange("(k p) n -> p k n", p=128), sem_og, writes=[kwo])
        for q4 in range(4):
            fw.dma(SY, X1[:, 4 * q4:4 * q4 + 4, :], xv[:, 4 * q4:4 * q4 + 4, :], sem_x[q4],
                   writes=[kX1[4 * q4 + j] for j in range(4)])
        def a4_s0(t):
            pis = [2 * (t % 4), 2 * (t % 4) + 1]
            for hf in range(2):
                pv = pbanks[pis[hf]][:, :]
                fns = []
                for k in range(8):
                    lhs = ssmT[:, k, t, :] if k < 4 else attT[:, k - 4, t::16]
                    fns.append(mmf(pv, lhs, wo[:, k, hf * 512:(hf + 1) * 512], k == 0, k == 7))
                fw.mm(fns, [kssm, kat, kwo], [pk[pis[hf]]])
                sl4 = t % 4
                c0 = 36 + 3 * sl4
                fw.op(A, sqa(oj, pv, stat[:, c0 + hf:c0 + hf + 1]), [pk[pis[hf]]], [kst4[sl4], koj])

        def a4_s1(t):
            pis = [2 * (t % 4), 2 * (t % 4) + 1]
            sl4 = t % 4
            c0 = 36 + 3 * sl4
            rs4 = stat[:, c0 + 2:c0 + 3]
            fw.op(V, tt(rs4, stat[:, c0:c0 + 1], stat[:, c0 + 1:c0 + 2], ALU.add), [kst4[sl4]], [kst4[sl4]])
            fw.op(V, ts(rs4, rs4, 1.0 / D, ALU.mult, EPS, ALU.add), [kst4[sl4]], [kst4[sl4]])
            fw.op(A, act(rs4, rs4, AF.Ln), [kst4[sl4]], [kst4[sl4]])
            fw.op(A, act(rs4, rs4, AF.Exp, scale=-0.5), [kst4[sl4]], [kst4[sl4]])
            for hf in range(2):
                pv = pbanks[pis[hf]][:, :]
                sl = slice(hf * 512, (hf + 1) * 512)
                fw.op(V, stt(otmp[sl4][:, sl], pv, rs4, g2[:, sl], ALU.mult, ALU.mult),
                      [pk[pis[hf]], kst4[sl4], kC], [kot[sl4]])
            fw.op(G_, tt(X1[:, t, :], X1[:, t, :], otmp[sl4], ALU.add), [kot[sl4], kX1[t]], [kX1[t]])

        skew(16, [a4_s0, a4_s1])
        if dbg and b == 0:
            fw.dma(SY, dbg_t['x1'], X1, sem_d, kX1, [kOut])
        fw.barrier()

        if STAGE < 6:
            raise _Stop()
        f0 = Bump(86 * K, ARENA)
        h2T = f0([128, 8, 16, 128], BF16)
        fT = f0([128, NFF, 16, 128], BF16)
        hb2 = [reg(118 * K + 4096 + 2048 * i_, [128, 1024], BF16) for i_ in range(2)]
        kfj, kh2T, kfT = Tk(), Tk(), Tk()
        khb = [Tk(), Tk()]
        kst5 = [Tk(), Tk()]
        ov = out[b].rearrange("(c t) d -> c t d", t=16)
        fj1 = reg(118 * K, [128, 1024], F32)
        def f1_s0(t):
            i5 = t % 2
            q0, q1 = stat[:, 52 + 2 * i5:53 + 2 * i5], stat[:, 53 + 2 * i5:54 + 2 * i5]
            fw.op(A, ttr(fj1, X1[:, t, :], X1[:, t, :], q0), [kX1[t]], [kst5[i5], kfj])
            fw.op(V, ts(q1, q0, 1.0 / D, ALU.mult, EPS, ALU.add), [kst5[i5]], [kst5[i5]])
            fw.op(A, act(q1, q1, AF.Ln), [kst5[i5]], [kst5[i5]])
            fw.op(A, act(q1, q1, AF.Exp, scale=-0.5), [kst5[i5]], [kst5[i5]])
            fw.op(A, act(hb2[i5], X1[:, t, :], AF.Copy, scale=q1), [kX1[t], kst5[i5]], [khb[i5]])

        def f1_s1(t):
            i5 = t % 2
            for kk_ in range(2):
                pi = kk_ + 2 * (t % 2)
                pv = pbanks[pi][:, 0:256].bitcast(BF16).rearrange("p (f c) -> p f c", f=4)
                fns = [trf(pv[:, f, :], hb2[i5][:, (4 * kk_ + f) * 128:(4 * kk_ + f + 1) * 128], idb) for f in range(4)]
                fw.mm(fns, [khb[i5], kC], [pk[pi]])
                for f in range(4):
                    k = 4 * kk_ + f
                    if f % 2 == 0:
                        fw.op(V, ts(h2T[:, k, t, :], pv[:, f, :], g3[:, k:k + 1], ALU.mult), [pk[pi], kC], [kh2T])
                    else:
                        fw.op(A, act(h2T[:, k, t, :], pv[:, f, :], AF.Copy, scale=g3[:, k:k + 1]),
                              [pk[pi], kC], [kh2T])

        skew(16, [f1_s0, f1_s1])
        kOt = [Tk() for _ in range(16)]
        fw.dma(SY, ov, X1, sem_o, kX1, kOt)
        fw.barrier()
        f2 = Bump(64 * K, 86 * K)
        wg = [f2([128, 8, 128], BF16) for _ in range(2)]
        wu = [f2([128, 8, 128], BF16) for _ in range(2)]
        sg = [f2([128, 512], F32) for _ in range(2)]
        wd = reg(20 * K, [128, NFF, 1024], BF16)
        kwg = [Tk(), Tk()]
        kwd = Tk()
        ksg = [Tk(), Tk()]
        wg_v = w_gate.rearrange("(k p) n -> p k n", p=128)
        wu_v = w_up.rearrange("(k p) n -> p k n", p=128)
        wd_v = w_down.rearrange("(j p) n -> p j n", p=128)
        hv = h2T.rearrange("p k t c -> p k (t c)")
        pc = 0
        for j in range(NFF):
            i = j % 2
            fw.dma(G_, wg[i], wg_v[:, :, j * 128:(j + 1) * 128], sem_g[i], writes=[kwg[i]])
            fw.dma(G_, wu[i], wu_v[:, :, j * 128:(j + 1) * 128], sem_g[i], writes=[kwg[i]])
            fw.dma(G_, wd[:, j, :], wd_v[:, j, :], sem_dn, writes=[kwd])
            for n in range(4):
                pg_, pu_ = 2 * (pc % 4), 2 * (pc % 4) + 1
                si = pc % 2
                pc += 1
                pgv, puv = pbanks[pg_][:, :], pbanks[pu_][:, :]
                fns = [mmf(pgv, wg[i][:, k, :], hv[:, k, n * 512:(n + 1) * 512], k == 0, k == 7) for k in range(8)]
                fw.mm(fns, [kh2T, kwg[i]], [pk[pg_]])
                fns = [mmf(puv, wu[i][:, k, :], hv[:, k, n * 512:(n + 1) * 512], k == 0, k == 7) for k in range(8)]
                fw.mm(fns, [kh2T, kwg[i]], [pk[pu_]])
                fw.op(A, act(sg[si], pgv, AF.Silu), [pk[pg_]], [ksg[si]])
                fw.op(V, tt(fT[:, j].rearrange("p t c -> p (t c)")[:, n * 512:(n + 1) * 512], sg[si], puv, ALU.mult),
                      [ksg[si], pk[pu_]], [kfT])
        fw.barrier()
        f3 = Bump(64 * K, 118 * K)
        xr = [f3([128, 1024], F32) for _ in range(4)]
        ot2 = [f3([128, 1024], F32) for _ in range(2)]
        fj3 = f3([128, 512], F32)
        kxr = [Tk() for _ in range(4)]
        kot2 = [Tk(), Tk()]
        kst6 = [Tk(), Tk()]
        for grp in range(8):
            base = 4 * (grp % 2)
            for tl in range(2):
                t = 2 * grp + tl
                fw.dma(SY, xr[t % 4], ov[:, t, :], sem_x[t % 4], [kOt[t]], [kxr[t % 4]])
            for j in range(NFF):
                fns = []
                for tl in range(2):
                    t = 2 * grp + tl
                    for hf in range(2):
                        fns.append(mmf(pbanks[base + 2 * tl + hf][:, :], fT[:, j, t, :], wd[:, j, hf * 512:(hf + 1) * 512],
                                       j == 0, j == NFF - 1))
                fw.mm(fns, [kfT, kwd], [pk[base + q_] for q_ in range(4)])
            for tl in range(2):
                t = 2 * grp + tl
                xi = t % 4
                i6 = t % 2
                c0 = 56 + 3 * i6
                rs6 = stat[:, c0 + 2:c0 + 3]
                for hf in range(2):
                    bk = base + 2 * tl + hf
                    fw.op(A, sqa(fj3, pbanks[bk][:, :], stat[:, c0 + hf:c0 + hf + 1]), [pk[bk]], [kst6[i6], kfj])
                fw.op(V, tt(rs6, stat[:, c0:c0 + 1], stat[:, c0 + 1:c0 + 2], ALU.add), [kst6[i6]], [kst6[i6]])
                fw.op(V, ts(rs6, rs6, 1.0 / D, ALU.mult, EPS, ALU.add), [kst6[i6]], [kst6[i6]])
                fw.op(A, act(rs6, rs6, AF.Ln), [kst6[i6]], [kst6[i6]])
                fw.op(A, act(rs6, rs6, AF.Exp, scale=-0.5), [kst6[i6]], [kst6[i6]])
                for hf in range(2):
                    bk = base + 2 * tl + hf
                    sl = slice(hf * 512, (hf + 1) * 512)
                    fw.op(V, stt(ot2[i6][:, sl], pbanks[bk][:, :], rs6, g4[:, sl], ALU.mult, ALU.mult),
                          [pk[bk], kst6[i6], kC], [kot2[i6]])
                fw.op(G_, tt(xr[xi], xr[xi], ot2[i6], ALU.add), [kot2[i6], kxr[xi]], [kxr[xi]])
                fw.dma(SY, ov[:, t, :], xr[xi], sem_st[t % 4], [kxr[xi]], [kOt[t]])
        fw.barrier()
    except _Stop:
        pass
    fw.barrier()
    fw.emit()
    return nc, es


_CACHE = {}


def _consts():
    idf = np.eye(128, dtype=np.float32)
    cb = np.zeros((128, 2, 256), np.float32)
    for r in range(2):
        kpos = r * 128 + np.arange(128)[:, None]
        qpos = np.arange(256)[None, :]
        cb[:, r, :] = np.where(kpos <= qpos, 0.0, NEG)
    tm = np.zeros((128, 2, 256), np.float32)
    dm = np.zeros((128, 2, 256), np.float32)
    for j in range(2):
        for s8 in range(8):
            for hi in range(16):
                sp = 8 * j + s8
                row = s8 * 16 + hi
                for jj in range(2):
                    pass
                for tp in range(16):
                    if tp >= sp:
                        tm[row, j, tp * 16:(tp + 1) * 16] = 1.0
                dm[row, j, sp * 16 + hi] = 1.0
    nv = np.tile(np.arange(1, 33, dtype=np.float32)[None, :], (128, 1))
    return idf, cb, tm, dm, nv


def kernel(**inp):
    f32 = np.float32
    x = np.ascontiguousarray(inp['x'], dtype=f32)

    def sq(k):
        return np.ascontiguousarray(inp[k][0], dtype=f32)

    def gp(a):
        a = a.reshape((2, 16) + a.shape[1:])
        a = np.moveaxis(a, 2, 1)
        return np.ascontiguousarray(a.reshape((128, 16) + a.shape[3:]))
    idf, cb, tm, dm, nv = _consts()
    shared = dict(
        w_in=sq('w_in'), w_glu=sq('w_glu'), w_out=sq('w_out'), w_gate=sq('w_gate'), w_up=sq('w_up'),
        w_down=sq('w_down'),
        areT=gp(sq('ssm_a_re')), aimT=gp(sq('ssm_a_im')),
        ldtT=gp(np.ascontiguousarray(np.broadcast_to(sq('ssm_log_dt')[:, None], (32, 64)))),
        breT=gp(sq('ssm_b_re')), bimT=gp(sq('ssm_b_im')),
        creT=gp(np.ascontiguousarray(sq('ssm_c_re').transpose(0, 2, 1))),
        cimT=gp(np.ascontiguousarray(sq('ssm_c_im').transpose(0, 2, 1))),
        drep=np.ascontiguousarray(np.broadcast_to(sq('ssm_d')[None], (128, 32, 16))),
        g1c=np.ascontiguousarray(sq('g_pre_mix').reshape(8, 128).T),
        g3c=np.ascontiguousarray(sq('g_pre_ffn').reshape(8, 128).T),
        g2r=np.ascontiguousarray(np.broadcast_to(sq('g_post_mix')[None], (128, 1024))),
        g4r=np.ascontiguousarray(np.broadcast_to(sq('g_post_ffn')[None], (128, 1024))),
        gsr=np.ascontiguousarray(np.broadcast_to(sq('g_ssm_out')[None], (128, 512))),
        gar=np.ascontiguousarray(np.broadcast_to(sq('g_attn_out')[None], (128, 512))),
        bgr=np.ascontiguousarray(np.broadcast_to(sq('b_glu')[None], (128, 512))),
        c_idf=idf, c_cb=cb, c_tm=tm, c_dm=dm, c_nv=nv,
    )
    if 'nc' not in _CACHE:
        _CACHE['nc'] = build(DEBUG)
    nc, _es = _CACHE['nc']
    in_maps = []
    for c in range(8):
        m = dict(shared)
        m['x'] = np.ascontiguousarray(x[2 * c:2 * c + 2])
        in_maps.append(m)
    res = run_bass_kernel_spmd(nc, in_maps, core_ids=list(range(8)))
    _CACHE['res'] = res
    return np.concatenate([np.asarray(r['out'], dtype=f32) for r in res.results], axis=0)
```
